# Optimizing a Trainium2 kernel written in Bass

```python
import jax, jax.numpy as jnp
from jax import lax
import numpy as np

D_MODEL = 1024
BATCH = 16
SEQ = 4096
DEPTH = 1

HEAD_DIM = 64
NSA_HEADS = 8
NSA_KV_GROUPS = 2
NSA_HPG = NSA_HEADS // NSA_KV_GROUPS
NSA_WIDTH = NSA_HEADS * HEAD_DIM
NSA_KV_WIDTH = NSA_KV_GROUPS * HEAD_DIM
CMP_LEN = 32
CMP_STRIDE = 16
CMP_HIDDEN = 2 * HEAD_DIM
SLC_BLOCK = 64
SLC_TOPN = 16
WINDOW = 512
MOBA_HEADS = 8
MOBA_WIDTH = MOBA_HEADS * HEAD_DIM
MOBA_BLOCK = 256
MOBA_TOPK = 3
TOTAL_HEADS = NSA_HEADS + MOBA_HEADS
Q_CHUNK = 32
RMS_EPS = 1e-6
COL_WIDTHS = (NSA_WIDTH, 6 * NSA_KV_WIDTH, 3 * NSA_HEADS, NSA_WIDTH, 3 * MOBA_WIDTH, MOBA_WIDTH, 2 * D_MODEL)
IN_COLS = NSA_WIDTH + 6 * NSA_KV_WIDTH + 3 * NSA_HEADS + NSA_WIDTH + 3 * MOBA_WIDTH + MOBA_WIDTH + 2 * D_MODEL

kernel_name = 'hybrid_nsa_moba_gated_block'


def rms_norm(x, g):
    xf = x.astype(jnp.float32)
    y = xf * lax.rsqrt(jnp.mean(xf * xf, axis=-1, keepdims=True) + RMS_EPS)
    return (y * g.astype(jnp.float32)).astype(x.dtype)


def alibi_slopes(n):
    return jnp.asarray((2.0 ** (-8.0 * np.arange(1, n + 1) / n)).astype(np.float32))


def masked_softmax(s, mask):
    s = jnp.where(mask, s, -jnp.inf)
    m = jnp.max(s, axis=-1, keepdims=True)
    m = jnp.where(jnp.isfinite(m), m, 0.0)
    p = jnp.where(mask, jnp.exp(s - m), 0.0)
    return p / jnp.maximum(jnp.sum(p, axis=-1, keepdims=True), 1e-30)


def cmp_to_slc_matrix(n_cmp, n_slc):
    cs = np.arange(n_cmp) * CMP_STRIDE
    ce = cs + CMP_LEN - 1
    ss = np.arange(n_slc) * SLC_BLOCK
    se = ss + SLC_BLOCK - 1
    return ((cs[:, None] <= se[None, :]) & (ce[:, None] >= ss[None, :])).astype(np.float32)


def compress(k, pe, w1, w2):
    S = k.shape[2]
    n_cmp = (S - CMP_LEN) // CMP_STRIDE + 1
    idx = np.arange(n_cmp)[:, None] * CMP_STRIDE + np.arange(CMP_LEN)[None, :]
    blocks = k[:, :, idx] + pe
    h = jax.nn.silu(jnp.einsum('bgnld,lde->bgne', blocks, w1))
    return jnp.einsum('bgne,ef->bgnf', h, w2)


def nsa_attention(q, kc, vc, ks, vs, kw, vw, gates, slopes):
    B, G, P, S, hd = q.shape
    n_cmp = kc.shape[2]
    n_slc = S // SLC_BLOCK
    k_sel = min(SLC_TOPN, n_slc)
    scale = hd ** -0.5
    cmp_end = jnp.asarray(np.arange(n_cmp) * CMP_STRIDE + CMP_LEN - 1, jnp.int32)
    overlap = jnp.asarray(cmp_to_slc_matrix(n_cmp, n_slc))
    ks_b = ks.reshape(B, G, n_slc, SLC_BLOCK, hd)
    vs_b = vs.reshape(B, G, n_slc, SLC_BLOCK, hd)
    kw_p = jnp.pad(kw, ((0, 0), (0, 0), (WINDOW, 0), (0, 0)))
    vw_p = jnp.pad(vw, ((0, 0), (0, 0), (WINDOW, 0), (0, 0)))
    bi = jnp.arange(B)[:, None, None, None]
    gi = jnp.arange(G)[None, :, None, None]
    blk = jnp.arange(n_slc)
    in_blk = jnp.arange(SLC_BLOCK)
    win_off = jnp.arange(WINDOW + Q_CHUNK) - WINDOW
    sl = slopes.astype(jnp.float32)[None, :, :, None, None]

    def chunk(ci):
        c0 = ci * Q_CHUNK
        t = c0 + jnp.arange(Q_CHUNK)
        qc = lax.dynamic_slice_in_dim(q, c0, Q_CHUNK, axis=3)
        g = lax.dynamic_slice_in_dim(gates, c0, Q_CHUNK, axis=4)
        dist = t[:, None] - cmp_end[None, :]
        s = jnp.einsum('bgpqd,bgnd->bgpqn', qc, kc).astype(jnp.float32) * scale - sl * dist.astype(jnp.float32)
        p_cmp = masked_softmax(s, dist >= 0)
        o_cmp = jnp.einsum('bgpqn,bgnd->bgpqd', p_cmp.astype(vc.dtype), vc)
        imp = jnp.einsum('bgpqn,nj->bgqj', p_cmp, overlap)
        cur = t // SLC_BLOCK
        causal_blk = blk[None, :] <= cur[:, None]
        forced = (blk[None, :] == 0) | (blk[None, :] == cur[:, None]) | (blk[None, :] == cur[:, None] - 1)
        imp = jnp.where(forced, jnp.inf, imp)
        imp = jnp.where(causal_blk, imp, -jnp.inf)
        _, sel = lax.top_k(imp, k_sel)
        kg = ks_b[bi, gi, sel]
        vg = vs_b[bi, gi, sel]
        pos = (sel[..., None] * SLC_BLOCK + in_blk).reshape(B, G, 1, Q_CHUNK, k_sel * SLC_BLOCK)
        dist = t[:, None] - pos
        s = jnp.einsum('bgpqd,bgqkld->bgpqkl', qc, kg).reshape(B, G, P, Q_CHUNK, k_sel * SLC_BLOCK)
        s = s.astype(jnp.float32) * scale - sl * dist.astype(jnp.float32)
        p = masked_softmax(s, dist >= 0).astype(vg.dtype).reshape(B, G, P, Q_CHUNK, k_sel, SLC_BLOCK)
        o_slc = jnp.einsum('bgpqkl,bgqkld->bgpqd', p, vg)
        kwin = lax.dynamic_slice_in_dim(kw_p, c0, WINDOW + Q_CHUNK, axis=2)
        vwin = lax.dynamic_slice_in_dim(vw_p, c0, WINDOW + Q_CHUNK, axis=2)
        posw = c0 + win_off
        dist = t[:, None] - posw[None, :]
        mask = (posw[None, :] >= 0) & (dist >= 0) & (dist < WINDOW)
        s = jnp.einsum('bgpqd,bgkd->bgpqk', qc, kwin).astype(jnp.float32) * scale - sl * dist.astype(jnp.float32)
        p = masked_softmax(s, mask).astype(vwin.dtype)
        o_win = jnp.einsum('bgpqk,bgkd->bgpqd', p, vwin)
        return g[0][..., None] * o_cmp + g[1][..., None] * o_slc + g[2][..., None] * o_win

    out = lax.map(chunk, jnp.arange(S // Q_CHUNK))
    return out.transpose(1, 0, 4, 2, 3, 5).reshape(B, S, G * P * hd)


def moba_attention(q, k, v, slopes):
    B, H, S, hd = q.shape
    nb = -(-S // MOBA_BLOCK)
    pad = nb * MOBA_BLOCK - S
    kb = jnp.pad(k, ((0, 0), (0, 0), (0, pad), (0, 0))).reshape(B, H, nb, MOBA_BLOCK, hd)
    vb = jnp.pad(v, ((0, 0), (0, 0), (0, pad), (0, 0))).reshape(B, H, nb, MOBA_BLOCK, hd)
    kbar = jnp.mean(kb.astype(jnp.float32), axis=3)
    k_sel = min(MOBA_TOPK, nb)
    scale = hd ** -0.5
    bi = jnp.arange(B)[:, None, None, None]
    hi = jnp.arange(H)[None, :, None, None]
    offs = jnp.arange(MOBA_BLOCK)
    blocks = jnp.arange(nb)
    sl = slopes.astype(jnp.float32)[None, :, None, None]

    def chunk(ci):
        c0 = ci * Q_CHUNK
        t = c0 + jnp.arange(Q_CHUNK)
        cur = c0 // MOBA_BLOCK
        qc = lax.dynamic_slice_in_dim(q, c0, Q_CHUNK, axis=2)
        gate = jnp.einsum('bhqd,bhnd->bhqn', qc.astype(jnp.float32), kbar)
        gate = jnp.where(blocks < cur, gate, -jnp.inf)
        _, sel = lax.top_k(gate, k_sel)
        valid = sel < cur
        kg = kb[bi, hi, sel]
        vg = vb[bi, hi, sel]
        ko = lax.dynamic_index_in_dim(kb, cur, axis=2, keepdims=False)
        vo = lax.dynamic_index_in_dim(vb, cur, axis=2, keepdims=False)
        n_sel = k_sel * MOBA_BLOCK
        s_sel = jnp.einsum('bhqd,bhqkld->bhqkl', qc, kg).reshape(B, H, Q_CHUNK, n_sel)
        s_own = jnp.einsum('bhqd,bhld->bhql', qc, ko)
        s = jnp.concatenate([s_sel, s_own], axis=-1).astype(jnp.float32) * scale
        pos_sel = (sel[..., None] * MOBA_BLOCK + offs).reshape(B, H, Q_CHUNK, n_sel)
        pos_own = jnp.broadcast_to(cur * MOBA_BLOCK + offs, (B, H, Q_CHUNK, MOBA_BLOCK))
        pos = jnp.concatenate([pos_sel, pos_own], axis=-1)
        dist = t[:, None] - pos
        mask_sel = jnp.broadcast_to(valid[..., None], (B, H, Q_CHUNK, k_sel, MOBA_BLOCK)).reshape(B, H, Q_CHUNK, n_sel)
        mask = jnp.concatenate([mask_sel, dist[..., n_sel:] >= 0], axis=-1)
        s = s - sl * dist.astype(jnp.float32)
        p = masked_softmax(s, mask).astype(v.dtype)
        p_sel = p[..., :n_sel].reshape(B, H, Q_CHUNK, k_sel, MOBA_BLOCK)
        p_own = p[..., n_sel:]
        return jnp.einsum('bhqkl,bhqkld->bhqd', p_sel, vg) + jnp.einsum('bhql,bhld->bhqd', p_own, vo)

    out = lax.map(chunk, jnp.arange(S // Q_CHUNK))
    return out.transpose(1, 0, 3, 2, 4).reshape(B, S, H * hd)


def hybrid_layer(x, norm_pre, w_in, pe_k, w1_k, w2_k, pe_v, w1_v, w2_v, w_a, w_b, w_o, norm_post):
    B, S, _ = x.shape
    G, P, hd = NSA_KV_GROUPS, NSA_HPG, HEAD_DIM
    u = rms_norm(x, norm_pre)
    proj = u @ w_in
    splits = [int(c) for c in np.cumsum(COL_WIDTHS)[:-1]]
    q_a, kv_a, g_a, z_a, qkv_b, z_b, merge = jnp.split(proj, splits, axis=-1)
    slopes = alibi_slopes(TOTAL_HEADS)
    q_a = q_a.reshape(B, S, G, P, hd).transpose(0, 2, 3, 1, 4)
    kv_a = kv_a.reshape(B, S, 6, G, hd).transpose(2, 0, 3, 1, 4)
    kc = compress(kv_a[0], pe_k, w1_k, w2_k)
    vc = compress(kv_a[1], pe_v, w1_v, w2_v)
    gates_a = jax.nn.sigmoid(g_a).reshape(B, S, 3, G, P).transpose(2, 0, 3, 4, 1)
    o_a = nsa_attention(q_a, kc, vc, kv_a[2], kv_a[3], kv_a[4], kv_a[5], gates_a,
                        slopes[0::2].reshape(G, P))
    y_a = o_a * jax.nn.silu(z_a)
    qkv_b = qkv_b.reshape(B, S, 3, MOBA_HEADS, hd).transpose(2, 0, 3, 1, 4)
    o_b = moba_attention(qkv_b[0], qkv_b[1], qkv_b[2], slopes[1::2])
    y_b = o_b * jax.nn.silu(z_b)
    gate_a, gate_b = jnp.split(merge, 2, axis=-1)
    m = jax.nn.sigmoid(gate_a) * (y_a @ w_a) + jax.nn.sigmoid(gate_b) * (y_b @ w_b)
    return x + rms_norm(m @ w_o, norm_post)


def setup_inputs(seed: int = 0) -> dict:
    key = jax.random.key(seed)
    ks = jax.random.split(key, 13)

    def nrm(k, shape, s):
        return jax.random.normal(k, shape, jnp.float32) * s

    return {
        'x': nrm(ks[0], (BATCH, SEQ, D_MODEL), 1.0),
        'norm_pre': 1.0 + nrm(ks[1], (DEPTH, D_MODEL), 0.1),
        'w_in': nrm(ks[2], (DEPTH, D_MODEL, IN_COLS), D_MODEL ** -0.5),
        'cmp_pe_k': nrm(ks[3], (DEPTH, CMP_LEN, HEAD_DIM), 0.1),
        'cmp_w1_k': nrm(ks[4], (DEPTH, CMP_LEN, HEAD_DIM, CMP_HIDDEN), (CMP_LEN * HEAD_DIM) ** -0.5),
        'cmp_w2_k': nrm(ks[5], (DEPTH, CMP_HIDDEN, HEAD_DIM), CMP_HIDDEN ** -0.5),
        'cmp_pe_v': nrm(ks[6], (DEPTH, CMP_LEN, HEAD_DIM), 0.1),
        'cmp_w1_v': nrm(ks[7], (DEPTH, CMP_LEN, HEAD_DIM, CMP_HIDDEN), (CMP_LEN * HEAD_DIM) ** -0.5),
        'cmp_w2_v': nrm(ks[8], (DEPTH, CMP_HIDDEN, HEAD_DIM), CMP_HIDDEN ** -0.5),
        'w_branch_a': nrm(ks[9], (DEPTH, NSA_WIDTH, D_MODEL), NSA_WIDTH ** -0.5),
        'w_branch_b': nrm(ks[10], (DEPTH, MOBA_WIDTH, D_MODEL), MOBA_WIDTH ** -0.5),
        'w_o': nrm(ks[11], (DEPTH, D_MODEL, D_MODEL), D_MODEL ** -0.5),
        'norm_post': 1.0 + nrm(ks[12], (DEPTH, D_MODEL), 0.1),
    }


def reference(x, norm_pre, w_in, cmp_pe_k, cmp_w1_k, cmp_w2_k, cmp_pe_v, cmp_w1_v, cmp_w2_v,
              w_branch_a, w_branch_b, w_o, norm_post):
    h = x
    for l in range(DEPTH):
        h = hybrid_layer(h, norm_pre[l], w_in[l], cmp_pe_k[l], cmp_w1_k[l], cmp_w2_k[l],
                         cmp_pe_v[l], cmp_w1_v[l], cmp_w2_v[l], w_branch_a[l], w_branch_b[l],
                         w_o[l], norm_post[l])
    return h
```

```python
import contextlib
import numpy as np
import ml_dtypes
import concourse.bass as bass
import concourse.mybir as mybir
from concourse.bass_utils import run_bass_kernel_spmd

F32 = mybir.dt.float32
BF16 = mybir.dt.bfloat16
ALU = mybir.AluOpType
AF = mybir.ActivationFunctionType
AX = mybir.AxisListType
NPBF = ml_dtypes.bfloat16

D = 1024
NCOL = 5912
NEG = -30000.0
SCALE = 0.125
BIGF = 1.0e4
EPSL = 1.0e-30
C_QA, C_KV, C_GA, C_ZA, C_QB, C_ZB, C_MG = 0, 512, 1280, 1304, 1816, 3352, 3864
SLOPES = [2.0 ** (-(i + 1) / 2.0) for i in range(16)]
SL_HH = [SLOPES[2 * h] for h in range(8)] + [SLOPES[2 * h + 1] for h in range(8)]


class Prog:
    EPOCH = 20000
    NDMA = 8

    def __init__(self, nc):
        self.nc = nc
        self.ops = []
        self.lastw = {}
        self.readers = {}
        self.stack = contextlib.ExitStack()
        self.sb_bytes = 0

    def sb(self, name, shape, dtype):
        n = 1
        for s in shape[1:]:
            n *= s
        self.sb_bytes += n * (4 if dtype == F32 else 2)
        return self.stack.enter_context(self.nc.sbuf_tensor('sb_' + name, list(shape), dtype))

    def ps(self, name, shape=(128, 512), dtype=F32):
        return self.stack.enter_context(self.nc.psum_tensor(name, list(shape), dtype))

    def _deps(self, idx, reads, writes):
        deps = set()
        for k in reads:
            w = self.lastw.get(k)
            if w is not None:
                deps.add(w)
        for k in writes:
            w = self.lastw.get(k)
            if w is not None:
                deps.add(w)
            for r in self.readers.get(k, ()):
                deps.add(r)
        for k in reads:
            self.readers.setdefault(k, []).append(idx)
        for k in writes:
            self.lastw[k] = idx
            self.readers[k] = []
        deps.discard(idx)
        return deps

    def op(self, eng, fn, reads=(), writes=()):
        idx = len(self.ops)
        deps = self._deps(idx, reads, writes)
        self.ops.append(dict(eng=eng, fn=fn, deps=deps, dma=False, wkeys=set(writes), rkeys=set(reads)))
        return idx

    def dma(self, eng, out, in_, reads=(), writes=()):
        idx = len(self.ops)
        deps = self._deps(idx, reads, writes)
        self.ops.append(dict(eng=eng, fn=lambda e: e.dma_start(out=out, in_=in_), deps=deps, dma=True,
                             wkeys=set(writes), rkeys=set(reads)))
        return idx

    def finish(self):
        nc = self.nc
        ops = self.ops
        needed = set()
        for i, o in enumerate(ops):
            nd = set()
            for d in o['deps']:
                od = ops[d]
                if od['eng'] == o['eng'] and not od['dma'] and not o['dma']:
                    if o['eng'] == 'tensor':
                        continue
                    if not ((od['wkeys'] & o['rkeys']) or (od['wkeys'] & o['wkeys'])):
                        continue
                nd.add(d)
            o['deps'] = nd
            for d in nd:
                if not ops[d]['dma']:
                    needed.add(d)
        engs = ['tensor', 'vector', 'scalar', 'gpsimd', 'sync']
        cnt = {e: 0 for e in engs}
        dcnt = {e: 0 for e in engs}
        nep = {e: 0 for e in engs}
        for i, o in enumerate(ops):
            e = o['eng']
            if o['dma']:
                d = dcnt[e]
                dcnt[e] += 1
                o['sig'] = (('dma', e, d % self.NDMA), 16 * (d // self.NDMA + 1), 16)
                o['prev'] = (('dma', e, d % self.NDMA), 16 * (d // self.NDMA)) if d >= self.NDMA else None
            elif i in needed:
                c = cnt[e]
                cnt[e] += 1
                ep = c // self.EPOCH
                nep[e] = max(nep[e], ep + 1)
                o['sig'] = (('cmp', e, ep), c % self.EPOCH + 1, 1)
            else:
                o['sig'] = None
        sems = {}
        for e in engs:
            for ep in range(nep[e]):
                sems[('cmp', e, ep)] = self.stack.enter_context(nc.semaphore(f"s_{e}_{ep}"))
            for j in range(min(self.NDMA, dcnt[e])):
                sems[('dma', e, j)] = self.stack.enter_context(nc.semaphore(f"d_{e}_{j}"))
        self.n_instr = {e: 0 for e in engs}
        with nc.Block() as block:
            def make(ename):
                def body(eng):
                    waited = {}
                    lastdma = {}
                    for o in ops:
                        if o['eng'] != ename:
                            continue
                        best = {}
                        for d in o['deps']:
                            s = ops[d]['sig']
                            if s[1] > best.get(s[0], 0):
                                best[s[0]] = s[1]
                        if o['dma'] and o['prev'] is not None:
                            k, v = o['prev']
                            if v > best.get(k, 0):
                                best[k] = v
                        for k, v in best.items():
                            if waited.get(k, 0) >= v:
                                continue
                            eng.wait_ge(sems[k], v)
                            waited[k] = v
                            self.n_instr[ename] += 1
                        ins = o['fn'](eng)
                        self.n_instr[ename] += 1
                        if o['sig'] is not None:
                            ins.then_inc(sems[o['sig'][0]], o['sig'][2])
                            if o['dma']:
                                lastdma[o['sig'][0]] = o['sig'][1]
                    for k, v in lastdma.items():
                        if waited.get(k, 0) < v:
                            eng.wait_ge(sems[k], v)
                return body
            for ename in engs:
                if any(o['eng'] == ename for o in ops):
                    getattr(block, ename)(make(ename))
        self.stack.close()


def make_consts():
    c = {}
    c['ident_f'] = np.eye(128, dtype=np.float32)
    c['ident_b'] = np.eye(128).astype(NPBF)
    p = np.arange(128)[:, None]
    fp = np.arange(896)[None, :]
    c['cmb'] = np.where(p > fp - 384, NEG, 0.0).astype(NPBF)
    c['omb'] = np.where(fp - 384 >= p, NEG, 0.0).astype(NPBF)
    kk = np.arange(4)[None, :, None] * 128 + np.arange(128)[:, None, None]
    f = np.arange(512)[None, None, :]
    mm = np.where(kk // 256 == f // 256, np.where(kk > f, NEG, 0.0), np.where(f // 256 > kk // 256, 0.0, NEG))
    c['mmb'] = mm.astype(NPBF)
    jj = np.arange(128)[:, None]
    k2 = np.arange(4096)[None, :]
    c['e128'] = ((k2 // 64 == jj) & (jj < 64)).astype(NPBF)
    oh = np.zeros((128, 16, 128), np.float32)
    oh[0:16] = np.broadcast_to(np.eye(16)[:, :, None], (16, 16, 128))
    c['ohm'] = oh.astype(NPBF)
    alb = np.zeros((128, 16, 36), np.float32)
    for hh in range(16):
        for di in range(36):
            alb[:, hh, di] = SL_HH[hh] * (np.arange(128) + 128.0 * (di - 32))
    c['alb'] = alb
    clb = np.zeros((128, 8, 2, 8), np.float32)
    for ha in range(8):
        for a in range(2):
            for i in range(8):
                clb[:, ha, a, i] = SL_HH[ha] * (2048.0 * a + 16.0 * np.arange(128) + 31.0 - 512.0 * i)
    c['clb'] = clb
    cq = np.zeros((16, 512), np.float32)
    for hh in range(16):
        cq[hh] = -SL_HH[hh] * np.arange(512) / SCALE
    c['cq'] = cq.astype(NPBF)
    goh = np.zeros((56, 12, 128), np.float32)
    for g in range(2):
        for pp in range(2):
            for k in range(3):
                idx = (g * 2 + pp) * 3 + k
                ra = k * 8 + g * 4 + 2 * pp
                goh[ra, idx, 0:64] = 1.0
                goh[32 + ra, idx, 0:64] = 1.0
                goh[ra + 1, idx, 64:128] = 1.0
                goh[32 + ra + 1, idx, 64:128] = 1.0
    c['goh'] = goh.astype(NPBF)
    n_cmp, n_slc = 255, 64
    cs = np.arange(n_cmp) * 16
    ce = cs + 31
    ss = np.arange(n_slc) * 64
    se = ss + 63
    ov = ((cs[:, None] <= se[None, :]) & (ce[:, None] >= ss[None, :])).astype(np.float32)
    ovp = np.zeros((256, 64), np.float32)
    ovp[:255] = ov
    c['ov'] = np.ascontiguousarray(ovp.reshape(2, 128, 64).transpose(1, 0, 2)).astype(NPBF)
    q = np.arange(128)[:, None]
    m = np.arange(128)[None, :]
    jr = m - 64
    cr = q // 64
    ab = np.where((jr == cr) | (jr == cr - 1), BIGF, np.where(jr > cr, -BIGF, 0.0))
    c['abase'] = ab.astype(np.float32)
    m2 = np.arange(32)[None, :]
    c['bbase'] = np.broadcast_to(np.where(m2 >= 16, -BIGF, 0.0), (128, 32)).astype(np.float32).copy()
    c['obase'] = np.broadcast_to(np.where(m2 == 16, 0.0, 2 * NEG), (128, 32)).astype(np.float32).copy()
    c['xg'] = (16.0 * np.arange(128)[:, None] + 31.0 - np.arange(512)[None, :]).astype(np.float32)
    return c


CONST_DT = dict(ident_f=F32, ident_b=BF16, cmb=BF16, omb=BF16, mmb=BF16, e128=BF16, ohm=BF16, alb=F32, clb=F32,
                cq=BF16, goh=BF16, ov=BF16, abase=F32, bbase=F32, obase=F32, xg=F32)


def build(S, NSEQ, dbg=()):
    nc = bass.Bass("TRN2", target_bir_lowering=False)
    NT = S // 128
    NQT = S // 512
    consts = make_consts()

    def din(name, shape, dt=F32):
        return nc.dram_tensor(name, list(shape), dt, kind="ExternalInput").ap()

    x_d = din("x", [NSEQ, S, D])
    win_d = din("w_in", [D, NCOL])
    w1s_d = din("w1s", [128, 32, 128])
    w2s_d = din("w2s", [128, 2, 64])
    pet_d = din("pet", [128, 32])
    wa_d = din("w_a", [512, D])
    wb_d = din("w_b", [512, D])
    wo_d = din("w_o", [D, D])
    npre_d = din("npre_bc", [128, D])
    npost_d = din("npost_bc", [128, D])
    cd = {k: din("c_" + k, list(v.shape), CONST_DT[k]) for k, v in consts.items()}
    out_d = nc.dram_tensor("out", [NSEQ, S, D], F32, kind="ExternalOutput").ap()
    wbf_d = nc.dram_tensor("wbf", [D, NCOL], BF16, kind="Internal").ap()
    ysc_d = nc.dram_tensor("yscr", [8, 128, S], BF16, kind="Internal").ap()
    wabf_d = nc.dram_tensor("wabf", [2, 512, D], BF16, kind="Internal").ap()
    dbg_d = {}
    for name, shape in dbg:
        dbg_d[name] = nc.dram_tensor("dbg_" + name, list(shape), F32, kind="ExternalOutput").ap()

    P = Prog(nc)
    cs = {k: P.sb("k_" + k, list(v.shape), CONST_DT[k]) for k, v in consts.items()}
    uT = P.sb("uT", [128, 8, S], BF16)
    SA = max(S, 4096)
    kT0 = P.sb("kT0", [128, SA], BF16)
    kT1 = P.sb("kT1", [128, SA], BF16)
    Vp = P.sb("Vp", [128, SA], BF16)
    kcT = P.sb("kcT", [128, 256], BF16)
    vc = P.sb("vc", [128, 2, 64], BF16)
    hcm = P.sb("hcm", [128, 2, 256], BF16)
    QA = [[P.sb(f"QA{b}_{h}", [65, 512], BF16) for h in range(4)] for b in range(2)]
    PT = [P.sb(f"PT{j}", [128, 512], BF16) for j in range(4)]
    XM = [P.sb(f"XM{j}", [128, 512], BF16) for j in range(2)]
    WK = [P.sb(f"WK{j}", [128, 512], F32) for j in range(5)]
    YST = [P.sb(f"YST{j}", [128, 512], BF16) for j in range(2)]
    SGT = P.sb("SGT", [64, 512], BF16)
    selT = P.sb("selT", [128, 512], BF16)
    selTm = [P.sb(f"selTm{j}", [128, 512], BF16) for j in range(2)]
    T1 = P.sb("T1", [128, 4, 64], F32)
    T2 = P.sb("T2", [128, 4, 64], F32)
    M8a = P.sb("M8a", [128, 8], F32)
    M8b = P.sb("M8b", [128, 8], F32)
    GT = P.sb("GT", [128, 8, 16], F32)
    kbarf = P.sb("kbarf", [128, 16], F32)
    kbar = P.sb("kbar", [64, 2, 16], BF16)
    wst = [P.sb(f"wst{j}", [128, 8, 128], BF16) for j in range(2)]
    WQZ = P.sb("WQZ", [128, 8, 512], BF16)
    wg24 = P.sb("wg24", [128, 8, 24], BF16)
    w2s = P.sb("w2s", [128, 2, 64], BF16)
    pet = P.sb("pet", [128, 32], BF16)
    hb = P.sb("hb", [128, 2], F32)
    xt = [P.sb(f"xt{j}", [128, D], F32) for j in range(2)]
    xn = [P.sb(f"xn{j}", [128, D], F32) for j in range(2)]
    gpre = P.sb("gpre", [128, D], F32)
    gpost = gpre
    ACC = [xt[j][:, 0:512] for j in range(2)]
    SZ = [xn[j][:, 0:512] for j in range(2)]
    ssq = P.sb("ssq", [128, 2], F32)
    eps_t = P.sb("eps_t", [128, 1], F32)
    ones_b = P.sb("ones_b", [128, 128], BF16)
    wab = P.sb("wab", [128, 2, 8, 128], BF16)
    wo_s = P.sb("wo_s", [128, 8, D], BF16)
    ps = [P.ps(f"ps{j}") for j in range(8)]
    ST = [0, 1, 2]
    OB, LB, X0, X1, X2 = 3, 4, 5, 6, 7
    Vp3 = Vp[:].rearrange("p (t c) -> p t c", c=128)
    mT = kT0[:].rearrange("p (c t) -> p c t", c=8)
    ysl = kT1[:].rearrange("p (c t) -> p c t", c=8)
    wgs = Vp[:].rearrange("p (b c n) -> p b c n", b=2, c=8)
    assert SA // 8 >= 512 and SA // 16 >= 256

    rr = {'misc': 0, 'st': 0, 'pt': 0, 'wst': 0, 'wk': 0}

    def MM(out, lhsT, rhs, start, stop, reads, writes, tp=None):
        if tp is None:
            P.op('tensor', lambda e: e.matmul(out, lhsT=lhsT, rhs=rhs, start=start, stop=stop), reads, writes)
        else:
            P.op('tensor', lambda e: e.matmul(out, lhsT=lhsT, rhs=rhs, start=start, stop=stop, tile_position=tp),
                 reads, writes)

    def TR(out, in_, ident, reads, writes):
        P.op('tensor', lambda e: e.transpose(out, in_, ident), reads, writes)

    def ACT(out, in_, func, reads, writes, bias=None, scale=1.0):
        if bias is None:
            P.op('scalar', lambda e: e.activation(out=out, in_=in_, func=func, scale=scale), reads, writes)
        else:
            P.op('scalar', lambda e: e.activation(out=out, in_=in_, func=func, bias=bias, scale=scale), reads, writes)

    def TT(eng, out, in0, in1, op, reads, writes):
        P.op(eng, lambda e: e.tensor_tensor(out=out, in0=in0, in1=in1, op=op), reads, writes)

    def TS(eng, out, in0, s1, s2, op0, op1, reads, writes):
        if op1 is None:
            P.op(eng, lambda e: e.tensor_scalar(out=out, in0=in0, scalar1=s1, scalar2=None, op0=op0), reads, writes)
        else:
            P.op(eng, lambda e: e.tensor_scalar(out=out, in0=in0, scalar1=s1, scalar2=s2, op0=op0, op1=op1),
                 reads, writes)

    def STT(out, in0, scalar, in1, op0, op1, reads, writes):
        P.op('vector', lambda e: e.scalar_tensor_tensor(out=out, in0=in0, scalar=scalar, in1=in1, op0=op0, op1=op1),
             reads, writes)

    def CP(eng, out, in_, reads, writes):
        P.op(eng, lambda e: e.tensor_copy(out=out, in_=in_), reads, writes)

    def RCP(eng, out, in_, reads, writes):
        P.op(eng, lambda e: e.reciprocal(out=out, in_=in_), reads, writes)

    def MS(eng, ap, val, writes):
        P.op(eng, lambda e: e.memset(ap, val), (), writes)

    def pk(j):
        return ('ps', j)

    def misc_bank():
        j = [X0, X1, X2][rr['misc'] % 3]
        rr['misc'] += 1
        return j

    def next_wst():
        j = rr['wst'] % 2
        rr['wst'] += 1
        return j

    wcols = wbf_d.rearrange("(c p) n -> p c n", p=128)
    WBK = [('wbf', r) for r in range(8)]

    def load_w(dst, col0, ncols, key):
        P.dma('sync', dst, wcols[:, :, col0:col0 + ncols], reads=WBK, writes=[key])

    def dump(name, src, reads):
        if name in dbg_d:
            P.dma('sync', dbg_d[name], src, reads=reads, writes=['dbg_' + name])

    for k in consts:
        P.dma('sync', cs[k][:], cd[k], reads=(), writes=['c_' + k])
    CK = ['c_' + k for k in consts]
    for r in range(8):
        P.dma('gpsimd', wbf_d[r * 128:(r + 1) * 128, :], win_d[r * 128:(r + 1) * 128, :], reads=(), writes=[('wbf', r)])
    P.dma('gpsimd', wabf_d[0], wa_d, reads=(), writes=['wabf0'])
    P.dma('gpsimd', wabf_d[1], wb_d, reads=(), writes=['wabf1'])
    P.dma('gpsimd', wo_s[:], wo_d.rearrange("(j p) n -> p j n", p=128), reads=(), writes=['wo'])
    P.dma('gpsimd', w2s[:], w2s_d, reads=(), writes=['w2s'])
    P.dma('gpsimd', pet[:], pet_d, reads=(), writes=['pet'])
    load_w(wg24[:], C_GA, 24, 'wg24')
    MS('vector', eps_t[:], 1e-6, ['eps'])
    MS('vector', ones_b[:], 1.0, ['ones'])
    MS('vector', SGT[:], 0.0, ['SGT'])
    MS('vector', kcT[64:65, :], 1.0, ['kcT'])
    MS('vector', hcm[:], 0.0, ['hcm'])
    MS('vector', selT[:], 0.0, ['selT'])
    MS('vector', selTm[0][:], 0.0, [('selTm', 0)])
    MS('vector', selTm[1][:], 0.0, [('selTm', 1)])

    def attend(tiles):
        pend = None
        for t in tiles:
            sb_ = ST[rr['st'] % 3]
            rr['st'] += 1
            pj = rr['pt'] % 4
            rr['pt'] += 1
            n = len(t['smm'])
            for m, (lh, rh, rd) in enumerate(t['smm']):
                MM(ps[sb_][:, :], lh, rh, m == 0, m == n - 1, rd, [pk(sb_)])
            ACT(PT[pj][:], ps[sb_][:, :], AF.Exp, [pk(sb_)] + CK, [('PT', pj)], bias=t['bias'], scale=SCALE)
            if pend is not None:
                for (o_, lh, tp, st_, sp_, rd, wk) in pend[0]:
                    MM(o_, lh, PT[pend[1]][:], st_, sp_, rd + [('PT', pend[1])], [wk], tp=tp)
            pend = (t['pv'], pj)
        if pend is not None:
            for (o_, lh, tp, st_, sp_, rd, wk) in pend[0]:
                MM(o_, lh, PT[pend[1]][:], st_, sp_, rd + [('PT', pend[1])], [wk], tp=tp)

    def proj_fm(wtile, wkey, wc0, ncol, blk, bank):
        for c in range(8):
            MM(ps[bank][0:ncol, :], wtile[:, c, wc0:wc0 + ncol], uT[:, c, blk * 512:(blk + 1) * 512],
               c == 0, c == 7, [wkey, ('uT', blk)], [pk(bank)])

    def silu_pair(bank, dst, dkey):
        w = WK[0]
        ACT(w[:], ps[bank][:, :], AF.Exp, [pk(bank)], [('WK', 0)], scale=-1.0)
        TS('gpsimd', w[:], w[:], 1.0, None, ALU.add, None, [('WK', 0)], [('WK', 0)])
        RCP('vector', w[:], w[:], [('WK', 0)], [('WK', 0)])
        TT('vector', dst[:], ps[bank][:, :], w[:], ALU.mult, [pk(bank), ('WK', 0)], [dkey])

    def combine_branch(acc, akey, first, gbank, lrows):
        w = WK[1]
        for (r0, r1, lb) in lrows:
            TS('vector', w[r0:r1, :], ps[lb][r0:r1, :], EPSL, None, ALU.max, None, [pk(lb)], [('WK', 1)])
        RCP('vector', w[:], w[:], [('WK', 1)], [('WK', 1)])
        if gbank is not None:
            TT('vector', w[:], ps[gbank][:, :], w[:], ALU.mult, [pk(gbank), ('WK', 1)], [('WK', 1)])
        if first:
            TT('vector', acc[:], ps[OB][:, :], w[:], ALU.mult, [pk(OB), ('WK', 1)], [akey])
        else:
            w2 = WK[2]
            TT('vector', w2[:], ps[OB][:, :], w[:], ALU.mult, [pk(OB), ('WK', 1)], [('WK', 2)])
            TT('gpsimd', acc[:], acc[:], w2[:], ALU.add, [akey, ('WK', 2)], [akey])

    for s in range(NSEQ):
        P.dma('sync', gpre[:], npre_d, reads=(), writes=['gpre'])
        for tt in range(NT):
            b = tt % 2
            P.dma('sync', xt[b][:], x_d[s, tt * 128:(tt + 1) * 128, :], reads=(), writes=[('xt', b)])
            TT('vector', xn[b][:], xt[b][:], xt[b][:], ALU.mult, [('xt', b)], [('xn', b)])
            P.op('vector', (lambda bb: lambda e: e.reduce_sum(out=ssq[:, bb:bb + 1], in_=xn[bb][:], axis=AX.X))(b),
                 [('xn', b)], [('ssq', b)])
            ACT(ssq[:, b:b + 1], ssq[:, b:b + 1], AF.Ln, [('ssq', b), 'eps'], [('ssq', b)], bias=eps_t[:, 0:1],
                scale=1.0 / D)
            ACT(ssq[:, b:b + 1], ssq[:, b:b + 1], AF.Exp, [('ssq', b)], [('ssq', b)], scale=-0.5)
            STT(xn[b][:], xt[b][:], ssq[:, b:b + 1], gpre[:], ALU.mult, ALU.mult, [('xt', b), ('ssq', b), 'gpre'],
                [('xn', b)])
            for half in range(2):
                bk = misc_bank()
                for cc in range(4):
                    c = half * 4 + cc
                    TR(ps[bk][:, cc * 128:(cc + 1) * 128], xn[b][:, c * 128:(c + 1) * 128], cs['ident_f'][:],
                       [('xn', b), 'c_ident_f'], [pk(bk)])
                CP('vector' if half == 0 else 'gpsimd' if False else 'vector',
                   uT[:, half * 4:(half + 1) * 4, tt * 128:(tt + 1) * 128],
                   ps[bk][:, :].rearrange("p (c t) -> p c t", c=4), [pk(bk)], [('uT', tt // 4)])
        if 'uT' in dbg_d and s == 0:
            CP('vector', WK[0][:], uT[:, 0, 0:512], [('uT', 0)], [('WK', 0)])
            dump('uT', WK[0][:], [('WK', 0)])

        for g in range(2):
            wj = next_wst()
            load_w(wst[wj][:, :, 0:64], C_KV + 0 * 128 + g * 64, 64, ('wst', wj))
            load_w(wst[wj][:, :, 64:128], C_KV + 1 * 128 + g * 64, 64, ('wst', wj))
            for blk in range(NQT):
                bk = misc_bank()
                proj_fm(wst[wj], ('wst', wj), 0, 128, blk, bk)
                CP('vector', kT1[:, blk * 512:(blk + 1) * 512], ps[bk][:, :], [pk(bk)], ['kT1'])
            W1 = Vp[:].rearrange("p (l e) -> p l e", e=128)[:, 0:32, :]
            P.dma('gpsimd', W1, w1s_d, reads=(), writes=['Vp'])
            ncm = (S - 32) // 16 + 1
            for wh in range(2):
                r0 = 64 * wh
                bk = misc_bank()
                for l in range(32):
                    MM(ps[bk][:, 0:ncm], W1[r0:r0 + 64, l, :], kT1[r0:r0 + 64, l:l + 16 * (ncm - 1) + 1:16],
                       l == 0, l == 31, ['Vp', 'kT1'], [pk(bk)])
                bk2 = misc_bank()
                for l in range(32):
                    MM(ps[bk2][:, 0:1], W1[r0:r0 + 64, l, :], pet[r0:r0 + 64, l:l + 1], l == 0, l == 31,
                       ['Vp', 'pet'], [pk(bk2)])
                CP('vector', hb[:, 0:1], ps[bk2][:, 0:1], [pk(bk2)], ['hb'])
                TS('vector', hb[:, 1:2], hb[:, 0:1], -1.0, None, ALU.mult, None, ['hb'], ['hb'])
                w = WK[0]
                ACT(w[:, 0:ncm], ps[bk][:, 0:ncm], AF.Exp, [pk(bk), 'hb'], [('WK', 0)], bias=hb[:, 1:2], scale=-1.0)
                TS('gpsimd', w[:, 0:ncm], w[:, 0:ncm], 1.0, None, ALU.add, None, [('WK', 0)], [('WK', 0)])
                RCP('vector', w[:, 0:ncm], w[:, 0:ncm], [('WK', 0)], [('WK', 0)])
                STT(hcm[:, wh, 0:ncm], ps[bk][:, 0:ncm], hb[:, 0:1], w[:, 0:ncm], ALU.add, ALU.mult,
                    [pk(bk), 'hb', ('WK', 0)], ['hcm'])
                if wh == 0:
                    bk3 = misc_bank()
                    MM(ps[bk3][0:64, 0:256], w2s[:, 0, :], hcm[:, 0, :], True, True, ['w2s', 'hcm'], [pk(bk3)])
                    CP('vector', kcT[0:64, :], ps[bk3][0:64, 0:256], [pk(bk3)], ['kcT'])
                else:
                    bk3 = misc_bank()
                    for a in range(2):
                        MM(ps[bk3][:, a * 64:(a + 1) * 64], hcm[:, 1, a * 128:(a + 1) * 128], w2s[:, 1, :], True, True,
                           ['w2s', 'hcm'], [pk(bk3)])
                    CP('vector', vc[:], ps[bk3][:, 0:128].rearrange("p (a d) -> p a d", a=2), [pk(bk3)], ['vc'])
            wj = next_wst()
            load_w(wst[wj][:, :, 0:64], C_KV + 2 * 128 + g * 64, 64, ('wst', wj))
            load_w(wst[wj][:, :, 64:128], C_KV + 4 * 128 + g * 64, 64, ('wst', wj))
            for blk in range(NQT):
                bk = misc_bank()
                proj_fm(wst[wj], ('wst', wj), 0, 128, blk, bk)
                CP('vector', kT0[0:64, blk * 512:(blk + 1) * 512], ps[bk][0:64, :], [pk(bk)], ['kT0'])
                CP('vector', kT1[0:64, blk * 512:(blk + 1) * 512], ps[bk][64:128, :], [pk(bk)], ['kT1'])
            MS('vector', kT0[64:65, :], 1.0, ['kT0'])
            MS('vector', kT1[64:65, :], 1.0, ['kT1'])
            wj = next_wst()
            load_w(wst[wj][:, :, 0:64], C_KV + 3 * 128 + g * 64, 64, ('wst', wj))
            load_w(wst[wj][:, :, 64:128], C_KV + 5 * 128 + g * 64, 64, ('wst', wj))
            for t4 in range(NT // 4):
                bk = misc_bank()
                for q4 in range(4):
                    tt = t4 * 4 + q4
                    for c in range(8):
                        MM(ps[bk][:, q4 * 128:(q4 + 1) * 128], uT[:, c, tt * 128:(tt + 1) * 128], wst[wj][:, c, :],
                           c == 0, c == 7, [('wst', wj), ('uT', t4)], [pk(bk)])
                CP('vector', Vp3[:, t4 * 4:(t4 + 1) * 4, :], ps[bk][:, :].rearrange("p (t c) -> p t c", c=128),
                   [pk(bk)], ['Vp'])
            load_w(WQZ[:, :, 0:256], C_QA + g * 256, 256, 'WQZ')
            load_w(WQZ[:, :, 256:512], C_ZA + g * 256, 256, 'WQZ')
            for h4 in range(4):
                for b2 in range(2):
                    P.dma('sync', QA[b2][h4][64:65, :], cd['cq'][4 * g + h4:4 * g + h4 + 1, :], reads=(),
                          writes=[('QA', b2, h4)])
            for i in range(NQT):
                b2 = i % 2
                for pp in range(2):
                    bk = misc_bank()
                    proj_fm(WQZ, 'WQZ', pp * 128, 128, i, bk)
                    CP('vector', QA[b2][2 * pp][0:64, :], ps[bk][0:64, :], [pk(bk)], [('QA', b2, 2 * pp)])
                    CP('vector', QA[b2][2 * pp + 1][0:64, :], ps[bk][64:128, :], [pk(bk)], [('QA', b2, 2 * pp + 1)])
                for pp in range(2):
                    bk = misc_bank()
                    proj_fm(WQZ, 'WQZ', 256 + pp * 128, 128, i, bk)
                    silu_pair(bk, SZ[pp], ('xn', pp))
                bk = misc_bank()
                for c in range(8):
                    MM(ps[bk][0:24, :], wg24[:, c, :], uT[:, c, i * 512:(i + 1) * 512], c == 0, c == 7,
                       ['wg24', ('uT', i)], [pk(bk)])
                w = WK[0]
                ACT(w[0:24, :], ps[bk][0:24, :], AF.Exp, [pk(bk)], [('WK', 0)], scale=-1.0)
                TS('gpsimd', w[0:24, :], w[0:24, :], 1.0, None, ALU.add, None, [('WK', 0)], [('WK', 0)])
                RCP('vector', w[0:24, :], w[0:24, :], [('WK', 0)], [('WK', 0)])
                CP('vector', SGT[0:24, :], w[0:24, :], [('WK', 0)], ['SGT'])
                TT('vector', SGT[32:56, :], w[0:24, :], SGT[0:24, :], ALU.subtract, [('WK', 0), 'SGT'], ['SGT'])
                a_list = [0] if (512 * i + 511) < (2048 + 31) else [0, 1]
                a_list = [a for a in a_list if a * 128 < ncm]
                for a in a_list:
                    thr = 512.0 * i - 2048.0 * a
                    TS('vector', XM[a][:], cs['xg'][:], thr, NEG, ALU.is_gt, ALU.mult, ['c_xg'], [('XM', a)])
                nimp = 0
                tot_imp = 4 * len(a_list)
                for pp in range(2):
                    lbank = [LB, X1]
                    for hl in range(2):
                        h4 = 2 * pp + hl
                        ha = 4 * g + h4
                        for ai, a in enumerate(a_list):
                            sb_ = ST[rr['st'] % 3]
                            rr['st'] += 1
                            pj = hl * 2 + a
                            MM(ps[sb_][:, :], kcT[0:65, a * 128:(a + 1) * 128], QA[b2][h4][0:65, :], True, False,
                               ['kcT', ('QA', b2, h4)], [pk(sb_)])
                            MM(ps[sb_][:, :], cs['ident_b'][:], XM[a][:], False, True, ['c_ident_b', ('XM', a)],
                               [pk(sb_)])
                            ACT(PT[pj][:], ps[sb_][:, :], AF.Exp, [pk(sb_)] + CK, [('PT', pj)],
                                bias=cs['clb'][:, ha, a, i:i + 1], scale=SCALE)
                            la = len(a_list)
                            MM(ps[lbank[hl]][:, :], ones_b[:], PT[pj][:], ai == 0, ai == la - 1, ['ones', ('PT', pj)],
                               [pk(lbank[hl])])
                            MM(ps[OB][64 * hl:64 * hl + 64, :], vc[:, a, :], PT[pj][:], ai == 0, ai == la - 1,
                               ['vc', ('PT', pj)], [pk(OB)], tp=(0, 64 * hl))
                        wr = WK[3 + hl]
                        TS('vector', wr[:], ps[lbank[hl]][:, :], EPSL, None, ALU.max, None, [pk(lbank[hl])],
                           [('WK', 3 + hl)])
                        RCP('vector', wr[:], wr[:], [('WK', 3 + hl)], [('WK', 3 + hl)])
                        for ai, a in enumerate(a_list):
                            pj = hl * 2 + a
                            TT('gpsimd', PT[pj][:], PT[pj][:], wr[:], ALU.mult, [('PT', pj), ('WK', 3 + hl)],
                               [('PT', pj)])
                            MM(ps[X2][0:64, :], cs['ov'][:, a, :], PT[pj][:], nimp == 0, nimp == tot_imp - 1,
                               ['c_ov', ('PT', pj)], [pk(X2)])
                            nimp += 1
                    gb = X0
                    MM(ps[gb][:, :], cs['goh'][0:56, (g * 2 + pp) * 3 + 0, :], SGT[0:56, :], True, True,
                       ['c_goh', 'SGT'], [pk(gb)])
                    w = WK[1]
                    TT('vector', w[0:64, :], ps[gb][0:64, :], WK[3][0:64, :], ALU.mult, [pk(gb), ('WK', 3)], [('WK', 1)])
                    TT('vector', w[64:128, :], ps[gb][64:128, :], WK[4][64:128, :], ALU.mult, [pk(gb), ('WK', 4)],
                       [('WK', 1)])
                    TT('vector', ACC[pp][:], ps[OB][:, :], w[:], ALU.mult, [pk(OB), ('WK', 1)], [('xt', pp)])
                w = WK[0]
                CP('vector', w[0:64, :], ps[X2][0:64, :], [pk(X2)], [('WK', 0)])
                bk = X0
                for qs in range(4):
                    TR(ps[bk][:, qs * 64:(qs + 1) * 64], w[0:64, qs * 128:(qs + 1) * 128], cs['ident_f'][0:64, 0:64],
                       [('WK', 0), 'c_ident_f'], [pk(bk)])
                for qs in range(4):
                    ti = 4 * i + qs
                    TT('vector', T1[:, qs, :], ps[bk][:, qs * 64:(qs + 1) * 64],
                       cs['abase'][:, 64 - 2 * ti:128 - 2 * ti], ALU.add, [pk(bk), 'c_abase'], ['T1'])
                MS('vector', T1[:, :, 0:1], BIGF, ['T1'])
                for qs in range(4):
                    P.op('vector', (lambda q_: lambda e: e.max(out=M8a[:], in_=T1[:, q_, :]))(qs), ['T1'], ['M8a'])
                    P.op('vector', (lambda q_: lambda e: e.match_replace(out=T2[:, q_, :], in_to_replace=M8a[:],
                                                                        in_values=T1[:, q_, :], imm_value=-1e9))(qs),
                         ['T1', 'M8a'], ['T2'])
                    P.op('vector', (lambda q_: lambda e: e.max(out=M8b[:], in_=T2[:, q_, :]))(qs), ['T2'], ['M8b'])
                    TS('vector', T2[:, qs, :], T1[:, qs, :], M8b[:, 7:8], NEG, ALU.is_lt, ALU.mult, ['T1', 'M8b'], ['T2'])
                bk = X1
                for qs in range(4):
                    TR(ps[bk][0:64, qs * 128:(qs + 1) * 128], T2[:, qs, :], cs['ident_f'][:], ['T2', 'c_ident_f'],
                       [pk(bk)])
                CP('vector', selT[0:64, :], ps[bk][0:64, :], [pk(bk)], ['selT'])
                for pp in range(2):
                    for br in (1, 2):
                        tiles = []
                        if br == 1:
                            kts = list(range(0, 4 * i + 4))
                        else:
                            kts = [kt for kt in range(4 * i - 4, 4 * i + 4) if kt >= 0]
                        nk = len(kts)
                        for ki, kt in enumerate(kts):
                            for hl in range(2):
                                h4 = 2 * pp + hl
                                ha = 4 * g + h4
                                kT = kT0 if br == 1 else kT1
                                kkey = 'kT0' if br == 1 else 'kT1'
                                smm = [(kT[0:65, kt * 128:(kt + 1) * 128], QA[b2][h4][0:65, :], [kkey, ('QA', b2, h4)])]
                                if br == 1:
                                    smm.append((cs['e128'][:, kt * 128:(kt + 1) * 128], selT[:, :], ['c_e128', 'selT']))
                                    if kt >= 4 * i:
                                        r = kt - 4 * i
                                        smm.append((cs['ident_b'][:], cs['cmb'][:, 384 - 128 * r:896 - 128 * r],
                                                    ['c_ident_b', 'c_cmb']))
                                else:
                                    r = kt - (4 * i - 4)
                                    if r < 4:
                                        smm.append((cs['ident_b'][:], cs['omb'][:, 384 - 128 * r:896 - 128 * r],
                                                    ['c_ident_b', 'c_omb']))
                                    else:
                                        r -= 4
                                        smm.append((cs['ident_b'][:], cs['cmb'][:, 384 - 128 * r:896 - 128 * r],
                                                    ['c_ident_b', 'c_cmb']))
                                vcol = 0 if br == 1 else 64
                                pv = [(ps[OB][64 * hl:64 * hl + 64, :], Vp3[:, kt, vcol:vcol + 64], (0, 64 * hl),
                                       ki == 0, ki == nk - 1, ['Vp'], pk(OB)),
                                      (ps[LB][64 * hl:64 * hl + 64, :], ones_b[:, 0:64], (0, 64 * hl),
                                       ki == 0, ki == nk - 1, ['ones'], pk(LB))]
                                tiles.append(dict(smm=smm, bias=cs['alb'][:, ha, kt - 4 * i + 32:kt - 4 * i + 33], pv=pv))
                        attend(tiles)
                        gb = X0
                        MM(ps[gb][:, :], cs['goh'][0:56, (g * 2 + pp) * 3 + br, :], SGT[0:56, :], True, True,
                           ['c_goh', 'SGT'], [pk(gb)])
                        combine_branch(ACC[pp], ('xt', pp), False, gb, [(0, 128, LB)])
                    yj = rr['wk'] % 2
                    rr['wk'] += 1
                    TT('vector', YST[yj][:], ACC[pp][:], SZ[pp][:], ALU.mult, [('xt', pp), ('xn', pp)], [('YST', yj)])
                    P.dma('sync', ysc_d[g * 2 + pp, :, i * 512:(i + 1) * 512], YST[yj][:], reads=[('YST', yj)],
                          writes=[('ysc', g * 2 + pp, i)])

        for j in range(4):
            wj = next_wst()
            load_w(wst[wj][:], C_QB + 512 + j * 128, 128, ('wst', wj))
            for blk in range(NQT):
                bk = misc_bank()
                proj_fm(wst[wj], ('wst', wj), 0, 128, blk, bk)
                CP('vector', kT0[0:64, blk * 512:(blk + 1) * 512], ps[bk][0:64, :], [pk(bk)], ['kT0'])
                CP('vector', kT1[0:64, blk * 512:(blk + 1) * 512], ps[bk][64:128, :], [pk(bk)], ['kT1'])
                P.op('vector', (lambda bk_, blk_: lambda e: e.reduce_sum(
                    out=kbarf[:, 2 * blk_:2 * blk_ + 2], in_=ps[bk_][:, :].rearrange("p (b t) -> p b t", b=2),
                    axis=AX.X))(bk, blk), [pk(bk)], ['kbarf'])
            MS('vector', kT0[64:65, :], 1.0, ['kT0'])
            MS('vector', kT1[64:65, :], 1.0, ['kT1'])
            nb = S // 256
            TS('vector', kbar[0:64, 0, 0:nb], kbarf[0:64, 0:nb], 1.0 / 256, None, ALU.mult, None, ['kbarf'], ['kbar'])
            TS('vector', kbar[0:64, 1, 0:nb], kbarf[64:128, 0:nb], 1.0 / 256, None, ALU.mult, None, ['kbarf'], ['kbar'])
            wj = next_wst()
            load_w(wst[wj][:], C_QB + 1024 + j * 128, 128, ('wst', wj))
            for t4 in range(NT // 4):
                bk = misc_bank()
                for q4 in range(4):
                    tt = t4 * 4 + q4
                    for c in range(8):
                        MM(ps[bk][:, q4 * 128:(q4 + 1) * 128], uT[:, c, tt * 128:(tt + 1) * 128], wst[wj][:, c, :],
                           c == 0, c == 7, [('wst', wj), ('uT', t4)], [pk(bk)])
                CP('vector', Vp3[:, t4 * 4:(t4 + 1) * 4, :], ps[bk][:, :].rearrange("p (t c) -> p t c", c=128),
                   [pk(bk)], ['Vp'])
            load_w(WQZ[:, :, 0:128], C_QB + j * 128, 128, 'WQZ')
            load_w(WQZ[:, :, 128:256], C_ZB + j * 128, 128, 'WQZ')
            for hl in range(2):
                for b2 in range(2):
                    P.dma('sync', QA[b2][hl][64:65, :], cd['cq'][8 + 2 * j + hl:8 + 2 * j + hl + 1, :], reads=(),
                          writes=[('QA', b2, hl)])
            for i in range(NQT):
                b2 = i % 2
                bk = misc_bank()
                proj_fm(WQZ, 'WQZ', 0, 128, i, bk)
                CP('vector', QA[b2][0][0:64, :], ps[bk][0:64, :], [pk(bk)], [('QA', b2, 0)])
                CP('vector', QA[b2][1][0:64, :], ps[bk][64:128, :], [pk(bk)], [('QA', b2, 1)])
                bk = misc_bank()
                proj_fm(WQZ, 'WQZ', 128, 128, i, bk)
                silu_pair(bk, SZ[0], ('xn', 0))
                bk = misc_bank()
                for hl in range(2):
                    for qs in range(4):
                        MM(ps[bk][:, (hl * 4 + qs) * 16:(hl * 4 + qs) * 16 + nb], QA[b2][hl][0:64, qs * 128:(qs + 1) * 128],
                           kbar[0:64, hl, 0:nb], True, True, ['kbar', ('QA', b2, hl)], [pk(bk)])
                for hl in range(2):
                    for qs in range(4):
                        cur = (4 * i + qs) // 2
                        e8 = hl * 4 + qs
                        TT('vector', GT[:, e8, 0:nb], ps[bk][:, e8 * 16:e8 * 16 + nb], cs['bbase'][:, 16 - cur:16 - cur + nb],
                           ALU.add, [pk(bk), 'c_bbase'], ['GT'])
                        if nb < 16:
                            MS('vector', GT[:, e8, nb:16], -BIGF, ['GT'])
                        P.op('vector', (lambda e_: lambda e: e.max(out=M8a[:], in_=GT[:, e_, :]))(e8), ['GT'], ['M8a'])
                        TS('vector', GT[:, e8, :], GT[:, e8, :], M8a[:, 2:3], NEG, ALU.is_lt, ALU.mult, ['GT', 'M8a'], ['GT'])
                        TT('vector', GT[:, e8, :], GT[:, e8, :], cs['obase'][:, 16 - cur:32 - cur], ALU.max,
                           ['GT', 'c_obase'], ['GT'])
                for hl in range(2):
                    bk = misc_bank()
                    for qs in range(4):
                        TR(ps[bk][0:16, qs * 128:(qs + 1) * 128], GT[:, hl * 4 + qs, :], cs['ident_f'][:],
                           ['GT', 'c_ident_f'], [pk(bk)])
                    CP('vector', selTm[hl][0:16, :], ps[bk][0:16, :], [pk(bk)], [('selTm', hl)])
                tiles = []
                kts = list(range(0, 4 * i + 4))
                nk = len(kts)
                for ki, kt in enumerate(kts):
                    for hl in range(2):
                        hh = 8 + 2 * j + hl
                        kT = kT0 if hl == 0 else kT1
                        kkey = 'kT0' if hl == 0 else 'kT1'
                        smm = [(kT[0:65, kt * 128:(kt + 1) * 128], QA[b2][hl][0:65, :], [kkey, ('QA', b2, hl)]),
                               (cs['ohm'][:, kt // 2, :], selTm[hl][:, :], ['c_ohm', ('selTm', hl)])]
                        if kt >= 4 * i:
                            smm.append((cs['ident_b'][:], cs['mmb'][:, kt - 4 * i, :], ['c_ident_b', 'c_mmb']))
                        pv = [(ps[OB][64 * hl:64 * hl + 64, :], Vp3[:, kt, 64 * hl:64 * hl + 64], (0, 64 * hl),
                               ki == 0, ki == nk - 1, ['Vp'], pk(OB)),
                              (ps[LB][64 * hl:64 * hl + 64, :], ones_b[:, 0:64], (0, 64 * hl),
                               ki == 0, ki == nk - 1, ['ones'], pk(LB))]
                        tiles.append(dict(smm=smm, bias=cs['alb'][:, hh, kt - 4 * i + 32:kt - 4 * i + 33], pv=pv))
                attend(tiles)
                combine_branch(ACC[0], ('xt', 0), True, None, [(0, 128, LB)])
                yj = rr['wk'] % 2
                rr['wk'] += 1
                TT('vector', YST[yj][:], ACC[0][:], SZ[0][:], ALU.mult, [('xt', 0), ('xn', 0)], [('YST', yj)])
                P.dma('sync', ysc_d[4 + j, :, i * 512:(i + 1) * 512], YST[yj][:], reads=[('YST', yj)],
                      writes=[('ysc', 4 + j, i)])

        P.op('sync', lambda e: e.nop(), (), ['Vp', ('wgs', 0), ('wgs', 1)])
        P.dma('sync', gpost[:], npost_d, reads=(), writes=['gpre'])
        for i in range(NQT):
            P.dma('sync', ysl[:, :, 0:512], ysc_d[:, :, i * 512:(i + 1) * 512].rearrange("c p t -> p c t"),
                  reads=[('ysc', jj, i) for jj in range(8)], writes=['kT1'])
            for fc in range(8):
                gj = fc % 2
                P.dma('sync', wgs[:, gj, :, 0:128], wcols[:, :, C_MG + fc * 128:C_MG + (fc + 1) * 128], reads=WBK,
                      writes=[('wgs', gj)])
                P.dma('sync', wgs[:, gj, :, 128:256], wcols[:, :, C_MG + 1024 + fc * 128:C_MG + 1024 + (fc + 1) * 128],
                      reads=WBK, writes=[('wgs', gj)])
                for ab in range(2):
                    P.dma('sync', wab[:, gj, 4 * ab:4 * ab + 4, :],
                          wabf_d[ab].rearrange("(j p) n -> p j n", p=128)[:, :, fc * 128:(fc + 1) * 128],
                          reads=['wabf0', 'wabf1'], writes=[('wab', gj)])
                for ab in range(2):
                    bg = misc_bank()
                    for c in range(8):
                        MM(ps[bg][:, :], wgs[:, gj, c, ab * 128:(ab + 1) * 128], uT[:, c, i * 512:(i + 1) * 512],
                           c == 0, c == 7, [('wgs', gj), ('uT', i)], [pk(bg)])
                    w = WK[ab]
                    ACT(w[:], ps[bg][:, :], AF.Exp, [pk(bg)], [('WK', ab)], scale=-1.0)
                    TS('gpsimd', w[:], w[:], 1.0, None, ALU.add, None, [('WK', ab)], [('WK', ab)])
                    RCP('vector', w[:], w[:], [('WK', ab)], [('WK', ab)])
                    bm = OB if ab == 0 else LB
                    for jj in range(4):
                        MM(ps[bm][:, :], wab[:, gj, 4 * ab + jj, :], ysl[:, 4 * ab + jj, 0:512], jj == 0, jj == 3,
                           [('wab', gj), 'kT1'], [pk(bm)])
                    TT('vector', w[:], ps[bm][:, :], w[:], ALU.mult, [pk(bm), ('WK', ab)], [('WK', ab)])
                TT('gpsimd', mT[:, fc, 0:512], WK[0][:], WK[1][:], ALU.add, [('WK', 0), ('WK', 1)], ['kT0'])
            for ts_ in range(4):
                tt = i * 4 + ts_
                b = tt % 2
                P.dma('sync', xt[b][:], x_d[s, tt * 128:(tt + 1) * 128, :], reads=(), writes=[('xt', b)])
                banks = [ST[0], ST[1]]
                for hf in range(2):
                    for fc in range(8):
                        MM(ps[banks[hf]][:, :], mT[:, fc, ts_ * 128:(ts_ + 1) * 128], wo_s[:, fc, hf * 512:(hf + 1) * 512],
                           fc == 0, fc == 7, ['wo', 'kT0'], [pk(banks[hf])])
                for hf in range(2):
                    CP('vector', WK[2 + hf][:], ps[banks[hf]][:, :], [pk(banks[hf])], [('WK', 2 + hf)])
                    TT('gpsimd', xn[b][:, hf * 512:(hf + 1) * 512], WK[2 + hf][:], WK[2 + hf][:], ALU.mult,
                       [('WK', 2 + hf)], [('xn', b)])
                P.op('vector', (lambda bb: lambda e: e.reduce_sum(out=ssq[:, bb:bb + 1], in_=xn[bb][:], axis=AX.X))(b),
                     [('xn', b)], [('ssq', b)])
                ACT(ssq[:, b:b + 1], ssq[:, b:b + 1], AF.Ln, [('ssq', b), 'eps'], [('ssq', b)], bias=eps_t[:, 0:1],
                    scale=1.0 / D)
                ACT(ssq[:, b:b + 1], ssq[:, b:b + 1], AF.Exp, [('ssq', b)], [('ssq', b)], scale=-0.5)
                for hf in range(2):
                    STT(xn[b][:, hf * 512:(hf + 1) * 512], WK[2 + hf][:], ssq[:, b:b + 1],
                        gpost[:, hf * 512:(hf + 1) * 512], ALU.mult, ALU.mult, [('WK', 2 + hf), ('ssq', b), 'gpre'],
                        [('xn', b)])
                TT('gpsimd', xn[b][:], xn[b][:], xt[b][:], ALU.add, [('xn', b), ('xt', b)], [('xn', b)])
                P.dma('sync', out_d[s, tt * 128:(tt + 1) * 128, :], xn[b][:], reads=[('xn', b)], writes=[('out', tt)])
        P.op('sync', lambda e: e.nop(), (), ['Vp', ('wgs', 0), ('wgs', 1)])
    P.finish()
    return nc, P


def host_inputs(inputs, S):
    c = make_consts()
    f = lambda a: np.ascontiguousarray(np.asarray(a, dtype=np.float32))
    w1k = f(inputs['cmp_w1_k'])[0].transpose(1, 0, 2)
    w1v = f(inputs['cmp_w1_v'])[0].transpose(1, 0, 2)
    shared = {
        'w_in': f(inputs['w_in'])[0],
        'w1s': np.ascontiguousarray(np.concatenate([w1k, w1v], 0)),
        'w2s': np.ascontiguousarray(np.stack([f(inputs['cmp_w2_k'])[0], f(inputs['cmp_w2_v'])[0]], 1)),
        'pet': np.ascontiguousarray(np.concatenate([f(inputs['cmp_pe_k'])[0].T, f(inputs['cmp_pe_v'])[0].T], 0)),
        'w_a': f(inputs['w_branch_a'])[0],
        'w_b': f(inputs['w_branch_b'])[0],
        'w_o': f(inputs['w_o'])[0],
        'npre_bc': np.ascontiguousarray(np.broadcast_to(f(inputs['norm_pre'])[0][None, :], (128, D))),
        'npost_bc': np.ascontiguousarray(np.broadcast_to(f(inputs['norm_post'])[0][None, :], (128, D))),
    }
    for k, v in c.items():
        shared['c_' + k] = v
    return shared


def kernel(**inputs):
    x = np.ascontiguousarray(np.asarray(inputs['x'], dtype=np.float32))
    B, S, _ = x.shape
    ncores = 8
    nseq = B // ncores
    nc, _ = build(S, nseq)
    shared = host_inputs(inputs, S)
    in_maps = []
    for c in range(ncores):
        m = dict(shared)
        m['x'] = np.ascontiguousarray(x[c * nseq:(c + 1) * nseq])
        in_maps.append(m)
    res = run_bass_kernel_spmd(nc, in_maps, core_ids=list(range(ncores)))
    return np.concatenate([np.asarray(r['out']) for r in res.results], axis=0).astype(np.float32)
```

```python
import contextlib
import numpy as np
import ml_dtypes
import concourse.bass as bass
import concourse.mybir as mybir
from concourse.bass_utils import run_bass_kernel_spmd

F32 = mybir.dt.float32
BF16 = mybir.dt.bfloat16
ALU = mybir.AluOpType
AF = mybir.ActivationFunctionType
AX = mybir.AxisListType
NPBF = ml_dtypes.bfloat16

D = 1024
NCOL = 5912
NEG = -30000.0
SCALE = 0.125
BIGF = 1.0e4
EPSL = 1.0e-30
C_QA, C_KV, C_GA, C_ZA, C_QB, C_ZB, C_MG = 0, 512, 1280, 1304, 1816, 3352, 3864
SLOPES = [2.0 ** (-(i + 1) / 2.0) for i in range(16)]
SL_HH = [SLOPES[2 * h] for h in range(8)] + [SLOPES[2 * h + 1] for h in range(8)]


class Prog:
    EPOCH = 20000
    NDMA = 8

    def __init__(self, nc):
        self.nc = nc
        self.ops = []
        self.lastw = {}
        self.readers = {}
        self.stack = contextlib.ExitStack()
        self.sb_bytes = 0

    def sb(self, name, shape, dtype):
        n = 1
        for s in shape[1:]:
            n *= s
        self.sb_bytes += n * (4 if dtype == F32 else 2)
        return self.stack.enter_context(self.nc.sbuf_tensor('sb_' + name, list(shape), dtype))

    def ps(self, name, shape=(128, 512), dtype=F32):
        return self.stack.enter_context(self.nc.psum_tensor(name, list(shape), dtype))

    def _deps(self, idx, reads, writes):
        deps = set()
        for k in reads:
            w = self.lastw.get(k)
            if w is not None:
                deps.add(w)
        for k in writes:
            w = self.lastw.get(k)
            if w is not None:
                deps.add(w)
            for r in self.readers.get(k, ()):
                deps.add(r)
        for k in reads:
            self.readers.setdefault(k, []).append(idx)
        for k in writes:
            self.lastw[k] = idx
            self.readers[k] = []
        deps.discard(idx)
        return deps

    def op(self, eng, fn, reads=(), writes=()):
        idx = len(self.ops)
        deps = self._deps(idx, reads, writes)
        self.ops.append(dict(eng=eng, fn=fn, deps=deps, dma=False, wkeys=set(writes), rkeys=set(reads)))
        return idx

    def dma(self, eng, out, in_, reads=(), writes=()):
        idx = len(self.ops)
        deps = self._deps(idx, reads, writes)
        self.ops.append(dict(eng=eng, fn=lambda e: e.dma_start(out=out, in_=in_), deps=deps, dma=True,
                             wkeys=set(writes), rkeys=set(reads)))
        return idx

    def finish(self):
        nc = self.nc
        ops = self.ops
        needed = set()
        for i, o in enumerate(ops):
            nd = set()
            for d in o['deps']:
                od = ops[d]
                if od['eng'] == o['eng'] and not od['dma'] and not o['dma']:
                    if o['eng'] == 'tensor':
                        continue
                    if not ((od['wkeys'] & o['rkeys']) or (od['wkeys'] & o['wkeys'])):
                        continue
                nd.add(d)
            o['deps'] = nd
            for d in nd:
                if not ops[d]['dma']:
                    needed.add(d)
        engs = ['tensor', 'vector', 'scalar', 'gpsimd', 'sync']
        cnt = {e: 0 for e in engs}
        dcnt = {e: 0 for e in engs}
        nep = {e: 0 for e in engs}
        for i, o in enumerate(ops):
            e = o['eng']
            if o['dma']:
                d = dcnt[e]
                dcnt[e] += 1
                o['sig'] = (('dma', e, d % self.NDMA), 16 * (d // self.NDMA + 1), 16)
                o['prev'] = (('dma', e, d % self.NDMA), 16 * (d // self.NDMA)) if d >= self.NDMA else None
            elif i in needed:
                c = cnt[e]
                cnt[e] += 1
                ep = c // self.EPOCH
                nep[e] = max(nep[e], ep + 1)
                o['sig'] = (('cmp', e, ep), c % self.EPOCH + 1, 1)
            else:
                o['sig'] = None
        sems = {}
        for e in engs:
            for ep in range(nep[e]):
                sems[('cmp', e, ep)] = self.stack.enter_context(nc.semaphore(f"s_{e}_{ep}"))
            for j in range(min(self.NDMA, dcnt[e])):
                sems[('dma', e, j)] = self.stack.enter_context(nc.semaphore(f"d_{e}_{j}"))
        self.n_instr = {e: 0 for e in engs}
        with nc.Block() as block:
            def make(ename):
                def body(eng):
                    waited = {}
                    lastdma = {}
                    for o in ops:
                        if o['eng'] != ename:
                            continue
                        best = {}
                        for d in o['deps']:
                            s = ops[d]['sig']
                            if s[1] > best.get(s[0], 0):
                                best[s[0]] = s[1]
                        if o['dma'] and o['prev'] is not None:
                            k, v = o['prev']
                            if v > best.get(k, 0):
                                best[k] = v
                        for k, v in best.items():
                            if waited.get(k, 0) >= v:
                                continue
                            eng.wait_ge(sems[k], v)
                            waited[k] = v
                            self.n_instr[ename] += 1
                        ins = o['fn'](eng)
                        self.n_instr[ename] += 1
                        if o['sig'] is not None:
                            ins.then_inc(sems[o['sig'][0]], o['sig'][2])
                            if o['dma']:
                                lastdma[o['sig'][0]] = o['sig'][1]
                    for k, v in lastdma.items():
                        if waited.get(k, 0) < v:
                            eng.wait_ge(sems[k], v)
                return body
            for ename in engs:
                if any(o['eng'] == ename for o in ops):
                    getattr(block, ename)(make(ename))
        self.stack.close()


def make_consts():
    c = {}
    c['ident_f'] = np.eye(128, dtype=np.float32)
    c['ident_b'] = np.eye(128).astype(NPBF)
    p = np.arange(128)[:, None]
    fp = np.arange(896)[None, :]
    c['cmb'] = np.where(p > fp - 384, NEG, 0.0).astype(NPBF)
    c['omb'] = np.where(fp - 384 >= p, NEG, 0.0).astype(NPBF)
    kk = np.arange(4)[None, :, None] * 128 + np.arange(128)[:, None, None]
    f = np.arange(512)[None, None, :]
    mm = np.where(kk // 256 == f // 256, np.where(kk > f, NEG, 0.0), np.where(f // 256 > kk // 256, 0.0, NEG))
    c['mmb'] = mm.astype(NPBF)
    jj = np.arange(128)[:, None]
    k2 = np.arange(4096)[None, :]
    c['e128'] = ((k2 // 64 == jj) & (jj < 64)).astype(NPBF)
    oh = np.zeros((128, 16, 128), np.float32)
    oh[0:16] = np.broadcast_to(np.eye(16)[:, :, None], (16, 16, 128))
    c['ohm'] = oh.astype(NPBF)
    alb = np.zeros((128, 16, 36), np.float32)
    for hh in range(16):
        for di in range(36):
            alb[:, hh, di] = SL_HH[hh] * (np.arange(128) + 128.0 * (di - 32))
    c['alb'] = alb
    clb = np.zeros((128, 8, 2, 8), np.float32)
    for ha in range(8):
        for a in range(2):
            for i in range(8):
                clb[:, ha, a, i] = SL_HH[ha] * (2048.0 * a + 16.0 * np.arange(128) + 31.0 - 512.0 * i)
    c['clb'] = clb
    cq = np.zeros((16, 512), np.float32)
    for hh in range(16):
        cq[hh] = -SL_HH[hh] * np.arange(512) / SCALE
    c['cq'] = cq.astype(NPBF)
    goh = np.zeros((56, 12, 128), np.float32)
    for g in range(2):
        for pp in range(2):
            for k in range(3):
                idx = (g * 2 + pp) * 3 + k
                ra = k * 8 + g * 4 + 2 * pp
                goh[ra, idx, 0:64] = 1.0
                goh[32 + ra, idx, 0:64] = 1.0
                goh[ra + 1, idx, 64:128] = 1.0
                goh[32 + ra + 1, idx, 64:128] = 1.0
    c['goh'] = goh.astype(NPBF)
    n_cmp, n_slc = 255, 64
    cs = np.arange(n_cmp) * 16
    ce = cs + 31
    ss = np.arange(n_slc) * 64
    se = ss + 63
    ov = ((cs[:, None] <= se[None, :]) & (ce[:, None] >= ss[None, :])).astype(np.float32)
    ovp = np.zeros((256, 64), np.float32)
    ovp[:255] = ov
    c['ov'] = np.ascontiguousarray(ovp.reshape(2, 128, 64).transpose(1, 0, 2)).astype(NPBF)
    q = np.arange(128)[:, None]
    m = np.arange(128)[None, :]
    jr = m - 64
    cr = q // 64
    ab = np.where((jr == cr) | (jr == cr - 1), BIGF, np.where(jr > cr, -BIGF, 0.0))
    c['abase'] = ab.astype(np.float32)
    m2 = np.arange(32)[None, :]
    c['bbase'] = np.broadcast_to(np.where(m2 >= 16, -BIGF, 0.0), (128, 32)).astype(np.float32).copy()
    c['obase'] = np.broadcast_to(np.where(m2 == 16, 0.0, 2 * NEG), (128, 32)).astype(np.float32).copy()
    c['xg'] = (16.0 * np.arange(128)[:, None] + 31.0 - np.arange(512)[None, :]).astype(np.float32)
    return c


CONST_DT = dict(ident_f=F32, ident_b=BF16, cmb=BF16, omb=BF16, mmb=BF16, e128=BF16, ohm=BF16, alb=F32, clb=F32,
                cq=BF16, goh=BF16, ov=BF16, abase=F32, bbase=F32, obase=F32, xg=F32)


def build(S, NSEQ, dbg=()):
    nc = bass.Bass("TRN2", target_bir_lowering=False)
    NT = S // 128
    NQT = S // 512
    consts = make_consts()

    def din(name, shape, dt=F32):
        return nc.dram_tensor(name, list(shape), dt, kind="ExternalInput").ap()

    x_d = din("x", [NSEQ, S, D])
    win_d = din("w_in", [D, NCOL])
    w1s_d = din("w1s", [128, 32, 128])
    w2s_d = din("w2s", [128, 2, 64])
    pet_d = din("pet", [128, 32])
    wa_d = din("w_a", [512, D])
    wb_d = din("w_b", [512, D])
    wo_d = din("w_o", [D, D])
    npre_d = din("npre_bc", [128, D])
    npost_d = din("npost_bc", [128, D])
    cd = {k: din("c_" + k, list(v.shape), CONST_DT[k]) for k, v in consts.items()}
    out_d = nc.dram_tensor("out", [NSEQ, S, D], F32, kind="ExternalOutput").ap()
    wbf_d = nc.dram_tensor("wbf", [D, NCOL], BF16, kind="Internal").ap()
    ysc_d = nc.dram_tensor("yscr", [8, 128, S], BF16, kind="Internal").ap()
    wabf_d = nc.dram_tensor("wabf", [2, 512, D], BF16, kind="Internal").ap()
    dbg_d = {}
    for name, shape in dbg:
        dbg_d[name] = nc.dram_tensor("dbg_" + name, list(shape), F32, kind="ExternalOutput").ap()

    P = Prog(nc)
    cs = {k: P.sb("k_" + k, list(v.shape), CONST_DT[k]) for k, v in consts.items()}
    uT = P.sb("uT", [128, 8, S], BF16)
    SA = max(S, 4096)
    kT0 = P.sb("kT0", [128, SA], BF16)
    kT1 = P.sb("kT1", [128, SA], BF16)
    Vp = P.sb("Vp", [128, SA], BF16)
    kcT = P.sb("kcT", [128, 256], BF16)
    vc = P.sb("vc", [128, 2, 64], BF16)
    hcm = P.sb("hcm", [128, 2, 256], BF16)
    QA = [[P.sb(f"QA{b}_{h}", [65, 512], BF16) for h in range(4)] for b in range(2)]
    PT = [P.sb(f"PT{j}", [128, 512], BF16) for j in range(4)]
    XM = [P.sb(f"XM{j}", [128, 512], BF16) for j in range(2)]
    WK = [P.sb(f"WK{j}", [128, 512], F32) for j in range(5)]
    YST = [P.sb(f"YST{j}", [128, 512], BF16) for j in range(2)]
    SGT = P.sb("SGT", [64, 512], BF16)
    selT = P.sb("selT", [128, 512], BF16)
    selTm = [P.sb(f"selTm{j}", [128, 512], BF16) for j in range(2)]
    T1 = P.sb("T1", [128, 4, 64], F32)
    T2 = P.sb("T2", [128, 4, 64], F32)
    M8a = P.sb("M8a", [128, 8], F32)
    M8b = P.sb("M8b", [128, 8], F32)
    GT = P.sb("GT", [128, 8, 16], F32)
    kbarf = P.sb("kbarf", [128, 16], F32)
    kbar = P.sb("kbar", [64, 2, 16], BF16)
    wst = [P.sb(f"wst{j}", [128, 8, 128], BF16) for j in range(2)]
    WQZ = P.sb("WQZ", [128, 8, 512], BF16)
    wg24 = P.sb("wg24", [128, 8, 24], BF16)
    w2s = P.sb("w2s", [128, 2, 64], BF16)
    pet = P.sb("pet", [128, 32], BF16)
    hb = P.sb("hb", [128, 2], F32)
    xt = [P.sb(f"xt{j}", [128, D], F32) for j in range(2)]
    xn = [P.sb(f"xn{j}", [128, D], F32) for j in range(2)]
    gpre = P.sb("gpre", [128, D], F32)
    gpost = gpre
    ACC = [xt[j][:, 0:512] for j in range(2)]
    SZ = [xn[j][:, 0:512] for j in range(2)]
    ssq = P.sb("ssq", [128, 2], F32)
    ssq2 = P.sb("ssq2", [128, 2], F32)
    eps_t = P.sb("eps_t", [128, 1], F32)
    ones_b = P.sb("ones_b", [128, 128], BF16)
    wab = P.sb("wab", [128, 2, 8, 128], BF16)
    wo_s = P.sb("wo_s", [128, 8, D], BF16)
    ps = [P.ps(f"ps{j}") for j in range(8)]
    ST = [0, 1]
    OLS = [(2, 3), (4, 5)]
    MISC = [6, 7]
    Vp3 = Vp[:].rearrange("p (t c) -> p t c", c=128)
    mT = kT0[:].rearrange("p (c t) -> p c t", c=8)
    ysl = kT1[:].rearrange("p (c t) -> p c t", c=8)
    wgs = Vp[:].rearrange("p (b c n) -> p b c n", b=2, c=8)
    assert SA // 8 >= 512 and SA // 16 >= 256

    rr = {'misc': 0, 'st': 0, 'pt': 0, 'wst': 0, 'wk': 0, 'ol': 0}

    def kt_lo(hh, i):
        lo = 0
        while SL_HH[hh] * (512 * i - 128 * lo - 127) > 110.0:
            lo += 1
        return lo

    def MM(out, lhsT, rhs, start, stop, reads, writes, tp=None):
        if tp is None:
            P.op('tensor', lambda e: e.matmul(out, lhsT=lhsT, rhs=rhs, start=start, stop=stop), reads, writes)
        else:
            P.op('tensor', lambda e: e.matmul(out, lhsT=lhsT, rhs=rhs, start=start, stop=stop, tile_position=tp),
                 reads, writes)

    def TR(out, in_, ident, reads, writes):
        P.op('tensor', lambda e: e.transpose(out, in_, ident), reads, writes)

    def ACT(out, in_, func, reads, writes, bias=None, scale=1.0):
        if bias is None:
            P.op('scalar', lambda e: e.activation(out=out, in_=in_, func=func, scale=scale), reads, writes)
        else:
            P.op('scalar', lambda e: e.activation(out=out, in_=in_, func=func, bias=bias, scale=scale), reads, writes)

    def TT(eng, out, in0, in1, op, reads, writes):
        P.op(eng, lambda e: e.tensor_tensor(out=out, in0=in0, in1=in1, op=op), reads, writes)

    def TS(eng, out, in0, s1, s2, op0, op1, reads, writes):
        if op1 is None:
            P.op(eng, lambda e: e.tensor_scalar(out=out, in0=in0, scalar1=s1, scalar2=None, op0=op0), reads, writes)
        else:
            P.op(eng, lambda e: e.tensor_scalar(out=out, in0=in0, scalar1=s1, scalar2=s2, op0=op0, op1=op1),
                 reads, writes)

    def STT(out, in0, scalar, in1, op0, op1, reads, writes):
        P.op('vector', lambda e: e.scalar_tensor_tensor(out=out, in0=in0, scalar=scalar, in1=in1, op0=op0, op1=op1),
             reads, writes)

    def CP(eng, out, in_, reads, writes):
        P.op(eng, lambda e: e.tensor_copy(out=out, in_=in_), reads, writes)

    def RCP(eng, out, in_, reads, writes):
        P.op(eng, lambda e: e.reciprocal(out=out, in_=in_), reads, writes)

    def MS(eng, ap, val, writes):
        P.op(eng, lambda e: e.memset(ap, val), (), writes)

    def pk(j):
        return ('ps', j)

    def misc_bank():
        j = MISC[rr['misc'] % 2]
        rr['misc'] += 1
        return j

    def next_wst():
        j = rr['wst'] % 2
        rr['wst'] += 1
        return j

    wcols = wbf_d.rearrange("(c p) n -> p c n", p=128)
    WBK = [('wbf', r) for r in range(8)]

    def load_w(dst, col0, ncols, key):
        P.dma('sync', dst, wcols[:, :, col0:col0 + ncols], reads=WBK, writes=[key])

    def dump(name, src, reads):
        if name in dbg_d:
            P.dma('sync', dbg_d[name], src, reads=reads, writes=['dbg_' + name])

    for k in consts:
        P.dma('sync', cs[k][:], cd[k], reads=(), writes=['c_' + k])
    CK = ['c_' + k for k in consts]
    for r in range(8):
        P.dma('gpsimd', wbf_d[r * 128:(r + 1) * 128, :], win_d[r * 128:(r + 1) * 128, :], reads=(), writes=[('wbf', r)])
    P.dma('gpsimd', wabf_d[0], wa_d, reads=(), writes=['wabf0'])
    P.dma('gpsimd', wabf_d[1], wb_d, reads=(), writes=['wabf1'])
    P.dma('gpsimd', wo_s[:], wo_d.rearrange("(j p) n -> p j n", p=128), reads=(), writes=['wo'])
    P.dma('gpsimd', w2s[:], w2s_d, reads=(), writes=['w2s'])
    P.dma('gpsimd', pet[:], pet_d, reads=(), writes=['pet'])
    load_w(wg24[:], C_GA, 24, 'wg24')
    MS('vector', eps_t[:], 1e-6, ['eps'])
    MS('vector', ones_b[:], 1.0, ['ones'])
    MS('vector', SGT[:], 0.0, ['SGT'])
    MS('vector', kcT[64:65, :], 1.0, ['kcT'])
    MS('vector', hcm[:], 0.0, ['hcm'])
    MS('vector', selT[:], 0.0, ['selT'])
    MS('vector', selTm[0][:], 0.0, [('selTm', 0)])
    MS('vector', selTm[1][:], 0.0, [('selTm', 1)])

    def attend(tiles):
        pend = []
        for t in tiles:
            sb_ = ST[rr['st'] % 2]
            rr['st'] += 1
            pj = rr['pt'] % 4
            rr['pt'] += 1
            n = len(t['smm'])
            for m, (lh, rh, rd) in enumerate(t['smm']):
                MM(ps[sb_][:, :], lh, rh, m == 0, m == n - 1, rd, [pk(sb_)])
            ACT(PT[pj][:], ps[sb_][:, :], AF.Exp, [pk(sb_)] + CK, [('PT', pj)], bias=t['bias'], scale=SCALE)
            pend.append((t['pv'], pj))
            if len(pend) > 2:
                pv_, pj_ = pend.pop(0)
                for (o_, lh, tp, st_, sp_, rd, wk) in pv_:
                    MM(o_, lh, PT[pj_][:], st_, sp_, rd + [('PT', pj_)], [wk], tp=tp)
        for pv_, pj_ in pend:
            for (o_, lh, tp, st_, sp_, rd, wk) in pv_:
                MM(o_, lh, PT[pj_][:], st_, sp_, rd + [('PT', pj_)], [wk], tp=tp)

    def proj_fm(wtile, wkey, wc0, ncol, blk, bank):
        for c in range(8):
            MM(ps[bank][0:ncol, :], wtile[:, c, wc0:wc0 + ncol], uT[:, c, blk * 512:(blk + 1) * 512],
               c == 0, c == 7, [wkey, ('uT', blk)], [pk(bank)])

    def silu_pair(bank, dst, dkey):
        w = WK[0]
        ACT(w[:], ps[bank][:, :], AF.Exp, [pk(bank)], [('WK', 0)], scale=-1.0)
        TS('vector', w[:], w[:], 1.0, None, ALU.add, None, [('WK', 0)], [('WK', 0)])
        RCP('vector', w[:], w[:], [('WK', 0)], [('WK', 0)])
        TT('vector', dst[:], ps[bank][:, :], w[:], ALU.mult, [pk(bank), ('WK', 0)], [dkey])

    def combine_branch(acc, akey, first, gbank, lrows, OB):
        w = WK[1]
        for (r0, r1, lb) in lrows:
            TS('vector', w[r0:r1, :], ps[lb][r0:r1, :], EPSL, None, ALU.max, None, [pk(lb)], [('WK', 1)])
        RCP('vector', w[:], w[:], [('WK', 1)], [('WK', 1)])
        if gbank is not None:
            TT('vector', w[:], ps[gbank][:, :], w[:], ALU.mult, [pk(gbank), ('WK', 1)], [('WK', 1)])
        if first:
            TT('vector', acc[:], ps[OB][:, :], w[:], ALU.mult, [pk(OB), ('WK', 1)], [akey])
        else:
            w2 = WK[2]
            TT('vector', w2[:], ps[OB][:, :], w[:], ALU.mult, [pk(OB), ('WK', 1)], [('WK', 2)])
            TT('vector', acc[:], acc[:], w2[:], ALU.add, [akey, ('WK', 2)], [akey])

    for s in range(NSEQ):
        P.dma('sync', gpre[:], npre_d, reads=(), writes=['gpre'])
        for tt in range(NT):
            b = tt % 2
            P.dma('sync', xt[b][:], x_d[s, tt * 128:(tt + 1) * 128, :], reads=(), writes=[('xt', b)])
            TT('vector', xn[b][:], xt[b][:], xt[b][:], ALU.mult, [('xt', b)], [('xn', b)])
            P.op('vector', (lambda bb: lambda e: e.reduce_sum(out=ssq[:, bb:bb + 1], in_=xn[bb][:], axis=AX.X))(b),
                 [('xn', b)], [('ssq', b)])
            ACT(ssq[:, b:b + 1], ssq[:, b:b + 1], AF.Ln, [('ssq', b), 'eps'], [('ssq', b)], bias=eps_t[:, 0:1],
                scale=1.0 / D)
            ACT(ssq[:, b:b + 1], ssq[:, b:b + 1], AF.Exp, [('ssq', b)], [('ssq', b)], scale=-0.5)
            STT(xn[b][:], xt[b][:], ssq[:, b:b + 1], gpre[:], ALU.mult, ALU.mult, [('xt', b), ('ssq', b), 'gpre'],
                [('xn', b)])
            for half in range(2):
                bk = misc_bank()
                for cc in range(4):
                    c = half * 4 + cc
                    TR(ps[bk][:, cc * 128:(cc + 1) * 128], xn[b][:, c * 128:(c + 1) * 128], cs['ident_f'][:],
                       [('xn', b), 'c_ident_f'], [pk(bk)])
                CP('vector' if half == 0 else 'gpsimd' if False else 'vector',
                   uT[:, half * 4:(half + 1) * 4, tt * 128:(tt + 1) * 128],
                   ps[bk][:, :].rearrange("p (c t) -> p c t", c=4), [pk(bk)], [('uT', tt // 4)])
        if 'uT' in dbg_d and s == 0:
            CP('vector', WK[0][:], uT[:, 0, 0:512], [('uT', 0)], [('WK', 0)])
            dump('uT', WK[0][:], [('WK', 0)])

        for g in range(2):
            wj = next_wst()
            load_w(wst[wj][:, :, 0:64], C_KV + 0 * 128 + g * 64, 64, ('wst', wj))
            load_w(wst[wj][:, :, 64:128], C_KV + 1 * 128 + g * 64, 64, ('wst', wj))
            for blk in range(NQT):
                bk = misc_bank()
                proj_fm(wst[wj], ('wst', wj), 0, 128, blk, bk)
                CP('vector', kT1[:, blk * 512:(blk + 1) * 512], ps[bk][:, :], [pk(bk)], ['kT1'])
            W1 = Vp[:].rearrange("p (l e) -> p l e", e=128)[:, 0:32, :]
            P.dma('gpsimd', W1, w1s_d, reads=(), writes=['Vp'])
            ncm = (S - 32) // 16 + 1
            for wh in range(2):
                r0 = 64 * wh
                bk = misc_bank()
                for l in range(32):
                    MM(ps[bk][:, 0:ncm], W1[r0:r0 + 64, l, :], kT1[r0:r0 + 64, l:l + 16 * (ncm - 1) + 1:16],
                       l == 0, l == 31, ['Vp', 'kT1'], [pk(bk)])
                bk2 = misc_bank()
                for l in range(32):
                    MM(ps[bk2][:, 0:1], W1[r0:r0 + 64, l, :], pet[r0:r0 + 64, l:l + 1], l == 0, l == 31,
                       ['Vp', 'pet'], [pk(bk2)])
                CP('vector', hb[:, 0:1], ps[bk2][:, 0:1], [pk(bk2)], ['hb'])
                TS('vector', hb[:, 1:2], hb[:, 0:1], -1.0, None, ALU.mult, None, ['hb'], ['hb'])
                w = WK[0]
                ACT(w[:, 0:ncm], ps[bk][:, 0:ncm], AF.Exp, [pk(bk), 'hb'], [('WK', 0)], bias=hb[:, 1:2], scale=-1.0)
                TS('vector', w[:, 0:ncm], w[:, 0:ncm], 1.0, None, ALU.add, None, [('WK', 0)], [('WK', 0)])
                RCP('vector', w[:, 0:ncm], w[:, 0:ncm], [('WK', 0)], [('WK', 0)])
                STT(hcm[:, wh, 0:ncm], ps[bk][:, 0:ncm], hb[:, 0:1], w[:, 0:ncm], ALU.add, ALU.mult,
                    [pk(bk), 'hb', ('WK', 0)], ['hcm'])
                if wh == 0:
                    bk3 = misc_bank()
                    MM(ps[bk3][0:64, 0:256], w2s[:, 0, :], hcm[:, 0, :], True, True, ['w2s', 'hcm'], [pk(bk3)])
                    CP('vector', kcT[0:64, :], ps[bk3][0:64, 0:256], [pk(bk3)], ['kcT'])
                else:
                    bk3 = misc_bank()
                    for a in range(2):
                        MM(ps[bk3][:, a * 64:(a + 1) * 64], hcm[:, 1, a * 128:(a + 1) * 128], w2s[:, 1, :], True, True,
                           ['w2s', 'hcm'], [pk(bk3)])
                    CP('vector', vc[:], ps[bk3][:, 0:128].rearrange("p (a d) -> p a d", a=2), [pk(bk3)], ['vc'])
            wj = next_wst()
            load_w(wst[wj][:, :, 0:64], C_KV + 2 * 128 + g * 64, 64, ('wst', wj))
            load_w(wst[wj][:, :, 64:128], C_KV + 4 * 128 + g * 64, 64, ('wst', wj))
            for blk in range(NQT):
                bk = misc_bank()
                proj_fm(wst[wj], ('wst', wj), 0, 128, blk, bk)
                CP('vector', kT0[0:64, blk * 512:(blk + 1) * 512], ps[bk][0:64, :], [pk(bk)], ['kT0'])
                CP('vector', kT1[0:64, blk * 512:(blk + 1) * 512], ps[bk][64:128, :], [pk(bk)], ['kT1'])
            MS('vector', kT0[64:65, :], 1.0, ['kT0'])
            MS('vector', kT1[64:65, :], 1.0, ['kT1'])
            wj = next_wst()
            load_w(wst[wj][:, :, 0:64], C_KV + 3 * 128 + g * 64, 64, ('wst', wj))
            load_w(wst[wj][:, :, 64:128], C_KV + 5 * 128 + g * 64, 64, ('wst', wj))
            for t4 in range(NT // 4):
                bk = misc_bank()
                for q4 in range(4):
                    tt = t4 * 4 + q4
                    for c in range(8):
                        MM(ps[bk][:, q4 * 128:(q4 + 1) * 128], uT[:, c, tt * 128:(tt + 1) * 128], wst[wj][:, c, :],
                           c == 0, c == 7, [('wst', wj), ('uT', t4)], [pk(bk)])
                CP('vector', Vp3[:, t4 * 4:(t4 + 1) * 4, :], ps[bk][:, :].rearrange("p (t c) -> p t c", c=128),
                   [pk(bk)], ['Vp'])
            load_w(WQZ[:, :, 0:256], C_QA + g * 256, 256, 'WQZ')
            load_w(WQZ[:, :, 256:512], C_ZA + g * 256, 256, 'WQZ')
            for h4 in range(4):
                for b2 in range(2):
                    P.dma('sync', QA[b2][h4][64:65, :], cd['cq'][4 * g + h4:4 * g + h4 + 1, :], reads=(),
                          writes=[('QA', b2, h4)])
            for i in range(NQT):
                b2 = i % 2
                for pp in range(2):
                    bk = misc_bank()
                    proj_fm(WQZ, 'WQZ', pp * 128, 128, i, bk)
                    CP('vector', QA[b2][2 * pp][0:64, :], ps[bk][0:64, :], [pk(bk)], [('QA', b2, 2 * pp)])
                    CP('vector', QA[b2][2 * pp + 1][0:64, :], ps[bk][64:128, :], [pk(bk)], [('QA', b2, 2 * pp + 1)])
                for pp in range(2):
                    bk = misc_bank()
                    proj_fm(WQZ, 'WQZ', 256 + pp * 128, 128, i, bk)
                    silu_pair(bk, SZ[pp], ('xn', pp))
                bk = misc_bank()
                for c in range(8):
                    MM(ps[bk][0:24, :], wg24[:, c, :], uT[:, c, i * 512:(i + 1) * 512], c == 0, c == 7,
                       ['wg24', ('uT', i)], [pk(bk)])
                w = WK[0]
                ACT(w[0:24, :], ps[bk][0:24, :], AF.Exp, [pk(bk)], [('WK', 0)], scale=-1.0)
                TS('vector', w[0:24, :], w[0:24, :], 1.0, None, ALU.add, None, [('WK', 0)], [('WK', 0)])
                RCP('vector', w[0:24, :], w[0:24, :], [('WK', 0)], [('WK', 0)])
                CP('vector', SGT[0:24, :], w[0:24, :], [('WK', 0)], ['SGT'])
                TT('vector', SGT[32:56, :], w[0:24, :], SGT[0:24, :], ALU.subtract, [('WK', 0), 'SGT'], ['SGT'])
                a_list = [0] if (512 * i + 511) < (2048 + 31) else [0, 1]
                a_list = [a for a in a_list if a * 128 < ncm]
                for a in a_list:
                    thr = 512.0 * i - 2048.0 * a
                    TS('vector', XM[a][:], cs['xg'][:], thr, NEG, ALU.is_gt, ALU.mult, ['c_xg'], [('XM', a)])
                nimp = 0
                tot_imp = 4 * len(a_list)
                for pp in range(2):
                    OB = OLS[0][0]
                    X2 = OLS[1][0]
                    lbank = [OLS[0][1], OLS[1][1]]
                    for hl in range(2):
                        h4 = 2 * pp + hl
                        ha = 4 * g + h4
                        for ai, a in enumerate(a_list):
                            sb_ = ST[rr['st'] % 2]
                            rr['st'] += 1
                            pj = hl * 2 + a
                            MM(ps[sb_][:, :], kcT[0:65, a * 128:(a + 1) * 128], QA[b2][h4][0:65, :], True, False,
                               ['kcT', ('QA', b2, h4)], [pk(sb_)])
                            MM(ps[sb_][:, :], cs['ident_b'][:], XM[a][:], False, True, ['c_ident_b', ('XM', a)],
                               [pk(sb_)])
                            ACT(PT[pj][:], ps[sb_][:, :], AF.Exp, [pk(sb_)] + CK, [('PT', pj)],
                                bias=cs['clb'][:, ha, a, i:i + 1], scale=SCALE)
                            la = len(a_list)
                            MM(ps[lbank[hl]][:, :], ones_b[:], PT[pj][:], ai == 0, ai == la - 1, ['ones', ('PT', pj)],
                               [pk(lbank[hl])])
                            MM(ps[OB][64 * hl:64 * hl + 64, :], vc[:, a, :], PT[pj][:], ai == 0, ai == la - 1,
                               ['vc', ('PT', pj)], [pk(OB)], tp=(0, 64 * hl))
                        wr = WK[3 + hl]
                        TS('vector', wr[:], ps[lbank[hl]][:, :], EPSL, None, ALU.max, None, [pk(lbank[hl])],
                           [('WK', 3 + hl)])
                        RCP('vector', wr[:], wr[:], [('WK', 3 + hl)], [('WK', 3 + hl)])
                        for ai, a in enumerate(a_list):
                            pj = hl * 2 + a
                            TT('vector', PT[pj][:], PT[pj][:], wr[:], ALU.mult, [('PT', pj), ('WK', 3 + hl)],
                               [('PT', pj)])
                            MM(ps[X2][0:64, :], cs['ov'][:, a, :], PT[pj][:], nimp == 0, nimp == tot_imp - 1,
                               ['c_ov', ('PT', pj)], [pk(X2)])
                            nimp += 1
                    gb = misc_bank()
                    MM(ps[gb][:, :], cs['goh'][0:56, (g * 2 + pp) * 3 + 0, :], SGT[0:56, :], True, True,
                       ['c_goh', 'SGT'], [pk(gb)])
                    w = WK[1]
                    TT('vector', w[0:64, :], ps[gb][0:64, :], WK[3][0:64, :], ALU.mult, [pk(gb), ('WK', 3)], [('WK', 1)])
                    TT('vector', w[64:128, :], ps[gb][64:128, :], WK[4][64:128, :], ALU.mult, [pk(gb), ('WK', 4)],
                       [('WK', 1)])
                    TT('vector', ACC[pp][:], ps[OB][:, :], w[:], ALU.mult, [pk(OB), ('WK', 1)], [('xt', pp)])
                w = WK[0]
                CP('vector', w[0:64, :], ps[X2][0:64, :], [pk(X2)], [('WK', 0)])
                bk = misc_bank()
                for qs in range(4):
                    TR(ps[bk][:, qs * 64:(qs + 1) * 64], w[0:64, qs * 128:(qs + 1) * 128], cs['ident_f'][0:64, 0:64],
                       [('WK', 0), 'c_ident_f'], [pk(bk)])
                for qs in range(4):
                    ti = 4 * i + qs
                    TT('vector', T1[:, qs, :], ps[bk][:, qs * 64:(qs + 1) * 64],
                       cs['abase'][:, 64 - 2 * ti:128 - 2 * ti], ALU.add, [pk(bk), 'c_abase'], ['T1'])
                MS('vector', T1[:, :, 0:1], BIGF, ['T1'])
                for qs in range(4):
                    P.op('vector', (lambda q_: lambda e: e.max(out=M8a[:], in_=T1[:, q_, :]))(qs), ['T1'], ['M8a'])
                    P.op('vector', (lambda q_: lambda e: e.match_replace(out=T2[:, q_, :], in_to_replace=M8a[:],
                                                                        in_values=T1[:, q_, :], imm_value=-1e9))(qs),
                         ['T1', 'M8a'], ['T2'])
                    P.op('vector', (lambda q_: lambda e: e.max(out=M8b[:], in_=T2[:, q_, :]))(qs), ['T2'], ['M8b'])
                    TS('vector', T2[:, qs, :], T1[:, qs, :], M8b[:, 7:8], NEG, ALU.is_lt, ALU.mult, ['T1', 'M8b'], ['T2'])
                bk = misc_bank()
                for qs in range(4):
                    TR(ps[bk][0:64, qs * 128:(qs + 1) * 128], T2[:, qs, :], cs['ident_f'][:], ['T2', 'c_ident_f'],
                       [pk(bk)])
                CP('vector', selT[0:64, :], ps[bk][0:64, :], [pk(bk)], ['selT'])
                for pp in range(2):
                    for br in (1, 2):
                        tiles = []
                        OB, LB = OLS[rr['ol'] % 2]
                        rr['ol'] += 1
                        if br == 1:
                            kts = list(range(0, 4 * i + 4))
                        else:
                            kts = [kt for kt in range(4 * i - 4, 4 * i + 4) if kt >= 0]
                        for kt in kts:
                            for hl in range(2):
                                h4 = 2 * pp + hl
                                ha = 4 * g + h4
                                hk = [k_ for k_ in kts if k_ >= kt_lo(ha, i)]
                                if kt not in hk:
                                    continue
                                ki = hk.index(kt)
                                nk = len(hk)
                                kT = kT0 if br == 1 else kT1
                                kkey = 'kT0' if br == 1 else 'kT1'
                                smm = [(kT[0:65, kt * 128:(kt + 1) * 128], QA[b2][h4][0:65, :], [kkey, ('QA', b2, h4)])]
                                if br == 1:
                                    smm.append((cs['e128'][:, kt * 128:(kt + 1) * 128], selT[:, :], ['c_e128', 'selT']))
                                    if kt >= 4 * i:
                                        r = kt - 4 * i
                                        smm.append((cs['ident_b'][:], cs['cmb'][:, 384 - 128 * r:896 - 128 * r],
                                                    ['c_ident_b', 'c_cmb']))
                                else:
                                    r = kt - (4 * i - 4)
                                    if r < 4:
                                        smm.append((cs['ident_b'][:], cs['omb'][:, 384 - 128 * r:896 - 128 * r],
                                                    ['c_ident_b', 'c_omb']))
                                    else:
                                        r -= 4
                                        smm.append((cs['ident_b'][:], cs['cmb'][:, 384 - 128 * r:896 - 128 * r],
                                                    ['c_ident_b', 'c_cmb']))
                                vcol = 0 if br == 1 else 64
                                pv = [(ps[OB][64 * hl:64 * hl + 64, :], Vp3[:, kt, vcol:vcol + 64], (0, 64 * hl),
                                       ki == 0, ki == nk - 1, ['Vp'], pk(OB)),
                                      (ps[LB][64 * hl:64 * hl + 64, :], ones_b[:, 0:64], (0, 64 * hl),
                                       ki == 0, ki == nk - 1, ['ones'], pk(LB))]
                                tiles.append(dict(smm=smm, bias=cs['alb'][:, ha, kt - 4 * i + 32:kt - 4 * i + 33], pv=pv))
                        attend(tiles)
                        gb = misc_bank()
                        MM(ps[gb][:, :], cs['goh'][0:56, (g * 2 + pp) * 3 + br, :], SGT[0:56, :], True, True,
                           ['c_goh', 'SGT'], [pk(gb)])
                        combine_branch(ACC[pp], ('xt', pp), False, gb, [(0, 128, LB)], OB)
                    yj = rr['wk'] % 2
                    rr['wk'] += 1
                    TT('vector', YST[yj][:], ACC[pp][:], SZ[pp][:], ALU.mult, [('xt', pp), ('xn', pp)], [('YST', yj)])
                    P.dma('sync', ysc_d[g * 2 + pp, :, i * 512:(i + 1) * 512], YST[yj][:], reads=[('YST', yj)],
                          writes=[('ysc', g * 2 + pp, i)])

        for j in range(4):
            wj = next_wst()
            load_w(wst[wj][:], C_QB + 512 + j * 128, 128, ('wst', wj))
            for blk in range(NQT):
                bk = misc_bank()
                proj_fm(wst[wj], ('wst', wj), 0, 128, blk, bk)
                CP('vector', kT0[0:64, blk * 512:(blk + 1) * 512], ps[bk][0:64, :], [pk(bk)], ['kT0'])
                CP('vector', kT1[0:64, blk * 512:(blk + 1) * 512], ps[bk][64:128, :], [pk(bk)], ['kT1'])
                P.op('vector', (lambda bk_, blk_: lambda e: e.reduce_sum(
                    out=kbarf[:, 2 * blk_:2 * blk_ + 2], in_=ps[bk_][:, :].rearrange("p (b t) -> p b t", b=2),
                    axis=AX.X))(bk, blk), [pk(bk)], ['kbarf'])
            MS('vector', kT0[64:65, :], 1.0, ['kT0'])
            MS('vector', kT1[64:65, :], 1.0, ['kT1'])
            nb = S // 256
            TS('vector', kbar[0:64, 0, 0:nb], kbarf[0:64, 0:nb], 1.0 / 256, None, ALU.mult, None, ['kbarf'], ['kbar'])
            TS('vector', kbar[0:64, 1, 0:nb], kbarf[64:128, 0:nb], 1.0 / 256, None, ALU.mult, None, ['kbarf'], ['kbar'])
            wj = next_wst()
            load_w(wst[wj][:], C_QB + 1024 + j * 128, 128, ('wst', wj))
            for t4 in range(NT // 4):
                bk = misc_bank()
                for q4 in range(4):
                    tt = t4 * 4 + q4
                    for c in range(8):
                        MM(ps[bk][:, q4 * 128:(q4 + 1) * 128], uT[:, c, tt * 128:(tt + 1) * 128], wst[wj][:, c, :],
                           c == 0, c == 7, [('wst', wj), ('uT', t4)], [pk(bk)])
                CP('vector', Vp3[:, t4 * 4:(t4 + 1) * 4, :], ps[bk][:, :].rearrange("p (t c) -> p t c", c=128),
                   [pk(bk)], ['Vp'])
            load_w(WQZ[:, :, 0:128], C_QB + j * 128, 128, 'WQZ')
            load_w(WQZ[:, :, 128:256], C_ZB + j * 128, 128, 'WQZ')
            for hl in range(2):
                for b2 in range(2):
                    P.dma('sync', QA[b2][hl][64:65, :], cd['cq'][8 + 2 * j + hl:8 + 2 * j + hl + 1, :], reads=(),
                          writes=[('QA', b2, hl)])
            for i in range(NQT):
                b2 = i % 2
                bk = misc_bank()
                proj_fm(WQZ, 'WQZ', 0, 128, i, bk)
                CP('vector', QA[b2][0][0:64, :], ps[bk][0:64, :], [pk(bk)], [('QA', b2, 0)])
                CP('vector', QA[b2][1][0:64, :], ps[bk][64:128, :], [pk(bk)], [('QA', b2, 1)])
                bk = misc_bank()
                proj_fm(WQZ, 'WQZ', 128, 128, i, bk)
                silu_pair(bk, SZ[0], ('xn', 0))
                bk = misc_bank()
                for hl in range(2):
                    for qs in range(4):
                        MM(ps[bk][:, (hl * 4 + qs) * 16:(hl * 4 + qs) * 16 + nb], QA[b2][hl][0:64, qs * 128:(qs + 1) * 128],
                           kbar[0:64, hl, 0:nb], True, True, ['kbar', ('QA', b2, hl)], [pk(bk)])
                for hl in range(2):
                    for qs in range(4):
                        cur = (4 * i + qs) // 2
                        e8 = hl * 4 + qs
                        TT('vector', GT[:, e8, 0:nb], ps[bk][:, e8 * 16:e8 * 16 + nb], cs['bbase'][:, 16 - cur:16 - cur + nb],
                           ALU.add, [pk(bk), 'c_bbase'], ['GT'])
                        if nb < 16:
                            MS('vector', GT[:, e8, nb:16], -BIGF, ['GT'])
                        P.op('vector', (lambda e_: lambda e: e.max(out=M8a[:], in_=GT[:, e_, :]))(e8), ['GT'], ['M8a'])
                        TS('vector', GT[:, e8, :], GT[:, e8, :], M8a[:, 2:3], NEG, ALU.is_lt, ALU.mult, ['GT', 'M8a'], ['GT'])
                        TT('vector', GT[:, e8, :], GT[:, e8, :], cs['obase'][:, 16 - cur:32 - cur], ALU.max,
                           ['GT', 'c_obase'], ['GT'])
                for hl in range(2):
                    bk = misc_bank()
                    for qs in range(4):
                        TR(ps[bk][0:16, qs * 128:(qs + 1) * 128], GT[:, hl * 4 + qs, :], cs['ident_f'][:],
                           ['GT', 'c_ident_f'], [pk(bk)])
                    CP('vector', selTm[hl][0:16, :], ps[bk][0:16, :], [pk(bk)], [('selTm', hl)])
                tiles = []
                OB, LB = OLS[rr['ol'] % 2]
                rr['ol'] += 1
                kts = list(range(0, 4 * i + 4))
                for kt in kts:
                    for hl in range(2):
                        hh = 8 + 2 * j + hl
                        hk = [k_ for k_ in kts if k_ >= kt_lo(hh, i)]
                        if kt not in hk:
                            continue
                        ki = hk.index(kt)
                        nk = len(hk)
                        kT = kT0 if hl == 0 else kT1
                        kkey = 'kT0' if hl == 0 else 'kT1'
                        smm = [(kT[0:65, kt * 128:(kt + 1) * 128], QA[b2][hl][0:65, :], [kkey, ('QA', b2, hl)]),
                               (cs['ohm'][:, kt // 2, :], selTm[hl][:, :], ['c_ohm', ('selTm', hl)])]
                        if kt >= 4 * i:
                            smm.append((cs['ident_b'][:], cs['mmb'][:, kt - 4 * i, :], ['c_ident_b', 'c_mmb']))
                        pv = [(ps[OB][64 * hl:64 * hl + 64, :], Vp3[:, kt, 64 * hl:64 * hl + 64], (0, 64 * hl),
                               ki == 0, ki == nk - 1, ['Vp'], pk(OB)),
                              (ps[LB][64 * hl:64 * hl + 64, :], ones_b[:, 0:64], (0, 64 * hl),
                               ki == 0, ki == nk - 1, ['ones'], pk(LB))]
                        tiles.append(dict(smm=smm, bias=cs['alb'][:, hh, kt - 4 * i + 32:kt - 4 * i + 33], pv=pv))
                attend(tiles)
                combine_branch(ACC[0], ('xt', 0), True, None, [(0, 128, LB)], OB)
                yj = rr['wk'] % 2
                rr['wk'] += 1
                TT('vector', YST[yj][:], ACC[0][:], SZ[0][:], ALU.mult, [('xt', 0), ('xn', 0)], [('YST', yj)])
                P.dma('sync', ysc_d[4 + j, :, i * 512:(i + 1) * 512], YST[yj][:], reads=[('YST', yj)],
                      writes=[('ysc', 4 + j, i)])

        P.op('sync', lambda e: e.nop(), (), ['Vp', ('wgs', 0), ('wgs', 1)])
        P.dma('sync', gpost[:], npost_d, reads=(), writes=['gpre'])
        for i in range(NQT):
            P.dma('sync', ysl[:, :, 0:512], ysc_d[:, :, i * 512:(i + 1) * 512].rearrange("c p t -> p c t"),
                  reads=[('ysc', jj, i) for jj in range(8)], writes=['kT1'])
            for fc in range(8):
                gj = fc % 2
                P.dma('sync', wgs[:, gj, :, 0:128], wcols[:, :, C_MG + fc * 128:C_MG + (fc + 1) * 128], reads=WBK,
                      writes=[('wgs', gj)])
                P.dma('sync', wgs[:, gj, :, 128:256], wcols[:, :, C_MG + 1024 + fc * 128:C_MG + 1024 + (fc + 1) * 128],
                      reads=WBK, writes=[('wgs', gj)])
                for ab in range(2):
                    P.dma('sync', wab[:, gj, 4 * ab:4 * ab + 4, :],
                          wabf_d[ab].rearrange("(j p) n -> p j n", p=128)[:, :, fc * 128:(fc + 1) * 128],
                          reads=['wabf0', 'wabf1'], writes=[('wab', gj)])
                for ab in range(2):
                    bg = misc_bank()
                    for c in range(8):
                        MM(ps[bg][:, :], wgs[:, gj, c, ab * 128:(ab + 1) * 128], uT[:, c, i * 512:(i + 1) * 512],
                           c == 0, c == 7, [('wgs', gj), ('uT', i)], [pk(bg)])
                    w = WK[ab]
                    ACT(w[:], ps[bg][:, :], AF.Exp, [pk(bg)], [('WK', ab)], scale=-1.0)
                    TS('vector', w[:], w[:], 1.0, None, ALU.add, None, [('WK', ab)], [('WK', ab)])
                    RCP('vector', w[:], w[:], [('WK', ab)], [('WK', ab)])
                    bm = OLS[fc % 2][ab]
                    for jj in range(4):
                        MM(ps[bm][:, :], wab[:, gj, 4 * ab + jj, :], ysl[:, 4 * ab + jj, 0:512], jj == 0, jj == 3,
                           [('wab', gj), 'kT1'], [pk(bm)])
                    TT('vector', w[:], ps[bm][:, :], w[:], ALU.mult, [pk(bm), ('WK', ab)], [('WK', ab)])
                TT('vector', mT[:, fc, 0:512], WK[0][:], WK[1][:], ALU.add, [('WK', 0), ('WK', 1)], ['kT0'])
            for ts_ in range(4):
                tt = i * 4 + ts_
                b = tt % 2
                P.dma('sync', xt[b][:], x_d[s, tt * 128:(tt + 1) * 128, :], reads=(), writes=[('xt', b)])
                banks = [ST[0], ST[1]]
                for hf in range(2):
                    for fc in range(8):
                        MM(ps[banks[hf]][:, :], mT[:, fc, ts_ * 128:(ts_ + 1) * 128], wo_s[:, fc, hf * 512:(hf + 1) * 512],
                           fc == 0, fc == 7, ['wo', 'kT0'], [pk(banks[hf])])
                for hf in range(2):
                    P.op('scalar', (lambda o_, i_: lambda e: e.copy(out=o_, in_=i_))(WK[2 + hf][:], ps[banks[hf]][:, :]),
                         [pk(banks[hf])], [('WK', 2 + hf)])
                    P.op('vector', (lambda o_, i_, a_: lambda e: e.scalar_tensor_tensor(
                        out=o_, in0=i_, scalar=1.0, in1=i_, op0=ALU.mult, op1=ALU.mult, accum_out=a_))(
                        YST[hf][:], WK[2 + hf][:], ssq2[:, hf:hf + 1]), [('WK', 2 + hf)], [('YST', hf), ('ssq2', hf)])
                TT('vector', ssq[:, b:b + 1], ssq2[:, 0:1], ssq2[:, 1:2], ALU.add, [('ssq2', 0), ('ssq2', 1)], [('ssq', b)])
                ACT(ssq[:, b:b + 1], ssq[:, b:b + 1], AF.Ln, [('ssq', b), 'eps'], [('ssq', b)], bias=eps_t[:, 0:1],
                    scale=1.0 / D)
                ACT(ssq[:, b:b + 1], ssq[:, b:b + 1], AF.Exp, [('ssq', b)], [('ssq', b)], scale=-0.5)
                for hf in range(2):
                    STT(xn[b][:, hf * 512:(hf + 1) * 512], WK[2 + hf][:], ssq[:, b:b + 1],
                        gpost[:, hf * 512:(hf + 1) * 512], ALU.mult, ALU.mult, [('WK', 2 + hf), ('ssq', b), 'gpre'],
                        [('xn', b)])
                TT('vector', xn[b][:], xn[b][:], xt[b][:], ALU.add, [('xn', b), ('xt', b)], [('xn', b)])
                P.dma('sync', out_d[s, tt * 128:(tt + 1) * 128, :], xn[b][:], reads=[('xn', b)], writes=[('out', tt)])
        P.op('sync', lambda e: e.nop(), (), ['Vp', ('wgs', 0), ('wgs', 1)])
    P.finish()
    return nc, P


def host_inputs(inputs, S):
    c = make_consts()
    f = lambda a: np.ascontiguousarray(np.asarray(a, dtype=np.float32))
    w1k = f(inputs['cmp_w1_k'])[0].transpose(1, 0, 2)
    w1v = f(inputs['cmp_w1_v'])[0].transpose(1, 0, 2)
    shared = {
        'w_in': f(inputs['w_in'])[0],
        'w1s': np.ascontiguousarray(np.concatenate([w1k, w1v], 0)),
        'w2s': np.ascontiguousarray(np.stack([f(inputs['cmp_w2_k'])[0], f(inputs['cmp_w2_v'])[0]], 1)),
        'pet': np.ascontiguousarray(np.concatenate([f(inputs['cmp_pe_k'])[0].T, f(inputs['cmp_pe_v'])[0].T], 0)),
        'w_a': f(inputs['w_branch_a'])[0],
        'w_b': f(inputs['w_branch_b'])[0],
        'w_o': f(inputs['w_o'])[0],
        'npre_bc': np.ascontiguousarray(np.broadcast_to(f(inputs['norm_pre'])[0][None, :], (128, D))),
        'npost_bc': np.ascontiguousarray(np.broadcast_to(f(inputs['norm_post'])[0][None, :], (128, D))),
    }
    for k, v in c.items():
        shared['c_' + k] = v
    return shared


def kernel(**inputs):
    x = np.ascontiguousarray(np.asarray(inputs['x'], dtype=np.float32))
    B, S, _ = x.shape
    ncores = 8
    nseq = B // ncores
    nc, _ = build(S, nseq)
    shared = host_inputs(inputs, S)
    in_maps = []
    for c in range(ncores):
        m = dict(shared)
        m['x'] = np.ascontiguousarray(x[c * nseq:(c + 1) * nseq])
        in_maps.append(m)
    res = run_bass_kernel_spmd(nc, in_maps, core_ids=list(range(ncores)))
    return np.concatenate([np.asarray(r['out']) for r in res.results], axis=0).astype(np.float32)
```

```python
import contextlib
import numpy as np
import ml_dtypes
import concourse.bass as bass
import concourse.mybir as mybir
from concourse.bass_utils import run_bass_kernel_spmd

F32 = mybir.dt.float32
BF16 = mybir.dt.bfloat16
ALU = mybir.AluOpType
AF = mybir.ActivationFunctionType
AX = mybir.AxisListType
NPBF = ml_dtypes.bfloat16

D = 1024
DEBUG_SEP = False
NCOL = 5912
NEG = -30000.0
SCALE = 0.125
BIGF = 1.0e4
EPSL = 1.0e-30
C_QA, C_KV, C_GA, C_ZA, C_QB, C_ZB, C_MG = 0, 512, 1280, 1304, 1816, 3352, 3864
SLOPES = [2.0 ** (-(i + 1) / 2.0) for i in range(16)]
SL_HH = [SLOPES[2 * h] for h in range(8)] + [SLOPES[2 * h + 1] for h in range(8)]


class Prog:
    EPOCH = 20000
    NDMA = 8

    def __init__(self, nc):
        self.nc = nc
        self.ops = []
        self.lastw = {}
        self.readers = {}
        self.stack = contextlib.ExitStack()
        self.sb_bytes = 0

    def sb(self, name, shape, dtype):
        n = 1
        for s in shape[1:]:
            n *= s
        self.sb_bytes += n * (4 if dtype == F32 else 2)
        return self.stack.enter_context(self.nc.sbuf_tensor('sb_' + name, list(shape), dtype))

    def ps(self, name, shape=(128, 512), dtype=F32):
        return self.stack.enter_context(self.nc.psum_tensor(name, list(shape), dtype))

    def _deps(self, idx, reads, writes):
        deps = set()
        for k in reads:
            w = self.lastw.get(k)
            if w is not None:
                deps.add(w)
        for k in writes:
            w = self.lastw.get(k)
            if w is not None:
                deps.add(w)
            for r in self.readers.get(k, ()):
                deps.add(r)
        for k in reads:
            self.readers.setdefault(k, []).append(idx)
        for k in writes:
            self.lastw[k] = idx
            self.readers[k] = []
        deps.discard(idx)
        return deps

    def op(self, eng, fn, reads=(), writes=()):
        idx = len(self.ops)
        deps = self._deps(idx, reads, writes)
        self.ops.append(dict(eng=eng, fn=fn, deps=deps, dma=False, wkeys=set(writes), rkeys=set(reads)))
        return idx

    def dma(self, eng, out, in_, reads=(), writes=()):
        idx = len(self.ops)
        deps = self._deps(idx, reads, writes)
        self.ops.append(dict(eng=eng, fn=lambda e: e.dma_start(out=out, in_=in_), deps=deps, dma=True,
                             wkeys=set(writes), rkeys=set(reads)))
        return idx

    def finish(self):
        nc = self.nc
        ops = self.ops
        needed = set()
        for i, o in enumerate(ops):
            nd = set()
            for d in o['deps']:
                od = ops[d]
                if od['eng'] == o['eng'] and not od['dma'] and not o['dma']:
                    if o['eng'] == 'tensor':
                        continue
                    if not ((od['wkeys'] & o['rkeys']) or (od['wkeys'] & o['wkeys'])):
                        continue
                nd.add(d)
            o['deps'] = nd
            for d in nd:
                if not ops[d]['dma']:
                    needed.add(d)
        engs = ['tensor', 'vector', 'scalar', 'gpsimd', 'sync']
        cnt = {e: 0 for e in engs}
        dcnt = {e: 0 for e in engs}
        nep = {e: 0 for e in engs}
        for i, o in enumerate(ops):
            e = o['eng']
            if o['dma']:
                d = dcnt[e]
                dcnt[e] += 1
                o['sig'] = (('dma', e, d % self.NDMA), 16 * (d // self.NDMA + 1), 16)
                o['prev'] = (('dma', e, d % self.NDMA), 16 * (d // self.NDMA)) if d >= self.NDMA else None
            elif i in needed:
                c = cnt[e]
                cnt[e] += 1
                ep = c // self.EPOCH
                nep[e] = max(nep[e], ep + 1)
                o['sig'] = (('cmp', e, ep), c % self.EPOCH + 1, 1)
            else:
                o['sig'] = None
        sems = {}
        for e in engs:
            for ep in range(nep[e]):
                sems[('cmp', e, ep)] = self.stack.enter_context(nc.semaphore(f"s_{e}_{ep}"))
            for j in range(min(self.NDMA, dcnt[e])):
                sems[('dma', e, j)] = self.stack.enter_context(nc.semaphore(f"d_{e}_{j}"))
        self.n_instr = {e: 0 for e in engs}
        with nc.Block() as block:
            def make(ename):
                def body(eng):
                    waited = {}
                    lastdma = {}
                    for o in ops:
                        if o['eng'] != ename:
                            continue
                        best = {}
                        for d in o['deps']:
                            s = ops[d]['sig']
                            if s[1] > best.get(s[0], 0):
                                best[s[0]] = s[1]
                        if o['dma'] and o['prev'] is not None:
                            k, v = o['prev']
                            if v > best.get(k, 0):
                                best[k] = v
                        for k, v in best.items():
                            if waited.get(k, 0) >= v:
                                continue
                            eng.wait_ge(sems[k], v)
                            waited[k] = v
                            self.n_instr[ename] += 1
                        ins = o['fn'](eng)
                        self.n_instr[ename] += 1
                        if o['sig'] is not None:
                            ins.then_inc(sems[o['sig'][0]], o['sig'][2])
                            if o['dma']:
                                lastdma[o['sig'][0]] = o['sig'][1]
                    for k, v in lastdma.items():
                        if waited.get(k, 0) < v:
                            eng.wait_ge(sems[k], v)
                return body
            for ename in engs:
                if any(o['eng'] == ename for o in ops):
                    getattr(block, ename)(make(ename))
        self.stack.close()


def make_consts():
    c = {}
    c['ident_f'] = np.eye(128, dtype=np.float32)
    c['ident_b'] = np.eye(128).astype(NPBF)
    p = np.arange(128)[:, None]
    fp = np.arange(896)[None, :]
    c['cmb'] = np.where(p > fp - 384, NEG, 0.0).astype(NPBF)
    c['omb'] = np.where(fp - 384 >= p, NEG, 0.0).astype(NPBF)
    kk = np.arange(4)[None, :, None] * 128 + np.arange(128)[:, None, None]
    f = np.arange(512)[None, None, :]
    mm = np.where(kk // 256 == f // 256, np.where(kk > f, NEG, 0.0), np.where(f // 256 > kk // 256, 0.0, NEG))
    c['mmb'] = mm.astype(NPBF)
    jj = np.arange(128)[:, None]
    k2 = np.arange(4096)[None, :]
    c['e128'] = ((k2 // 64 == jj) & (jj < 64)).astype(NPBF)
    oh = np.zeros((128, 16, 128), np.float32)
    oh[0:16] = np.broadcast_to(np.eye(16)[:, :, None], (16, 16, 128))
    c['ohm'] = oh.astype(NPBF)
    alb = np.zeros((128, 16, 36), np.float32)
    for hh in range(16):
        for di in range(36):
            alb[:, hh, di] = SL_HH[hh] * (np.arange(128) + 128.0 * (di - 32))
    c['alb'] = alb
    clb = np.zeros((128, 8, 2, 8), np.float32)
    for ha in range(8):
        for a in range(2):
            for i in range(8):
                clb[:, ha, a, i] = SL_HH[ha] * (2048.0 * a + 16.0 * np.arange(128) + 31.0 - 512.0 * i)
    c['clb'] = clb
    cq = np.zeros((16, 512), np.float32)
    for hh in range(16):
        cq[hh] = -SL_HH[hh] * np.arange(512) / SCALE
    c['cq'] = cq.astype(NPBF)
    goh = np.zeros((56, 12, 128), np.float32)
    for g in range(2):
        for pp in range(2):
            for k in range(3):
                idx = (g * 2 + pp) * 3 + k
                ra = k * 8 + g * 4 + 2 * pp
                goh[ra, idx, 0:64] = 1.0
                goh[32 + ra, idx, 0:64] = 1.0
                goh[ra + 1, idx, 64:128] = 1.0
                goh[32 + ra + 1, idx, 64:128] = 1.0
    c['goh'] = goh.astype(NPBF)
    n_cmp, n_slc = 255, 64
    cs = np.arange(n_cmp) * 16
    ce = cs + 31
    ss = np.arange(n_slc) * 64
    se = ss + 63
    ov = ((cs[:, None] <= se[None, :]) & (ce[:, None] >= ss[None, :])).astype(np.float32)
    ovp = np.zeros((256, 64), np.float32)
    ovp[:255] = ov
    c['ov'] = np.ascontiguousarray(ovp.reshape(2, 128, 64).transpose(1, 0, 2)).astype(NPBF)
    q = np.arange(128)[:, None]
    m = np.arange(128)[None, :]
    jr = m - 64
    cr = q // 64
    ab = np.where((jr == cr) | (jr == cr - 1), BIGF, np.where(jr > cr, -BIGF, 0.0))
    c['abase'] = ab.astype(np.float32)
    m2 = np.arange(32)[None, :]
    c['bbase'] = np.broadcast_to(np.where(m2 >= 16, -BIGF, 0.0), (128, 32)).astype(np.float32).copy()
    c['obase'] = np.broadcast_to(np.where(m2 == 16, 0.0, 2 * NEG), (128, 32)).astype(np.float32).copy()
    c['xg'] = (16.0 * np.arange(128)[:, None] + 31.0 - np.arange(512)[None, :]).astype(np.float32)
    return c


CONST_DT = dict(ident_f=F32, ident_b=BF16, cmb=BF16, omb=BF16, mmb=BF16, e128=BF16, ohm=BF16, alb=F32, clb=F32,
                cq=BF16, goh=BF16, ov=BF16, abase=F32, bbase=F32, obase=F32, xg=F32)


def build(S, NSEQ, dbg=()):
    nc = bass.Bass("TRN2", target_bir_lowering=False)
    NT = S // 128
    NQT = S // 512
    consts = make_consts()

    def din(name, shape, dt=F32):
        return nc.dram_tensor(name, list(shape), dt, kind="ExternalInput").ap()

    x_d = din("x", [NSEQ, S, D])
    win_d = din("w_in", [D, NCOL])
    w1s_d = din("w1s", [128, 32, 128])
    w2s_d = din("w2s", [128, 2, 64])
    pet_d = din("pet", [128, 32])
    wa_d = din("w_a", [512, D])
    wb_d = din("w_b", [512, D])
    wo_d = din("w_o", [D, D])
    npre_d = din("npre_bc", [128, D])
    npost_d = din("npost_bc", [128, D])
    cd = {k: din("c_" + k, list(v.shape), CONST_DT[k]) for k, v in consts.items()}
    out_d = nc.dram_tensor("out", [NSEQ, S, D], F32, kind="ExternalOutput").ap()
    wbf_d = nc.dram_tensor("wbf", [D, NCOL], BF16, kind="Internal").ap()
    ysc_d = nc.dram_tensor("yscr", [8, 128, S], BF16, kind="Internal").ap()
    wabf_d = nc.dram_tensor("wabf", [2, 512, D], BF16, kind="Internal").ap()
    dbg_d = {}
    for name, shape in dbg:
        dbg_d[name] = nc.dram_tensor("dbg_" + name, list(shape), F32, kind="ExternalOutput").ap()

    P = Prog(nc)
    cs = {k: P.sb("k_" + k, list(v.shape), CONST_DT[k]) for k, v in consts.items()}
    uT = P.sb("uT", [128, 8, S], BF16)
    SA = max(S, 4096)
    kT0 = P.sb("kT0", [128, SA], BF16)
    kT1 = P.sb("kT1", [128, SA], BF16)
    Vp = P.sb("Vp", [128, SA], BF16)
    kcT = P.sb("kcT", [128, 256], BF16)
    vcx = P.sb("vcx", [128, 2, 128], BF16)
    hcm = P.sb("hcm", [128, 2, 256], BF16)
    QA = [[P.sb(f"QA{b}_{h}", [65, 512], BF16) for h in range(4)] for b in range(2)]
    PT = [P.sb(f"PT{j}", [128, 512], BF16) for j in range(6)]
    XM = [P.sb(f"XM{j}", [128, 512], BF16) for j in range(2)]
    WKB = P.sb("WKB", [128, 7, 512], F32)
    WK = [WKB[:, j, :] for j in range(5)]
    YST = [P.sb(f"YST{j}", [128, 512], BF16) for j in range(2)]
    SGT = [P.sb(f"SGT{j}", [64, 512], BF16) for j in range(2)]
    selT = [P.sb(f"selT{j}", [128, 512], BF16) for j in range(2)]
    selTm = [[P.sb(f"selTm{b_}_{j}", [128, 512], BF16) for j in range(2)] for b_ in range(2)]
    T1 = P.sb("T1", [128, 4, 64], F32)
    T2 = P.sb("T2", [128, 4, 64], F32)
    M8a = P.sb("M8a", [128, 8], F32)
    M8b = P.sb("M8b", [128, 8], F32)
    GT = P.sb("GT", [128, 8, 16], F32)
    kbarf = P.sb("kbarf", [128, 16], F32)
    kbar = P.sb("kbar", [64, 2, 16], BF16)
    wst0 = P.sb("wst0", [128, 8, 128], BF16)
    wst = [wst0, wst0]
    WQZ = P.sb("WQZ", [128, 8, 512], BF16)
    wg24 = P.sb("wg24", [128, 8, 24], BF16)
    w2s = P.sb("w2s", [128, 2, 64], BF16)
    pet = P.sb("pet", [128, 32], BF16)
    hb = P.sb("hb", [128, 2], F32)
    xt = [P.sb(f"xt{j}", [128, D], F32) for j in range(2)]
    xn = [P.sb(f"xn{j}", [128, D], F32) for j in range(2)]
    gpre = WKB[:, 5:7, :].rearrange("p a n -> p (a n)")
    gpost = gpre
    ACC = [[xt[j][:, b_ * 512:(b_ + 1) * 512] for j in range(2)] for b_ in range(2)]
    SZ = [[xn[j][:, b_ * 512:(b_ + 1) * 512] for j in range(2)] for b_ in range(2)]
    if DEBUG_SEP:
        ACC = [[P.sb(f"ACCd{b_}{j}", [128, 512], F32)[:] for j in range(2)] for b_ in range(2)]
        SZ = [[P.sb(f"SZd{b_}{j}", [128, 512], F32)[:] for j in range(2)] for b_ in range(2)]
    impacc = WKB[:, 5, :]
    WKg = WKB[:, 6, :]
    ones1 = P.sb("ones1", [128, 1], F32)

    def XK(b_):
        return [('xt', b_, 0), ('xt', b_, 1)]

    def NK(b_):
        return [('xn', b_, 0), ('xn', b_, 1)]
    ssq = P.sb("ssq", [128, 2], F32)
    ssq2 = P.sb("ssq2", [128, 2], F32)
    eps_t = P.sb("eps_t", [128, 1], F32)
    ones_b = P.sb("ones_b", [128, 128], BF16)
    wab = WQZ[:].rearrange("p c (b n) -> p b c n", b=4)[:, 0:2]
    wo_s = P.sb("wo_s", [128, 8, D], BF16)
    ps = [P.ps(f"ps{j}") for j in range(8)]
    ST = [0, 1]
    OLS = [(2, 3), (4, 5)]
    MISC = [6, 7]
    Vp3 = Vp[:].rearrange("p (t c) -> p t c", c=128)
    mT = kT0[:].rearrange("p (c t) -> p c t", c=8)
    ysl = kT1[:].rearrange("p (c t) -> p c t", c=8)
    wgs = Vp[:].rearrange("p (b c n) -> p b c n", b=2, c=8)
    assert SA // 8 >= 512 and SA // 16 >= 256

    rr = {'misc': 0, 'st': 0, 'pt': 0, 'wst': 0, 'wk': 0, 'ol': 0, 'ptc': 0}

    def kt_lo(hh, i):
        lo = 0
        while SL_HH[hh] * (512 * i - 128 * lo - 127) > 110.0:
            lo += 1
        return lo

    def MM(out, lhsT, rhs, start, stop, reads, writes, tp=None):
        if tp is None:
            P.op('tensor', lambda e: e.matmul(out, lhsT=lhsT, rhs=rhs, start=start, stop=stop), reads, writes)
        else:
            P.op('tensor', lambda e: e.matmul(out, lhsT=lhsT, rhs=rhs, start=start, stop=stop, tile_position=tp),
                 reads, writes)

    def TR(out, in_, ident, reads, writes):
        P.op('tensor', lambda e: e.transpose(out, in_, ident), reads, writes)

    def ACT(out, in_, func, reads, writes, bias=None, scale=1.0):
        if bias is None:
            P.op('scalar', lambda e: e.activation(out=out, in_=in_, func=func, scale=scale), reads, writes)
        else:
            P.op('scalar', lambda e: e.activation(out=out, in_=in_, func=func, bias=bias, scale=scale), reads, writes)

    def TT(eng, out, in0, in1, op, reads, writes):
        P.op(eng, lambda e: e.tensor_tensor(out=out, in0=in0, in1=in1, op=op), reads, writes)

    def TS(eng, out, in0, s1, s2, op0, op1, reads, writes):
        if op1 is None:
            P.op(eng, lambda e: e.tensor_scalar(out=out, in0=in0, scalar1=s1, scalar2=None, op0=op0), reads, writes)
        else:
            P.op(eng, lambda e: e.tensor_scalar(out=out, in0=in0, scalar1=s1, scalar2=s2, op0=op0, op1=op1),
                 reads, writes)

    def STT(out, in0, scalar, in1, op0, op1, reads, writes):
        P.op('vector', lambda e: e.scalar_tensor_tensor(out=out, in0=in0, scalar=scalar, in1=in1, op0=op0, op1=op1),
             reads, writes)

    def CP(eng, out, in_, reads, writes):
        P.op(eng, lambda e: e.tensor_copy(out=out, in_=in_), reads, writes)

    def RCP(eng, out, in_, reads, writes):
        P.op(eng, lambda e: e.reciprocal(out=out, in_=in_), reads, writes)

    def MS(eng, ap, val, writes):
        P.op(eng, lambda e: e.memset(ap, val), (), writes)

    def pk(j):
        return ('ps', j)

    def misc_bank():
        j = MISC[rr['misc'] % 2]
        rr['misc'] += 1
        return j

    def next_wst():
        return 0

    wcols = wbf_d.rearrange("(c p) n -> p c n", p=128)
    WBK = [('wbf', r) for r in range(8)]

    def load_w(dst, col0, ncols, key):
        P.dma('sync', dst, wcols[:, :, col0:col0 + ncols], reads=WBK, writes=[key])

    def dump(name, src, reads):
        if name in dbg_d:
            P.dma('sync', dbg_d[name], src, reads=reads, writes=['dbg_' + name])

    for k in consts:
        P.dma('sync', cs[k][:], cd[k], reads=(), writes=['c_' + k])
    CK = ['c_' + k for k in consts]
    for r in range(8):
        P.dma('gpsimd', wbf_d[r * 128:(r + 1) * 128, :], win_d[r * 128:(r + 1) * 128, :], reads=(), writes=[('wbf', r)])
    P.dma('gpsimd', wabf_d[0], wa_d, reads=(), writes=['wabf0'])
    P.dma('gpsimd', wabf_d[1], wb_d, reads=(), writes=['wabf1'])
    P.dma('gpsimd', wo_s[:], wo_d.rearrange("(j p) n -> p j n", p=128), reads=(), writes=['wo'])
    P.dma('gpsimd', w2s[:], w2s_d, reads=(), writes=['w2s'])
    P.dma('gpsimd', pet[:], pet_d, reads=(), writes=['pet'])
    load_w(wg24[:], C_GA, 24, 'wg24')
    MS('vector', eps_t[:], 1e-6, ['eps'])
    MS('vector', ones_b[:], 1.0, ['ones'])
    for b_ in range(2):
        MS('vector', SGT[b_][:], 0.0, [('SGT', b_)])
        MS('vector', selT[b_][:], 0.0, [('selT', b_)])
        for j_ in range(2):
            MS('vector', selTm[b_][j_][:], 0.0, [('selTm', b_, j_)])
    MS('vector', vcx[:], 1.0, ['vcx'])
    MS('vector', ones1[:], 1.0, ['ones1'])
    MS('vector', kcT[64:65, :], 1.0, ['kcT'])
    MS('vector', hcm[:], 0.0, ['hcm'])

    def adv(gen):
        if gen is not None:
            try:
                next(gen)
            except StopIteration:
                pass

    def exhaust(gen):
        if gen is not None:
            for _ in gen:
                pass

    def attend(tiles, gen=None, step=3):
        pend = []
        for t in tiles:
            sb_ = ST[rr['st'] % 2]
            rr['st'] += 1
            pj = rr['pt'] % 4
            rr['pt'] += 1
            n = len(t['smm'])
            for m, (lh, rh, rd) in enumerate(t['smm']):
                MM(ps[sb_][:, :], lh, rh, m == 0, m == n - 1, rd, [pk(sb_)])
            ACT(PT[pj][:], ps[sb_][:, :], AF.Exp, [pk(sb_)] + CK, [('PT', pj)], bias=t['bias'], scale=SCALE)
            pend.append((t['pv'], pj))
            rr['tc'] = rr.get('tc', 0) + 1
            if rr['tc'] % step == 0:
                adv(gen)
            if len(pend) > 2:
                pv_, pj_ = pend.pop(0)
                for (o_, lh, tp, st_, sp_, rd, wk) in pv_:
                    MM(o_, lh, PT[pj_][:], st_, sp_, rd + [('PT', pj_)], [wk], tp=tp)
        for pv_, pj_ in pend:
            for (o_, lh, tp, st_, sp_, rd, wk) in pv_:
                MM(o_, lh, PT[pj_][:], st_, sp_, rd + [('PT', pj_)], [wk], tp=tp)

    def proj_fm(wtile, wkey, wc0, ncol, blk, bank):
        for c in range(8):
            MM(ps[bank][0:ncol, :], wtile[:, c, wc0:wc0 + ncol], uT[:, c, blk * 512:(blk + 1) * 512],
               c == 0, c == 7, [wkey, ('uT', blk)], [pk(bank)])

    def silu_pair(bank, dst, dkey):
        w = WK[0]
        ACT(w[:], ps[bank][:, :], AF.Exp, [pk(bank)], [('WK', 0)], scale=-1.0)
        TS('vector', w[:], w[:], 1.0, None, ALU.add, None, [('WK', 0)], [('WK', 0)])
        RCP('vector', w[:], w[:], [('WK', 0)], [('WK', 0)])
        TT('vector', dst, ps[bank][:, :], w[:], ALU.mult, [pk(bank), ('WK', 0)], [dkey])

    def combine_branch(acc, akey, first, gbank, lrows, OB):
        w = WK[1]
        for (r0, r1, lb) in lrows:
            TS('vector', w[r0:r1, :], ps[lb][r0:r1, :], EPSL, None, ALU.max, None, [pk(lb)], [('WK', 1)])
        RCP('vector', w[:], w[:], [('WK', 1)], [('WK', 1)])
        if gbank is not None:
            TT('vector', w[:], ps[gbank][:, :], w[:], ALU.mult, [pk(gbank), ('WK', 1)], [('WK', 1)])
        if first:
            TT('vector', acc, ps[OB][:, :], w[:], ALU.mult, [pk(OB), ('WK', 1)], [akey])
        else:
            w2 = WK[2]
            TT('vector', w2[:], ps[OB][:, :], w[:], ALU.mult, [pk(OB), ('WK', 1)], [('WK', 2)])
            TT('vector', acc, acc, w2[:], ALU.add, [akey, ('WK', 2)], [akey])

    for s in range(NSEQ):
        P.dma('sync', gpre, npre_d, reads=(), writes=['impacc', 'WKg'])
        for tt in range(NT):
            b = tt % 2
            P.dma('sync', xt[b][:], x_d[s, tt * 128:(tt + 1) * 128, :], reads=(), writes=XK(b))
            TT('vector', xn[b][:], xt[b][:], xt[b][:], ALU.mult, XK(b), NK(b))
            P.op('vector', (lambda bb: lambda e: e.reduce_sum(out=ssq[:, bb:bb + 1], in_=xn[bb][:], axis=AX.X))(b),
                 NK(b), [('ssq', b)])
            ACT(ssq[:, b:b + 1], ssq[:, b:b + 1], AF.Ln, [('ssq', b), 'eps'], [('ssq', b)], bias=eps_t[:, 0:1],
                scale=1.0 / D)
            ACT(ssq[:, b:b + 1], ssq[:, b:b + 1], AF.Exp, [('ssq', b)], [('ssq', b)], scale=-0.5)
            STT(xn[b][:], xt[b][:], ssq[:, b:b + 1], gpre, ALU.mult, ALU.mult, XK(b) + [('ssq', b), 'impacc', 'WKg'],
                NK(b))
            for half in range(2):
                bk = misc_bank()
                for cc in range(4):
                    c = half * 4 + cc
                    TR(ps[bk][:, cc * 128:(cc + 1) * 128], xn[b][:, c * 128:(c + 1) * 128], cs['ident_f'][:],
                       NK(b) + ['c_ident_f'], [pk(bk)])
                CP('vector' if half == 0 else 'gpsimd' if False else 'vector',
                   uT[:, half * 4:(half + 1) * 4, tt * 128:(tt + 1) * 128],
                   ps[bk][:, :].rearrange("p (c t) -> p c t", c=4), [pk(bk)], [('uT', tt // 4)])
        if 'uT' in dbg_d and s == 0:
            CP('vector', WK[0][:], uT[:, 0, 0:512], [('uT', 0)], [('WK', 0)])
            dump('uT', WK[0][:], [('WK', 0)])

        ncm = (S - 32) // 16 + 1
        for g in range(2):
            wj = next_wst()
            load_w(wst[wj][:, :, 0:64], C_KV + 0 * 128 + g * 64, 64, ('wst', wj))
            load_w(wst[wj][:, :, 64:128], C_KV + 1 * 128 + g * 64, 64, ('wst', wj))
            for blk in range(NQT):
                bk = misc_bank()
                proj_fm(wst[wj], ('wst', wj), 0, 128, blk, bk)
                CP('vector', kT1[:, blk * 512:(blk + 1) * 512], ps[bk][:, :], [pk(bk)], ['kT1'])
            W1 = Vp[:].rearrange("p (l e) -> p l e", e=128)[:, 0:32, :]
            P.dma('gpsimd', W1, w1s_d, reads=(), writes=['Vp'])
            for wh in range(2):
                r0 = 64 * wh
                bk = misc_bank()
                for l in range(32):
                    MM(ps[bk][:, 0:ncm], W1[r0:r0 + 64, l, :], kT1[r0:r0 + 64, l:l + 16 * (ncm - 1) + 1:16],
                       l == 0, l == 31, ['Vp', 'kT1'], [pk(bk)])
                bk2 = misc_bank()
                for l in range(32):
                    MM(ps[bk2][:, 0:1], W1[r0:r0 + 64, l, :], pet[r0:r0 + 64, l:l + 1], l == 0, l == 31,
                       ['Vp', 'pet'], [pk(bk2)])
                CP('vector', hb[:, 0:1], ps[bk2][:, 0:1], [pk(bk2)], ['hb'])
                TS('vector', hb[:, 1:2], hb[:, 0:1], -1.0, None, ALU.mult, None, ['hb'], ['hb'])
                w = WK[0]
                ACT(w[:, 0:ncm], ps[bk][:, 0:ncm], AF.Exp, [pk(bk), 'hb'], [('WK', 0)], bias=hb[:, 1:2], scale=-1.0)
                TS('vector', w[:, 0:ncm], w[:, 0:ncm], 1.0, None, ALU.add, None, [('WK', 0)], [('WK', 0)])
                RCP('vector', w[:, 0:ncm], w[:, 0:ncm], [('WK', 0)], [('WK', 0)])
                STT(hcm[:, wh, 0:ncm], ps[bk][:, 0:ncm], hb[:, 0:1], w[:, 0:ncm], ALU.add, ALU.mult,
                    [pk(bk), 'hb', ('WK', 0)], ['hcm'])
                if wh == 0:
                    bk3 = misc_bank()
                    MM(ps[bk3][0:64, 0:256], w2s[:, 0, :], hcm[:, 0, :], True, True, ['w2s', 'hcm'], [pk(bk3)])
                    CP('vector', kcT[0:64, :], ps[bk3][0:64, 0:256], [pk(bk3)], ['kcT'])
                else:
                    bk3 = misc_bank()
                    for a in range(2):
                        MM(ps[bk3][:, a * 64:(a + 1) * 64], hcm[:, 1, a * 128:(a + 1) * 128], w2s[:, 1, :], True, True,
                           ['w2s', 'hcm'], [pk(bk3)])
                    CP('vector', vcx[:, :, 0:64], ps[bk3][:, 0:128].rearrange("p (a d) -> p a d", a=2), [pk(bk3)],
                       ['vcx'])
            wj = next_wst()
            load_w(wst[wj][:, :, 0:64], C_KV + 2 * 128 + g * 64, 64, ('wst', wj))
            load_w(wst[wj][:, :, 64:128], C_KV + 4 * 128 + g * 64, 64, ('wst', wj))
            for blk in range(NQT):
                bk = misc_bank()
                proj_fm(wst[wj], ('wst', wj), 0, 128, blk, bk)
                CP('vector', kT0[0:64, blk * 512:(blk + 1) * 512], ps[bk][0:64, :], [pk(bk)], ['kT0'])
                CP('vector', kT1[0:64, blk * 512:(blk + 1) * 512], ps[bk][64:128, :], [pk(bk)], ['kT1'])
            MS('vector', kT0[64:65, :], 1.0, ['kT0'])
            MS('vector', kT1[64:65, :], 1.0, ['kT1'])
            wj = next_wst()
            load_w(wst[wj][:, :, 0:64], C_KV + 3 * 128 + g * 64, 64, ('wst', wj))
            load_w(wst[wj][:, :, 64:128], C_KV + 5 * 128 + g * 64, 64, ('wst', wj))
            for t4 in range(NT // 4):
                bk = misc_bank()
                for q4 in range(4):
                    tt = t4 * 4 + q4
                    for c in range(8):
                        MM(ps[bk][:, q4 * 128:(q4 + 1) * 128], uT[:, c, tt * 128:(tt + 1) * 128], wst[wj][:, c, :],
                           c == 0, c == 7, [('wst', wj), ('uT', t4)], [pk(bk)])
                CP('vector', Vp3[:, t4 * 4:(t4 + 1) * 4, :], ps[bk][:, :].rearrange("p (t c) -> p t c", c=128),
                   [pk(bk)], ['Vp'])
            load_w(WQZ[:, :, 0:256], C_QA + g * 256, 256, 'WQZ')
            load_w(WQZ[:, :, 256:512], C_ZA + g * 256, 256, 'WQZ')
            for h4 in range(4):
                for b2 in range(2):
                    P.dma('sync', QA[b2][h4][64:65, :], cd['cq'][4 * g + h4:4 * g + h4 + 1, :], reads=(),
                          writes=[('QA', b2, h4)])

            def nsa_pre(g, i):
                b2 = i % 2
                for pp in range(2):
                    bk = misc_bank()
                    proj_fm(WQZ, 'WQZ', pp * 128, 128, i, bk)
                    CP('vector', QA[b2][2 * pp][0:64, :], ps[bk][0:64, :], [pk(bk)], [('QA', b2, 2 * pp)])
                    CP('vector', QA[b2][2 * pp + 1][0:64, :], ps[bk][64:128, :], [pk(bk)], [('QA', b2, 2 * pp + 1)])
                    yield
                for pp in range(2):
                    bk = misc_bank()
                    proj_fm(WQZ, 'WQZ', 256 + pp * 128, 128, i, bk)
                    silu_pair(bk, SZ[b2][pp], ('xn', pp, b2))
                    yield
                bk = misc_bank()
                for c in range(8):
                    MM(ps[bk][0:24, :], wg24[:, c, :], uT[:, c, i * 512:(i + 1) * 512], c == 0, c == 7,
                       ['wg24', ('uT', i)], [pk(bk)])
                w = WK[0]
                ACT(w[0:24, :], ps[bk][0:24, :], AF.Exp, [pk(bk)], [('WK', 0)], scale=-1.0)
                TS('vector', w[0:24, :], w[0:24, :], 1.0, None, ALU.add, None, [('WK', 0)], [('WK', 0)])
                RCP('vector', w[0:24, :], w[0:24, :], [('WK', 0)], [('WK', 0)])
                CP('vector', SGT[b2][0:24, :], w[0:24, :], [('WK', 0)], [('SGT', b2)])
                TT('vector', SGT[b2][32:56, :], w[0:24, :], SGT[b2][0:24, :], ALU.subtract, [('WK', 0), ('SGT', b2)],
                   [('SGT', b2)])
                yield
                a_list = [0] if (512 * i + 511) < (2048 + 31) else [0, 1]
                a_list = [a for a in a_list if a * 128 < ncm]
                for a in a_list:
                    thr = 512.0 * i - 2048.0 * a
                    TS('vector', XM[a][:], cs['xg'][:], thr, NEG, ALU.is_gt, ALU.mult, ['c_xg'], [('XM', a)])
                la = len(a_list)
                for h4 in range(4):
                    hl = h4 % 2
                    pp = h4 // 2
                    ha = 4 * g + h4
                    if hl == 0:
                        gb = misc_bank()
                        MM(ps[gb][:, :], cs['goh'][0:56, (g * 2 + pp) * 3 + 0, :], SGT[b2][0:56, :], True, True,
                           ['c_goh', ('SGT', b2)], [pk(gb)])
                        CP('vector', WKg[:], ps[gb][:, :], [pk(gb)], ['WKg'])
                    bkA = misc_bank()
                    bkB = misc_bank()
                    for ai, a in enumerate(a_list):
                        sb_ = ST[rr['st'] % 2]
                        rr['st'] += 1
                        pj = 4 + rr['ptc'] % 2
                        rr['ptc'] += 1
                        MM(ps[sb_][:, :], kcT[0:65, a * 128:(a + 1) * 128], QA[b2][h4][0:65, :], True, False,
                           ['kcT', ('QA', b2, h4)], [pk(sb_)])
                        MM(ps[sb_][:, :], cs['ident_b'][:], XM[a][:], False, True, ['c_ident_b', ('XM', a)], [pk(sb_)])
                        ACT(PT[pj][:], ps[sb_][:, :], AF.Exp, [pk(sb_)] + CK, [('PT', pj)],
                            bias=cs['clb'][:, ha, a, i:i + 1], scale=SCALE)
                        MM(ps[bkA][:, :], vcx[:, a, :], PT[pj][:], ai == 0, ai == la - 1, ['vcx', ('PT', pj)], [pk(bkA)])
                        MM(ps[bkB][0:64, :], cs['ov'][:, a, :], PT[pj][:], ai == 0, ai == la - 1, ['c_ov', ('PT', pj)],
                           [pk(bkB)])
                    wr = WK[3]
                    TS('vector', wr[0:64, :], ps[bkA][64:128, :], EPSL, None, ALU.max, None, [pk(bkA)], [('WK', 3)])
                    TS('vector', wr[64:128, :], ps[bkA][64:128, :], EPSL, None, ALU.max, None, [pk(bkA)], [('WK', 3)])
                    RCP('vector', wr[:, :], wr[:, :], [('WK', 3)], [('WK', 3)])
                    if h4 == 0:
                        TT('vector', impacc[0:64, :], ps[bkB][0:64, :], wr[0:64, :], ALU.mult, [pk(bkB), ('WK', 3)],
                           ['impacc'])
                    else:
                        TT('vector', WK[4][0:64, :], ps[bkB][0:64, :], wr[0:64, :], ALU.mult, [pk(bkB), ('WK', 3)],
                           [('WK', 4)])
                        TT('vector', impacc[0:64, :], impacc[0:64, :], WK[4][0:64, :], ALU.add, ['impacc', ('WK', 4)],
                           ['impacc'])
                    r0 = 64 * hl
                    TT('vector', WK[1][r0:r0 + 64, :], WKg[r0:r0 + 64, :], wr[r0:r0 + 64, :], ALU.mult, ['WKg', ('WK', 3)],
                       [('WK', 1)])
                    TT('vector', ACC[b2][pp][r0:r0 + 64, :], ps[bkA][0:64, :], WK[1][r0:r0 + 64, :], ALU.mult,
                       [pk(bkA), ('WK', 1)], [('xt', pp, b2)])
                    yield
                bk = misc_bank()
                for qs in range(4):
                    TR(ps[bk][:, qs * 64:(qs + 1) * 64], impacc[0:64, qs * 128:(qs + 1) * 128], cs['ident_f'][0:64, 0:64],
                       ['impacc', 'c_ident_f'], [pk(bk)])
                for qs in range(4):
                    ti = 4 * i + qs
                    TT('vector', T1[:, qs, :], ps[bk][:, qs * 64:(qs + 1) * 64],
                       cs['abase'][:, 64 - 2 * ti:128 - 2 * ti], ALU.add, [pk(bk), 'c_abase'], ['T1'])
                MS('vector', T1[:, :, 0:1], BIGF, ['T1'])
                for qs in range(4):
                    P.op('vector', (lambda q_: lambda e: e.max(out=M8a[:], in_=T1[:, q_, :]))(qs), ['T1'], ['M8a'])
                    P.op('vector', (lambda q_: lambda e: e.match_replace(out=T2[:, q_, :], in_to_replace=M8a[:],
                                                                        in_values=T1[:, q_, :], imm_value=-1e9))(qs),
                         ['T1', 'M8a'], ['T2'])
                    P.op('vector', (lambda q_: lambda e: e.max(out=M8b[:], in_=T2[:, q_, :]))(qs), ['T2'], ['M8b'])
                    TS('vector', T2[:, qs, :], T1[:, qs, :], M8b[:, 7:8], NEG, ALU.is_lt, ALU.mult, ['T1', 'M8b'], ['T2'])
                yield
                yield
                bk = misc_bank()
                for qs in range(4):
                    TR(ps[bk][0:64, qs * 128:(qs + 1) * 128], T2[:, qs, :], cs['ident_f'][:], ['T2', 'c_ident_f'],
                       [pk(bk)])
                CP('vector', selT[b2][0:64, :], ps[bk][0:64, :], [pk(bk)], [('selT', b2)])
                yield

            gen = nsa_pre(g, 0)
            exhaust(gen)
            for i in range(NQT):
                b2 = i % 2
                gen = nsa_pre(g, i + 1) if i + 1 < NQT else None
                for pp in range(2):
                    for br in (1, 2):
                        tiles = []
                        OB, LB = OLS[rr['ol'] % 2]
                        rr['ol'] += 1
                        if br == 1:
                            kts = list(range(0, 4 * i + 4))
                        else:
                            kts = [kt for kt in range(4 * i - 4, 4 * i + 4) if kt >= 0]
                        for kt in kts:
                            for hl in range(2):
                                h4 = 2 * pp + hl
                                ha = 4 * g + h4
                                hk = [k_ for k_ in kts if k_ >= kt_lo(ha, i)]
                                if kt not in hk:
                                    continue
                                ki = hk.index(kt)
                                nk = len(hk)
                                kT = kT0 if br == 1 else kT1
                                kkey = 'kT0' if br == 1 else 'kT1'
                                smm = [(kT[0:65, kt * 128:(kt + 1) * 128], QA[b2][h4][0:65, :], [kkey, ('QA', b2, h4)])]
                                if br == 1:
                                    smm.append((cs['e128'][:, kt * 128:(kt + 1) * 128], selT[b2][:, :],
                                                ['c_e128', ('selT', b2)]))
                                    if kt >= 4 * i:
                                        r = kt - 4 * i
                                        smm.append((cs['ident_b'][:], cs['cmb'][:, 384 - 128 * r:896 - 128 * r],
                                                    ['c_ident_b', 'c_cmb']))
                                else:
                                    r = kt - (4 * i - 4)
                                    if r < 4:
                                        smm.append((cs['ident_b'][:], cs['omb'][:, 384 - 128 * r:896 - 128 * r],
                                                    ['c_ident_b', 'c_omb']))
                                    else:
                                        r -= 4
                                        smm.append((cs['ident_b'][:], cs['cmb'][:, 384 - 128 * r:896 - 128 * r],
                                                    ['c_ident_b', 'c_cmb']))
                                vcol = 0 if br == 1 else 64
                                pv = [(ps[OB][64 * hl:64 * hl + 64, :], Vp3[:, kt, vcol:vcol + 64], (0, 64 * hl),
                                       ki == 0, ki == nk - 1, ['Vp'], pk(OB)),
                                      (ps[LB][64 * hl:64 * hl + 64, :], ones_b[:, 0:64], (0, 64 * hl),
                                       ki == 0, ki == nk - 1, ['ones'], pk(LB))]
                                tiles.append(dict(smm=smm, bias=cs['alb'][:, ha, kt - 4 * i + 32:kt - 4 * i + 33], pv=pv))
                        attend(tiles, gen)
                        gb = misc_bank()
                        MM(ps[gb][:, :], cs['goh'][0:56, (g * 2 + pp) * 3 + br, :], SGT[b2][0:56, :], True, True,
                           ['c_goh', ('SGT', b2)], [pk(gb)])
                        combine_branch(ACC[b2][pp], ('xt', pp, b2), False, gb, [(0, 128, LB)], OB)
                    yj = rr['wk'] % 2
                    rr['wk'] += 1
                    TT('vector', YST[yj][:], ACC[b2][pp], SZ[b2][pp], ALU.mult, [('xt', pp, b2), ('xn', pp, b2)],
                       [('YST', yj)])
                    P.dma('sync', ysc_d[g * 2 + pp, :, i * 512:(i + 1) * 512], YST[yj][:], reads=[('YST', yj)],
                          writes=[('ysc', g * 2 + pp, i)])
                exhaust(gen)

        nb = S // 256
        for j in range(4):
            wj = next_wst()
            load_w(wst[wj][:], C_QB + 512 + j * 128, 128, ('wst', wj))
            for blk in range(NQT):
                bk = misc_bank()
                proj_fm(wst[wj], ('wst', wj), 0, 128, blk, bk)
                CP('vector', kT0[0:64, blk * 512:(blk + 1) * 512], ps[bk][0:64, :], [pk(bk)], ['kT0'])
                CP('vector', kT1[0:64, blk * 512:(blk + 1) * 512], ps[bk][64:128, :], [pk(bk)], ['kT1'])
                P.op('vector', (lambda bk_, blk_: lambda e: e.reduce_sum(
                    out=kbarf[:, 2 * blk_:2 * blk_ + 2], in_=ps[bk_][:, :].rearrange("p (b t) -> p b t", b=2),
                    axis=AX.X))(bk, blk), [pk(bk)], ['kbarf'])
            MS('vector', kT0[64:65, :], 1.0, ['kT0'])
            MS('vector', kT1[64:65, :], 1.0, ['kT1'])
            TS('vector', kbar[0:64, 0, 0:nb], kbarf[0:64, 0:nb], 1.0 / 256, None, ALU.mult, None, ['kbarf'], ['kbar'])
            TS('vector', kbar[0:64, 1, 0:nb], kbarf[64:128, 0:nb], 1.0 / 256, None, ALU.mult, None, ['kbarf'], ['kbar'])
            wj = next_wst()
            load_w(wst[wj][:], C_QB + 1024 + j * 128, 128, ('wst', wj))
            for t4 in range(NT // 4):
                bk = misc_bank()
                for q4 in range(4):
                    tt = t4 * 4 + q4
                    for c in range(8):
                        MM(ps[bk][:, q4 * 128:(q4 + 1) * 128], uT[:, c, tt * 128:(tt + 1) * 128], wst[wj][:, c, :],
                           c == 0, c == 7, [('wst', wj), ('uT', t4)], [pk(bk)])
                CP('vector', Vp3[:, t4 * 4:(t4 + 1) * 4, :], ps[bk][:, :].rearrange("p (t c) -> p t c", c=128),
                   [pk(bk)], ['Vp'])
            load_w(WQZ[:, :, 0:128], C_QB + j * 128, 128, 'WQZ')
            load_w(WQZ[:, :, 128:256], C_ZB + j * 128, 128, 'WQZ')
            for hl in range(2):
                for b2 in range(2):
                    P.dma('sync', QA[b2][hl][64:65, :], cd['cq'][8 + 2 * j + hl:8 + 2 * j + hl + 1, :], reads=(),
                          writes=[('QA', b2, hl)])

            def moba_pre(j, i):
                b2 = i % 2
                bk = misc_bank()
                proj_fm(WQZ, 'WQZ', 0, 128, i, bk)
                CP('vector', QA[b2][0][0:64, :], ps[bk][0:64, :], [pk(bk)], [('QA', b2, 0)])
                CP('vector', QA[b2][1][0:64, :], ps[bk][64:128, :], [pk(bk)], [('QA', b2, 1)])
                yield
                bk = misc_bank()
                proj_fm(WQZ, 'WQZ', 128, 128, i, bk)
                silu_pair(bk, SZ[b2][0], ('xn', 0, b2))
                yield
                bk = misc_bank()
                for hl in range(2):
                    for qs in range(4):
                        MM(ps[bk][:, (hl * 4 + qs) * 16:(hl * 4 + qs) * 16 + nb], QA[b2][hl][0:64, qs * 128:(qs + 1) * 128],
                           kbar[0:64, hl, 0:nb], True, True, ['kbar', ('QA', b2, hl)], [pk(bk)])
                for hl in range(2):
                    for qs in range(4):
                        cur = (4 * i + qs) // 2
                        e8 = hl * 4 + qs
                        TT('vector', GT[:, e8, 0:nb], ps[bk][:, e8 * 16:e8 * 16 + nb], cs['bbase'][:, 16 - cur:16 - cur + nb],
                           ALU.add, [pk(bk), 'c_bbase'], ['GT'])
                        if nb < 16:
                            MS('vector', GT[:, e8, nb:16], -BIGF, ['GT'])
                        P.op('vector', (lambda e_: lambda e: e.max(out=M8a[:], in_=GT[:, e_, :]))(e8), ['GT'], ['M8a'])
                        TS('vector', GT[:, e8, :], GT[:, e8, :], M8a[:, 2:3], NEG, ALU.is_lt, ALU.mult, ['GT', 'M8a'], ['GT'])
                        TT('vector', GT[:, e8, :], GT[:, e8, :], cs['obase'][:, 16 - cur:32 - cur], ALU.max,
                           ['GT', 'c_obase'], ['GT'])
                yield
                yield
                for hl in range(2):
                    bk = misc_bank()
                    for qs in range(4):
                        TR(ps[bk][0:16, qs * 128:(qs + 1) * 128], GT[:, hl * 4 + qs, :], cs['ident_f'][:],
                           ['GT', 'c_ident_f'], [pk(bk)])
                    CP('vector', selTm[b2][hl][0:16, :], ps[bk][0:16, :], [pk(bk)], [('selTm', b2, hl)])
                    yield

            gen = moba_pre(j, 0)
            exhaust(gen)
            for i in range(NQT):
                b2 = i % 2
                gen = moba_pre(j, i + 1) if i + 1 < NQT else None
                tiles = []
                OB, LB = OLS[rr['ol'] % 2]
                rr['ol'] += 1
                kts = list(range(0, 4 * i + 4))
                for kt in kts:
                    for hl in range(2):
                        hh = 8 + 2 * j + hl
                        hk = [k_ for k_ in kts if k_ >= kt_lo(hh, i)]
                        if kt not in hk:
                            continue
                        ki = hk.index(kt)
                        nk = len(hk)
                        kT = kT0 if hl == 0 else kT1
                        kkey = 'kT0' if hl == 0 else 'kT1'
                        smm = [(kT[0:65, kt * 128:(kt + 1) * 128], QA[b2][hl][0:65, :], [kkey, ('QA', b2, hl)]),
                               (cs['ohm'][:, kt // 2, :], selTm[b2][hl][:, :], ['c_ohm', ('selTm', b2, hl)])]
                        if kt >= 4 * i:
                            smm.append((cs['ident_b'][:], cs['mmb'][:, kt - 4 * i, :], ['c_ident_b', 'c_mmb']))
                        pv = [(ps[OB][64 * hl:64 * hl + 64, :], Vp3[:, kt, 64 * hl:64 * hl + 64], (0, 64 * hl),
                               ki == 0, ki == nk - 1, ['Vp'], pk(OB)),
                              (ps[LB][64 * hl:64 * hl + 64, :], ones_b[:, 0:64], (0, 64 * hl),
                               ki == 0, ki == nk - 1, ['ones'], pk(LB))]
                        tiles.append(dict(smm=smm, bias=cs['alb'][:, hh, kt - 4 * i + 32:kt - 4 * i + 33], pv=pv))
                attend(tiles, gen, step=4)
                combine_branch(ACC[b2][0], ('xt', 0, b2), True, None, [(0, 128, LB)], OB)
                yj = rr['wk'] % 2
                rr['wk'] += 1
                TT('vector', YST[yj][:], ACC[b2][0], SZ[b2][0], ALU.mult, [('xt', 0, b2), ('xn', 0, b2)], [('YST', yj)])
                P.dma('sync', ysc_d[4 + j, :, i * 512:(i + 1) * 512], YST[yj][:], reads=[('YST', yj)],
                      writes=[('ysc', 4 + j, i)])
                exhaust(gen)

        P.op('sync', lambda e: e.nop(), (), ['Vp', ('wgs', 0), ('wgs', 1), 'WQZ', ('wab', 0), ('wab', 1)])
        P.dma('sync', gpost, npost_d, reads=(), writes=['impacc', 'WKg'])
        for i in range(NQT):
            P.dma('sync', ysl[:, :, 0:512], ysc_d[:, :, i * 512:(i + 1) * 512].rearrange("c p t -> p c t"),
                  reads=[('ysc', jj, i) for jj in range(8)], writes=['kT1'])
            for fc in range(8):
                gj = fc % 2
                P.dma('sync', wgs[:, gj, :, 0:128], wcols[:, :, C_MG + fc * 128:C_MG + (fc + 1) * 128], reads=WBK,
                      writes=[('wgs', gj)])
                P.dma('sync', wgs[:, gj, :, 128:256], wcols[:, :, C_MG + 1024 + fc * 128:C_MG + 1024 + (fc + 1) * 128],
                      reads=WBK, writes=[('wgs', gj)])
                for ab in range(2):
                    P.dma('sync', wab[:, gj, 4 * ab:4 * ab + 4, :],
                          wabf_d[ab].rearrange("(j p) n -> p j n", p=128)[:, :, fc * 128:(fc + 1) * 128],
                          reads=['wabf0', 'wabf1'], writes=[('wab', gj)])
                for ab in range(2):
                    bg = misc_bank()
                    for c in range(8):
                        MM(ps[bg][:, :], wgs[:, gj, c, ab * 128:(ab + 1) * 128], uT[:, c, i * 512:(i + 1) * 512],
                           c == 0, c == 7, [('wgs', gj), ('uT', i)], [pk(bg)])
                    w = WK[ab]
                    ACT(w[:], ps[bg][:, :], AF.Exp, [pk(bg)], [('WK', ab)], scale=-1.0)
                    ACT(w[:], w[:], AF.Ln, [('WK', ab), 'ones1'], [('WK', ab)], bias=ones1[:, 0:1], scale=1.0)
                    ACT(w[:], w[:], AF.Exp, [('WK', ab)], [('WK', ab)], scale=-1.0)
                    bm = OLS[fc % 2][ab]
                    for jj in range(4):
                        MM(ps[bm][:, :], wab[:, gj, 4 * ab + jj, :], ysl[:, 4 * ab + jj, 0:512], jj == 0, jj == 3,
                           [('wab', gj), 'kT1'], [pk(bm)])
                    TT('vector', w[:], ps[bm][:, :], w[:], ALU.mult, [pk(bm), ('WK', ab)], [('WK', ab)])
                TT('vector', mT[:, fc, 0:512], WK[0][:], WK[1][:], ALU.add, [('WK', 0), ('WK', 1)], ['kT0'])
            for ts_ in range(4):
                tt = i * 4 + ts_
                b = tt % 2
                P.dma('sync', xt[b][:], x_d[s, tt * 128:(tt + 1) * 128, :], reads=(), writes=XK(b))
                banks = [ST[0], ST[1]]
                for hf in range(2):
                    for fc in range(8):
                        MM(ps[banks[hf]][:, :], mT[:, fc, ts_ * 128:(ts_ + 1) * 128], wo_s[:, fc, hf * 512:(hf + 1) * 512],
                           fc == 0, fc == 7, ['wo', 'kT0'], [pk(banks[hf])])
                for hf in range(2):
                    P.op('scalar', (lambda o_, i_: lambda e: e.copy(out=o_, in_=i_))(WK[2 + hf][:], ps[banks[hf]][:, :]),
                         [pk(banks[hf])], [('WK', 2 + hf)])
                    P.op('vector', (lambda o_, i_, a_: lambda e: e.scalar_tensor_tensor(
                        out=o_, in0=i_, scalar=1.0, in1=i_, op0=ALU.mult, op1=ALU.mult, accum_out=a_))(
                        YST[hf][:], WK[2 + hf][:], ssq2[:, hf:hf + 1]), [('WK', 2 + hf)], [('YST', hf), ('ssq2', hf)])
                TT('vector', ssq[:, b:b + 1], ssq2[:, 0:1], ssq2[:, 1:2], ALU.add, [('ssq2', 0), ('ssq2', 1)], [('ssq', b)])
                ACT(ssq[:, b:b + 1], ssq[:, b:b + 1], AF.Ln, [('ssq', b), 'eps'], [('ssq', b)], bias=eps_t[:, 0:1],
                    scale=1.0 / D)
                ACT(ssq[:, b:b + 1], ssq[:, b:b + 1], AF.Exp, [('ssq', b)], [('ssq', b)], scale=-0.5)
                for hf in range(2):
                    STT(xn[b][:, hf * 512:(hf + 1) * 512], WK[2 + hf][:], ssq[:, b:b + 1],
                        gpost[:, hf * 512:(hf + 1) * 512], ALU.mult, ALU.mult, [('WK', 2 + hf), ('ssq', b), 'impacc', 'WKg'],
                        [('xn', b, hf)])
                TT('gpsimd', xn[b][:], xn[b][:], xt[b][:], ALU.add, NK(b) + XK(b), NK(b))
                P.dma('sync', out_d[s, tt * 128:(tt + 1) * 128, :], xn[b][:], reads=NK(b), writes=[('out', tt)])
        P.op('sync', lambda e: e.nop(), (), ['Vp', ('wgs', 0), ('wgs', 1), 'WQZ', ('wab', 0), ('wab', 1)])
    P.finish()
    return nc, P


def host_inputs(inputs, S):
    c = make_consts()
    f = lambda a: np.ascontiguousarray(np.asarray(a, dtype=np.float32))
    w1k = f(inputs['cmp_w1_k'])[0].transpose(1, 0, 2)
    w1v = f(inputs['cmp_w1_v'])[0].transpose(1, 0, 2)
    shared = {
        'w_in': f(inputs['w_in'])[0],
        'w1s': np.ascontiguousarray(np.concatenate([w1k, w1v], 0)),
        'w2s': np.ascontiguousarray(np.stack([f(inputs['cmp_w2_k'])[0], f(inputs['cmp_w2_v'])[0]], 1)),
        'pet': np.ascontiguousarray(np.concatenate([f(inputs['cmp_pe_k'])[0].T, f(inputs['cmp_pe_v'])[0].T], 0)),
        'w_a': f(inputs['w_branch_a'])[0],
        'w_b': f(inputs['w_branch_b'])[0],
        'w_o': f(inputs['w_o'])[0],
        'npre_bc': np.ascontiguousarray(np.broadcast_to(f(inputs['norm_pre'])[0][None, :], (128, D))),
        'npost_bc': np.ascontiguousarray(np.broadcast_to(f(inputs['norm_post'])[0][None, :], (128, D))),
    }
    for k, v in c.items():
        shared['c_' + k] = v
    return shared


def kernel(**inputs):
    x = np.ascontiguousarray(np.asarray(inputs['x'], dtype=np.float32))
    B, S, _ = x.shape
    ncores = 8
    nseq = B // ncores
    nc, _ = build(S, nseq)
    shared = host_inputs(inputs, S)
    in_maps = []
    for c in range(ncores):
        m = dict(shared)
        m['x'] = np.ascontiguousarray(x[c * nseq:(c + 1) * nseq])
        in_maps.append(m)
    res = run_bass_kernel_spmd(nc, in_maps, core_ids=list(range(ncores)))
    return np.concatenate([np.asarray(r['out']) for r in res.results], axis=0).astype(np.float32)
```

```python
import contextlib
import numpy as np
import ml_dtypes
import concourse.bass as bass
import concourse.mybir as mybir
from concourse.bass_utils import run_bass_kernel_spmd

F32 = mybir.dt.float32
BF16 = mybir.dt.bfloat16
ALU = mybir.AluOpType
AF = mybir.ActivationFunctionType
AX = mybir.AxisListType
NPBF = ml_dtypes.bfloat16

D = 1024
DEBUG_SEP = False
NCOL = 5912
NEG = -30000.0
SCALE = 0.125
BIGF = 1.0e4
EPSL = 1.0e-30
C_QA, C_KV, C_GA, C_ZA, C_QB, C_ZB, C_MG = 0, 512, 1280, 1304, 1816, 3352, 3864
SLOPES = [2.0 ** (-(i + 1) / 2.0) for i in range(16)]
SL_HH = [SLOPES[2 * h] for h in range(8)] + [SLOPES[2 * h + 1] for h in range(8)]


class Prog:
    EPOCH = 20000
    NDMA = 8

    def __init__(self, nc):
        self.nc = nc
        self.ops = []
        self.lastw = {}
        self.readers = {}
        self.stack = contextlib.ExitStack()
        self.sb_bytes = 0

    def sb(self, name, shape, dtype):
        n = 1
        for s in shape[1:]:
            n *= s
        self.sb_bytes += n * (4 if dtype == F32 else 2)
        return self.stack.enter_context(self.nc.sbuf_tensor('sb_' + name, list(shape), dtype))

    def ps(self, name, shape=(128, 512), dtype=F32):
        return self.stack.enter_context(self.nc.psum_tensor(name, list(shape), dtype))

    def _deps(self, idx, reads, writes):
        deps = set()
        for k in reads:
            w = self.lastw.get(k)
            if w is not None:
                deps.add(w)
        for k in writes:
            w = self.lastw.get(k)
            if w is not None:
                deps.add(w)
            for r in self.readers.get(k, ()):
                deps.add(r)
        for k in reads:
            self.readers.setdefault(k, []).append(idx)
        for k in writes:
            self.lastw[k] = idx
            self.readers[k] = []
        deps.discard(idx)
        return deps

    def op(self, eng, fn, reads=(), writes=()):
        idx = len(self.ops)
        deps = self._deps(idx, reads, writes)
        self.ops.append(dict(eng=eng, fn=fn, deps=deps, dma=False, wkeys=set(writes), rkeys=set(reads)))
        return idx

    def dma(self, eng, out, in_, reads=(), writes=()):
        idx = len(self.ops)
        deps = self._deps(idx, reads, writes)
        self.ops.append(dict(eng=eng, fn=lambda e: e.dma_start(out=out, in_=in_), deps=deps, dma=True,
                             wkeys=set(writes), rkeys=set(reads)))
        return idx

    def finish(self):
        nc = self.nc
        ops = self.ops
        needed = set()
        for i, o in enumerate(ops):
            nd = set()
            for d in o['deps']:
                od = ops[d]
                if od['eng'] == o['eng'] and not od['dma'] and not o['dma']:
                    if o['eng'] == 'tensor':
                        continue
                    if not ((od['wkeys'] & o['rkeys']) or (od['wkeys'] & o['wkeys'])):
                        continue
                nd.add(d)
            o['deps'] = nd
            for d in nd:
                if not ops[d]['dma']:
                    needed.add(d)
        engs = ['tensor', 'vector', 'scalar', 'gpsimd', 'sync']
        cnt = {e: 0 for e in engs}
        dcnt = {e: 0 for e in engs}
        nep = {e: 0 for e in engs}
        for i, o in enumerate(ops):
            e = o['eng']
            if o['dma']:
                d = dcnt[e]
                dcnt[e] += 1
                o['sig'] = (('dma', e, d % self.NDMA), 16 * (d // self.NDMA + 1), 16)
                o['prev'] = (('dma', e, d % self.NDMA), 16 * (d // self.NDMA)) if d >= self.NDMA else None
            elif i in needed:
                c = cnt[e]
                cnt[e] += 1
                ep = c // self.EPOCH
                nep[e] = max(nep[e], ep + 1)
                o['sig'] = (('cmp', e, ep), c % self.EPOCH + 1, 1)
            else:
                o['sig'] = None
        sems = {}
        for e in engs:
            for ep in range(nep[e]):
                sems[('cmp', e, ep)] = self.stack.enter_context(nc.semaphore(f"s_{e}_{ep}"))
            for j in range(min(self.NDMA, dcnt[e])):
                sems[('dma', e, j)] = self.stack.enter_context(nc.semaphore(f"d_{e}_{j}"))
        self.n_instr = {e: 0 for e in engs}
        with nc.Block() as block:
            def make(ename):
                def body(eng):
                    waited = {}
                    lastdma = {}
                    for o in ops:
                        if o['eng'] != ename:
                            continue
                        best = {}
                        for d in o['deps']:
                            s = ops[d]['sig']
                            if s[1] > best.get(s[0], 0):
                                best[s[0]] = s[1]
                        if o['dma'] and o['prev'] is not None:
                            k, v = o['prev']
                            if v > best.get(k, 0):
                                best[k] = v
                        for k, v in best.items():
                            if waited.get(k, 0) >= v:
                                continue
                            eng.wait_ge(sems[k], v)
                            waited[k] = v
                            self.n_instr[ename] += 1
                        ins = o['fn'](eng)
                        self.n_instr[ename] += 1
                        if o['sig'] is not None:
                            ins.then_inc(sems[o['sig'][0]], o['sig'][2])
                            if o['dma']:
                                lastdma[o['sig'][0]] = o['sig'][1]
                    for k, v in lastdma.items():
                        if waited.get(k, 0) < v:
                            eng.wait_ge(sems[k], v)
                return body
            for ename in engs:
                if any(o['eng'] == ename for o in ops):
                    getattr(block, ename)(make(ename))
        self.stack.close()


def make_consts():
    c = {}
    c['ident_f'] = np.eye(128, dtype=np.float32)
    c['ident_b'] = np.eye(128).astype(NPBF)
    p = np.arange(128)[:, None]
    fp = np.arange(896)[None, :]
    c['cmb'] = np.where(p > fp - 384, NEG, 0.0).astype(NPBF)
    c['omb'] = np.where(fp - 384 >= p, NEG, 0.0).astype(NPBF)
    kk = np.arange(4)[None, :, None] * 128 + np.arange(128)[:, None, None]
    f = np.arange(512)[None, None, :]
    mm = np.where(kk // 256 == f // 256, np.where(kk > f, NEG, 0.0), np.where(f // 256 > kk // 256, 0.0, NEG))
    c['mmb'] = mm.astype(NPBF)
    k2 = np.arange(4096)[None, :]
    c['eslc'] = (k2 // 64 == np.arange(64)[:, None]).astype(NPBF)
    c['emoba'] = (k2 // 256 == np.arange(16)[:, None]).astype(NPBF)
    c['cqb'] = np.ascontiguousarray(np.broadcast_to(np.arange(512, dtype=np.float32)[None, :], (128, 512)))
    alb = np.zeros((128, 16, 36), np.float32)
    for hh in range(16):
        for di in range(36):
            alb[:, hh, di] = SL_HH[hh] * (np.arange(128) + 128.0 * (di - 32))
    c['alb'] = alb
    clb = np.zeros((128, 8, 2, 8), np.float32)
    for ha in range(8):
        for a in range(2):
            for i in range(8):
                clb[:, ha, a, i] = SL_HH[ha] * (2048.0 * a + 16.0 * np.arange(128) + 31.0 - 512.0 * i)
    c['clb'] = clb
    cq = np.zeros((16, 512), np.float32)
    for hh in range(16):
        cq[hh] = -SL_HH[hh] * np.arange(512) / SCALE
    c['cq'] = cq.astype(NPBF)
    goh = np.zeros((56, 12, 128), np.float32)
    for g in range(2):
        for pp in range(2):
            for k in range(3):
                idx = (g * 2 + pp) * 3 + k
                ra = k * 8 + g * 4 + 2 * pp
                goh[ra, idx, 0:64] = 1.0
                goh[32 + ra, idx, 0:64] = 1.0
                goh[ra + 1, idx, 64:128] = 1.0
                goh[32 + ra + 1, idx, 64:128] = 1.0
    c['goh'] = goh.astype(NPBF)
    n_cmp, n_slc = 255, 64
    cs = np.arange(n_cmp) * 16
    ce = cs + 31
    ss = np.arange(n_slc) * 64
    se = ss + 63
    ov = ((cs[:, None] <= se[None, :]) & (ce[:, None] >= ss[None, :])).astype(np.float32)
    ovp = np.zeros((256, 64), np.float32)
    ovp[:255] = ov
    c['ov'] = np.ascontiguousarray(ovp.reshape(2, 128, 64).transpose(1, 0, 2)).astype(NPBF)
    q = np.arange(128)[:, None]
    m = np.arange(128)[None, :]
    jr = m - 64
    cr = q // 64
    ab = np.where((jr == cr) | (jr == cr - 1), BIGF, np.where(jr > cr, -BIGF, 0.0))
    c['abase'] = ab.astype(np.float32)
    m2 = np.arange(32)[None, :]
    c['bbase'] = np.broadcast_to(np.where(m2 >= 16, -BIGF, 0.0), (128, 32)).astype(np.float32).copy()
    c['obase'] = np.broadcast_to(np.where(m2 == 16, 0.0, 2 * NEG), (128, 32)).astype(np.float32).copy()
    c['xg'] = (16.0 * np.arange(128)[:, None] + 31.0 - np.arange(512)[None, :]).astype(np.float32)
    return c


DRAM_ONLY = ('eslc', 'emoba', 'cq')
CONST_DT = dict(ident_f=F32, ident_b=BF16, cmb=BF16, omb=BF16, mmb=BF16, eslc=BF16, emoba=BF16, cqb=F32, alb=F32, clb=F32,
                cq=BF16, goh=BF16, ov=BF16, abase=F32, bbase=F32, obase=F32, xg=F32)


def build(S, NSEQ, dbg=()):
    nc = bass.Bass("TRN2", target_bir_lowering=False)
    NT = S // 128
    NQT = S // 512
    consts = make_consts()

    def din(name, shape, dt=F32):
        return nc.dram_tensor(name, list(shape), dt, kind="ExternalInput").ap()

    x_d = din("x", [NSEQ, S, D])
    win_d = din("w_in", [D, NCOL])
    w1s_d = din("w1s", [128, 32, 128])
    w2s_d = din("w2s", [128, 2, 64])
    pet_d = din("pet", [128, 32])
    wa_d = din("w_a", [512, D])
    wb_d = din("w_b", [512, D])
    wo_d = din("w_o", [D, D])
    npre_d = din("npre_bc", [128, D])
    npost_d = din("npost_bc", [128, D])
    cd = {k: din("c_" + k, list(v.shape), CONST_DT[k]) for k, v in consts.items()}
    out_d = nc.dram_tensor("out", [NSEQ, S, D], F32, kind="ExternalOutput").ap()
    wbf_d = nc.dram_tensor("wbf", [D, NCOL], BF16, kind="Internal").ap()
    ysc_d = nc.dram_tensor("yscr", [8, 128, S], BF16, kind="Internal").ap()
    wabf_d = nc.dram_tensor("wabf", [2, 512, D], BF16, kind="Internal").ap()
    dbg_d = {}
    for name, shape in dbg:
        dbg_d[name] = nc.dram_tensor("dbg_" + name, list(shape), F32, kind="ExternalOutput").ap()

    P = Prog(nc)
    cs = {k: P.sb("k_" + k, list(v.shape), CONST_DT[k]) for k, v in consts.items() if k not in DRAM_ONLY}
    uT = P.sb("uT", [128, 8, S], BF16)
    SA = max(S, 4096)
    kT0 = P.sb("kT0", [128, SA], BF16)
    kT1 = P.sb("kT1", [128, SA], BF16)
    Vp = P.sb("Vp", [128, 2 * SA], BF16)
    kcT = P.sb("kcT", [128, 256], BF16)
    vcx = P.sb("vcx", [128, 2, 128], BF16)
    hcm = P.sb("hcm", [128, 2, 256], BF16)
    QA = [[P.sb(f"QA{b}_{h}", [128, 512], BF16) for h in range(4)] for b in range(2)]
    PT = [P.sb(f"PT{j}", [128, 512], BF16) for j in range(6)]
    XM = [P.sb(f"XM{j}", [128, 512], BF16) for j in range(2)]
    WKB = P.sb("WKB", [128, 7, 512], F32)
    WK = [WKB[:, j, :] for j in range(5)]
    YST = [P.sb(f"YST{j}", [128, 512], BF16) for j in range(2)]
    SGT = [P.sb(f"SGT{j}", [64, 512], BF16) for j in range(2)]
    T1 = P.sb("T1", [128, 4, 64], F32)
    T2 = P.sb("T2", [128, 4, 64], F32)
    M8a = P.sb("M8a", [128, 8], F32)
    M8b = P.sb("M8b", [128, 8], F32)
    GT = P.sb("GT", [128, 8, 16], F32)
    kbarf = P.sb("kbarf", [128, 16], F32)
    kbar = P.sb("kbar", [64, 2, 16], BF16)
    wst0 = P.sb("wst0", [128, 8, 128], BF16)
    wst = [wst0, wst0]
    WQZ = P.sb("WQZ", [128, 8, 512], BF16)
    wg24 = P.sb("wg24", [128, 8, 24], BF16)
    w2s = P.sb("w2s", [128, 2, 64], BF16)
    pet = P.sb("pet", [128, 32], BF16)
    hb = P.sb("hb", [128, 2], F32)
    xt = [P.sb(f"xt{j}", [128, D], F32) for j in range(2)]
    xn = [P.sb(f"xn{j}", [128, D], F32) for j in range(2)]
    gpre = WKB[:, 5:7, :].rearrange("p a n -> p (a n)")
    gpost = gpre
    ACC = [[xt[j][:, b_ * 512:(b_ + 1) * 512] for j in range(2)] for b_ in range(2)]
    SZ = [[xn[j][:, b_ * 512:(b_ + 1) * 512] for j in range(2)] for b_ in range(2)]
    if DEBUG_SEP:
        ACC = [[P.sb(f"ACCd{b_}{j}", [128, 512], F32)[:] for j in range(2)] for b_ in range(2)]
        SZ = [[P.sb(f"SZd{b_}{j}", [128, 512], F32)[:] for j in range(2)] for b_ in range(2)]
    impacc = WKB[:, 5, :]
    WKg = WKB[:, 6, :]
    ones1 = P.sb("ones1", [128, 1], F32)

    def XK(b_):
        return [('xt', b_, 0), ('xt', b_, 1)]

    def NK(b_):
        return [('xn', b_, 0), ('xn', b_, 1)]
    ssq = P.sb("ssq", [128, 2], F32)
    ssq2 = P.sb("ssq2", [128, 2], F32)
    eps_t = P.sb("eps_t", [128, 1], F32)
    ones_b = P.sb("ones_b", [128, 128], BF16)
    wab = WQZ[:].rearrange("p c (b n) -> p b c n", b=4)[:, 0:2]
    wo_s = P.sb("wo_s", [128, 8, D], BF16)
    ps = [P.ps(f"ps{j}") for j in range(8)]
    ST = [0, 1]
    OLS = [(2, 3), (4, 5)]
    MISC = [6, 7]
    Vp3 = Vp[:].rearrange("p (t c) -> p t c", c=256)
    mT = kT0[:].rearrange("p (c t) -> p c t", c=8)
    ysl = kT1[:].rearrange("p (c t) -> p c t", c=8)
    wgs = Vp[:, 0:SA].rearrange("p (b c n) -> p b c n", b=2, c=8)
    assert SA // 8 >= 512 and SA // 16 >= 256

    rr = {'misc': 0, 'st': 0, 'pt': 0, 'wst': 0, 'wk': 0, 'ol': 0, 'ptc': 0}

    def kt_lo(hh, i):
        lo = 0
        while SL_HH[hh] * (512 * i - 128 * lo - 127) > 110.0:
            lo += 1
        return lo

    def MM(out, lhsT, rhs, start, stop, reads, writes, tp=None):
        if tp is None:
            P.op('tensor', lambda e: e.matmul(out, lhsT=lhsT, rhs=rhs, start=start, stop=stop), reads, writes)
        else:
            P.op('tensor', lambda e: e.matmul(out, lhsT=lhsT, rhs=rhs, start=start, stop=stop, tile_position=tp),
                 reads, writes)

    def TR(out, in_, ident, reads, writes):
        P.op('tensor', lambda e: e.transpose(out, in_, ident), reads, writes)

    def ACT(out, in_, func, reads, writes, bias=None, scale=1.0):
        if bias is None:
            P.op('scalar', lambda e: e.activation(out=out, in_=in_, func=func, scale=scale), reads, writes)
        else:
            P.op('scalar', lambda e: e.activation(out=out, in_=in_, func=func, bias=bias, scale=scale), reads, writes)

    def TT(eng, out, in0, in1, op, reads, writes):
        P.op(eng, lambda e: e.tensor_tensor(out=out, in0=in0, in1=in1, op=op), reads, writes)

    def TS(eng, out, in0, s1, s2, op0, op1, reads, writes):
        if op1 is None:
            P.op(eng, lambda e: e.tensor_scalar(out=out, in0=in0, scalar1=s1, scalar2=None, op0=op0), reads, writes)
        else:
            P.op(eng, lambda e: e.tensor_scalar(out=out, in0=in0, scalar1=s1, scalar2=s2, op0=op0, op1=op1),
                 reads, writes)

    def STT(out, in0, scalar, in1, op0, op1, reads, writes):
        P.op('vector', lambda e: e.scalar_tensor_tensor(out=out, in0=in0, scalar=scalar, in1=in1, op0=op0, op1=op1),
             reads, writes)

    def CP(eng, out, in_, reads, writes):
        P.op(eng, lambda e: e.tensor_copy(out=out, in_=in_), reads, writes)

    def RCP(eng, out, in_, reads, writes):
        P.op(eng, lambda e: e.reciprocal(out=out, in_=in_), reads, writes)

    def MS(eng, ap, val, writes):
        P.op(eng, lambda e: e.memset(ap, val), (), writes)

    def pk(j):
        return ('ps', j)

    def misc_bank():
        j = MISC[rr['misc'] % 2]
        rr['misc'] += 1
        return j

    def next_wst():
        return 0

    wcols = wbf_d.rearrange("(c p) n -> p c n", p=128)
    WBK = [('wbf', r) for r in range(8)]

    def load_w(dst, col0, ncols, key):
        P.dma('sync', dst, wcols[:, :, col0:col0 + ncols], reads=WBK, writes=[key])

    def dump(name, src, reads):
        if name in dbg_d:
            P.dma('sync', dbg_d[name], src, reads=reads, writes=['dbg_' + name])

    for k in cs:
        P.dma('sync', cs[k][:], cd[k], reads=(), writes=['c_' + k])
    CK = ['c_alb', 'c_clb']
    for r in range(8):
        P.dma('gpsimd', wbf_d[r * 128:(r + 1) * 128, :], win_d[r * 128:(r + 1) * 128, :], reads=(), writes=[('wbf', r)])
    P.dma('gpsimd', wabf_d[0], wa_d, reads=(), writes=['wabf0'])
    P.dma('gpsimd', wabf_d[1], wb_d, reads=(), writes=['wabf1'])
    P.dma('gpsimd', wo_s[:], wo_d.rearrange("(j p) n -> p j n", p=128), reads=(), writes=['wo'])
    P.dma('gpsimd', w2s[:], w2s_d, reads=(), writes=['w2s'])
    P.dma('gpsimd', pet[:], pet_d, reads=(), writes=['pet'])
    load_w(wg24[:], C_GA, 24, 'wg24')
    MS('vector', eps_t[:], 1e-6, ['eps'])
    MS('vector', ones_b[:], 1.0, ['ones'])
    for b_ in range(2):
        MS('vector', SGT[b_][:], 0.0, [('SGT', b_)])
    MS('vector', vcx[:], 1.0, ['vcx'])
    MS('vector', ones1[:], 1.0, ['ones1'])
    MS('vector', kcT[64:65, :], 1.0, ['kcT'])
    MS('vector', hcm[:], 0.0, ['hcm'])

    def adv(gen):
        if gen is not None:
            try:
                next(gen)
            except StopIteration:
                pass

    def exhaust(gen):
        if gen is not None:
            for _ in gen:
                pass

    def attend(tiles, gen=None, step=3):
        pend = []
        for t in tiles:
            sb_ = ST[rr['st'] % 2]
            rr['st'] += 1
            pj = rr['pt'] % 4
            rr['pt'] += 1
            n = len(t['smm'])
            for m, (lh, rh, rd) in enumerate(t['smm']):
                MM(ps[sb_][:, :], lh, rh, m == 0, m == n - 1, rd, [pk(sb_)])
            ACT(PT[pj][:], ps[sb_][:, :], AF.Exp, [pk(sb_)] + CK, [('PT', pj)], bias=t['bias'], scale=SCALE)
            pend.append((t['pv'], pj))
            rr['tc'] = rr.get('tc', 0) + 1
            if rr['tc'] % step == 0:
                adv(gen)
            if len(pend) > 2:
                pv_, pj_ = pend.pop(0)
                for (o_, lh, tp, st_, sp_, rd, wk) in pv_:
                    MM(o_, lh, PT[pj_][:], st_, sp_, rd + [('PT', pj_)], [wk], tp=tp)
        for pv_, pj_ in pend:
            for (o_, lh, tp, st_, sp_, rd, wk) in pv_:
                MM(o_, lh, PT[pj_][:], st_, sp_, rd + [('PT', pj_)], [wk], tp=tp)

    def proj_fm(wtile, wkey, wc0, ncol, blk, bank):
        for c in range(8):
            MM(ps[bank][0:ncol, :], wtile[:, c, wc0:wc0 + ncol], uT[:, c, blk * 512:(blk + 1) * 512],
               c == 0, c == 7, [wkey, ('uT', blk)], [pk(bank)])

    def silu_pair(bank, dst, dkey):
        w = WK[0]
        ACT(w[:], ps[bank][:, :], AF.Exp, [pk(bank)], [('WK', 0)], scale=-1.0)
        TS('vector', w[:], w[:], 1.0, None, ALU.add, None, [('WK', 0)], [('WK', 0)])
        RCP('vector', w[:], w[:], [('WK', 0)], [('WK', 0)])
        TT('vector', dst, ps[bank][:, :], w[:], ALU.mult, [pk(bank), ('WK', 0)], [dkey])

    def combine_branch(acc, akey, first, gbank, banks):
        w = WK[1]
        for hl in range(2):
            r0 = 64 * hl
            TS('vector', w[r0:r0 + 64, :], ps[banks[hl]][64:128, :], EPSL, None, ALU.max, None, [pk(banks[hl])],
               [('WK', 1)])
        RCP('vector', w[:], w[:], [('WK', 1)], [('WK', 1)])
        if gbank is not None:
            TT('vector', w[:], ps[gbank][:, :], w[:], ALU.mult, [pk(gbank), ('WK', 1)], [('WK', 1)])
        dst = acc if first else WK[2]
        dkey = akey if first else ('WK', 2)
        for hl in range(2):
            r0 = 64 * hl
            TT('vector', dst[r0:r0 + 64, :], ps[banks[hl]][0:64, :], w[r0:r0 + 64, :], ALU.mult,
               [pk(banks[hl]), ('WK', 1)], [dkey])
        if not first:
            TT('vector', acc, acc, WK[2][:], ALU.add, [akey, ('WK', 2)], [akey])

    for s in range(NSEQ):
        P.dma('sync', gpre, npre_d, reads=(), writes=['impacc', 'WKg'])
        for tt in range(NT):
            b = tt % 2
            P.dma('sync', xt[b][:], x_d[s, tt * 128:(tt + 1) * 128, :], reads=(), writes=XK(b))
            TT('vector', xn[b][:], xt[b][:], xt[b][:], ALU.mult, XK(b), NK(b))
            P.op('vector', (lambda bb: lambda e: e.reduce_sum(out=ssq[:, bb:bb + 1], in_=xn[bb][:], axis=AX.X))(b),
                 NK(b), [('ssq', b)])
            ACT(ssq[:, b:b + 1], ssq[:, b:b + 1], AF.Ln, [('ssq', b), 'eps'], [('ssq', b)], bias=eps_t[:, 0:1],
                scale=1.0 / D)
            ACT(ssq[:, b:b + 1], ssq[:, b:b + 1], AF.Exp, [('ssq', b)], [('ssq', b)], scale=-0.5)
            STT(xn[b][:], xt[b][:], ssq[:, b:b + 1], gpre, ALU.mult, ALU.mult, XK(b) + [('ssq', b), 'impacc', 'WKg'],
                NK(b))
            for half in range(2):
                bk = misc_bank()
                for cc in range(4):
                    c = half * 4 + cc
                    TR(ps[bk][:, cc * 128:(cc + 1) * 128], xn[b][:, c * 128:(c + 1) * 128], cs['ident_f'][:],
                       NK(b) + ['c_ident_f'], [pk(bk)])
                CP('vector' if half == 0 else 'gpsimd' if False else 'vector',
                   uT[:, half * 4:(half + 1) * 4, tt * 128:(tt + 1) * 128],
                   ps[bk][:, :].rearrange("p (c t) -> p c t", c=4), [pk(bk)], [('uT', tt // 4)])
        if 'uT' in dbg_d and s == 0:
            CP('vector', WK[0][:], uT[:, 0, 0:512], [('uT', 0)], [('WK', 0)])
            dump('uT', WK[0][:], [('WK', 0)])

        ncm = (S - 32) // 16 + 1
        for g in range(2):
            wj = next_wst()
            load_w(wst[wj][:, :, 0:64], C_KV + 0 * 128 + g * 64, 64, ('wst', wj))
            load_w(wst[wj][:, :, 64:128], C_KV + 1 * 128 + g * 64, 64, ('wst', wj))
            for blk in range(NQT):
                bk = misc_bank()
                proj_fm(wst[wj], ('wst', wj), 0, 128, blk, bk)
                CP('vector', kT1[:, blk * 512:(blk + 1) * 512], ps[bk][:, :], [pk(bk)], ['kT1'])
            W1 = Vp[:, 0:4096].rearrange("p (l e) -> p l e", e=128)
            P.dma('gpsimd', W1, w1s_d, reads=(), writes=['Vp'])
            for wh in range(2):
                r0 = 64 * wh
                bk = misc_bank()
                for l in range(32):
                    MM(ps[bk][:, 0:ncm], W1[r0:r0 + 64, l, :], kT1[r0:r0 + 64, l:l + 16 * (ncm - 1) + 1:16],
                       l == 0, l == 31, ['Vp', 'kT1'], [pk(bk)])
                bk2 = misc_bank()
                for l in range(32):
                    MM(ps[bk2][:, 0:1], W1[r0:r0 + 64, l, :], pet[r0:r0 + 64, l:l + 1], l == 0, l == 31,
                       ['Vp', 'pet'], [pk(bk2)])
                CP('vector', hb[:, 0:1], ps[bk2][:, 0:1], [pk(bk2)], ['hb'])
                TS('vector', hb[:, 1:2], hb[:, 0:1], -1.0, None, ALU.mult, None, ['hb'], ['hb'])
                w = WK[0]
                ACT(w[:, 0:ncm], ps[bk][:, 0:ncm], AF.Exp, [pk(bk), 'hb'], [('WK', 0)], bias=hb[:, 1:2], scale=-1.0)
                TS('vector', w[:, 0:ncm], w[:, 0:ncm], 1.0, None, ALU.add, None, [('WK', 0)], [('WK', 0)])
                RCP('vector', w[:, 0:ncm], w[:, 0:ncm], [('WK', 0)], [('WK', 0)])
                STT(hcm[:, wh, 0:ncm], ps[bk][:, 0:ncm], hb[:, 0:1], w[:, 0:ncm], ALU.add, ALU.mult,
                    [pk(bk), 'hb', ('WK', 0)], ['hcm'])
                if wh == 0:
                    bk3 = misc_bank()
                    MM(ps[bk3][0:64, 0:256], w2s[:, 0, :], hcm[:, 0, :], True, True, ['w2s', 'hcm'], [pk(bk3)])
                    CP('vector', kcT[0:64, :], ps[bk3][0:64, 0:256], [pk(bk3)], ['kcT'])
                else:
                    bk3 = misc_bank()
                    for a in range(2):
                        MM(ps[bk3][:, a * 64:(a + 1) * 64], hcm[:, 1, a * 128:(a + 1) * 128], w2s[:, 1, :], True, True,
                           ['w2s', 'hcm'], [pk(bk3)])
                    CP('vector', vcx[:, :, 0:64], ps[bk3][:, 0:128].rearrange("p (a d) -> p a d", a=2), [pk(bk3)],
                       ['vcx'])
            wj = next_wst()
            load_w(wst[wj][:, :, 0:64], C_KV + 2 * 128 + g * 64, 64, ('wst', wj))
            load_w(wst[wj][:, :, 64:128], C_KV + 4 * 128 + g * 64, 64, ('wst', wj))
            for blk in range(NQT):
                bk = misc_bank()
                proj_fm(wst[wj], ('wst', wj), 0, 128, blk, bk)
                CP('vector', kT0[0:64, blk * 512:(blk + 1) * 512], ps[bk][0:64, :], [pk(bk)], ['kT0'])
                CP('vector', kT1[0:64, blk * 512:(blk + 1) * 512], ps[bk][64:128, :], [pk(bk)], ['kT1'])
            P.dma('sync', kT0[64:128, 0:S], cd['eslc'][:, 0:S], reads=(), writes=['kT0'])
            MS('vector', kT1[64:128, :], 0.0, ['kT1'])
            MS('vector', kT1[64:65, :], 1.0, ['kT1'])
            wj = next_wst()
            load_w(wst[wj][:, :, 0:64], C_KV + 3 * 128 + g * 64, 64, ('wst', wj))
            load_w(wst[wj][:, :, 64:128], C_KV + 5 * 128 + g * 64, 64, ('wst', wj))
            MS('vector', Vp3[:, :, :].rearrange("p t (h c) -> p t h c", h=2)[:, :, :, 64:128], 1.0, ['Vp'])
            for t4 in range(NT // 4):
                bk = misc_bank()
                for q4 in range(4):
                    tt = t4 * 4 + q4
                    for c in range(8):
                        MM(ps[bk][:, q4 * 128:(q4 + 1) * 128], uT[:, c, tt * 128:(tt + 1) * 128], wst[wj][:, c, :],
                           c == 0, c == 7, [('wst', wj), ('uT', t4)], [pk(bk)])
                CP('vector', Vp3[:, t4 * 4:(t4 + 1) * 4, :].rearrange("p t (h c) -> p t h c", h=2)[:, :, :, 0:64],
                   ps[bk][:, :].rearrange("p (t h c) -> p t h c", h=2, c=64), [pk(bk)], ['Vp'])
            load_w(WQZ[:, :, 0:256], C_QA + g * 256, 256, 'WQZ')
            load_w(WQZ[:, :, 256:512], C_ZA + g * 256, 256, 'WQZ')
            for h4 in range(4):
                for b2 in range(2):
                    P.dma('sync', QA[b2][h4][64:65, :], cd['cq'][4 * g + h4:4 * g + h4 + 1, :], reads=(),
                          writes=[('QA', b2, h4)])

            def nsa_pre(g, i):
                b2 = i % 2
                for pp in range(2):
                    bk = misc_bank()
                    proj_fm(WQZ, 'WQZ', pp * 128, 128, i, bk)
                    CP('vector', QA[b2][2 * pp][0:64, :], ps[bk][0:64, :], [pk(bk)], [('QA', b2, 2 * pp)])
                    CP('vector', QA[b2][2 * pp + 1][0:64, :], ps[bk][64:128, :], [pk(bk)], [('QA', b2, 2 * pp + 1)])
                    yield
                for pp in range(2):
                    bk = misc_bank()
                    proj_fm(WQZ, 'WQZ', 256 + pp * 128, 128, i, bk)
                    silu_pair(bk, SZ[b2][pp], ('xn', pp, b2))
                    yield
                bk = misc_bank()
                for c in range(8):
                    MM(ps[bk][0:24, :], wg24[:, c, :], uT[:, c, i * 512:(i + 1) * 512], c == 0, c == 7,
                       ['wg24', ('uT', i)], [pk(bk)])
                w = WK[0]
                ACT(w[0:24, :], ps[bk][0:24, :], AF.Exp, [pk(bk)], [('WK', 0)], scale=-1.0)
                TS('vector', w[0:24, :], w[0:24, :], 1.0, None, ALU.add, None, [('WK', 0)], [('WK', 0)])
                RCP('vector', w[0:24, :], w[0:24, :], [('WK', 0)], [('WK', 0)])
                CP('vector', SGT[b2][0:24, :], w[0:24, :], [('WK', 0)], [('SGT', b2)])
                TT('vector', SGT[b2][32:56, :], w[0:24, :], SGT[b2][0:24, :], ALU.subtract, [('WK', 0), ('SGT', b2)],
                   [('SGT', b2)])
                yield
                a_list = [0] if (512 * i + 511) < (2048 + 31) else [0, 1]
                a_list = [a for a in a_list if a * 128 < ncm]
                for a in a_list:
                    thr = 512.0 * i - 2048.0 * a
                    TS('vector', XM[a][:], cs['xg'][:], thr, NEG, ALU.is_gt, ALU.mult, ['c_xg'], [('XM', a)])
                la = len(a_list)
                for h4 in range(4):
                    hl = h4 % 2
                    pp = h4 // 2
                    ha = 4 * g + h4
                    if hl == 0:
                        gb = misc_bank()
                        MM(ps[gb][:, :], cs['goh'][0:56, (g * 2 + pp) * 3 + 0, :], SGT[b2][0:56, :], True, True,
                           ['c_goh', ('SGT', b2)], [pk(gb)])
                        CP('vector', WKg[:], ps[gb][:, :], [pk(gb)], ['WKg'])
                    bkA = misc_bank()
                    bkB = misc_bank()
                    for ai, a in enumerate(a_list):
                        sb_ = ST[rr['st'] % 2]
                        rr['st'] += 1
                        pj = 4 + rr['ptc'] % 2
                        rr['ptc'] += 1
                        MM(ps[sb_][:, :], kcT[0:65, a * 128:(a + 1) * 128], QA[b2][h4][0:65, :], True, False,
                           ['kcT', ('QA', b2, h4)], [pk(sb_)])
                        MM(ps[sb_][:, :], cs['ident_b'][:], XM[a][:], False, True, ['c_ident_b', ('XM', a)], [pk(sb_)])
                        ACT(PT[pj][:], ps[sb_][:, :], AF.Exp, [pk(sb_)] + CK, [('PT', pj)],
                            bias=cs['clb'][:, ha, a, i:i + 1], scale=SCALE)
                        MM(ps[bkA][:, :], vcx[:, a, :], PT[pj][:], ai == 0, ai == la - 1, ['vcx', ('PT', pj)], [pk(bkA)])
                        MM(ps[bkB][0:64, :], cs['ov'][:, a, :], PT[pj][:], ai == 0, ai == la - 1, ['c_ov', ('PT', pj)],
                           [pk(bkB)])
                    wr = WK[3]
                    TS('vector', wr[0:64, :], ps[bkA][64:128, :], EPSL, None, ALU.max, None, [pk(bkA)], [('WK', 3)])
                    TS('vector', wr[64:128, :], ps[bkA][64:128, :], EPSL, None, ALU.max, None, [pk(bkA)], [('WK', 3)])
                    RCP('vector', wr[:, :], wr[:, :], [('WK', 3)], [('WK', 3)])
                    if h4 == 0:
                        TT('vector', impacc[0:64, :], ps[bkB][0:64, :], wr[0:64, :], ALU.mult, [pk(bkB), ('WK', 3)],
                           ['impacc'])
                    else:
                        TT('vector', WK[4][0:64, :], ps[bkB][0:64, :], wr[0:64, :], ALU.mult, [pk(bkB), ('WK', 3)],
                           [('WK', 4)])
                        TT('vector', impacc[0:64, :], impacc[0:64, :], WK[4][0:64, :], ALU.add, ['impacc', ('WK', 4)],
                           ['impacc'])
                    r0 = 64 * hl
                    TT('vector', WK[1][r0:r0 + 64, :], WKg[r0:r0 + 64, :], wr[r0:r0 + 64, :], ALU.mult, ['WKg', ('WK', 3)],
                       [('WK', 1)])
                    TT('vector', ACC[b2][pp][r0:r0 + 64, :], ps[bkA][0:64, :], WK[1][r0:r0 + 64, :], ALU.mult,
                       [pk(bkA), ('WK', 1)], [('xt', pp, b2)])
                    yield
                bk = misc_bank()
                for qs in range(4):
                    TR(ps[bk][:, qs * 64:(qs + 1) * 64], impacc[0:64, qs * 128:(qs + 1) * 128], cs['ident_f'][0:64, 0:64],
                       ['impacc', 'c_ident_f'], [pk(bk)])
                for qs in range(4):
                    ti = 4 * i + qs
                    TT('vector', T1[:, qs, :], ps[bk][:, qs * 64:(qs + 1) * 64],
                       cs['abase'][:, 64 - 2 * ti:128 - 2 * ti], ALU.add, [pk(bk), 'c_abase'], ['T1'])
                MS('vector', T1[:, :, 0:1], BIGF, ['T1'])
                for qs in range(4):
                    P.op('vector', (lambda q_: lambda e: e.max(out=M8a[:], in_=T1[:, q_, :]))(qs), ['T1'], ['M8a'])
                    P.op('vector', (lambda q_: lambda e: e.match_replace(out=T2[:, q_, :], in_to_replace=M8a[:],
                                                                        in_values=T1[:, q_, :], imm_value=-1e9))(qs),
                         ['T1', 'M8a'], ['T2'])
                    P.op('vector', (lambda q_: lambda e: e.max(out=M8b[:], in_=T2[:, q_, :]))(qs), ['T2'], ['M8b'])
                    TS('vector', T2[:, qs, :], T1[:, qs, :], M8b[:, 7:8], NEG, ALU.is_lt, ALU.mult, ['T1', 'M8b'], ['T2'])
                yield
                yield
                bk = misc_bank()
                for qs in range(4):
                    TR(ps[bk][0:64, qs * 128:(qs + 1) * 128], T2[:, qs, :], cs['ident_f'][:], ['T2', 'c_ident_f'],
                       [pk(bk)])
                for h4 in range(4):
                    STT(QA[b2][h4][64:128, :], cs['cqb'][64:128, :], -SL_HH[4 * g + h4] / SCALE, ps[bk][0:64, :],
                        ALU.mult, ALU.add, [pk(bk), 'c_cqb'], [('QA', b2, h4)])
                yield

            gen = nsa_pre(g, 0)
            exhaust(gen)
            for i in range(NQT):
                b2 = i % 2
                gen = nsa_pre(g, i + 1) if i + 1 < NQT else None
                for pp in range(2):
                    for br in (1, 2):
                        tiles = []
                        OB, LB = OLS[rr['ol'] % 2]
                        rr['ol'] += 1
                        if br == 1:
                            kts = list(range(0, 4 * i + 4))
                        else:
                            kts = [kt for kt in range(4 * i - 4, 4 * i + 4) if kt >= 0]
                        for kt in kts:
                            for hl in range(2):
                                h4 = 2 * pp + hl
                                ha = 4 * g + h4
                                hk = [k_ for k_ in kts if k_ >= kt_lo(ha, i)]
                                if kt not in hk:
                                    continue
                                ki = hk.index(kt)
                                nk = len(hk)
                                kT = kT0 if br == 1 else kT1
                                kkey = 'kT0' if br == 1 else 'kT1'
                                smm = [(kT[:, kt * 128:(kt + 1) * 128], QA[b2][h4][:, :], [kkey, ('QA', b2, h4)])]
                                if br == 1:
                                    if kt >= 4 * i:
                                        r = kt - 4 * i
                                        smm.append((cs['ident_b'][:], cs['cmb'][:, 384 - 128 * r:896 - 128 * r],
                                                    ['c_ident_b', 'c_cmb']))
                                else:
                                    r = kt - (4 * i - 4)
                                    if r < 4:
                                        smm.append((cs['ident_b'][:], cs['omb'][:, 384 - 128 * r:896 - 128 * r],
                                                    ['c_ident_b', 'c_omb']))
                                    else:
                                        r -= 4
                                        smm.append((cs['ident_b'][:], cs['cmb'][:, 384 - 128 * r:896 - 128 * r],
                                                    ['c_ident_b', 'c_cmb']))
                                vcol = 0 if br == 1 else 128
                                bnk = (OB, LB)[hl]
                                pv = [(ps[bnk][:, :], Vp3[:, kt, vcol:vcol + 128], None,
                                       ki == 0, ki == nk - 1, ['Vp'], pk(bnk))]
                                tiles.append(dict(smm=smm, bias=cs['alb'][:, ha, kt - 4 * i + 32:kt - 4 * i + 33], pv=pv))
                        attend(tiles, gen)
                        gb = misc_bank()
                        MM(ps[gb][:, :], cs['goh'][0:56, (g * 2 + pp) * 3 + br, :], SGT[b2][0:56, :], True, True,
                           ['c_goh', ('SGT', b2)], [pk(gb)])
                        combine_branch(ACC[b2][pp], ('xt', pp, b2), False, gb, (OB, LB))
                    yj = rr['wk'] % 2
                    rr['wk'] += 1
                    TT('vector', YST[yj][:], ACC[b2][pp], SZ[b2][pp], ALU.mult, [('xt', pp, b2), ('xn', pp, b2)],
                       [('YST', yj)])
                    P.dma('sync', ysc_d[g * 2 + pp, :, i * 512:(i + 1) * 512], YST[yj][:], reads=[('YST', yj)],
                          writes=[('ysc', g * 2 + pp, i)])
                exhaust(gen)

        nb = S // 256
        for j in range(4):
            wj = next_wst()
            load_w(wst[wj][:], C_QB + 512 + j * 128, 128, ('wst', wj))
            for blk in range(NQT):
                bk = misc_bank()
                proj_fm(wst[wj], ('wst', wj), 0, 128, blk, bk)
                CP('vector', kT0[0:64, blk * 512:(blk + 1) * 512], ps[bk][0:64, :], [pk(bk)], ['kT0'])
                CP('vector', kT1[0:64, blk * 512:(blk + 1) * 512], ps[bk][64:128, :], [pk(bk)], ['kT1'])
                P.op('vector', (lambda bk_, blk_: lambda e: e.reduce_sum(
                    out=kbarf[:, 2 * blk_:2 * blk_ + 2], in_=ps[bk_][:, :].rearrange("p (b t) -> p b t", b=2),
                    axis=AX.X))(bk, blk), [pk(bk)], ['kbarf'])
            for kT_, kk_ in ((kT0, 'kT0'), (kT1, 'kT1')):
                MS('vector', kT_[64:128, :], 0.0, [kk_])
                MS('vector', kT_[64:65, :], 1.0, [kk_])
                P.dma('sync', kT_[96:112, 0:S], cd['emoba'][:, 0:S], reads=(), writes=[kk_])
            TS('vector', kbar[0:64, 0, 0:nb], kbarf[0:64, 0:nb], 1.0 / 256, None, ALU.mult, None, ['kbarf'], ['kbar'])
            TS('vector', kbar[0:64, 1, 0:nb], kbarf[64:128, 0:nb], 1.0 / 256, None, ALU.mult, None, ['kbarf'], ['kbar'])
            wj = next_wst()
            load_w(wst[wj][:], C_QB + 1024 + j * 128, 128, ('wst', wj))
            MS('vector', Vp3[:, :, :].rearrange("p t (h c) -> p t h c", h=2)[:, :, :, 64:128], 1.0, ['Vp'])
            for t4 in range(NT // 4):
                bk = misc_bank()
                for q4 in range(4):
                    tt = t4 * 4 + q4
                    for c in range(8):
                        MM(ps[bk][:, q4 * 128:(q4 + 1) * 128], uT[:, c, tt * 128:(tt + 1) * 128], wst[wj][:, c, :],
                           c == 0, c == 7, [('wst', wj), ('uT', t4)], [pk(bk)])
                CP('vector', Vp3[:, t4 * 4:(t4 + 1) * 4, :].rearrange("p t (h c) -> p t h c", h=2)[:, :, :, 0:64],
                   ps[bk][:, :].rearrange("p (t h c) -> p t h c", h=2, c=64), [pk(bk)], ['Vp'])
            load_w(WQZ[:, :, 0:128], C_QB + j * 128, 128, 'WQZ')
            load_w(WQZ[:, :, 128:256], C_ZB + j * 128, 128, 'WQZ')
            for hl in range(2):
                for b2 in range(2):
                    MS('vector', QA[b2][hl][64:128, :], 0.0, [('QA', b2, hl)])
                    P.dma('sync', QA[b2][hl][64:65, :], cd['cq'][8 + 2 * j + hl:8 + 2 * j + hl + 1, :], reads=(),
                          writes=[('QA', b2, hl)])

            def moba_pre(j, i):
                b2 = i % 2
                bk = misc_bank()
                proj_fm(WQZ, 'WQZ', 0, 128, i, bk)
                CP('vector', QA[b2][0][0:64, :], ps[bk][0:64, :], [pk(bk)], [('QA', b2, 0)])
                CP('vector', QA[b2][1][0:64, :], ps[bk][64:128, :], [pk(bk)], [('QA', b2, 1)])
                yield
                bk = misc_bank()
                proj_fm(WQZ, 'WQZ', 128, 128, i, bk)
                silu_pair(bk, SZ[b2][0], ('xn', 0, b2))
                yield
                bk = misc_bank()
                for hl in range(2):
                    for qs in range(4):
                        MM(ps[bk][:, (hl * 4 + qs) * 16:(hl * 4 + qs) * 16 + nb], QA[b2][hl][0:64, qs * 128:(qs + 1) * 128],
                           kbar[0:64, hl, 0:nb], True, True, ['kbar', ('QA', b2, hl)], [pk(bk)])
                for hl in range(2):
                    for qs in range(4):
                        cur = (4 * i + qs) // 2
                        e8 = hl * 4 + qs
                        TT('vector', GT[:, e8, 0:nb], ps[bk][:, e8 * 16:e8 * 16 + nb], cs['bbase'][:, 16 - cur:16 - cur + nb],
                           ALU.add, [pk(bk), 'c_bbase'], ['GT'])
                        if nb < 16:
                            MS('vector', GT[:, e8, nb:16], -BIGF, ['GT'])
                        P.op('vector', (lambda e_: lambda e: e.max(out=M8a[:], in_=GT[:, e_, :]))(e8), ['GT'], ['M8a'])
                        TS('vector', GT[:, e8, :], GT[:, e8, :], M8a[:, 2:3], NEG, ALU.is_lt, ALU.mult, ['GT', 'M8a'], ['GT'])
                        TT('vector', GT[:, e8, :], GT[:, e8, :], cs['obase'][:, 16 - cur:32 - cur], ALU.max,
                           ['GT', 'c_obase'], ['GT'])
                yield
                yield
                for hl in range(2):
                    bk = misc_bank()
                    for qs in range(4):
                        TR(ps[bk][0:16, qs * 128:(qs + 1) * 128], GT[:, hl * 4 + qs, :], cs['ident_f'][:],
                           ['GT', 'c_ident_f'], [pk(bk)])
                    CP('vector', QA[b2][hl][96:112, :], ps[bk][0:16, :], [pk(bk)], [('QA', b2, hl)])
                    yield

            gen = moba_pre(j, 0)
            exhaust(gen)
            for i in range(NQT):
                b2 = i % 2
                gen = moba_pre(j, i + 1) if i + 1 < NQT else None
                tiles = []
                OB, LB = OLS[rr['ol'] % 2]
                rr['ol'] += 1
                kts = list(range(0, 4 * i + 4))
                for kt in kts:
                    for hl in range(2):
                        hh = 8 + 2 * j + hl
                        hk = [k_ for k_ in kts if k_ >= kt_lo(hh, i)]
                        if kt not in hk:
                            continue
                        ki = hk.index(kt)
                        nk = len(hk)
                        kT = kT0 if hl == 0 else kT1
                        kkey = 'kT0' if hl == 0 else 'kT1'
                        smm = [(kT[:, kt * 128:(kt + 1) * 128], QA[b2][hl][:, :], [kkey, ('QA', b2, hl)])]
                        if kt >= 4 * i:
                            smm.append((cs['ident_b'][:], cs['mmb'][:, kt - 4 * i, :], ['c_ident_b', 'c_mmb']))
                        bnk = (OB, LB)[hl]
                        pv = [(ps[bnk][:, :], Vp3[:, kt, 128 * hl:128 * hl + 128], None,
                               ki == 0, ki == nk - 1, ['Vp'], pk(bnk))]
                        tiles.append(dict(smm=smm, bias=cs['alb'][:, hh, kt - 4 * i + 32:kt - 4 * i + 33], pv=pv))
                attend(tiles, gen, step=4)
                combine_branch(ACC[b2][0], ('xt', 0, b2), True, None, (OB, LB))
                yj = rr['wk'] % 2
                rr['wk'] += 1
                TT('vector', YST[yj][:], ACC[b2][0], SZ[b2][0], ALU.mult, [('xt', 0, b2), ('xn', 0, b2)], [('YST', yj)])
                P.dma('sync', ysc_d[4 + j, :, i * 512:(i + 1) * 512], YST[yj][:], reads=[('YST', yj)],
                      writes=[('ysc', 4 + j, i)])
                exhaust(gen)

        P.op('sync', lambda e: e.nop(), (), ['Vp', ('wgs', 0), ('wgs', 1), 'WQZ', ('wab', 0), ('wab', 1)])
        P.dma('sync', gpost, npost_d, reads=(), writes=['impacc', 'WKg'])
        for i in range(NQT):
            P.dma('sync', ysl[:, :, 0:512], ysc_d[:, :, i * 512:(i + 1) * 512].rearrange("c p t -> p c t"),
                  reads=[('ysc', jj, i) for jj in range(8)], writes=['kT1'])
            for fc in range(8):
                gj = fc % 2
                P.dma('sync', wgs[:, gj, :, 0:128], wcols[:, :, C_MG + fc * 128:C_MG + (fc + 1) * 128], reads=WBK,
                      writes=[('wgs', gj)])
                P.dma('sync', wgs[:, gj, :, 128:256], wcols[:, :, C_MG + 1024 + fc * 128:C_MG + 1024 + (fc + 1) * 128],
                      reads=WBK, writes=[('wgs', gj)])
                for ab in range(2):
                    P.dma('sync', wab[:, gj, 4 * ab:4 * ab + 4, :],
                          wabf_d[ab].rearrange("(j p) n -> p j n", p=128)[:, :, fc * 128:(fc + 1) * 128],
                          reads=['wabf0', 'wabf1'], writes=[('wab', gj)])
                for ab in range(2):
                    bg = misc_bank()
                    for c in range(8):
                        MM(ps[bg][:, :], wgs[:, gj, c, ab * 128:(ab + 1) * 128], uT[:, c, i * 512:(i + 1) * 512],
                           c == 0, c == 7, [('wgs', gj), ('uT', i)], [pk(bg)])
                    w = WK[ab]
                    ACT(w[:], ps[bg][:, :], AF.Exp, [pk(bg)], [('WK', ab)], scale=-1.0)
                    ACT(w[:], w[:], AF.Ln, [('WK', ab), 'ones1'], [('WK', ab)], bias=ones1[:, 0:1], scale=1.0)
                    ACT(w[:], w[:], AF.Exp, [('WK', ab)], [('WK', ab)], scale=-1.0)
                    bm = OLS[fc % 2][ab]
                    for jj in range(4):
                        MM(ps[bm][:, :], wab[:, gj, 4 * ab + jj, :], ysl[:, 4 * ab + jj, 0:512], jj == 0, jj == 3,
                           [('wab', gj), 'kT1'], [pk(bm)])
                    TT('vector', w[:], ps[bm][:, :], w[:], ALU.mult, [pk(bm), ('WK', ab)], [('WK', ab)])
                TT('vector', mT[:, fc, 0:512], WK[0][:], WK[1][:], ALU.add, [('WK', 0), ('WK', 1)], ['kT0'])
            for ts_ in range(4):
                tt = i * 4 + ts_
                b = tt % 2
                P.dma('sync', xt[b][:], x_d[s, tt * 128:(tt + 1) * 128, :], reads=(), writes=XK(b))
                banks = [ST[0], ST[1]]
                for hf in range(2):
                    for fc in range(8):
                        MM(ps[banks[hf]][:, :], mT[:, fc, ts_ * 128:(ts_ + 1) * 128], wo_s[:, fc, hf * 512:(hf + 1) * 512],
                           fc == 0, fc == 7, ['wo', 'kT0'], [pk(banks[hf])])
                for hf in range(2):
                    P.op('scalar', (lambda o_, i_: lambda e: e.copy(out=o_, in_=i_))(WK[2 + hf][:], ps[banks[hf]][:, :]),
                         [pk(banks[hf])], [('WK', 2 + hf)])
                    P.op('vector', (lambda o_, i_, a_: lambda e: e.scalar_tensor_tensor(
                        out=o_, in0=i_, scalar=1.0, in1=i_, op0=ALU.mult, op1=ALU.mult, accum_out=a_))(
                        YST[hf][:], WK[2 + hf][:], ssq2[:, hf:hf + 1]), [('WK', 2 + hf)], [('YST', hf), ('ssq2', hf)])
                TT('vector', ssq[:, b:b + 1], ssq2[:, 0:1], ssq2[:, 1:2], ALU.add, [('ssq2', 0), ('ssq2', 1)], [('ssq', b)])
                ACT(ssq[:, b:b + 1], ssq[:, b:b + 1], AF.Ln, [('ssq', b), 'eps'], [('ssq', b)], bias=eps_t[:, 0:1],
                    scale=1.0 / D)
                ACT(ssq[:, b:b + 1], ssq[:, b:b + 1], AF.Exp, [('ssq', b)], [('ssq', b)], scale=-0.5)
                for hf in range(2):
                    STT(xn[b][:, hf * 512:(hf + 1) * 512], WK[2 + hf][:], ssq[:, b:b + 1],
                        gpost[:, hf * 512:(hf + 1) * 512], ALU.mult, ALU.mult, [('WK', 2 + hf), ('ssq', b), 'impacc', 'WKg'],
                        [('xn', b, hf)])
                TT('gpsimd', xn[b][:], xn[b][:], xt[b][:], ALU.add, NK(b) + XK(b), NK(b))
                P.dma('sync', out_d[s, tt * 128:(tt + 1) * 128, :], xn[b][:], reads=NK(b), writes=[('out', tt)])
        P.op('sync', lambda e: e.nop(), (), ['Vp', ('wgs', 0), ('wgs', 1), 'WQZ', ('wab', 0), ('wab', 1)])
    P.finish()
    return nc, P


def host_inputs(inputs, S):
    c = make_consts()
    f = lambda a: np.ascontiguousarray(np.asarray(a, dtype=np.float32))
    w1k = f(inputs['cmp_w1_k'])[0].transpose(1, 0, 2)
    w1v = f(inputs['cmp_w1_v'])[0].transpose(1, 0, 2)
    shared = {
        'w_in': f(inputs['w_in'])[0],
        'w1s': np.ascontiguousarray(np.concatenate([w1k, w1v], 0)),
        'w2s': np.ascontiguousarray(np.stack([f(inputs['cmp_w2_k'])[0], f(inputs['cmp_w2_v'])[0]], 1)),
        'pet': np.ascontiguousarray(np.concatenate([f(inputs['cmp_pe_k'])[0].T, f(inputs['cmp_pe_v'])[0].T], 0)),
        'w_a': f(inputs['w_branch_a'])[0],
        'w_b': f(inputs['w_branch_b'])[0],
        'w_o': f(inputs['w_o'])[0],
        'npre_bc': np.ascontiguousarray(np.broadcast_to(f(inputs['norm_pre'])[0][None, :], (128, D))),
        'npost_bc': np.ascontiguousarray(np.broadcast_to(f(inputs['norm_post'])[0][None, :], (128, D))),
    }
    for k, v in c.items():
        shared['c_' + k] = v
    return shared


def kernel(**inputs):
    x = np.ascontiguousarray(np.asarray(inputs['x'], dtype=np.float32))
    B, S, _ = x.shape
    ncores = 8
    nseq = B // ncores
    nc, _ = build(S, nseq)
    shared = host_inputs(inputs, S)
    in_maps = []
    for c in range(ncores):
        m = dict(shared)
        m['x'] = np.ascontiguousarray(x[c * nseq:(c + 1) * nseq])
        in_maps.append(m)
    res = run_bass_kernel_spmd(nc, in_maps, core_ids=list(range(ncores)))
    return np.concatenate([np.asarray(r['out']) for r in res.results], axis=0).astype(np.float32)
```

```python
import contextlib
import numpy as np
import ml_dtypes
import concourse.bass as bass
import concourse.mybir as mybir
from concourse.bass_utils import run_bass_kernel_spmd

F32 = mybir.dt.float32
BF16 = mybir.dt.bfloat16
ALU = mybir.AluOpType
AF = mybir.ActivationFunctionType
AX = mybir.AxisListType
NPBF = ml_dtypes.bfloat16

D = 1024
DEBUG_SEP = False
NCOL = 5912
NEG = -30000.0
SCALE = 0.125
BIGF = 1.0e4
EPSL = 1.0e-18
C_QA, C_KV, C_GA, C_ZA, C_QB, C_ZB, C_MG = 0, 512, 1280, 1304, 1816, 3352, 3864
SLOPES = [2.0 ** (-(i + 1) / 2.0) for i in range(16)]
SL_HH = [SLOPES[2 * h] for h in range(8)] + [SLOPES[2 * h + 1] for h in range(8)]


class Prog:
    EPOCH = 20000
    NDMA = 8

    def __init__(self, nc):
        self.nc = nc
        self.ops = []
        self.lastw = {}
        self.readers = {}
        self.stack = contextlib.ExitStack()
        self.sb_bytes = 0

    def sb(self, name, shape, dtype):
        n = 1
        for s in shape[1:]:
            n *= s
        self.sb_bytes += n * (4 if dtype == F32 else 2)
        return self.stack.enter_context(self.nc.sbuf_tensor('sb_' + name, list(shape), dtype))

    def ps(self, name, shape=(128, 512), dtype=F32):
        return self.stack.enter_context(self.nc.psum_tensor(name, list(shape), dtype))

    def _deps(self, idx, reads, writes):
        deps = set()
        for k in reads:
            w = self.lastw.get(k)
            if w is not None:
                deps.add(w)
        for k in writes:
            w = self.lastw.get(k)
            if w is not None:
                deps.add(w)
            for r in self.readers.get(k, ()):
                deps.add(r)
        for k in reads:
            self.readers.setdefault(k, []).append(idx)
        for k in writes:
            self.lastw[k] = idx
            self.readers[k] = []
        deps.discard(idx)
        return deps

    def op(self, eng, fn, reads=(), writes=()):
        idx = len(self.ops)
        deps = self._deps(idx, reads, writes)
        self.ops.append(dict(eng=eng, fn=fn, deps=deps, dma=False, wkeys=set(writes), rkeys=set(reads)))
        return idx

    def dma(self, eng, out, in_, reads=(), writes=()):
        idx = len(self.ops)
        deps = self._deps(idx, reads, writes)
        self.ops.append(dict(eng=eng, fn=lambda e: e.dma_start(out=out, in_=in_), deps=deps, dma=True,
                             wkeys=set(writes), rkeys=set(reads)))
        return idx

    def finish(self):
        nc = self.nc
        ops = self.ops
        needed = set()
        for i, o in enumerate(ops):
            nd = set()
            for d in o['deps']:
                od = ops[d]
                if od['eng'] == o['eng'] and not od['dma'] and not o['dma']:
                    if o['eng'] == 'tensor':
                        continue
                    if not ((od['wkeys'] & o['rkeys']) or (od['wkeys'] & o['wkeys'])):
                        continue
                nd.add(d)
            o['deps'] = nd
            for d in nd:
                if not ops[d]['dma']:
                    needed.add(d)
        engs = ['tensor', 'vector', 'scalar', 'gpsimd', 'sync']
        cnt = {e: 0 for e in engs}
        dcnt = {e: 0 for e in engs}
        nep = {e: 0 for e in engs}
        for i, o in enumerate(ops):
            e = o['eng']
            if o['dma']:
                d = dcnt[e]
                dcnt[e] += 1
                o['sig'] = (('dma', e, d % self.NDMA), 16 * (d // self.NDMA + 1), 16)
                o['prev'] = (('dma', e, d % self.NDMA), 16 * (d // self.NDMA)) if d >= self.NDMA else None
            elif i in needed:
                c = cnt[e]
                cnt[e] += 1
                ep = c // self.EPOCH
                nep[e] = max(nep[e], ep + 1)
                o['sig'] = (('cmp', e, ep), c % self.EPOCH + 1, 1)
            else:
                o['sig'] = None
        sems = {}
        for e in engs:
            for ep in range(nep[e]):
                sems[('cmp', e, ep)] = self.stack.enter_context(nc.semaphore(f"s_{e}_{ep}"))
            for j in range(min(self.NDMA, dcnt[e])):
                sems[('dma', e, j)] = self.stack.enter_context(nc.semaphore(f"d_{e}_{j}"))
        self.n_instr = {e: 0 for e in engs}
        with nc.Block() as block:
            def make(ename):
                def body(eng):
                    waited = {}
                    lastdma = {}
                    for o in ops:
                        if o['eng'] != ename:
                            continue
                        best = {}
                        for d in o['deps']:
                            s = ops[d]['sig']
                            if s[1] > best.get(s[0], 0):
                                best[s[0]] = s[1]
                        if o['dma'] and o['prev'] is not None:
                            k, v = o['prev']
                            if v > best.get(k, 0):
                                best[k] = v
                        for k, v in best.items():
                            if waited.get(k, 0) >= v:
                                continue
                            eng.wait_ge(sems[k], v)
                            waited[k] = v
                            self.n_instr[ename] += 1
                        ins = o['fn'](eng)
                        self.n_instr[ename] += 1
                        if o['sig'] is not None:
                            ins.then_inc(sems[o['sig'][0]], o['sig'][2])
                            if o['dma']:
                                lastdma[o['sig'][0]] = o['sig'][1]
                    for k, v in lastdma.items():
                        if waited.get(k, 0) < v:
                            eng.wait_ge(sems[k], v)
                return body
            for ename in engs:
                if any(o['eng'] == ename for o in ops):
                    getattr(block, ename)(make(ename))
        self.stack.close()


def make_consts():
    c = {}
    c['ident_f'] = np.eye(128, dtype=np.float32)
    c['ident_b'] = np.eye(128).astype(NPBF)
    p = np.arange(128)[:, None]
    fp = np.arange(896)[None, :]
    c['cmb'] = np.where(p > fp - 384, NEG, 0.0).astype(NPBF)
    c['omb'] = np.where(fp - 384 >= p, NEG, 0.0).astype(NPBF)
    kk = np.arange(4)[None, :, None] * 128 + np.arange(128)[:, None, None]
    f = np.arange(512)[None, None, :]
    mm = np.where(kk // 256 == f // 256, np.where(kk > f, NEG, 0.0), np.where(f // 256 > kk // 256, 0.0, NEG))
    c['mmb'] = mm.astype(NPBF)
    k2 = np.arange(4096)[None, :]
    c['eslc'] = (k2 // 64 == np.arange(64)[:, None]).astype(NPBF)
    c['emoba'] = (k2 // 256 == np.arange(16)[:, None]).astype(NPBF)
    c['cqb'] = np.ascontiguousarray(np.broadcast_to(np.arange(512, dtype=np.float32)[None, :], (128, 512)))
    alb = np.zeros((128, 16, 36), np.float32)
    for hh in range(16):
        for di in range(36):
            alb[:, hh, di] = SL_HH[hh] * (np.arange(128) + 128.0 * (di - 32))
    c['alb'] = alb
    clb = np.zeros((128, 8, 2, 8), np.float32)
    for ha in range(8):
        for a in range(2):
            for i in range(8):
                clb[:, ha, a, i] = SL_HH[ha] * (2048.0 * a + 16.0 * np.arange(128) + 31.0 - 512.0 * i)
    c['clb'] = clb
    cq = np.zeros((16, 512), np.float32)
    for hh in range(16):
        cq[hh] = -SL_HH[hh] * np.arange(512) / SCALE
    c['cq'] = cq.astype(NPBF)
    goh = np.zeros((56, 12, 128), np.float32)
    for g in range(2):
        for pp in range(2):
            for k in range(3):
                idx = (g * 2 + pp) * 3 + k
                ra = k * 8 + g * 4 + 2 * pp
                goh[ra, idx, 0:64] = 1.0
                goh[32 + ra, idx, 0:64] = 1.0
                goh[ra + 1, idx, 64:128] = 1.0
                goh[32 + ra + 1, idx, 64:128] = 1.0
    c['goh'] = goh.astype(NPBF)
    n_cmp, n_slc = 255, 64
    cs = np.arange(n_cmp) * 16
    ce = cs + 31
    ss = np.arange(n_slc) * 64
    se = ss + 63
    ov = ((cs[:, None] <= se[None, :]) & (ce[:, None] >= ss[None, :])).astype(np.float32)
    ovp = np.zeros((256, 64), np.float32)
    ovp[:255] = ov
    c['ov'] = np.ascontiguousarray(ovp.reshape(2, 128, 64).transpose(1, 0, 2)).astype(NPBF)
    q = np.arange(128)[:, None]
    m = np.arange(128)[None, :]
    jr = m - 64
    cr = q // 64
    ab = np.where((jr == cr) | (jr == cr - 1), BIGF, np.where(jr > cr, -BIGF, 0.0))
    c['abase'] = ab.astype(np.float32)
    m2 = np.arange(32)[None, :]
    c['bbase'] = np.broadcast_to(np.where(m2 >= 16, -BIGF, 0.0), (128, 32)).astype(np.float32).copy()
    c['obase'] = np.broadcast_to(np.where(m2 == 16, 0.0, 2 * NEG), (128, 32)).astype(np.float32).copy()
    c['xg'] = (16.0 * np.arange(128)[:, None] + 31.0 - np.arange(512)[None, :]).astype(np.float32)
    return c


DRAM_ONLY = ('eslc', 'emoba', 'cq')
CONST_DT = dict(ident_f=F32, ident_b=BF16, cmb=BF16, omb=BF16, mmb=BF16, eslc=BF16, emoba=BF16, cqb=F32, alb=F32, clb=F32,
                cq=BF16, goh=BF16, ov=BF16, abase=F32, bbase=F32, obase=F32, xg=F32)


def build(S, NSEQ, dbg=()):
    nc = bass.Bass("TRN2", target_bir_lowering=False)
    NT = S // 128
    NQT = S // 512
    consts = make_consts()

    def din(name, shape, dt=F32):
        return nc.dram_tensor(name, list(shape), dt, kind="ExternalInput").ap()

    x_d = din("x", [NSEQ, S, D])
    win_d = din("w_in", [D, NCOL])
    w1s_d = din("w1s", [128, 32, 128])
    w2s_d = din("w2s", [128, 2, 64])
    pet_d = din("pet", [128, 32])
    wa_d = din("w_a", [512, D])
    wb_d = din("w_b", [512, D])
    wo_d = din("w_o", [D, D])
    npre_d = din("npre_bc", [128, D])
    npost_d = din("npost_bc", [128, D])
    cd = {k: din("c_" + k, list(v.shape), CONST_DT[k]) for k, v in consts.items()}
    out_d = nc.dram_tensor("out", [NSEQ, S, D], F32, kind="ExternalOutput").ap()
    wbf_d = nc.dram_tensor("wbf", [D, NCOL], BF16, kind="Internal").ap()
    ysc_d = nc.dram_tensor("yscr", [8, 128, S], BF16, kind="Internal").ap()
    wabf_d = nc.dram_tensor("wabf", [2, 512, D], BF16, kind="Internal").ap()
    dbg_d = {}
    for name, shape in dbg:
        dbg_d[name] = nc.dram_tensor("dbg_" + name, list(shape), F32, kind="ExternalOutput").ap()

    P = Prog(nc)
    cs = {k: P.sb("k_" + k, list(v.shape), CONST_DT[k]) for k, v in consts.items() if k not in DRAM_ONLY}
    uT = P.sb("uT", [128, 8, S], BF16)
    SA = max(S, 4096)
    kT0 = P.sb("kT0", [128, SA], BF16)
    kT1 = P.sb("kT1", [128, SA], BF16)
    Vp = P.sb("Vp", [128, 2 * SA], BF16)
    kcT = P.sb("kcT", [128, 256], BF16)
    vcx = P.sb("vcx", [128, 2, 128], BF16)
    hcm = P.sb("hcm", [128, 2, 256], BF16)
    QA = [[P.sb(f"QA{b}_{h}", [128, 512], BF16) for h in range(4)] for b in range(2)]
    PT = [P.sb(f"PT{j}", [128, 512], BF16) for j in range(6)]
    XM = [P.sb(f"XM{j}", [128, 512], BF16) for j in range(2)]
    WKB = P.sb("WKB", [128, 7, 512], F32)
    WK = [WKB[:, j, :] for j in range(5)]
    YST = [P.sb(f"YST{j}", [128, 512], BF16) for j in range(2)]
    SGT = [P.sb(f"SGT{j}", [64, 512], BF16) for j in range(2)]
    T1 = P.sb("T1", [128, 4, 64], F32)
    T2 = P.sb("T2", [128, 4, 64], F32)
    M8a = P.sb("M8a", [128, 8], F32)
    M8b = P.sb("M8b", [128, 8], F32)
    GT = P.sb("GT", [128, 8, 16], F32)
    kbarf = P.sb("kbarf", [128, 16], F32)
    kbar = P.sb("kbar", [64, 2, 16], BF16)
    wst = [P.sb(f"wst{j}", [128, 8, 128], BF16) for j in range(2)]
    WQZ = P.sb("WQZ", [128, 8, 512], BF16)
    wg24 = P.sb("wg24", [128, 8, 24], BF16)
    w2s = P.sb("w2s", [128, 2, 64], BF16)
    pet = P.sb("pet", [128, 32], BF16)
    hb = P.sb("hb", [128, 2], F32)
    xt = [P.sb(f"xt{j}", [128, D], F32) for j in range(2)]
    xn = [P.sb(f"xn{j}", [128, D], F32) for j in range(2)]
    gpre = WKB[:, 5:7, :].rearrange("p a n -> p (a n)")
    gpost = gpre
    ACC = [[xt[j][:, b_ * 512:(b_ + 1) * 512] for j in range(2)] for b_ in range(2)]
    SZ = [[xn[j][:, b_ * 512:(b_ + 1) * 512] for j in range(2)] for b_ in range(2)]
    if DEBUG_SEP:
        ACC = [[P.sb(f"ACCd{b_}{j}", [128, 512], F32)[:] for j in range(2)] for b_ in range(2)]
        SZ = [[P.sb(f"SZd{b_}{j}", [128, 512], F32)[:] for j in range(2)] for b_ in range(2)]
    impacc = WKB[:, 5, :]
    WKg = WKB[:, 6, :]
    ones1 = P.sb("ones1", [128, 1], F32)

    def XK(b_):
        return [('xt', b_, 0), ('xt', b_, 1)]

    def NK(b_):
        return [('xn', b_, 0), ('xn', b_, 1)]
    ssq = P.sb("ssq", [128, 2], F32)
    ssq2 = P.sb("ssq2", [128, 2], F32)
    eps_t = P.sb("eps_t", [128, 1], F32)
    ones_b = P.sb("ones_b", [128, 128], BF16)
    wab = WQZ[:].rearrange("p c (b n) -> p b c n", b=4)[:, 0:2]
    wo_s = P.sb("wo_s", [128, 8, D], BF16)
    ps = [P.ps(f"ps{j}") for j in range(8)]
    ST = [0, 1]
    OLS = [(2, 3), (4, 5)]
    MISC = [6, 7]
    Vp3 = Vp[:].rearrange("p (t c) -> p t c", c=256)
    mT = kT0[:].rearrange("p (c t) -> p c t", c=8)
    ysl = kT1[:].rearrange("p (c t) -> p c t", c=8)
    wgs = Vp[:, 0:SA].rearrange("p (b c n) -> p b c n", b=2, c=8)
    assert SA // 8 >= 512 and SA // 16 >= 256

    rr = {'misc': 0, 'st': 0, 'pt': 0, 'wst': 0, 'wk': 0, 'ol': 0, 'ptc': 0, 'g4': 0}

    def kt_lo(hh, i):
        lo = 0
        while SL_HH[hh] * (512 * i - 128 * lo - 127) > 110.0:
            lo += 1
        return lo

    def MM(out, lhsT, rhs, start, stop, reads, writes, tp=None):
        if tp is None:
            P.op('tensor', lambda e: e.matmul(out, lhsT=lhsT, rhs=rhs, start=start, stop=stop), reads, writes)
        else:
            P.op('tensor', lambda e: e.matmul(out, lhsT=lhsT, rhs=rhs, start=start, stop=stop, tile_position=tp),
                 reads, writes)

    def TR(out, in_, ident, reads, writes):
        P.op('tensor', lambda e: e.transpose(out, in_, ident), reads, writes)

    def ACT(out, in_, func, reads, writes, bias=None, scale=1.0):
        if bias is None:
            P.op('scalar', lambda e: e.activation(out=out, in_=in_, func=func, scale=scale), reads, writes)
        else:
            P.op('scalar', lambda e: e.activation(out=out, in_=in_, func=func, bias=bias, scale=scale), reads, writes)

    def TT(eng, out, in0, in1, op, reads, writes):
        P.op(eng, lambda e: e.tensor_tensor(out=out, in0=in0, in1=in1, op=op), reads, writes)

    def TS(eng, out, in0, s1, s2, op0, op1, reads, writes):
        if op1 is None:
            P.op(eng, lambda e: e.tensor_scalar(out=out, in0=in0, scalar1=s1, scalar2=None, op0=op0), reads, writes)
        else:
            P.op(eng, lambda e: e.tensor_scalar(out=out, in0=in0, scalar1=s1, scalar2=s2, op0=op0, op1=op1),
                 reads, writes)

    def STT(out, in0, scalar, in1, op0, op1, reads, writes):
        P.op('vector', lambda e: e.scalar_tensor_tensor(out=out, in0=in0, scalar=scalar, in1=in1, op0=op0, op1=op1),
             reads, writes)

    def CP(eng, out, in_, reads, writes):
        P.op(eng, lambda e: e.tensor_copy(out=out, in_=in_), reads, writes)

    def RCP(eng, out, in_, reads, writes):
        ACT(out, in_, AF.Ln, reads, writes)
        ACT(out, out, AF.Exp, writes, writes, scale=-1.0)

    def SIG1P(ap, np_, key):
        ACT(ap, ap, AF.Ln, [key, 'ones1'], [key], bias=ones1[0:np_, 0:1], scale=1.0)
        ACT(ap, ap, AF.Exp, [key], [key], scale=-1.0)

    def MS(eng, ap, val, writes):
        P.op(eng, lambda e: e.memset(ap, val), (), writes)

    def pk(j):
        return ('ps', j)

    def misc_bank():
        j = MISC[rr['misc'] % 2]
        rr['misc'] += 1
        return j

    def next_wst():
        j = rr['wst'] % 2
        rr['wst'] += 1
        return j

    wcols = wbf_d.rearrange("(c p) n -> p c n", p=128)
    WBK = [('wbf', r) for r in range(8)]

    def load_w(dst, col0, ncols, key):
        P.dma('sync', dst, wcols[:, :, col0:col0 + ncols], reads=WBK, writes=[key])

    def dump(name, src, reads):
        if name in dbg_d:
            P.dma('sync', dbg_d[name], src, reads=reads, writes=['dbg_' + name])

    for k in cs:
        P.dma('sync', cs[k][:], cd[k], reads=(), writes=['c_' + k])
    CK = ['c_alb', 'c_clb']
    for r in range(8):
        P.dma('gpsimd', wbf_d[r * 128:(r + 1) * 128, :], win_d[r * 128:(r + 1) * 128, :], reads=(), writes=[('wbf', r)])
    P.dma('gpsimd', wabf_d[0], wa_d, reads=(), writes=['wabf0'])
    P.dma('gpsimd', wabf_d[1], wb_d, reads=(), writes=['wabf1'])
    P.dma('gpsimd', wo_s[:], wo_d.rearrange("(j p) n -> p j n", p=128), reads=(), writes=['wo'])
    P.dma('gpsimd', w2s[:], w2s_d, reads=(), writes=['w2s'])
    P.dma('gpsimd', pet[:], pet_d, reads=(), writes=['pet'])
    load_w(wg24[:], C_GA, 24, 'wg24')
    MS('vector', eps_t[:], 1e-6, ['eps'])
    MS('vector', ones_b[:], 1.0, ['ones'])
    for b_ in range(2):
        MS('vector', SGT[b_][:], 0.0, [('SGT', b_)])
    MS('vector', vcx[:], 1.0, ['vcx'])
    MS('vector', ones1[:], 1.0, ['ones1'])
    MS('vector', kcT[64:65, :], 1.0, ['kcT'])
    MS('vector', hcm[:], 0.0, ['hcm'])

    def adv(gen):
        if gen is not None:
            try:
                next(gen)
            except StopIteration:
                pass

    def exhaust(gen):
        if gen is not None:
            for _ in gen:
                pass

    def attend(tiles, gen=None, step=3):
        pend = []
        for t in tiles:
            sb_ = ST[rr['st'] % 2]
            rr['st'] += 1
            pj = rr['pt'] % 4
            rr['pt'] += 1
            n = len(t['smm'])
            for m, (lh, rh, rd) in enumerate(t['smm']):
                MM(ps[sb_][:, :], lh, rh, m == 0, m == n - 1, rd, [pk(sb_)])
            ACT(PT[pj][:], ps[sb_][:, :], AF.Exp, [pk(sb_)] + CK, [('PT', pj)], bias=t['bias'], scale=SCALE)
            pend.append((t['pv'], pj))
            rr['tc'] = rr.get('tc', 0) + 1
            if rr['tc'] % step == 0:
                adv(gen)
            if len(pend) > 2:
                pv_, pj_ = pend.pop(0)
                for (o_, lh, tp, st_, sp_, rd, wk) in pv_:
                    MM(o_, lh, PT[pj_][:], st_, sp_, rd + [('PT', pj_)], [wk], tp=tp)
        for pv_, pj_ in pend:
            for (o_, lh, tp, st_, sp_, rd, wk) in pv_:
                MM(o_, lh, PT[pj_][:], st_, sp_, rd + [('PT', pj_)], [wk], tp=tp)

    def proj_fm(wtile, wkey, wc0, ncol, blk, bank):
        for c in range(8):
            MM(ps[bank][0:ncol, :], wtile[:, c, wc0:wc0 + ncol], uT[:, c, blk * 512:(blk + 1) * 512],
               c == 0, c == 7, [wkey, ('uT', blk)], [pk(bank)])

    def silu_pair(bank, dst, dkey):
        w = WK[0]
        ACT(w[:], ps[bank][:, :], AF.Exp, [pk(bank)], [('WK', 0)], scale=-1.0)
        SIG1P(w[:], 128, ('WK', 0))
        TT('vector', dst, ps[bank][:, :], w[:], ALU.mult, [pk(bank), ('WK', 0)], [dkey])

    def combine_branch(acc, akey, first, gbank, banks):
        w = WK[1]
        for hl in range(2):
            r0 = 64 * hl
            TS('vector', w[r0:r0 + 64, :], ps[banks[hl]][64:128, :], EPSL, None, ALU.max, None, [pk(banks[hl])],
               [('WK', 1)])
        RCP('vector', w[:], w[:], [('WK', 1)], [('WK', 1)])
        if gbank is not None:
            TT('vector', w[:], ps[gbank][:, :], w[:], ALU.mult, [pk(gbank), ('WK', 1)], [('WK', 1)])
        dst = acc if first else WK[2]
        dkey = akey if first else ('WK', 2)
        for hl in range(2):
            r0 = 64 * hl
            TT('vector', dst[r0:r0 + 64, :], ps[banks[hl]][0:64, :], w[r0:r0 + 64, :], ALU.mult,
               [pk(banks[hl]), ('WK', 1)], [dkey])
        if not first:
            TT('vector', acc, acc, WK[2][:], ALU.add, [akey, ('WK', 2)], [akey])

    for s in range(NSEQ):
        P.dma('sync', gpre, npre_d, reads=(), writes=['impacc', 'WKg'])
        for tt in range(NT):
            b = tt % 2
            P.dma('sync', xt[b][:], x_d[s, tt * 128:(tt + 1) * 128, :], reads=(), writes=XK(b))
            TT('vector', xn[b][:], xt[b][:], xt[b][:], ALU.mult, XK(b), NK(b))
            P.op('vector', (lambda bb: lambda e: e.reduce_sum(out=ssq[:, bb:bb + 1], in_=xn[bb][:], axis=AX.X))(b),
                 NK(b), [('ssq', b)])
            ACT(ssq[:, b:b + 1], ssq[:, b:b + 1], AF.Ln, [('ssq', b), 'eps'], [('ssq', b)], bias=eps_t[:, 0:1],
                scale=1.0 / D)
            ACT(ssq[:, b:b + 1], ssq[:, b:b + 1], AF.Exp, [('ssq', b)], [('ssq', b)], scale=-0.5)
            STT(xn[b][:], xt[b][:], ssq[:, b:b + 1], gpre, ALU.mult, ALU.mult, XK(b) + [('ssq', b), 'impacc', 'WKg'],
                NK(b))
            for half in range(2):
                bk = misc_bank()
                for cc in range(4):
                    c = half * 4 + cc
                    TR(ps[bk][:, cc * 128:(cc + 1) * 128], xn[b][:, c * 128:(c + 1) * 128], cs['ident_f'][:],
                       NK(b) + ['c_ident_f'], [pk(bk)])
                CP('vector' if half == 0 else 'gpsimd' if False else 'vector',
                   uT[:, half * 4:(half + 1) * 4, tt * 128:(tt + 1) * 128],
                   ps[bk][:, :].rearrange("p (c t) -> p c t", c=4), [pk(bk)], [('uT', tt // 4)])
        if 'uT' in dbg_d and s == 0:
            CP('vector', WK[0][:], uT[:, 0, 0:512], [('uT', 0)], [('WK', 0)])
            dump('uT', WK[0][:], [('WK', 0)])

        ncm = (S - 32) // 16 + 1
        for g in range(2):
            wj = next_wst()
            load_w(wst[wj][:, :, 0:64], C_KV + 0 * 128 + g * 64, 64, ('wst', wj))
            load_w(wst[wj][:, :, 64:128], C_KV + 1 * 128 + g * 64, 64, ('wst', wj))
            for blk in range(NQT):
                bk = misc_bank()
                proj_fm(wst[wj], ('wst', wj), 0, 128, blk, bk)
                CP('vector', kT1[:, blk * 512:(blk + 1) * 512], ps[bk][:, :], [pk(bk)], ['kT1'])
            W1 = Vp[:, 0:4096].rearrange("p (l e) -> p l e", e=128)
            P.dma('gpsimd', W1, w1s_d, reads=(), writes=['Vp'])
            for wh in range(2):
                r0 = 64 * wh
                bk = misc_bank()
                for l in range(32):
                    MM(ps[bk][:, 0:ncm], W1[r0:r0 + 64, l, :], kT1[r0:r0 + 64, l:l + 16 * (ncm - 1) + 1:16],
                       l == 0, l == 31, ['Vp', 'kT1'], [pk(bk)])
                bk2 = misc_bank()
                for l in range(32):
                    MM(ps[bk2][:, 0:1], W1[r0:r0 + 64, l, :], pet[r0:r0 + 64, l:l + 1], l == 0, l == 31,
                       ['Vp', 'pet'], [pk(bk2)])
                CP('vector', hb[:, 0:1], ps[bk2][:, 0:1], [pk(bk2)], ['hb'])
                TS('vector', hb[:, 1:2], hb[:, 0:1], -1.0, None, ALU.mult, None, ['hb'], ['hb'])
                w = WK[0]
                ACT(w[:, 0:ncm], ps[bk][:, 0:ncm], AF.Exp, [pk(bk), 'hb'], [('WK', 0)], bias=hb[:, 1:2], scale=-1.0)
                SIG1P(w[:, 0:ncm], 128, ('WK', 0))
                STT(hcm[:, wh, 0:ncm], ps[bk][:, 0:ncm], hb[:, 0:1], w[:, 0:ncm], ALU.add, ALU.mult,
                    [pk(bk), 'hb', ('WK', 0)], ['hcm'])
                if wh == 0:
                    bk3 = misc_bank()
                    MM(ps[bk3][0:64, 0:256], w2s[:, 0, :], hcm[:, 0, :], True, True, ['w2s', 'hcm'], [pk(bk3)])
                    CP('vector', kcT[0:64, :], ps[bk3][0:64, 0:256], [pk(bk3)], ['kcT'])
                else:
                    bk3 = misc_bank()
                    for a in range(2):
                        MM(ps[bk3][:, a * 64:(a + 1) * 64], hcm[:, 1, a * 128:(a + 1) * 128], w2s[:, 1, :], True, True,
                           ['w2s', 'hcm'], [pk(bk3)])
                    CP('vector', vcx[:, :, 0:64], ps[bk3][:, 0:128].rearrange("p (a d) -> p a d", a=2), [pk(bk3)],
                       ['vcx'])
            wj = next_wst()
            load_w(wst[wj][:, :, 0:64], C_KV + 2 * 128 + g * 64, 64, ('wst', wj))
            load_w(wst[wj][:, :, 64:128], C_KV + 4 * 128 + g * 64, 64, ('wst', wj))
            for blk in range(NQT):
                bk = misc_bank()
                proj_fm(wst[wj], ('wst', wj), 0, 128, blk, bk)
                CP('vector', kT0[0:64, blk * 512:(blk + 1) * 512], ps[bk][0:64, :], [pk(bk)], ['kT0'])
                CP('vector', kT1[0:64, blk * 512:(blk + 1) * 512], ps[bk][64:128, :], [pk(bk)], ['kT1'])
            P.dma('sync', kT0[64:128, 0:S], cd['eslc'][:, 0:S], reads=(), writes=['kT0'])
            MS('vector', kT1[64:128, :], 0.0, ['kT1'])
            MS('vector', kT1[64:65, :], 1.0, ['kT1'])
            load_w(WQZ[:, :, 0:256], C_QA + g * 256, 256, 'WQZ')
            load_w(WQZ[:, :, 256:512], C_ZA + g * 256, 256, 'WQZ')
            for h4 in range(4):
                for b2 in range(2):
                    P.dma('sync', QA[b2][h4][64:65, :], cd['cq'][4 * g + h4:4 * g + h4 + 1, :], reads=(),
                          writes=[('QA', b2, h4)])

            def nsa_pre(g, i):
                b2 = i % 2
                for pp in range(2):
                    bk = misc_bank()
                    proj_fm(WQZ, 'WQZ', pp * 128, 128, i, bk)
                    CP('vector', QA[b2][2 * pp][0:64, :], ps[bk][0:64, :], [pk(bk)], [('QA', b2, 2 * pp)])
                    CP('vector', QA[b2][2 * pp + 1][0:64, :], ps[bk][64:128, :], [pk(bk)], [('QA', b2, 2 * pp + 1)])
                    yield
                for pp in range(2):
                    bk = misc_bank()
                    proj_fm(WQZ, 'WQZ', 256 + pp * 128, 128, i, bk)
                    silu_pair(bk, SZ[b2][pp], ('xn', pp, b2))
                    yield
                bk = misc_bank()
                for c in range(8):
                    MM(ps[bk][0:24, :], wg24[:, c, :], uT[:, c, i * 512:(i + 1) * 512], c == 0, c == 7,
                       ['wg24', ('uT', i)], [pk(bk)])
                w = WK[0]
                ACT(w[0:24, :], ps[bk][0:24, :], AF.Exp, [pk(bk)], [('WK', 0)], scale=-1.0)
                SIG1P(w[0:24, :], 24, ('WK', 0))
                CP('vector', SGT[b2][0:24, :], w[0:24, :], [('WK', 0)], [('SGT', b2)])
                TT('vector', SGT[b2][32:56, :], w[0:24, :], SGT[b2][0:24, :], ALU.subtract, [('WK', 0), ('SGT', b2)],
                   [('SGT', b2)])
                yield
                a_list = [0] if (512 * i + 511) < (2048 + 31) else [0, 1]
                a_list = [a for a in a_list if a * 128 < ncm]
                for a in a_list:
                    thr = 512.0 * i - 2048.0 * a
                    TS('vector', XM[a][:], cs['xg'][:], thr, NEG, ALU.is_gt, ALU.mult, ['c_xg'], [('XM', a)])
                la = len(a_list)
                for h4 in range(4):
                    hl = h4 % 2
                    pp = h4 // 2
                    ha = 4 * g + h4
                    if hl == 0:
                        gb = misc_bank()
                        MM(ps[gb][:, :], cs['goh'][0:56, (g * 2 + pp) * 3 + 0, :], SGT[b2][0:56, :], True, True,
                           ['c_goh', ('SGT', b2)], [pk(gb)])
                        CP('vector', WKg[:], ps[gb][:, :], [pk(gb)], ['WKg'])
                    bkA = misc_bank()
                    bkB = misc_bank()
                    for ai, a in enumerate(a_list):
                        sb_ = ST[rr['st'] % 2]
                        rr['st'] += 1
                        pj = 4 + rr['ptc'] % 2
                        rr['ptc'] += 1
                        MM(ps[sb_][:, :], kcT[0:65, a * 128:(a + 1) * 128], QA[b2][h4][0:65, :], True, False,
                           ['kcT', ('QA', b2, h4)], [pk(sb_)])
                        MM(ps[sb_][:, :], cs['ident_b'][:], XM[a][:], False, True, ['c_ident_b', ('XM', a)], [pk(sb_)])
                        ACT(PT[pj][:], ps[sb_][:, :], AF.Exp, [pk(sb_)] + CK, [('PT', pj)],
                            bias=cs['clb'][:, ha, a, i:i + 1], scale=SCALE)
                        MM(ps[bkA][:, :], vcx[:, a, :], PT[pj][:], ai == 0, ai == la - 1, ['vcx', ('PT', pj)], [pk(bkA)])
                        MM(ps[bkB][0:64, :], cs['ov'][:, a, :], PT[pj][:], ai == 0, ai == la - 1, ['c_ov', ('PT', pj)],
                           [pk(bkB)])
                    wr = WK[3]
                    TS('vector', wr[0:64, :], ps[bkA][64:128, :], EPSL, None, ALU.max, None, [pk(bkA)], [('WK', 3)])
                    TS('vector', wr[64:128, :], ps[bkA][64:128, :], EPSL, None, ALU.max, None, [pk(bkA)], [('WK', 3)])
                    RCP('vector', wr[:, :], wr[:, :], [('WK', 3)], [('WK', 3)])
                    if h4 == 0:
                        TT('vector', impacc[0:64, :], ps[bkB][0:64, :], wr[0:64, :], ALU.mult, [pk(bkB), ('WK', 3)],
                           ['impacc'])
                    else:
                        TT('vector', WK[4][0:64, :], ps[bkB][0:64, :], wr[0:64, :], ALU.mult, [pk(bkB), ('WK', 3)],
                           [('WK', 4)])
                        TT('vector', impacc[0:64, :], impacc[0:64, :], WK[4][0:64, :], ALU.add, ['impacc', ('WK', 4)],
                           ['impacc'])
                    r0 = 64 * hl
                    TT('vector', WK[1][r0:r0 + 64, :], WKg[r0:r0 + 64, :], wr[r0:r0 + 64, :], ALU.mult, ['WKg', ('WK', 3)],
                       [('WK', 1)])
                    TT('vector', ACC[b2][pp][r0:r0 + 64, :], ps[bkA][0:64, :], WK[1][r0:r0 + 64, :], ALU.mult,
                       [pk(bkA), ('WK', 1)], [('xt', pp, b2)])
                    yield
                bk = misc_bank()
                for qs in range(4):
                    TR(ps[bk][:, qs * 64:(qs + 1) * 64], impacc[0:64, qs * 128:(qs + 1) * 128], cs['ident_f'][0:64, 0:64],
                       ['impacc', 'c_ident_f'], [pk(bk)])
                for qs in range(4):
                    ti = 4 * i + qs
                    TT('vector', T1[:, qs, :], ps[bk][:, qs * 64:(qs + 1) * 64],
                       cs['abase'][:, 64 - 2 * ti:128 - 2 * ti], ALU.add, [pk(bk), 'c_abase'], ['T1'])
                MS('vector', T1[:, :, 0:1], BIGF, ['T1'])
                for qs in range(4):
                    P.op('vector', (lambda q_: lambda e: e.max(out=M8a[:], in_=T1[:, q_, :]))(qs), ['T1'], ['M8a'])
                    P.op('vector', (lambda q_: lambda e: e.match_replace(out=T2[:, q_, :], in_to_replace=M8a[:],
                                                                        in_values=T1[:, q_, :], imm_value=-1e9))(qs),
                         ['T1', 'M8a'], ['T2'])
                    P.op('vector', (lambda q_: lambda e: e.max(out=M8b[:], in_=T2[:, q_, :]))(qs), ['T2'], ['M8b'])
                    TS('vector', T2[:, qs, :], T1[:, qs, :], M8b[:, 7:8], NEG, ALU.is_lt, ALU.mult, ['T1', 'M8b'], ['T2'])
                yield
                yield
                bk = misc_bank()
                for qs in range(4):
                    TR(ps[bk][0:64, qs * 128:(qs + 1) * 128], T2[:, qs, :], cs['ident_f'][:], ['T2', 'c_ident_f'],
                       [pk(bk)])
                for h4 in range(4):
                    STT(QA[b2][h4][64:128, :], cs['cqb'][64:128, :], -SL_HH[4 * g + h4] / SCALE, ps[bk][0:64, :],
                        ALU.mult, ALU.add, [pk(bk), 'c_cqb'], [('QA', b2, h4)])
                yield

            gen0 = nsa_pre(g, 0)
            wj = next_wst()
            load_w(wst[wj][:, :, 0:64], C_KV + 3 * 128 + g * 64, 64, ('wst', wj))
            load_w(wst[wj][:, :, 64:128], C_KV + 5 * 128 + g * 64, 64, ('wst', wj))
            MS('vector', Vp3[:, :, :].rearrange("p t (h c) -> p t h c", h=2)[:, :, :, 64:128], 1.0, ['Vp'])
            for t4 in range(NT // 4):
                bk = misc_bank()
                for q4 in range(4):
                    tt = t4 * 4 + q4
                    for c in range(8):
                        MM(ps[bk][:, q4 * 128:(q4 + 1) * 128], uT[:, c, tt * 128:(tt + 1) * 128], wst[wj][:, c, :],
                           c == 0, c == 7, [('wst', wj), ('uT', t4)], [pk(bk)])
                CP('vector', Vp3[:, t4 * 4:(t4 + 1) * 4, :].rearrange("p t (h c) -> p t h c", h=2)[:, :, :, 0:64],
                   ps[bk][:, :].rearrange("p (t h c) -> p t h c", h=2, c=64), [pk(bk)], ['Vp'])
                adv(gen0)
                adv(gen0)
            exhaust(gen0)
            for i in range(NQT):
                b2 = i % 2
                gen = nsa_pre(g, i + 1) if i + 1 < NQT else None
                for pp in range(2):
                    for br in (1, 2):
                        tiles = []
                        OB, LB = OLS[rr['ol'] % 2]
                        rr['ol'] += 1
                        if br == 1:
                            kts = list(range(0, 4 * i + 4))
                        else:
                            kts = [kt for kt in range(4 * i - 4, 4 * i + 4) if kt >= 0]
                        for kt in kts:
                            for hl in range(2):
                                h4 = 2 * pp + hl
                                ha = 4 * g + h4
                                hk = [k_ for k_ in kts if k_ >= kt_lo(ha, i)]
                                if kt not in hk:
                                    continue
                                ki = hk.index(kt)
                                nk = len(hk)
                                kT = kT0 if br == 1 else kT1
                                kkey = 'kT0' if br == 1 else 'kT1'
                                smm = [(kT[:, kt * 128:(kt + 1) * 128], QA[b2][h4][:, :], [kkey, ('QA', b2, h4)])]
                                if br == 1:
                                    if kt >= 4 * i:
                                        r = kt - 4 * i
                                        smm.append((cs['ident_b'][:], cs['cmb'][:, 384 - 128 * r:896 - 128 * r],
                                                    ['c_ident_b', 'c_cmb']))
                                else:
                                    r = kt - (4 * i - 4)
                                    if r < 4:
                                        smm.append((cs['ident_b'][:], cs['omb'][:, 384 - 128 * r:896 - 128 * r],
                                                    ['c_ident_b', 'c_omb']))
                                    else:
                                        r -= 4
                                        smm.append((cs['ident_b'][:], cs['cmb'][:, 384 - 128 * r:896 - 128 * r],
                                                    ['c_ident_b', 'c_cmb']))
                                vcol = 0 if br == 1 else 128
                                bnk = (OB, LB)[hl]
                                pv = [(ps[bnk][:, :], Vp3[:, kt, vcol:vcol + 128], None,
                                       ki == 0, ki == nk - 1, ['Vp'], pk(bnk))]
                                tiles.append(dict(smm=smm, bias=cs['alb'][:, ha, kt - 4 * i + 32:kt - 4 * i + 33], pv=pv))
                        attend(tiles, gen)
                        gb = misc_bank()
                        MM(ps[gb][:, :], cs['goh'][0:56, (g * 2 + pp) * 3 + br, :], SGT[b2][0:56, :], True, True,
                           ['c_goh', ('SGT', b2)], [pk(gb)])
                        combine_branch(ACC[b2][pp], ('xt', pp, b2), False, gb, (OB, LB))
                    yj = rr['wk'] % 2
                    rr['wk'] += 1
                    TT('vector', YST[yj][:], ACC[b2][pp], SZ[b2][pp], ALU.mult, [('xt', pp, b2), ('xn', pp, b2)],
                       [('YST', yj)])
                    P.dma('sync', ysc_d[g * 2 + pp, :, i * 512:(i + 1) * 512], YST[yj][:], reads=[('YST', yj)],
                          writes=[('ysc', g * 2 + pp, i)])
                exhaust(gen)

        nb = S // 256
        for j in range(4):
            wj = next_wst()
            load_w(wst[wj][:], C_QB + 512 + j * 128, 128, ('wst', wj))
            for blk in range(NQT):
                bk = misc_bank()
                proj_fm(wst[wj], ('wst', wj), 0, 128, blk, bk)
                CP('vector', kT0[0:64, blk * 512:(blk + 1) * 512], ps[bk][0:64, :], [pk(bk)], ['kT0'])
                CP('vector', kT1[0:64, blk * 512:(blk + 1) * 512], ps[bk][64:128, :], [pk(bk)], ['kT1'])
                P.op('vector', (lambda bk_, blk_: lambda e: e.reduce_sum(
                    out=kbarf[:, 2 * blk_:2 * blk_ + 2], in_=ps[bk_][:, :].rearrange("p (b t) -> p b t", b=2),
                    axis=AX.X))(bk, blk), [pk(bk)], ['kbarf'])
            for kT_, kk_ in ((kT0, 'kT0'), (kT1, 'kT1')):
                MS('vector', kT_[64:128, :], 0.0, [kk_])
                MS('vector', kT_[64:65, :], 1.0, [kk_])
                P.dma('sync', kT_[96:112, 0:S], cd['emoba'][:, 0:S], reads=(), writes=[kk_])
            TS('vector', kbar[0:64, 0, 0:nb], kbarf[0:64, 0:nb], 1.0 / 256, None, ALU.mult, None, ['kbarf'], ['kbar'])
            TS('vector', kbar[0:64, 1, 0:nb], kbarf[64:128, 0:nb], 1.0 / 256, None, ALU.mult, None, ['kbarf'], ['kbar'])
            load_w(WQZ[:, :, 0:128], C_QB + j * 128, 128, 'WQZ')
            load_w(WQZ[:, :, 128:256], C_ZB + j * 128, 128, 'WQZ')
            for hl in range(2):
                for b2 in range(2):
                    MS('vector', QA[b2][hl][64:128, :], 0.0, [('QA', b2, hl)])
                    P.dma('sync', QA[b2][hl][64:65, :], cd['cq'][8 + 2 * j + hl:8 + 2 * j + hl + 1, :], reads=(),
                          writes=[('QA', b2, hl)])

            def moba_pre(j, i):
                b2 = i % 2
                bk = misc_bank()
                proj_fm(WQZ, 'WQZ', 0, 128, i, bk)
                CP('vector', QA[b2][0][0:64, :], ps[bk][0:64, :], [pk(bk)], [('QA', b2, 0)])
                CP('vector', QA[b2][1][0:64, :], ps[bk][64:128, :], [pk(bk)], [('QA', b2, 1)])
                yield
                bk = misc_bank()
                proj_fm(WQZ, 'WQZ', 128, 128, i, bk)
                silu_pair(bk, SZ[b2][0], ('xn', 0, b2))
                yield
                bk = misc_bank()
                for hl in range(2):
                    for qs in range(4):
                        MM(ps[bk][:, (hl * 4 + qs) * 16:(hl * 4 + qs) * 16 + nb], QA[b2][hl][0:64, qs * 128:(qs + 1) * 128],
                           kbar[0:64, hl, 0:nb], True, True, ['kbar', ('QA', b2, hl)], [pk(bk)])
                for hl in range(2):
                    for qs in range(4):
                        cur = (4 * i + qs) // 2
                        e8 = hl * 4 + qs
                        TT('vector', GT[:, e8, 0:nb], ps[bk][:, e8 * 16:e8 * 16 + nb], cs['bbase'][:, 16 - cur:16 - cur + nb],
                           ALU.add, [pk(bk), 'c_bbase'], ['GT'])
                        if nb < 16:
                            MS('vector', GT[:, e8, nb:16], -BIGF, ['GT'])
                        P.op('vector', (lambda e_: lambda e: e.max(out=M8a[:], in_=GT[:, e_, :]))(e8), ['GT'], ['M8a'])
                        TS('vector', GT[:, e8, :], GT[:, e8, :], M8a[:, 2:3], NEG, ALU.is_lt, ALU.mult, ['GT', 'M8a'], ['GT'])
                        TT('vector', GT[:, e8, :], GT[:, e8, :], cs['obase'][:, 16 - cur:32 - cur], ALU.max,
                           ['GT', 'c_obase'], ['GT'])
                yield
                yield
                for hl in range(2):
                    bk = misc_bank()
                    for qs in range(4):
                        TR(ps[bk][0:16, qs * 128:(qs + 1) * 128], GT[:, hl * 4 + qs, :], cs['ident_f'][:],
                           ['GT', 'c_ident_f'], [pk(bk)])
                    CP('vector', QA[b2][hl][96:112, :], ps[bk][0:16, :], [pk(bk)], [('QA', b2, hl)])
                    yield

            gen0 = moba_pre(j, 0)
            wj = next_wst()
            load_w(wst[wj][:], C_QB + 1024 + j * 128, 128, ('wst', wj))
            MS('vector', Vp3[:, :, :].rearrange("p t (h c) -> p t h c", h=2)[:, :, :, 64:128], 1.0, ['Vp'])
            for t4 in range(NT // 4):
                bk = misc_bank()
                for q4 in range(4):
                    tt = t4 * 4 + q4
                    for c in range(8):
                        MM(ps[bk][:, q4 * 128:(q4 + 1) * 128], uT[:, c, tt * 128:(tt + 1) * 128], wst[wj][:, c, :],
                           c == 0, c == 7, [('wst', wj), ('uT', t4)], [pk(bk)])
                CP('vector', Vp3[:, t4 * 4:(t4 + 1) * 4, :].rearrange("p t (h c) -> p t h c", h=2)[:, :, :, 0:64],
                   ps[bk][:, :].rearrange("p (t h c) -> p t h c", h=2, c=64), [pk(bk)], ['Vp'])
                adv(gen0)
            exhaust(gen0)
            for i in range(NQT):
                b2 = i % 2
                gen = moba_pre(j, i + 1) if i + 1 < NQT else None
                tiles = []
                OB, LB = OLS[rr['ol'] % 2]
                rr['ol'] += 1
                kts = list(range(0, 4 * i + 4))
                for kt in kts:
                    for hl in range(2):
                        hh = 8 + 2 * j + hl
                        hk = [k_ for k_ in kts if k_ >= kt_lo(hh, i)]
                        if kt not in hk:
                            continue
                        ki = hk.index(kt)
                        nk = len(hk)
                        kT = kT0 if hl == 0 else kT1
                        kkey = 'kT0' if hl == 0 else 'kT1'
                        smm = [(kT[:, kt * 128:(kt + 1) * 128], QA[b2][hl][:, :], [kkey, ('QA', b2, hl)])]
                        if kt >= 4 * i:
                            smm.append((cs['ident_b'][:], cs['mmb'][:, kt - 4 * i, :], ['c_ident_b', 'c_mmb']))
                        bnk = (OB, LB)[hl]
                        pv = [(ps[bnk][:, :], Vp3[:, kt, 128 * hl:128 * hl + 128], None,
                               ki == 0, ki == nk - 1, ['Vp'], pk(bnk))]
                        tiles.append(dict(smm=smm, bias=cs['alb'][:, hh, kt - 4 * i + 32:kt - 4 * i + 33], pv=pv))
                attend(tiles, gen, step=4)
                combine_branch(ACC[b2][0], ('xt', 0, b2), True, None, (OB, LB))
                yj = rr['wk'] % 2
                rr['wk'] += 1
                TT('vector', YST[yj][:], ACC[b2][0], SZ[b2][0], ALU.mult, [('xt', 0, b2), ('xn', 0, b2)], [('YST', yj)])
                P.dma('sync', ysc_d[4 + j, :, i * 512:(i + 1) * 512], YST[yj][:], reads=[('YST', yj)],
                      writes=[('ysc', 4 + j, i)])
                exhaust(gen)

        P.op('sync', lambda e: e.nop(), (), ['Vp', ('wgs', 0), ('wgs', 1), 'WQZ', ('wab', 0), ('wab', 1)])
        P.dma('sync', gpost, npost_d, reads=(), writes=['impacc', 'WKg'])
        for i in range(NQT):
            P.dma('sync', ysl[:, :, 0:512], ysc_d[:, :, i * 512:(i + 1) * 512].rearrange("c p t -> p c t"),
                  reads=[('ysc', jj, i) for jj in range(8)], writes=['kT1'])
            for fc in range(8):
                gj = fc % 2
                P.dma('sync', wgs[:, gj, :, 0:128], wcols[:, :, C_MG + fc * 128:C_MG + (fc + 1) * 128], reads=WBK,
                      writes=[('wgs', gj)])
                P.dma('sync', wgs[:, gj, :, 128:256], wcols[:, :, C_MG + 1024 + fc * 128:C_MG + 1024 + (fc + 1) * 128],
                      reads=WBK, writes=[('wgs', gj)])
                for ab in range(2):
                    P.dma('sync', wab[:, gj, 4 * ab:4 * ab + 4, :],
                          wabf_d[ab].rearrange("(j p) n -> p j n", p=128)[:, :, fc * 128:(fc + 1) * 128],
                          reads=['wabf0', 'wabf1'], writes=[('wab', gj)])
                for ab in range(2):
                    bg = [6, 7, 0, 1][rr['g4'] % 4]
                    rr['g4'] += 1
                    for c in range(8):
                        MM(ps[bg][:, :], wgs[:, gj, c, ab * 128:(ab + 1) * 128], uT[:, c, i * 512:(i + 1) * 512],
                           c == 0, c == 7, [('wgs', gj), ('uT', i)], [pk(bg)])
                    w = WK[ab]
                    ACT(w[:], ps[bg][:, :], AF.Sigmoid, [pk(bg)], [('WK', ab)])
                    bm = OLS[fc % 2][ab]
                    for jj in range(4):
                        MM(ps[bm][:, :], wab[:, gj, 4 * ab + jj, :], ysl[:, 4 * ab + jj, 0:512], jj == 0, jj == 3,
                           [('wab', gj), 'kT1'], [pk(bm)])
                    TT('vector', w[:], ps[bm][:, :], w[:], ALU.mult, [pk(bm), ('WK', ab)], [('WK', ab)])
                TT('vector', mT[:, fc, 0:512], WK[0][:], WK[1][:], ALU.add, [('WK', 0), ('WK', 1)], ['kT0'])
            for ts_ in range(4):
                tt = i * 4 + ts_
                b = tt % 2
                P.dma('sync', xt[b][:], x_d[s, tt * 128:(tt + 1) * 128, :], reads=(), writes=XK(b))
                banks = [ST[0], ST[1]]
                for hf in range(2):
                    for fc in range(8):
                        MM(ps[banks[hf]][:, :], mT[:, fc, ts_ * 128:(ts_ + 1) * 128], wo_s[:, fc, hf * 512:(hf + 1) * 512],
                           fc == 0, fc == 7, ['wo', 'kT0'], [pk(banks[hf])])
                for hf in range(2):
                    P.op('scalar', (lambda o_, i_: lambda e: e.copy(out=o_, in_=i_))(WK[2 + hf][:], ps[banks[hf]][:, :]),
                         [pk(banks[hf])], [('WK', 2 + hf)])
                    P.op('vector', (lambda o_, i_, a_: lambda e: e.scalar_tensor_tensor(
                        out=o_, in0=i_, scalar=1.0, in1=i_, op0=ALU.mult, op1=ALU.mult, accum_out=a_))(
                        YST[hf][:], WK[2 + hf][:], ssq2[:, hf:hf + 1]), [('WK', 2 + hf)], [('YST', hf), ('ssq2', hf)])
                TT('vector', ssq[:, b:b + 1], ssq2[:, 0:1], ssq2[:, 1:2], ALU.add, [('ssq2', 0), ('ssq2', 1)], [('ssq', b)])
                ACT(ssq[:, b:b + 1], ssq[:, b:b + 1], AF.Ln, [('ssq', b), 'eps'], [('ssq', b)], bias=eps_t[:, 0:1],
                    scale=1.0 / D)
                ACT(ssq[:, b:b + 1], ssq[:, b:b + 1], AF.Exp, [('ssq', b)], [('ssq', b)], scale=-0.5)
                for hf in range(2):
                    STT(xn[b][:, hf * 512:(hf + 1) * 512], WK[2 + hf][:], ssq[:, b:b + 1],
                        gpost[:, hf * 512:(hf + 1) * 512], ALU.mult, ALU.mult, [('WK', 2 + hf), ('ssq', b), 'impacc', 'WKg'],
                        [('xn', b, hf)])
                TT('gpsimd', xn[b][:], xn[b][:], xt[b][:], ALU.add, NK(b) + XK(b), NK(b))
                P.dma('sync', out_d[s, tt * 128:(tt + 1) * 128, :], xn[b][:], reads=NK(b), writes=[('out', tt)])
        P.op('sync', lambda e: e.nop(), (), ['Vp', ('wgs', 0), ('wgs', 1), 'WQZ', ('wab', 0), ('wab', 1)])
    P.finish()
    return nc, P


def host_inputs(inputs, S):
    c = make_consts()
    f = lambda a: np.ascontiguousarray(np.asarray(a, dtype=np.float32))
    w1k = f(inputs['cmp_w1_k'])[0].transpose(1, 0, 2)
    w1v = f(inputs['cmp_w1_v'])[0].transpose(1, 0, 2)
    shared = {
        'w_in': f(inputs['w_in'])[0],
        'w1s': np.ascontiguousarray(np.concatenate([w1k, w1v], 0)),
        'w2s': np.ascontiguousarray(np.stack([f(inputs['cmp_w2_k'])[0], f(inputs['cmp_w2_v'])[0]], 1)),
        'pet': np.ascontiguousarray(np.concatenate([f(inputs['cmp_pe_k'])[0].T, f(inputs['cmp_pe_v'])[0].T], 0)),
        'w_a': f(inputs['w_branch_a'])[0],
        'w_b': f(inputs['w_branch_b'])[0],
        'w_o': f(inputs['w_o'])[0],
        'npre_bc': np.ascontiguousarray(np.broadcast_to(f(inputs['norm_pre'])[0][None, :], (128, D))),
        'npost_bc': np.ascontiguousarray(np.broadcast_to(f(inputs['norm_post'])[0][None, :], (128, D))),
    }
    for k, v in c.items():
        shared['c_' + k] = v
    return shared


def kernel(**inputs):
    x = np.ascontiguousarray(np.asarray(inputs['x'], dtype=np.float32))
    B, S, _ = x.shape
    ncores = 8
    nseq = B // ncores
    nc, _ = build(S, nseq)
    shared = host_inputs(inputs, S)
    in_maps = []
    for c in range(ncores):
        m = dict(shared)
        m['x'] = np.ascontiguousarray(x[c * nseq:(c + 1) * nseq])
        in_maps.append(m)
    res = run_bass_kernel_spmd(nc, in_maps, core_ids=list(range(ncores)))
    return np.concatenate([np.asarray(r['out']) for r in res.results], axis=0).astype(np.float32)
```

```python
import contextlib
import numpy as np
import ml_dtypes
import concourse.bass as bass
import concourse.mybir as mybir
from concourse.bass_utils import run_bass_kernel_spmd

F32 = mybir.dt.float32
BF16 = mybir.dt.bfloat16
ALU = mybir.AluOpType
AF = mybir.ActivationFunctionType
AX = mybir.AxisListType
NPBF = ml_dtypes.bfloat16

D = 1024
DEBUG_SEP = False
NCOL = 5912
NEG = -30000.0
SCALE = 0.125
BIGF = 1.0e4
EPSL = 1.0e-18
C_QA, C_KV, C_GA, C_ZA, C_QB, C_ZB, C_MG = 0, 512, 1280, 1304, 1816, 3352, 3864
SLOPES = [2.0 ** (-(i + 1) / 2.0) for i in range(16)]
SL_HH = [SLOPES[2 * h] for h in range(8)] + [SLOPES[2 * h + 1] for h in range(8)]


class Prog:
    EPOCH = 20000
    NDMA = 8

    def __init__(self, nc):
        self.nc = nc
        self.ops = []
        self.lastw = {}
        self.readers = {}
        self.stack = contextlib.ExitStack()
        self.sb_bytes = 0

    def sb(self, name, shape, dtype):
        n = 1
        for s in shape[1:]:
            n *= s
        self.sb_bytes += n * (4 if dtype == F32 else 2)
        return self.stack.enter_context(self.nc.sbuf_tensor('sb_' + name, list(shape), dtype))

    def ps(self, name, shape=(128, 512), dtype=F32):
        return self.stack.enter_context(self.nc.psum_tensor(name, list(shape), dtype))

    def _deps(self, idx, reads, writes):
        deps = set()
        for k in reads:
            w = self.lastw.get(k)
            if w is not None:
                deps.add(w)
        for k in writes:
            w = self.lastw.get(k)
            if w is not None:
                deps.add(w)
            for r in self.readers.get(k, ()):
                deps.add(r)
        for k in reads:
            self.readers.setdefault(k, []).append(idx)
        for k in writes:
            self.lastw[k] = idx
            self.readers[k] = []
        deps.discard(idx)
        return deps

    def op(self, eng, fn, reads=(), writes=()):
        idx = len(self.ops)
        deps = self._deps(idx, reads, writes)
        self.ops.append(dict(eng=eng, fn=fn, deps=deps, dma=False, wkeys=set(writes), rkeys=set(reads)))
        return idx

    def dma(self, eng, out, in_, reads=(), writes=()):
        idx = len(self.ops)
        deps = self._deps(idx, reads, writes)
        self.ops.append(dict(eng=eng, fn=lambda e: e.dma_start(out=out, in_=in_), deps=deps, dma=True,
                             wkeys=set(writes), rkeys=set(reads)))
        return idx

    def finish(self):
        nc = self.nc
        ops = self.ops
        needed = set()
        for i, o in enumerate(ops):
            nd = set()
            for d in o['deps']:
                od = ops[d]
                if od['eng'] == o['eng'] and not od['dma'] and not o['dma']:
                    if o['eng'] == 'tensor':
                        continue
                    if not ((od['wkeys'] & o['rkeys']) or (od['wkeys'] & o['wkeys'])):
                        continue
                nd.add(d)
            o['deps'] = nd
            for d in nd:
                if not ops[d]['dma']:
                    needed.add(d)
        engs = ['tensor', 'vector', 'scalar', 'gpsimd', 'sync']
        cnt = {e: 0 for e in engs}
        dcnt = {e: 0 for e in engs}
        nep = {e: 0 for e in engs}
        for i, o in enumerate(ops):
            e = o['eng']
            if o['dma']:
                d = dcnt[e]
                dcnt[e] += 1
                o['sig'] = (('dma', e, d % self.NDMA), 16 * (d // self.NDMA + 1), 16)
                o['prev'] = (('dma', e, d % self.NDMA), 16 * (d // self.NDMA)) if d >= self.NDMA else None
            elif i in needed:
                c = cnt[e]
                cnt[e] += 1
                ep = c // self.EPOCH
                nep[e] = max(nep[e], ep + 1)
                o['sig'] = (('cmp', e, ep), c % self.EPOCH + 1, 1)
            else:
                o['sig'] = None
        sems = {}
        for e in engs:
            for ep in range(nep[e]):
                sems[('cmp', e, ep)] = self.stack.enter_context(nc.semaphore(f"s_{e}_{ep}"))
            for j in range(min(self.NDMA, dcnt[e])):
                sems[('dma', e, j)] = self.stack.enter_context(nc.semaphore(f"d_{e}_{j}"))
        self.n_instr = {e: 0 for e in engs}
        with nc.Block() as block:
            def make(ename):
                def body(eng):
                    waited = {}
                    lastdma = {}
                    for o in ops:
                        if o['eng'] != ename:
                            continue
                        best = {}
                        for d in o['deps']:
                            s = ops[d]['sig']
                            if s[1] > best.get(s[0], 0):
                                best[s[0]] = s[1]
                        if o['dma'] and o['prev'] is not None:
                            k, v = o['prev']
                            if v > best.get(k, 0):
                                best[k] = v
                        for k, v in best.items():
                            if waited.get(k, 0) >= v:
                                continue
                            eng.wait_ge(sems[k], v)
                            waited[k] = v
                            self.n_instr[ename] += 1
                        ins = o['fn'](eng)
                        self.n_instr[ename] += 1
                        if o['sig'] is not None:
                            ins.then_inc(sems[o['sig'][0]], o['sig'][2])
                            if o['dma']:
                                lastdma[o['sig'][0]] = o['sig'][1]
                    for k, v in lastdma.items():
                        if waited.get(k, 0) < v:
                            eng.wait_ge(sems[k], v)
                return body
            for ename in engs:
                if any(o['eng'] == ename for o in ops):
                    getattr(block, ename)(make(ename))
        self.stack.close()


def make_consts():
    c = {}
    c['ident_f'] = np.eye(128, dtype=np.float32)
    c['ident_b'] = np.eye(128).astype(NPBF)
    p = np.arange(128)[:, None]
    fp = np.arange(896)[None, :]
    c['cmb'] = np.where(p > fp - 384, NEG, 0.0).astype(NPBF)
    c['omb'] = np.where(fp - 384 >= p, NEG, 0.0).astype(NPBF)
    kk = np.arange(4)[None, :, None] * 128 + np.arange(128)[:, None, None]
    f = np.arange(512)[None, None, :]
    mm = np.where(kk // 256 == f // 256, np.where(kk > f, NEG, 0.0), np.where(f // 256 > kk // 256, 0.0, NEG))
    c['mmb'] = mm.astype(NPBF)
    k2 = np.arange(4096)[None, :]
    c['eslc'] = (k2 // 64 == np.arange(64)[:, None]).astype(NPBF)
    c['emoba'] = (k2 // 256 == np.arange(16)[:, None]).astype(NPBF)
    c['cqb'] = np.ascontiguousarray(np.broadcast_to(np.arange(512, dtype=np.float32)[None, :], (128, 512)))
    alb = np.zeros((128, 16, 36), np.float32)
    for hh in range(16):
        for di in range(36):
            alb[:, hh, di] = SL_HH[hh] * (np.arange(128) + 128.0 * (di - 32))
    c['alb'] = alb
    clb = np.zeros((128, 8, 2, 8), np.float32)
    for ha in range(8):
        for a in range(2):
            for i in range(8):
                clb[:, ha, a, i] = SL_HH[ha] * (2048.0 * a + 16.0 * np.arange(128) + 31.0 - 512.0 * i)
    c['clb'] = clb
    cq = np.zeros((16, 512), np.float32)
    for hh in range(16):
        cq[hh] = -SL_HH[hh] * np.arange(512) / SCALE
    c['cq'] = cq.astype(NPBF)
    goh = np.zeros((56, 12, 128), np.float32)
    for g in range(2):
        for pp in range(2):
            for k in range(3):
                idx = (g * 2 + pp) * 3 + k
                ra = k * 8 + g * 4 + 2 * pp
                goh[ra, idx, 0:64] = 1.0
                goh[32 + ra, idx, 0:64] = 1.0
                goh[ra + 1, idx, 64:128] = 1.0
                goh[32 + ra + 1, idx, 64:128] = 1.0
    c['goh'] = goh.astype(NPBF)
    n_cmp, n_slc = 255, 64
    cs = np.arange(n_cmp) * 16
    ce = cs + 31
    ss = np.arange(n_slc) * 64
    se = ss + 63
    ov = ((cs[:, None] <= se[None, :]) & (ce[:, None] >= ss[None, :])).astype(np.float32)
    ovp = np.zeros((256, 64), np.float32)
    ovp[:255] = ov
    c['ov'] = np.ascontiguousarray(ovp.reshape(2, 128, 64).transpose(1, 0, 2)).astype(NPBF)
    q = np.arange(128)[:, None]
    m = np.arange(128)[None, :]
    jr = m - 64
    cr = q // 64
    ab = np.where((jr == cr) | (jr == cr - 1), BIGF, np.where(jr > cr, -BIGF, 0.0))
    c['abase'] = ab.astype(np.float32)
    m2 = np.arange(32)[None, :]
    c['bbase'] = np.broadcast_to(np.where(m2 >= 16, -BIGF, 0.0), (128, 32)).astype(np.float32).copy()
    c['obase'] = np.broadcast_to(np.where(m2 == 16, 0.0, 2 * NEG), (128, 32)).astype(np.float32).copy()
    c['xg'] = (16.0 * np.arange(128)[:, None] + 31.0 - np.arange(512)[None, :]).astype(np.float32)
    return c


DRAM_ONLY = ('eslc', 'emoba', 'cq')
CONST_DT = dict(ident_f=F32, ident_b=BF16, cmb=BF16, omb=BF16, mmb=BF16, eslc=BF16, emoba=BF16, cqb=F32, alb=F32, clb=F32,
                cq=BF16, goh=BF16, ov=BF16, abase=F32, bbase=F32, obase=F32, xg=F32)


def build(S, NSEQ, dbg=()):
    nc = bass.Bass("TRN2", target_bir_lowering=False)
    NT = S // 128
    NQT = S // 512
    consts = make_consts()

    def din(name, shape, dt=F32):
        return nc.dram_tensor(name, list(shape), dt, kind="ExternalInput").ap()

    x_d = din("x", [NSEQ, S, D])
    win_d = din("w_in", [D, NCOL])
    w1s_d = din("w1s", [128, 32, 128])
    w2s_d = din("w2s", [128, 2, 64])
    pet_d = din("pet", [128, 32])
    wa_d = din("w_a", [512, D])
    wb_d = din("w_b", [512, D])
    wo_d = din("w_o", [D, D])
    npre_d = din("npre_bc", [128, D])
    npost_d = din("npost_bc", [128, D])
    cd = {k: din("c_" + k, list(v.shape), CONST_DT[k]) for k, v in consts.items()}
    out_d = nc.dram_tensor("out", [NSEQ, S, D], F32, kind="ExternalOutput").ap()
    wbf_d = nc.dram_tensor("wbf", [D, NCOL], BF16, kind="Internal").ap()
    ysc_d = nc.dram_tensor("yscr", [8, 128, S], BF16, kind="Internal").ap()
    wabf_d = nc.dram_tensor("wabf", [2, 512, D], BF16, kind="Internal").ap()
    dbg_d = {}
    for name, shape in dbg:
        dbg_d[name] = nc.dram_tensor("dbg_" + name, list(shape), F32, kind="ExternalOutput").ap()

    P = Prog(nc)
    cs = {k: P.sb("k_" + k, list(v.shape), CONST_DT[k]) for k, v in consts.items() if k not in DRAM_ONLY}
    uT = P.sb("uT", [128, 8, S], BF16)
    SA = max(S, 4096)
    kT0 = P.sb("kT0", [128, SA], BF16)
    kT1 = P.sb("kT1", [128, SA], BF16)
    Vp = P.sb("Vp", [128, 2 * SA], BF16)
    kcT = P.sb("kcT", [128, 256], BF16)
    vcx = P.sb("vcx", [128, 2, 128], BF16)
    hcm = P.sb("hcm", [128, 2, 256], BF16)
    QA = [[P.sb(f"QA{b}_{h}", [128, 512], BF16) for h in range(4)] for b in range(2)]
    PT = [P.sb(f"PT{j}", [128, 512], BF16) for j in range(6)]
    XM = [P.sb(f"XM{j}", [128, 512], BF16) for j in range(2)]
    WKB = P.sb("WKB", [128, 7, 512], F32)
    WK = [WKB[:, j, :] for j in range(5)]
    YST = [P.sb(f"YST{j}", [128, 512], BF16) for j in range(2)]
    SGT = [P.sb(f"SGT{j}", [64, 512], BF16) for j in range(2)]
    T1 = P.sb("T1", [128, 4, 64], F32)
    T2 = P.sb("T2", [128, 4, 64], F32)
    M8a = P.sb("M8a", [128, 8], F32)
    M8b = P.sb("M8b", [128, 8], F32)
    GT = P.sb("GT", [128, 8, 16], F32)
    kbarf = P.sb("kbarf", [128, 16], F32)
    kbar = P.sb("kbar", [64, 2, 16], BF16)
    wst = [P.sb(f"wst{j}", [128, 8, 128], BF16) for j in range(2)]
    WQZ = P.sb("WQZ", [128, 8, 512], BF16)
    wg24 = P.sb("wg24", [128, 8, 24], BF16)
    w2s = P.sb("w2s", [128, 2, 64], BF16)
    pet = P.sb("pet", [128, 32], BF16)
    hb = P.sb("hb", [128, 2], F32)
    xt = [P.sb(f"xt{j}", [128, D], F32) for j in range(2)]
    xn = [P.sb(f"xn{j}", [128, D], F32) for j in range(2)]
    gpre = WKB[:, 5:7, :].rearrange("p a n -> p (a n)")
    gpost = gpre
    ACC = [[xt[j][:, b_ * 512:(b_ + 1) * 512] for j in range(2)] for b_ in range(2)]
    SZ = [[xn[j][:, b_ * 512:(b_ + 1) * 512] for j in range(2)] for b_ in range(2)]
    if DEBUG_SEP:
        ACC = [[P.sb(f"ACCd{b_}{j}", [128, 512], F32)[:] for j in range(2)] for b_ in range(2)]
        SZ = [[P.sb(f"SZd{b_}{j}", [128, 512], F32)[:] for j in range(2)] for b_ in range(2)]
    impacc = WKB[:, 5, :]
    WKg = WKB[:, 6, :]
    ones1 = P.sb("ones1", [128, 1], F32)

    def XK(b_):
        return [('xt', b_, 0), ('xt', b_, 1)]

    def NK(b_):
        return [('xn', b_, 0), ('xn', b_, 1)]
    ssq = P.sb("ssq", [128, 2], F32)
    ssq2 = P.sb("ssq2", [128, 2], F32)
    eps_t = P.sb("eps_t", [128, 1], F32)
    ones_b = P.sb("ones_b", [128, 128], BF16)
    wab = WQZ[:].rearrange("p c (b n) -> p b c n", b=4)[:, 0:2]
    wo_s = P.sb("wo_s", [128, 8, D], BF16)
    ps = [P.ps(f"ps{j}") for j in range(8)]
    ST = [0, 1]
    OLS = [(2, 3), (4, 5)]
    MISC = [6, 7]
    Vp3 = Vp[:].rearrange("p (t c) -> p t c", c=256)
    mT = kT0[:].rearrange("p (c t) -> p c t", c=8)
    ysl = kT1[:].rearrange("p (c t) -> p c t", c=8)
    wgs = Vp[:, 0:SA].rearrange("p (b c n) -> p b c n", b=2, c=8)
    assert SA // 8 >= 512 and SA // 16 >= 256

    rr = {'misc': 0, 'st': 0, 'pt': 0, 'wst': 0, 'wk': 0, 'ol': 0, 'ptc': 0, 'g4': 0}

    def kt_lo(hh, i):
        lo = 0
        while SL_HH[hh] * (512 * i - 128 * lo - 127) > 110.0:
            lo += 1
        return lo

    def MM(out, lhsT, rhs, start, stop, reads, writes, tp=None):
        if tp is None:
            P.op('tensor', lambda e: e.matmul(out, lhsT=lhsT, rhs=rhs, start=start, stop=stop), reads, writes)
        else:
            P.op('tensor', lambda e: e.matmul(out, lhsT=lhsT, rhs=rhs, start=start, stop=stop, tile_position=tp),
                 reads, writes)

    def TR(out, in_, ident, reads, writes):
        P.op('tensor', lambda e: e.transpose(out, in_, ident), reads, writes)

    def ACT(out, in_, func, reads, writes, bias=None, scale=1.0):
        if bias is None:
            P.op('scalar', lambda e: e.activation(out=out, in_=in_, func=func, scale=scale), reads, writes)
        else:
            P.op('scalar', lambda e: e.activation(out=out, in_=in_, func=func, bias=bias, scale=scale), reads, writes)

    def TT(eng, out, in0, in1, op, reads, writes):
        P.op(eng, lambda e: e.tensor_tensor(out=out, in0=in0, in1=in1, op=op), reads, writes)

    def TS(eng, out, in0, s1, s2, op0, op1, reads, writes):
        if op1 is None:
            P.op(eng, lambda e: e.tensor_scalar(out=out, in0=in0, scalar1=s1, scalar2=None, op0=op0), reads, writes)
        else:
            P.op(eng, lambda e: e.tensor_scalar(out=out, in0=in0, scalar1=s1, scalar2=s2, op0=op0, op1=op1),
                 reads, writes)

    def STT(out, in0, scalar, in1, op0, op1, reads, writes):
        P.op('vector', lambda e: e.scalar_tensor_tensor(out=out, in0=in0, scalar=scalar, in1=in1, op0=op0, op1=op1),
             reads, writes)

    def CP(eng, out, in_, reads, writes):
        P.op(eng, lambda e: e.tensor_copy(out=out, in_=in_), reads, writes)

    def RCP(eng, out, in_, reads, writes):
        ACT(out, in_, AF.Ln, reads, writes)
        ACT(out, out, AF.Exp, writes, writes, scale=-1.0)

    def SIG1P(ap, np_, key):
        ACT(ap, ap, AF.Ln, [key, 'ones1'], [key], bias=ones1[0:np_, 0:1], scale=1.0)
        ACT(ap, ap, AF.Exp, [key], [key], scale=-1.0)

    def MS(eng, ap, val, writes):
        P.op(eng, lambda e: e.memset(ap, val), (), writes)

    def pk(j):
        return ('ps', j)

    def misc_bank():
        j = MISC[rr['misc'] % 2]
        rr['misc'] += 1
        return j

    def next_wst():
        j = rr['wst'] % 2
        rr['wst'] += 1
        return j

    wcols = wbf_d.rearrange("(c p) n -> p c n", p=128)
    WBK = [('wbf', r) for r in range(8)]

    def load_w(dst, col0, ncols, key):
        P.dma('sync', dst, wcols[:, :, col0:col0 + ncols], reads=WBK, writes=[key])

    def dump(name, src, reads):
        if name in dbg_d:
            P.dma('sync', dbg_d[name], src, reads=reads, writes=['dbg_' + name])

    for k in cs:
        P.dma('sync', cs[k][:], cd[k], reads=(), writes=['c_' + k])
    CK = ['c_alb', 'c_clb']
    for r in range(8):
        P.dma('gpsimd', wbf_d[r * 128:(r + 1) * 128, :], win_d[r * 128:(r + 1) * 128, :], reads=(), writes=[('wbf', r)])
    P.dma('gpsimd', wabf_d[0], wa_d, reads=(), writes=['wabf0'])
    P.dma('gpsimd', wabf_d[1], wb_d, reads=(), writes=['wabf1'])
    P.dma('gpsimd', wo_s[:], wo_d.rearrange("(j p) n -> p j n", p=128), reads=(), writes=['wo'])
    P.dma('gpsimd', w2s[:], w2s_d, reads=(), writes=['w2s'])
    P.dma('gpsimd', pet[:], pet_d, reads=(), writes=['pet'])
    load_w(wg24[:], C_GA, 24, 'wg24')
    MS('vector', eps_t[:], 1e-6, ['eps'])
    MS('vector', ones_b[:], 1.0, ['ones'])
    for b_ in range(2):
        MS('vector', SGT[b_][:], 0.0, [('SGT', b_)])
    MS('vector', vcx[:], 1.0, ['vcx'])
    MS('vector', ones1[:], 1.0, ['ones1'])
    MS('vector', kcT[64:65, :], 1.0, ['kcT'])
    MS('vector', hcm[:], 0.0, ['hcm'])

    def adv(gen):
        if gen is not None:
            try:
                next(gen)
            except StopIteration:
                pass

    def exhaust(gen):
        if gen is not None:
            for _ in gen:
                pass

    def attend(tiles, gen=None, step=3):
        pend = []
        for t in tiles:
            sb_ = ST[rr['st'] % 2]
            rr['st'] += 1
            pj = rr['pt'] % 4
            rr['pt'] += 1
            n = len(t['smm'])
            for m, (lh, rh, rd) in enumerate(t['smm']):
                MM(ps[sb_][:, :], lh, rh, m == 0, m == n - 1, rd, [pk(sb_)])
            ACT(PT[pj][:], ps[sb_][:, :], AF.Exp, [pk(sb_)] + CK, [('PT', pj)], bias=t['bias'], scale=SCALE)
            pend.append((t['pv'], pj))
            rr['tc'] = rr.get('tc', 0) + 1
            if rr['tc'] % step == 0:
                adv(gen)
            if len(pend) > 2:
                pv_, pj_ = pend.pop(0)
                for (o_, lh, tp, st_, sp_, rd, wk) in pv_:
                    MM(o_, lh, PT[pj_][:], st_, sp_, rd + [('PT', pj_)], [wk], tp=tp)
        for pv_, pj_ in pend:
            for (o_, lh, tp, st_, sp_, rd, wk) in pv_:
                MM(o_, lh, PT[pj_][:], st_, sp_, rd + [('PT', pj_)], [wk], tp=tp)

    def proj_fm(wtile, wkey, wc0, ncol, blk, bank):
        for c in range(8):
            MM(ps[bank][0:ncol, :], wtile[:, c, wc0:wc0 + ncol], uT[:, c, blk * 512:(blk + 1) * 512],
               c == 0, c == 7, [wkey, ('uT', blk)], [pk(bank)])

    def silu_pair(bank, dst, dkey):
        w = WK[0]
        ACT(w[:], ps[bank][:, :], AF.Exp, [pk(bank)], [('WK', 0)], scale=-1.0)
        SIG1P(w[:], 128, ('WK', 0))
        TT('vector', dst, ps[bank][:, :], w[:], ALU.mult, [pk(bank), ('WK', 0)], [dkey])

    def combine_branch(acc, akey, first, gbank, banks):
        w = WK[1]
        for hl in range(2):
            r0 = 64 * hl
            TS('vector', w[r0:r0 + 64, :], ps[banks[hl]][64:128, :], EPSL, None, ALU.max, None, [pk(banks[hl])],
               [('WK', 1)])
        RCP('vector', w[:], w[:], [('WK', 1)], [('WK', 1)])
        if gbank is not None:
            TT('vector', w[:], ps[gbank][:, :], w[:], ALU.mult, [pk(gbank), ('WK', 1)], [('WK', 1)])
        dst = acc if first else WK[2]
        dkey = akey if first else ('WK', 2)
        for hl in range(2):
            r0 = 64 * hl
            TT('vector', dst[r0:r0 + 64, :], ps[banks[hl]][0:64, :], w[r0:r0 + 64, :], ALU.mult,
               [pk(banks[hl]), ('WK', 1)], [dkey])
        if not first:
            TT('vector', acc, acc, WK[2][:], ALU.add, [akey, ('WK', 2)], [akey])

    for s in range(NSEQ):
        P.dma('sync', gpre, npre_d, reads=(), writes=['impacc', 'WKg'])
        for tt in range(NT):
            b = tt % 2
            P.dma('sync', xt[b][:], x_d[s, tt * 128:(tt + 1) * 128, :], reads=(), writes=XK(b))
            TT('vector', xn[b][:], xt[b][:], xt[b][:], ALU.mult, XK(b), NK(b))
            P.op('vector', (lambda bb: lambda e: e.reduce_sum(out=ssq[:, bb:bb + 1], in_=xn[bb][:], axis=AX.X))(b),
                 NK(b), [('ssq', b)])
            ACT(ssq[:, b:b + 1], ssq[:, b:b + 1], AF.Ln, [('ssq', b), 'eps'], [('ssq', b)], bias=eps_t[:, 0:1],
                scale=1.0 / D)
            ACT(ssq[:, b:b + 1], ssq[:, b:b + 1], AF.Exp, [('ssq', b)], [('ssq', b)], scale=-0.5)
            STT(xn[b][:], xt[b][:], ssq[:, b:b + 1], gpre, ALU.mult, ALU.mult, XK(b) + [('ssq', b), 'impacc', 'WKg'],
                NK(b))
            for half in range(2):
                bk = misc_bank()
                for cc in range(4):
                    c = half * 4 + cc
                    TR(ps[bk][:, cc * 128:(cc + 1) * 128], xn[b][:, c * 128:(c + 1) * 128], cs['ident_f'][:],
                       NK(b) + ['c_ident_f'], [pk(bk)])
                CP('vector' if half == 0 else 'gpsimd' if False else 'vector',
                   uT[:, half * 4:(half + 1) * 4, tt * 128:(tt + 1) * 128],
                   ps[bk][:, :].rearrange("p (c t) -> p c t", c=4), [pk(bk)], [('uT', tt // 4)])
        if 'uT' in dbg_d and s == 0:
            CP('vector', WK[0][:], uT[:, 0, 0:512], [('uT', 0)], [('WK', 0)])
            dump('uT', WK[0][:], [('WK', 0)])

        ncm = (S - 32) // 16 + 1
        for g in range(2):
            wj = next_wst()
            load_w(wst[wj][:, :, 0:64], C_KV + 0 * 128 + g * 64, 64, ('wst', wj))
            load_w(wst[wj][:, :, 64:128], C_KV + 1 * 128 + g * 64, 64, ('wst', wj))
            for blk in range(NQT):
                bk = misc_bank()
                proj_fm(wst[wj], ('wst', wj), 0, 128, blk, bk)
                CP('vector', kT1[:, blk * 512:(blk + 1) * 512], ps[bk][:, :], [pk(bk)], ['kT1'])
            W1 = Vp[:, 0:4096].rearrange("p (l e) -> p l e", e=128)
            P.dma('gpsimd', W1, w1s_d, reads=(), writes=['Vp'])
            for wh in range(2):
                r0 = 64 * wh
                bk = misc_bank()
                for l in range(32):
                    MM(ps[bk][:, 0:ncm], W1[r0:r0 + 64, l, :], kT1[r0:r0 + 64, l:l + 16 * (ncm - 1) + 1:16],
                       l == 0, l == 31, ['Vp', 'kT1'], [pk(bk)])
                bk2 = misc_bank()
                for l in range(32):
                    MM(ps[bk2][:, 0:1], W1[r0:r0 + 64, l, :], pet[r0:r0 + 64, l:l + 1], l == 0, l == 31,
                       ['Vp', 'pet'], [pk(bk2)])
                CP('vector', hb[:, 0:1], ps[bk2][:, 0:1], [pk(bk2)], ['hb'])
                TS('vector', hb[:, 1:2], hb[:, 0:1], -1.0, None, ALU.mult, None, ['hb'], ['hb'])
                w = WK[0]
                ACT(w[:, 0:ncm], ps[bk][:, 0:ncm], AF.Exp, [pk(bk), 'hb'], [('WK', 0)], bias=hb[:, 1:2], scale=-1.0)
                SIG1P(w[:, 0:ncm], 128, ('WK', 0))
                STT(hcm[:, wh, 0:ncm], ps[bk][:, 0:ncm], hb[:, 0:1], w[:, 0:ncm], ALU.add, ALU.mult,
                    [pk(bk), 'hb', ('WK', 0)], ['hcm'])
                if wh == 0:
                    bk3 = misc_bank()
                    MM(ps[bk3][0:64, 0:256], w2s[:, 0, :], hcm[:, 0, :], True, True, ['w2s', 'hcm'], [pk(bk3)])
                    CP('vector', kcT[0:64, :], ps[bk3][0:64, 0:256], [pk(bk3)], ['kcT'])
                else:
                    bk3 = misc_bank()
                    for a in range(2):
                        MM(ps[bk3][:, a * 64:(a + 1) * 64], hcm[:, 1, a * 128:(a + 1) * 128], w2s[:, 1, :], True, True,
                           ['w2s', 'hcm'], [pk(bk3)])
                    CP('vector', vcx[:, :, 0:64], ps[bk3][:, 0:128].rearrange("p (a d) -> p a d", a=2), [pk(bk3)],
                       ['vcx'])
            wj = next_wst()
            load_w(wst[wj][:, :, 0:64], C_KV + 2 * 128 + g * 64, 64, ('wst', wj))
            load_w(wst[wj][:, :, 64:128], C_KV + 4 * 128 + g * 64, 64, ('wst', wj))
            for blk in range(NQT):
                bk = misc_bank()
                proj_fm(wst[wj], ('wst', wj), 0, 128, blk, bk)
                CP('vector', kT0[0:64, blk * 512:(blk + 1) * 512], ps[bk][0:64, :], [pk(bk)], ['kT0'])
                CP('vector', kT1[0:64, blk * 512:(blk + 1) * 512], ps[bk][64:128, :], [pk(bk)], ['kT1'])
            P.dma('sync', kT0[64:128, 0:S], cd['eslc'][:, 0:S], reads=(), writes=['kT0'])
            MS('vector', kT1[64:128, :], 0.0, ['kT1'])
            MS('vector', kT1[64:65, :], 1.0, ['kT1'])
            load_w(WQZ[:, :, 0:256], C_QA + g * 256, 256, 'WQZ')
            load_w(WQZ[:, :, 256:512], C_ZA + g * 256, 256, 'WQZ')
            for h4 in range(4):
                for b2 in range(2):
                    P.dma('sync', QA[b2][h4][64:65, :], cd['cq'][4 * g + h4:4 * g + h4 + 1, :], reads=(),
                          writes=[('QA', b2, h4)])

            def nsa_pre(g, i):
                b2 = i % 2
                for pp in range(2):
                    bk = misc_bank()
                    proj_fm(WQZ, 'WQZ', pp * 128, 128, i, bk)
                    CP('vector', QA[b2][2 * pp][0:64, :], ps[bk][0:64, :], [pk(bk)], [('QA', b2, 2 * pp)])
                    CP('vector', QA[b2][2 * pp + 1][0:64, :], ps[bk][64:128, :], [pk(bk)], [('QA', b2, 2 * pp + 1)])
                    yield
                bk = misc_bank()
                for c in range(8):
                    MM(ps[bk][0:24, :], wg24[:, c, :], uT[:, c, i * 512:(i + 1) * 512], c == 0, c == 7,
                       ['wg24', ('uT', i)], [pk(bk)])
                w = WK[0]
                ACT(w[0:24, :], ps[bk][0:24, :], AF.Exp, [pk(bk)], [('WK', 0)], scale=-1.0)
                SIG1P(w[0:24, :], 24, ('WK', 0))
                CP('vector', SGT[b2][0:24, :], w[0:24, :], [('WK', 0)], [('SGT', b2)])
                TT('vector', SGT[b2][32:56, :], w[0:24, :], SGT[b2][0:24, :], ALU.subtract, [('WK', 0), ('SGT', b2)],
                   [('SGT', b2)])
                yield
                a_list = [0] if (512 * i + 511) < (2048 + 31) else [0, 1]
                a_list = [a for a in a_list if a * 128 < ncm]
                for a in a_list:
                    thr = 512.0 * i - 2048.0 * a
                    TS('vector', XM[a][:], cs['xg'][:], thr, NEG, ALU.is_gt, ALU.mult, ['c_xg'], [('XM', a)])
                la = len(a_list)
                for h4 in range(4):
                    hl = h4 % 2
                    pp = h4 // 2
                    ha = 4 * g + h4
                    if hl == 0:
                        gb = misc_bank()
                        MM(ps[gb][:, :], cs['goh'][0:56, (g * 2 + pp) * 3 + 0, :], SGT[b2][0:56, :], True, True,
                           ['c_goh', ('SGT', b2)], [pk(gb)])
                        CP('vector', WKg[:], ps[gb][:, :], [pk(gb)], ['WKg'])
                    bkA = misc_bank()
                    bkB = misc_bank()
                    for ai, a in enumerate(a_list):
                        sb_ = ST[rr['st'] % 2]
                        rr['st'] += 1
                        pj = 4 + rr['ptc'] % 2
                        rr['ptc'] += 1
                        MM(ps[sb_][:, :], kcT[0:65, a * 128:(a + 1) * 128], QA[b2][h4][0:65, :], True, False,
                           ['kcT', ('QA', b2, h4)], [pk(sb_)])
                        MM(ps[sb_][:, :], cs['ident_b'][:], XM[a][:], False, True, ['c_ident_b', ('XM', a)], [pk(sb_)])
                        ACT(PT[pj][:], ps[sb_][:, :], AF.Exp, [pk(sb_)] + CK, [('PT', pj)],
                            bias=cs['clb'][:, ha, a, i:i + 1], scale=SCALE)
                        MM(ps[bkA][:, :], vcx[:, a, :], PT[pj][:], ai == 0, ai == la - 1, ['vcx', ('PT', pj)], [pk(bkA)])
                        MM(ps[bkB][0:64, :], cs['ov'][:, a, :], PT[pj][:], ai == 0, ai == la - 1, ['c_ov', ('PT', pj)],
                           [pk(bkB)])
                    wr = WK[3]
                    TS('vector', wr[0:64, :], ps[bkA][64:128, :], EPSL, None, ALU.max, None, [pk(bkA)], [('WK', 3)])
                    TS('vector', wr[64:128, :], ps[bkA][64:128, :], EPSL, None, ALU.max, None, [pk(bkA)], [('WK', 3)])
                    RCP('vector', wr[:, :], wr[:, :], [('WK', 3)], [('WK', 3)])
                    if h4 == 0:
                        TT('vector', impacc[0:64, :], ps[bkB][0:64, :], wr[0:64, :], ALU.mult, [pk(bkB), ('WK', 3)],
                           ['impacc'])
                    else:
                        TT('vector', WK[4][0:64, :], ps[bkB][0:64, :], wr[0:64, :], ALU.mult, [pk(bkB), ('WK', 3)],
                           [('WK', 4)])
                        TT('vector', impacc[0:64, :], impacc[0:64, :], WK[4][0:64, :], ALU.add, ['impacc', ('WK', 4)],
                           ['impacc'])
                    r0 = 64 * hl
                    TT('vector', WK[1][r0:r0 + 64, :], WKg[r0:r0 + 64, :], wr[r0:r0 + 64, :], ALU.mult, ['WKg', ('WK', 3)],
                       [('WK', 1)])
                    TT('vector', ACC[b2][pp][r0:r0 + 64, :], ps[bkA][0:64, :], WK[1][r0:r0 + 64, :], ALU.mult,
                       [pk(bkA), ('WK', 1)], [('xt', pp, b2)])
                    yield
                bk = misc_bank()
                for qs in range(4):
                    TR(ps[bk][:, qs * 64:(qs + 1) * 64], impacc[0:64, qs * 128:(qs + 1) * 128], cs['ident_f'][0:64, 0:64],
                       ['impacc', 'c_ident_f'], [pk(bk)])
                for qs in range(4):
                    ti = 4 * i + qs
                    TT('vector', T1[:, qs, :], ps[bk][:, qs * 64:(qs + 1) * 64],
                       cs['abase'][:, 64 - 2 * ti:128 - 2 * ti], ALU.add, [pk(bk), 'c_abase'], ['T1'])
                MS('vector', T1[:, :, 0:1], BIGF, ['T1'])
                yield
                for qs in range(4):
                    P.op('vector', (lambda q_: lambda e: e.max(out=M8a[:], in_=T1[:, q_, :]))(qs), ['T1'], ['M8a'])
                    P.op('vector', (lambda q_: lambda e: e.match_replace(out=T2[:, q_, :], in_to_replace=M8a[:],
                                                                        in_values=T1[:, q_, :], imm_value=-1e9))(qs),
                         ['T1', 'M8a'], ['T2'])
                    P.op('vector', (lambda q_: lambda e: e.max(out=M8b[:], in_=T2[:, q_, :]))(qs), ['T2'], ['M8b'])
                    TS('vector', T2[:, qs, :], T1[:, qs, :], M8b[:, 7:8], NEG, ALU.is_lt, ALU.mult, ['T1', 'M8b'], ['T2'])
                    yield
                for pp in range(2):
                    bk = misc_bank()
                    proj_fm(WQZ, 'WQZ', 256 + pp * 128, 128, i, bk)
                    silu_pair(bk, SZ[b2][pp], ('xn', pp, b2))
                    yield
                yield
                bk = misc_bank()
                for qs in range(4):
                    TR(ps[bk][0:64, qs * 128:(qs + 1) * 128], T2[:, qs, :], cs['ident_f'][:], ['T2', 'c_ident_f'],
                       [pk(bk)])
                for h4 in range(4):
                    STT(QA[b2][h4][64:128, :], cs['cqb'][64:128, :], -SL_HH[4 * g + h4] / SCALE, ps[bk][0:64, :],
                        ALU.mult, ALU.add, [pk(bk), 'c_cqb'], [('QA', b2, h4)])
                yield

            gen0 = nsa_pre(g, 0)
            wj = next_wst()
            load_w(wst[wj][:, :, 0:64], C_KV + 3 * 128 + g * 64, 64, ('wst', wj))
            load_w(wst[wj][:, :, 64:128], C_KV + 5 * 128 + g * 64, 64, ('wst', wj))
            MS('vector', Vp3[:, :, :].rearrange("p t (h c) -> p t h c", h=2)[:, :, :, 64:128], 1.0, ['Vp'])
            for t4 in range(NT // 4):
                bk = misc_bank()
                for q4 in range(4):
                    tt = t4 * 4 + q4
                    for c in range(8):
                        MM(ps[bk][:, q4 * 128:(q4 + 1) * 128], uT[:, c, tt * 128:(tt + 1) * 128], wst[wj][:, c, :],
                           c == 0, c == 7, [('wst', wj), ('uT', t4)], [pk(bk)])
                CP('vector', Vp3[:, t4 * 4:(t4 + 1) * 4, :].rearrange("p t (h c) -> p t h c", h=2)[:, :, :, 0:64],
                   ps[bk][:, :].rearrange("p (t h c) -> p t h c", h=2, c=64), [pk(bk)], ['Vp'])
                adv(gen0)
                adv(gen0)
            exhaust(gen0)
            for i in range(NQT):
                b2 = i % 2
                gen = nsa_pre(g, i + 1) if i + 1 < NQT else None
                for pp in range(2):
                    for br in (1, 2):
                        tiles = []
                        OB, LB = OLS[rr['ol'] % 2]
                        rr['ol'] += 1
                        if br == 1:
                            kts = list(range(0, 4 * i + 4))
                        else:
                            kts = [kt for kt in range(4 * i - 4, 4 * i + 4) if kt >= 0]
                        for kt in kts:
                            for hl in range(2):
                                h4 = 2 * pp + hl
                                ha = 4 * g + h4
                                hk = [k_ for k_ in kts if k_ >= kt_lo(ha, i)]
                                if kt not in hk:
                                    continue
                                ki = hk.index(kt)
                                nk = len(hk)
                                kT = kT0 if br == 1 else kT1
                                kkey = 'kT0' if br == 1 else 'kT1'
                                smm = [(kT[:, kt * 128:(kt + 1) * 128], QA[b2][h4][:, :], [kkey, ('QA', b2, h4)])]
                                if br == 1:
                                    if kt >= 4 * i:
                                        r = kt - 4 * i
                                        smm.append((cs['ident_b'][:], cs['cmb'][:, 384 - 128 * r:896 - 128 * r],
                                                    ['c_ident_b', 'c_cmb']))
                                else:
                                    r = kt - (4 * i - 4)
                                    if r < 4:
                                        smm.append((cs['ident_b'][:], cs['omb'][:, 384 - 128 * r:896 - 128 * r],
                                                    ['c_ident_b', 'c_omb']))
                                    else:
                                        r -= 4
                                        smm.append((cs['ident_b'][:], cs['cmb'][:, 384 - 128 * r:896 - 128 * r],
                                                    ['c_ident_b', 'c_cmb']))
                                vcol = 0 if br == 1 else 128
                                bnk = (OB, LB)[hl]
                                pv = [(ps[bnk][:, :], Vp3[:, kt, vcol:vcol + 128], None,
                                       ki == 0, ki == nk - 1, ['Vp'], pk(bnk))]
                                tiles.append(dict(smm=smm, bias=cs['alb'][:, ha, kt - 4 * i + 32:kt - 4 * i + 33], pv=pv))
                        attend(tiles, gen)
                        gb = misc_bank()
                        MM(ps[gb][:, :], cs['goh'][0:56, (g * 2 + pp) * 3 + br, :], SGT[b2][0:56, :], True, True,
                           ['c_goh', ('SGT', b2)], [pk(gb)])
                        combine_branch(ACC[b2][pp], ('xt', pp, b2), False, gb, (OB, LB))
                    yj = rr['wk'] % 2
                    rr['wk'] += 1
                    TT('vector', YST[yj][:], ACC[b2][pp], SZ[b2][pp], ALU.mult, [('xt', pp, b2), ('xn', pp, b2)],
                       [('YST', yj)])
                    P.dma('sync', ysc_d[g * 2 + pp, :, i * 512:(i + 1) * 512], YST[yj][:], reads=[('YST', yj)],
                          writes=[('ysc', g * 2 + pp, i)])
                exhaust(gen)

        nb = S // 256
        for j in range(4):
            wj = next_wst()
            load_w(wst[wj][:], C_QB + 512 + j * 128, 128, ('wst', wj))
            for blk in range(NQT):
                bk = misc_bank()
                proj_fm(wst[wj], ('wst', wj), 0, 128, blk, bk)
                CP('vector', kT0[0:64, blk * 512:(blk + 1) * 512], ps[bk][0:64, :], [pk(bk)], ['kT0'])
                CP('vector', kT1[0:64, blk * 512:(blk + 1) * 512], ps[bk][64:128, :], [pk(bk)], ['kT1'])
                P.op('vector', (lambda bk_, blk_: lambda e: e.reduce_sum(
                    out=kbarf[:, 2 * blk_:2 * blk_ + 2], in_=ps[bk_][:, :].rearrange("p (b t) -> p b t", b=2),
                    axis=AX.X))(bk, blk), [pk(bk)], ['kbarf'])
            for kT_, kk_ in ((kT0, 'kT0'), (kT1, 'kT1')):
                MS('vector', kT_[64:128, :], 0.0, [kk_])
                MS('vector', kT_[64:65, :], 1.0, [kk_])
                P.dma('sync', kT_[96:112, 0:S], cd['emoba'][:, 0:S], reads=(), writes=[kk_])
            TS('vector', kbar[0:64, 0, 0:nb], kbarf[0:64, 0:nb], 1.0 / 256, None, ALU.mult, None, ['kbarf'], ['kbar'])
            TS('vector', kbar[0:64, 1, 0:nb], kbarf[64:128, 0:nb], 1.0 / 256, None, ALU.mult, None, ['kbarf'], ['kbar'])
            load_w(WQZ[:, :, 0:128], C_QB + j * 128, 128, 'WQZ')
            load_w(WQZ[:, :, 128:256], C_ZB + j * 128, 128, 'WQZ')
            for hl in range(2):
                for b2 in range(2):
                    MS('vector', QA[b2][hl][64:128, :], 0.0, [('QA', b2, hl)])
                    P.dma('sync', QA[b2][hl][64:65, :], cd['cq'][8 + 2 * j + hl:8 + 2 * j + hl + 1, :], reads=(),
                          writes=[('QA', b2, hl)])

            def moba_pre(j, i):
                b2 = i % 2
                bk = misc_bank()
                proj_fm(WQZ, 'WQZ', 0, 128, i, bk)
                CP('vector', QA[b2][0][0:64, :], ps[bk][0:64, :], [pk(bk)], [('QA', b2, 0)])
                CP('vector', QA[b2][1][0:64, :], ps[bk][64:128, :], [pk(bk)], [('QA', b2, 1)])
                yield
                bk = misc_bank()
                for hl in range(2):
                    for qs in range(4):
                        MM(ps[bk][:, (hl * 4 + qs) * 16:(hl * 4 + qs) * 16 + nb], QA[b2][hl][0:64, qs * 128:(qs + 1) * 128],
                           kbar[0:64, hl, 0:nb], True, True, ['kbar', ('QA', b2, hl)], [pk(bk)])
                for hl in range(2):
                    for qs in range(4):
                        cur = (4 * i + qs) // 2
                        e8 = hl * 4 + qs
                        TT('vector', GT[:, e8, 0:nb], ps[bk][:, e8 * 16:e8 * 16 + nb], cs['bbase'][:, 16 - cur:16 - cur + nb],
                           ALU.add, [pk(bk), 'c_bbase'], ['GT'])
                        if nb < 16:
                            MS('vector', GT[:, e8, nb:16], -BIGF, ['GT'])
                yield
                for hl in range(2):
                    for qs in range(4):
                        cur = (4 * i + qs) // 2
                        e8 = hl * 4 + qs
                        P.op('vector', (lambda e_: lambda e: e.max(out=M8a[:], in_=GT[:, e_, :]))(e8), ['GT'], ['M8a'])
                        TS('vector', GT[:, e8, :], GT[:, e8, :], M8a[:, 2:3], NEG, ALU.is_lt, ALU.mult, ['GT', 'M8a'], ['GT'])
                        TT('vector', GT[:, e8, :], GT[:, e8, :], cs['obase'][:, 16 - cur:32 - cur], ALU.max,
                           ['GT', 'c_obase'], ['GT'])
                        yield
                bk = misc_bank()
                proj_fm(WQZ, 'WQZ', 128, 128, i, bk)
                silu_pair(bk, SZ[b2][0], ('xn', 0, b2))
                yield
                yield
                for hl in range(2):
                    bk = misc_bank()
                    for qs in range(4):
                        TR(ps[bk][0:16, qs * 128:(qs + 1) * 128], GT[:, hl * 4 + qs, :], cs['ident_f'][:],
                           ['GT', 'c_ident_f'], [pk(bk)])
                    CP('vector', QA[b2][hl][96:112, :], ps[bk][0:16, :], [pk(bk)], [('QA', b2, hl)])
                    yield

            gen0 = moba_pre(j, 0)
            wj = next_wst()
            load_w(wst[wj][:], C_QB + 1024 + j * 128, 128, ('wst', wj))
            MS('vector', Vp3[:, :, :].rearrange("p t (h c) -> p t h c", h=2)[:, :, :, 64:128], 1.0, ['Vp'])
            for t4 in range(NT // 4):
                bk = misc_bank()
                for q4 in range(4):
                    tt = t4 * 4 + q4
                    for c in range(8):
                        MM(ps[bk][:, q4 * 128:(q4 + 1) * 128], uT[:, c, tt * 128:(tt + 1) * 128], wst[wj][:, c, :],
                           c == 0, c == 7, [('wst', wj), ('uT', t4)], [pk(bk)])
                CP('vector', Vp3[:, t4 * 4:(t4 + 1) * 4, :].rearrange("p t (h c) -> p t h c", h=2)[:, :, :, 0:64],
                   ps[bk][:, :].rearrange("p (t h c) -> p t h c", h=2, c=64), [pk(bk)], ['Vp'])
                adv(gen0)
            exhaust(gen0)
            for i in range(NQT):
                b2 = i % 2
                gen = moba_pre(j, i + 1) if i + 1 < NQT else None
                tiles = []
                OB, LB = OLS[rr['ol'] % 2]
                rr['ol'] += 1
                kts = list(range(0, 4 * i + 4))
                for kt in kts:
                    for hl in range(2):
                        hh = 8 + 2 * j + hl
                        hk = [k_ for k_ in kts if k_ >= kt_lo(hh, i)]
                        if kt not in hk:
                            continue
                        ki = hk.index(kt)
                        nk = len(hk)
                        kT = kT0 if hl == 0 else kT1
                        kkey = 'kT0' if hl == 0 else 'kT1'
                        smm = [(kT[:, kt * 128:(kt + 1) * 128], QA[b2][hl][:, :], [kkey, ('QA', b2, hl)])]
                        if kt >= 4 * i:
                            smm.append((cs['ident_b'][:], cs['mmb'][:, kt - 4 * i, :], ['c_ident_b', 'c_mmb']))
                        bnk = (OB, LB)[hl]
                        pv = [(ps[bnk][:, :], Vp3[:, kt, 128 * hl:128 * hl + 128], None,
                               ki == 0, ki == nk - 1, ['Vp'], pk(bnk))]
                        tiles.append(dict(smm=smm, bias=cs['alb'][:, hh, kt - 4 * i + 32:kt - 4 * i + 33], pv=pv))
                attend(tiles, gen, step=2)
                combine_branch(ACC[b2][0], ('xt', 0, b2), True, None, (OB, LB))
                yj = rr['wk'] % 2
                rr['wk'] += 1
                TT('vector', YST[yj][:], ACC[b2][0], SZ[b2][0], ALU.mult, [('xt', 0, b2), ('xn', 0, b2)], [('YST', yj)])
                P.dma('sync', ysc_d[4 + j, :, i * 512:(i + 1) * 512], YST[yj][:], reads=[('YST', yj)],
                      writes=[('ysc', 4 + j, i)])
                exhaust(gen)

        P.op('sync', lambda e: e.nop(), (), ['Vp', ('wgs', 0), ('wgs', 1), 'WQZ', ('wab', 0), ('wab', 1)])
        P.dma('sync', gpost, npost_d, reads=(), writes=['impacc', 'WKg'])
        for i in range(NQT):
            P.dma('sync', ysl[:, :, 0:512], ysc_d[:, :, i * 512:(i + 1) * 512].rearrange("c p t -> p c t"),
                  reads=[('ysc', jj, i) for jj in range(8)], writes=['kT1'])
            for fc in range(8):
                gj = fc % 2
                P.dma('sync', wgs[:, gj, :, 0:128], wcols[:, :, C_MG + fc * 128:C_MG + (fc + 1) * 128], reads=WBK,
                      writes=[('wgs', gj)])
                P.dma('sync', wgs[:, gj, :, 128:256], wcols[:, :, C_MG + 1024 + fc * 128:C_MG + 1024 + (fc + 1) * 128],
                      reads=WBK, writes=[('wgs', gj)])
                for ab in range(2):
                    P.dma('sync', wab[:, gj, 4 * ab:4 * ab + 4, :],
                          wabf_d[ab].rearrange("(j p) n -> p j n", p=128)[:, :, fc * 128:(fc + 1) * 128],
                          reads=['wabf0', 'wabf1'], writes=[('wab', gj)])
                for ab in range(2):
                    bg = [6, 7, 0, 1][rr['g4'] % 4]
                    rr['g4'] += 1
                    for c in range(8):
                        MM(ps[bg][:, :], wgs[:, gj, c, ab * 128:(ab + 1) * 128], uT[:, c, i * 512:(i + 1) * 512],
                           c == 0, c == 7, [('wgs', gj), ('uT', i)], [pk(bg)])
                    w = WK[ab]
                    ACT(w[:], ps[bg][:, :], AF.Sigmoid, [pk(bg)], [('WK', ab)])
                    bm = OLS[fc % 2][ab]
                    for jj in range(4):
                        MM(ps[bm][:, :], wab[:, gj, 4 * ab + jj, :], ysl[:, 4 * ab + jj, 0:512], jj == 0, jj == 3,
                           [('wab', gj), 'kT1'], [pk(bm)])
                    TT('vector', w[:], ps[bm][:, :], w[:], ALU.mult, [pk(bm), ('WK', ab)], [('WK', ab)])
                TT('vector', mT[:, fc, 0:512], WK[0][:], WK[1][:], ALU.add, [('WK', 0), ('WK', 1)], ['kT0'])
            for ts_ in range(4):
                tt = i * 4 + ts_
                b = tt % 2
                P.dma('sync', xt[b][:], x_d[s, tt * 128:(tt + 1) * 128, :], reads=(), writes=XK(b))
                banks = [ST[0], ST[1]]
                for hf in range(2):
                    for fc in range(8):
                        MM(ps[banks[hf]][:, :], mT[:, fc, ts_ * 128:(ts_ + 1) * 128], wo_s[:, fc, hf * 512:(hf + 1) * 512],
                           fc == 0, fc == 7, ['wo', 'kT0'], [pk(banks[hf])])
                for hf in range(2):
                    P.op('scalar', (lambda o_, i_: lambda e: e.copy(out=o_, in_=i_))(WK[2 + hf][:], ps[banks[hf]][:, :]),
                         [pk(banks[hf])], [('WK', 2 + hf)])
                    P.op('vector', (lambda o_, i_, a_: lambda e: e.scalar_tensor_tensor(
                        out=o_, in0=i_, scalar=1.0, in1=i_, op0=ALU.mult, op1=ALU.mult, accum_out=a_))(
                        YST[hf][:], WK[2 + hf][:], ssq2[:, hf:hf + 1]), [('WK', 2 + hf)], [('YST', hf), ('ssq2', hf)])
                TT('vector', ssq[:, b:b + 1], ssq2[:, 0:1], ssq2[:, 1:2], ALU.add, [('ssq2', 0), ('ssq2', 1)], [('ssq', b)])
                ACT(ssq[:, b:b + 1], ssq[:, b:b + 1], AF.Ln, [('ssq', b), 'eps'], [('ssq', b)], bias=eps_t[:, 0:1],
                    scale=1.0 / D)
                ACT(ssq[:, b:b + 1], ssq[:, b:b + 1], AF.Exp, [('ssq', b)], [('ssq', b)], scale=-0.5)
                for hf in range(2):
                    STT(xn[b][:, hf * 512:(hf + 1) * 512], WK[2 + hf][:], ssq[:, b:b + 1],
                        gpost[:, hf * 512:(hf + 1) * 512], ALU.mult, ALU.mult, [('WK', 2 + hf), ('ssq', b), 'impacc', 'WKg'],
                        [('xn', b, hf)])
                TT('gpsimd', xn[b][:], xn[b][:], xt[b][:], ALU.add, NK(b) + XK(b), NK(b))
                P.dma('sync', out_d[s, tt * 128:(tt + 1) * 128, :], xn[b][:], reads=NK(b), writes=[('out', tt)])
        P.op('sync', lambda e: e.nop(), (), ['Vp', ('wgs', 0), ('wgs', 1), 'WQZ', ('wab', 0), ('wab', 1)])
    P.finish()
    return nc, P


def host_inputs(inputs, S):
    c = make_consts()
    f = lambda a: np.ascontiguousarray(np.asarray(a, dtype=np.float32))
    w1k = f(inputs['cmp_w1_k'])[0].transpose(1, 0, 2)
    w1v = f(inputs['cmp_w1_v'])[0].transpose(1, 0, 2)
    shared = {
        'w_in': f(inputs['w_in'])[0],
        'w1s': np.ascontiguousarray(np.concatenate([w1k, w1v], 0)),
        'w2s': np.ascontiguousarray(np.stack([f(inputs['cmp_w2_k'])[0], f(inputs['cmp_w2_v'])[0]], 1)),
        'pet': np.ascontiguousarray(np.concatenate([f(inputs['cmp_pe_k'])[0].T, f(inputs['cmp_pe_v'])[0].T], 0)),
        'w_a': f(inputs['w_branch_a'])[0],
        'w_b': f(inputs['w_branch_b'])[0],
        'w_o': f(inputs['w_o'])[0],
        'npre_bc': np.ascontiguousarray(np.broadcast_to(f(inputs['norm_pre'])[0][None, :], (128, D))),
        'npost_bc': np.ascontiguousarray(np.broadcast_to(f(inputs['norm_post'])[0][None, :], (128, D))),
    }
    for k, v in c.items():
        shared['c_' + k] = v
    return shared


def kernel(**inputs):
    x = np.ascontiguousarray(np.asarray(inputs['x'], dtype=np.float32))
    B, S, _ = x.shape
    ncores = 8
    nseq = B // ncores
    nc, _ = build(S, nseq)
    shared = host_inputs(inputs, S)
    in_maps = []
    for c in range(ncores):
        m = dict(shared)
        m['x'] = np.ascontiguousarray(x[c * nseq:(c + 1) * nseq])
        in_maps.append(m)
    res = run_bass_kernel_spmd(nc, in_maps, core_ids=list(range(ncores)))
    return np.concatenate([np.asarray(r['out']) for r in res.results], axis=0).astype(np.float32)
```

```python
import contextlib
import numpy as np
import ml_dtypes
import concourse.bass as bass
import concourse.mybir as mybir
from concourse.bass_utils import run_bass_kernel_spmd

F32 = mybir.dt.float32
BF16 = mybir.dt.bfloat16
ALU = mybir.AluOpType
AF = mybir.ActivationFunctionType
AX = mybir.AxisListType
NPBF = ml_dtypes.bfloat16

D = 1024
DEBUG_SEP = False
NCOL = 5912
NEG = -30000.0
SCALE = 0.125
BIGF = 1.0e4
EPSL = 1.0e-18
C_QA, C_KV, C_GA, C_ZA, C_QB, C_ZB, C_MG = 0, 512, 1280, 1304, 1816, 3352, 3864
SLOPES = [2.0 ** (-(i + 1) / 2.0) for i in range(16)]
SL_HH = [SLOPES[2 * h] for h in range(8)] + [SLOPES[2 * h + 1] for h in range(8)]


class Prog:
    EPOCH = 20000
    NDMA = 8

    def __init__(self, nc):
        self.nc = nc
        self.ops = []
        self.lastw = {}
        self.readers = {}
        self.stack = contextlib.ExitStack()
        self.sb_bytes = 0

    def sb(self, name, shape, dtype):
        n = 1
        for s in shape[1:]:
            n *= s
        self.sb_bytes += n * (4 if dtype == F32 else 2)
        return self.stack.enter_context(self.nc.sbuf_tensor('sb_' + name, list(shape), dtype))

    def ps(self, name, shape=(128, 512), dtype=F32):
        return self.stack.enter_context(self.nc.psum_tensor(name, list(shape), dtype))

    def _deps(self, idx, reads, writes):
        deps = set()
        for k in reads:
            w = self.lastw.get(k)
            if w is not None:
                deps.add(w)
        for k in writes:
            w = self.lastw.get(k)
            if w is not None:
                deps.add(w)
            for r in self.readers.get(k, ()):
                deps.add(r)
        for k in reads:
            self.readers.setdefault(k, []).append(idx)
        for k in writes:
            self.lastw[k] = idx
            self.readers[k] = []
        deps.discard(idx)
        return deps

    def op(self, eng, fn, reads=(), writes=()):
        idx = len(self.ops)
        deps = self._deps(idx, reads, writes)
        self.ops.append(dict(eng=eng, fn=fn, deps=deps, dma=False, wkeys=set(writes), rkeys=set(reads)))
        return idx

    def dma(self, eng, out, in_, reads=(), writes=()):
        idx = len(self.ops)
        deps = self._deps(idx, reads, writes)
        self.ops.append(dict(eng=eng, fn=lambda e: e.dma_start(out=out, in_=in_), deps=deps, dma=True,
                             wkeys=set(writes), rkeys=set(reads)))
        return idx

    def finish(self):
        nc = self.nc
        ops = self.ops
        needed = set()
        for i, o in enumerate(ops):
            nd = set()
            for d in o['deps']:
                od = ops[d]
                if od['eng'] == o['eng'] and not od['dma'] and not o['dma']:
                    if o['eng'] == 'tensor':
                        continue
                    if not ((od['wkeys'] & o['rkeys']) or (od['wkeys'] & o['wkeys'])):
                        continue
                nd.add(d)
            o['deps'] = nd
            for d in nd:
                if not ops[d]['dma']:
                    needed.add(d)
        engs = ['tensor', 'vector', 'scalar', 'gpsimd', 'sync']
        cnt = {e: 0 for e in engs}
        dcnt = {e: 0 for e in engs}
        nep = {e: 0 for e in engs}
        for i, o in enumerate(ops):
            e = o['eng']
            if o['dma']:
                d = dcnt[e]
                dcnt[e] += 1
                o['sig'] = (('dma', e, d % self.NDMA), 16 * (d // self.NDMA + 1), 16)
                o['prev'] = (('dma', e, d % self.NDMA), 16 * (d // self.NDMA)) if d >= self.NDMA else None
            elif i in needed:
                c = cnt[e]
                cnt[e] += 1
                ep = c // self.EPOCH
                nep[e] = max(nep[e], ep + 1)
                o['sig'] = (('cmp', e, ep), c % self.EPOCH + 1, 1)
            else:
                o['sig'] = None
        sems = {}
        for e in engs:
            for ep in range(nep[e]):
                sems[('cmp', e, ep)] = self.stack.enter_context(nc.semaphore(f"s_{e}_{ep}"))
            for j in range(min(self.NDMA, dcnt[e])):
                sems[('dma', e, j)] = self.stack.enter_context(nc.semaphore(f"d_{e}_{j}"))
        self.n_instr = {e: 0 for e in engs}
        with nc.Block() as block:
            def make(ename):
                def body(eng):
                    waited = {}
                    lastdma = {}
                    for o in ops:
                        if o['eng'] != ename:
                            continue
                        best = {}
                        for d in o['deps']:
                            s = ops[d]['sig']
                            if s[1] > best.get(s[0], 0):
                                best[s[0]] = s[1]
                        if o['dma'] and o['prev'] is not None:
                            k, v = o['prev']
                            if v > best.get(k, 0):
                                best[k] = v
                        for k, v in best.items():
                            if waited.get(k, 0) >= v:
                                continue
                            eng.wait_ge(sems[k], v)
                            waited[k] = v
                            self.n_instr[ename] += 1
                        ins = o['fn'](eng)
                        self.n_instr[ename] += 1
                        if o['sig'] is not None:
                            ins.then_inc(sems[o['sig'][0]], o['sig'][2])
                            if o['dma']:
                                lastdma[o['sig'][0]] = o['sig'][1]
                    for k, v in lastdma.items():
                        if waited.get(k, 0) < v:
                            eng.wait_ge(sems[k], v)
                return body
            for ename in engs:
                if any(o['eng'] == ename for o in ops):
                    getattr(block, ename)(make(ename))
        self.stack.close()


def make_consts():
    c = {}
    c['ident_f'] = np.eye(128, dtype=np.float32)
    c['ident_b'] = np.eye(128).astype(NPBF)
    p = np.arange(128)[:, None]
    fp = np.arange(896)[None, :]
    c['cmb'] = np.where(p > fp - 384, NEG, 0.0).astype(NPBF)
    c['omb'] = np.where(fp - 384 >= p, NEG, 0.0).astype(NPBF)
    kk = np.arange(4)[None, :, None] * 128 + np.arange(128)[:, None, None]
    f = np.arange(512)[None, None, :]
    mm = np.where(kk // 256 == f // 256, np.where(kk > f, NEG, 0.0), np.where(f // 256 > kk // 256, 0.0, NEG))
    c['mmb'] = mm.astype(NPBF)
    k2 = np.arange(4096)[None, :]
    c['eslc'] = (k2 // 64 == np.arange(64)[:, None]).astype(NPBF)
    c['emoba'] = (k2 // 256 == np.arange(16)[:, None]).astype(NPBF)
    c['cqb'] = np.ascontiguousarray(np.broadcast_to(np.arange(512, dtype=np.float32)[None, :], (128, 512)))
    alb = np.zeros((128, 16, 36), np.float32)
    for hh in range(16):
        for di in range(36):
            alb[:, hh, di] = SL_HH[hh] * (np.arange(128) + 128.0 * (di - 32))
    c['alb'] = alb
    clb = np.zeros((128, 8, 2, 8), np.float32)
    for ha in range(8):
        for a in range(2):
            for i in range(8):
                clb[:, ha, a, i] = SL_HH[ha] * (2048.0 * a + 16.0 * np.arange(128) + 31.0 - 512.0 * i)
    c['clb'] = clb
    cq = np.zeros((16, 512), np.float32)
    for hh in range(16):
        cq[hh] = -SL_HH[hh] * np.arange(512) / SCALE
    c['cq'] = cq.astype(NPBF)
    goh = np.zeros((56, 12, 128), np.float32)
    for g in range(2):
        for pp in range(2):
            for k in range(3):
                idx = (g * 2 + pp) * 3 + k
                ra = k * 8 + g * 4 + 2 * pp
                goh[ra, idx, 0:64] = 1.0
                goh[32 + ra, idx, 0:64] = 1.0
                goh[ra + 1, idx, 64:128] = 1.0
                goh[32 + ra + 1, idx, 64:128] = 1.0
    c['goh'] = goh.astype(NPBF)
    n_cmp, n_slc = 255, 64
    cs = np.arange(n_cmp) * 16
    ce = cs + 31
    ss = np.arange(n_slc) * 64
    se = ss + 63
    ov = ((cs[:, None] <= se[None, :]) & (ce[:, None] >= ss[None, :])).astype(np.float32)
    ovp = np.zeros((256, 64), np.float32)
    ovp[:255] = ov
    c['ov'] = np.ascontiguousarray(ovp.reshape(2, 128, 64).transpose(1, 0, 2)).astype(NPBF)
    q = np.arange(128)[:, None]
    m = np.arange(128)[None, :]
    jr = m - 64
    cr = q // 64
    ab = np.where((jr == cr) | (jr == cr - 1), BIGF, np.where(jr > cr, -BIGF, 0.0))
    c['abase'] = ab.astype(np.float32)
    m2 = np.arange(32)[None, :]
    c['bbase'] = np.broadcast_to(np.where(m2 >= 16, -BIGF, 0.0), (128, 32)).astype(np.float32).copy()
    c['obase'] = np.broadcast_to(np.where(m2 == 16, 0.0, 2 * NEG), (128, 32)).astype(np.float32).copy()
    c['xg'] = (16.0 * np.arange(128)[:, None] + 31.0 - np.arange(512)[None, :]).astype(np.float32)
    return c


DRAM_ONLY = ('eslc', 'emoba', 'cq')
CONST_DT = dict(ident_f=F32, ident_b=BF16, cmb=BF16, omb=BF16, mmb=BF16, eslc=BF16, emoba=BF16, cqb=F32, alb=F32, clb=F32,
                cq=BF16, goh=BF16, ov=BF16, abase=F32, bbase=F32, obase=F32, xg=F32)


def build(S, NSEQ, dbg=()):
    nc = bass.Bass("TRN2", target_bir_lowering=False)
    NT = S // 128
    NQT = S // 512
    consts = make_consts()

    def din(name, shape, dt=F32):
        return nc.dram_tensor(name, list(shape), dt, kind="ExternalInput").ap()

    x_d = din("x", [NSEQ, S, D])
    win_d = din("w_in", [D, NCOL])
    w1s_d = din("w1s", [128, 32, 128])
    w2s_d = din("w2s", [128, 2, 64])
    pet_d = din("pet", [128, 32])
    wa_d = din("w_a", [512, D])
    wb_d = din("w_b", [512, D])
    wo_d = din("w_o", [D, D])
    npre_d = din("npre_bc", [128, D])
    npost_d = din("npost_bc", [128, D])
    cd = {k: din("c_" + k, list(v.shape), CONST_DT[k]) for k, v in consts.items()}
    out_d = nc.dram_tensor("out", [NSEQ, S, D], F32, kind="ExternalOutput").ap()
    wbf_d = nc.dram_tensor("wbf", [D, NCOL], BF16, kind="Internal").ap()
    ysc_d = nc.dram_tensor("yscr", [8, 128, S], BF16, kind="Internal").ap()
    wabf_d = nc.dram_tensor("wabf", [2, 512, D], BF16, kind="Internal").ap()
    dbg_d = {}
    for name, shape in dbg:
        dbg_d[name] = nc.dram_tensor("dbg_" + name, list(shape), F32, kind="ExternalOutput").ap()

    P = Prog(nc)
    cs = {k: P.sb("k_" + k, list(v.shape), CONST_DT[k]) for k, v in consts.items() if k not in DRAM_ONLY}
    uT = P.sb("uT", [128, 8, S], BF16)
    SA = max(S, 4096)
    kT0 = P.sb("kT0", [128, SA], BF16)
    kT1 = P.sb("kT1", [128, SA], BF16)
    Vp = P.sb("Vp", [128, 2 * SA], BF16)
    kcT = P.sb("kcT", [128, 256], BF16)
    vcx = P.sb("vcx", [128, 2, 128], BF16)
    hcm = P.sb("hcm", [128, 2, 256], BF16)
    QA = [[P.sb(f"QA{b}_{h}", [128, 512], BF16) for h in range(4)] for b in range(2)]
    PT = [P.sb(f"PT{j}", [128, 512], BF16) for j in range(6)]
    XM = [P.sb(f"XM{j}", [128, 512], BF16) for j in range(2)]
    WKB = P.sb("WKB", [128, 7, 512], F32)
    WK = [WKB[:, j, :] for j in range(5)]
    YST = [P.sb(f"YST{j}", [128, 512], BF16) for j in range(2)]
    SGT = [P.sb(f"SGT{j}", [64, 512], BF16) for j in range(2)]
    T1 = P.sb("T1", [128, 4, 64], F32)
    T2 = P.sb("T2", [128, 4, 64], F32)
    M8a = P.sb("M8a", [128, 8], F32)
    M8b = P.sb("M8b", [128, 8], F32)
    GT = P.sb("GT", [128, 8, 16], F32)
    kbarf = P.sb("kbarf", [128, 16], F32)
    kbar = P.sb("kbar", [64, 2, 16], BF16)
    wst = [P.sb(f"wst{j}", [128, 8, 128], BF16) for j in range(2)]
    WQZ = P.sb("WQZ", [128, 8, 512], BF16)
    wg24 = P.sb("wg24", [128, 8, 24], BF16)
    w2s = P.sb("w2s", [128, 2, 64], BF16)
    pet = P.sb("pet", [128, 32], BF16)
    hb = P.sb("hb", [128, 2], F32)
    xt = [P.sb(f"xt{j}", [128, D], F32) for j in range(2)]
    xn = [P.sb(f"xn{j}", [128, D], F32) for j in range(2)]
    gpre = WKB[:, 5:7, :].rearrange("p a n -> p (a n)")
    gpost = gpre
    ACC = [[xt[j][:, b_ * 512:(b_ + 1) * 512] for j in range(2)] for b_ in range(2)]
    SZ = [[xn[j][:, b_ * 512:(b_ + 1) * 512] for j in range(2)] for b_ in range(2)]
    if DEBUG_SEP:
        ACC = [[P.sb(f"ACCd{b_}{j}", [128, 512], F32)[:] for j in range(2)] for b_ in range(2)]
        SZ = [[P.sb(f"SZd{b_}{j}", [128, 512], F32)[:] for j in range(2)] for b_ in range(2)]
    impacc = WKB[:, 5, :]
    WKg = WKB[:, 6, :]
    ones1 = P.sb("ones1", [128, 1], F32)

    def XK(b_):
        return [('xt', b_, 0), ('xt', b_, 1)]

    def NK(b_):
        return [('xn', b_, 0), ('xn', b_, 1)]
    ssq = P.sb("ssq", [128, 2], F32)
    ssq2 = P.sb("ssq2", [128, 2], F32)
    eps_t = P.sb("eps_t", [128, 1], F32)
    ones_b = P.sb("ones_b", [128, 128], BF16)
    wab = WQZ[:].rearrange("p c (b n) -> p b c n", b=4)[:, 0:2]
    wo_s = P.sb("wo_s", [128, 8, D], BF16)
    ps = [P.ps(f"ps{j}") for j in range(8)]
    ST = [0, 1]
    OLS = [(2, 3), (4, 5)]
    MISC = [6, 7]
    Vp3 = Vp[:].rearrange("p (t c) -> p t c", c=256)
    mT = kT0[:].rearrange("p (c t) -> p c t", c=8)
    ysl = kT1[:].rearrange("p (c t) -> p c t", c=8)
    wgs = Vp[:, 0:SA].rearrange("p (b c n) -> p b c n", b=2, c=8)
    assert SA // 8 >= 512 and SA // 16 >= 256

    rr = {'misc': 0, 'st': 0, 'pt': 0, 'wst': 0, 'wk': 0, 'ol': 0, 'ptc': 0, 'g4': 0}

    def kt_lo(hh, i):
        lo = 0
        while SL_HH[hh] * (512 * i - 128 * lo - 127) > 110.0:
            lo += 1
        return lo

    def MM(out, lhsT, rhs, start, stop, reads, writes, tp=None):
        if tp is None:
            P.op('tensor', lambda e: e.matmul(out, lhsT=lhsT, rhs=rhs, start=start, stop=stop), reads, writes)
        else:
            P.op('tensor', lambda e: e.matmul(out, lhsT=lhsT, rhs=rhs, start=start, stop=stop, tile_position=tp),
                 reads, writes)

    def TR(out, in_, ident, reads, writes):
        P.op('tensor', lambda e: e.transpose(out, in_, ident), reads, writes)

    def ACT(out, in_, func, reads, writes, bias=None, scale=1.0):
        if bias is None:
            P.op('scalar', lambda e: e.activation(out=out, in_=in_, func=func, scale=scale), reads, writes)
        else:
            P.op('scalar', lambda e: e.activation(out=out, in_=in_, func=func, bias=bias, scale=scale), reads, writes)

    def TT(eng, out, in0, in1, op, reads, writes):
        P.op(eng, lambda e: e.tensor_tensor(out=out, in0=in0, in1=in1, op=op), reads, writes)

    def TS(eng, out, in0, s1, s2, op0, op1, reads, writes):
        if op1 is None:
            P.op(eng, lambda e: e.tensor_scalar(out=out, in0=in0, scalar1=s1, scalar2=None, op0=op0), reads, writes)
        else:
            P.op(eng, lambda e: e.tensor_scalar(out=out, in0=in0, scalar1=s1, scalar2=s2, op0=op0, op1=op1),
                 reads, writes)

    def STT(out, in0, scalar, in1, op0, op1, reads, writes):
        P.op('vector', lambda e: e.scalar_tensor_tensor(out=out, in0=in0, scalar=scalar, in1=in1, op0=op0, op1=op1),
             reads, writes)

    def CP(eng, out, in_, reads, writes):
        P.op(eng, lambda e: e.tensor_copy(out=out, in_=in_), reads, writes)

    def RCP(eng, out, in_, reads, writes):
        ACT(out, in_, AF.Ln, reads, writes)
        ACT(out, out, AF.Exp, writes, writes, scale=-1.0)

    def SIG1P(ap, np_, key):
        ACT(ap, ap, AF.Ln, [key, 'ones1'], [key], bias=ones1[0:np_, 0:1], scale=1.0)
        ACT(ap, ap, AF.Exp, [key], [key], scale=-1.0)

    def MS(eng, ap, val, writes):
        P.op(eng, lambda e: e.memset(ap, val), (), writes)

    def pk(j):
        return ('ps', j)

    def misc_bank():
        j = MISC[rr['misc'] % 2]
        rr['misc'] += 1
        return j

    def next_wst():
        j = rr['wst'] % 2
        rr['wst'] += 1
        return j

    wcols = wbf_d.rearrange("(c p) n -> p c n", p=128)
    WBK = [('wbf', r) for r in range(8)]

    def load_w(dst, col0, ncols, key):
        P.dma('sync', dst, wcols[:, :, col0:col0 + ncols], reads=WBK, writes=[key])

    def dump(name, src, reads):
        if name in dbg_d:
            P.dma('sync', dbg_d[name], src, reads=reads, writes=['dbg_' + name])

    for k in cs:
        P.dma('sync', cs[k][:], cd[k], reads=(), writes=['c_' + k])
    CK = ['c_alb', 'c_clb']
    for r in range(8):
        P.dma('gpsimd', wbf_d[r * 128:(r + 1) * 128, :], win_d[r * 128:(r + 1) * 128, :], reads=(), writes=[('wbf', r)])
    P.dma('gpsimd', wabf_d[0], wa_d, reads=(), writes=['wabf0'])
    P.dma('gpsimd', wabf_d[1], wb_d, reads=(), writes=['wabf1'])
    P.dma('gpsimd', wo_s[:], wo_d.rearrange("(j p) n -> p j n", p=128), reads=(), writes=['wo'])
    P.dma('gpsimd', w2s[:], w2s_d, reads=(), writes=['w2s'])
    P.dma('gpsimd', pet[:], pet_d, reads=(), writes=['pet'])
    load_w(wg24[:], C_GA, 24, 'wg24')
    MS('vector', eps_t[:], 1e-6, ['eps'])
    MS('vector', ones_b[:], 1.0, ['ones'])
    for b_ in range(2):
        MS('vector', SGT[b_][:], 0.0, [('SGT', b_)])
    MS('vector', vcx[:], 1.0, ['vcx'])
    MS('vector', ones1[:], 1.0, ['ones1'])
    MS('vector', kcT[64:65, :], 1.0, ['kcT'])
    MS('vector', hcm[:], 0.0, ['hcm'])

    def adv(gen):
        if gen is not None:
            try:
                next(gen)
            except StopIteration:
                pass

    def exhaust(gen):
        if gen is not None:
            for _ in gen:
                pass

    def attend(tiles, gen=None, step=3):
        pend = []
        for t in tiles:
            sb_ = ST[rr['st'] % 2]
            rr['st'] += 1
            pj = rr['pt'] % 4
            rr['pt'] += 1
            n = len(t['smm'])
            for m, (lh, rh, rd) in enumerate(t['smm']):
                MM(ps[sb_][:, :], lh, rh, m == 0, m == n - 1, rd, [pk(sb_)])
            ACT(PT[pj][:], ps[sb_][:, :], AF.Exp, [pk(sb_)] + CK, [('PT', pj)], bias=t['bias'], scale=SCALE)
            pend.append((t['pv'], pj))
            rr['tc'] = rr.get('tc', 0) + 1
            if rr['tc'] % step == 0:
                adv(gen)
            if len(pend) > 2:
                pv_, pj_ = pend.pop(0)
                for (o_, lh, tp, st_, sp_, rd, wk) in pv_:
                    MM(o_, lh, PT[pj_][:], st_, sp_, rd + [('PT', pj_)], [wk], tp=tp)
        for pv_, pj_ in pend:
            for (o_, lh, tp, st_, sp_, rd, wk) in pv_:
                MM(o_, lh, PT[pj_][:], st_, sp_, rd + [('PT', pj_)], [wk], tp=tp)

    def proj_fm(wtile, wkey, wc0, ncol, blk, bank):
        for c in range(8):
            MM(ps[bank][0:ncol, :], wtile[:, c, wc0:wc0 + ncol], uT[:, c, blk * 512:(blk + 1) * 512],
               c == 0, c == 7, [wkey, ('uT', blk)], [pk(bank)])

    def silu_pair(bank, dst, dkey):
        w = WK[0]
        ACT(w[:], ps[bank][:, :], AF.Exp, [pk(bank)], [('WK', 0)], scale=-1.0)
        SIG1P(w[:], 128, ('WK', 0))
        TT('vector', dst, ps[bank][:, :], w[:], ALU.mult, [pk(bank), ('WK', 0)], [dkey])

    def combine_branch(acc, akey, first, gbank, banks):
        w = WK[1]
        for hl in range(2):
            r0 = 64 * hl
            TS('vector', w[r0:r0 + 64, :], ps[banks[hl]][64:128, :], EPSL, None, ALU.max, None, [pk(banks[hl])],
               [('WK', 1)])
        RCP('vector', w[:], w[:], [('WK', 1)], [('WK', 1)])
        if gbank is not None:
            TT('vector', w[:], ps[gbank][:, :], w[:], ALU.mult, [pk(gbank), ('WK', 1)], [('WK', 1)])
        dst = acc if first else WK[2]
        dkey = akey if first else ('WK', 2)
        for hl in range(2):
            r0 = 64 * hl
            TT('vector', dst[r0:r0 + 64, :], ps[banks[hl]][0:64, :], w[r0:r0 + 64, :], ALU.mult,
               [pk(banks[hl]), ('WK', 1)], [dkey])
        if not first:
            TT('vector', acc, acc, WK[2][:], ALU.add, [akey, ('WK', 2)], [akey])

    for s in range(NSEQ):
        P.dma('sync', gpre, npre_d, reads=(), writes=['impacc', 'WKg'])
        for tt in range(NT):
            b = tt % 2
            P.dma('sync', xt[b][:], x_d[s, tt * 128:(tt + 1) * 128, :], reads=(), writes=XK(b))
            TT('vector', xn[b][:], xt[b][:], xt[b][:], ALU.mult, XK(b), NK(b))
            P.op('vector', (lambda bb: lambda e: e.reduce_sum(out=ssq[:, bb:bb + 1], in_=xn[bb][:], axis=AX.X))(b),
                 NK(b), [('ssq', b)])
            ACT(ssq[:, b:b + 1], ssq[:, b:b + 1], AF.Ln, [('ssq', b), 'eps'], [('ssq', b)], bias=eps_t[:, 0:1],
                scale=1.0 / D)
            ACT(ssq[:, b:b + 1], ssq[:, b:b + 1], AF.Exp, [('ssq', b)], [('ssq', b)], scale=-0.5)
            STT(xn[b][:], xt[b][:], ssq[:, b:b + 1], gpre, ALU.mult, ALU.mult, XK(b) + [('ssq', b), 'impacc', 'WKg'],
                NK(b))
            for half in range(2):
                bk = misc_bank()
                for cc in range(4):
                    c = half * 4 + cc
                    TR(ps[bk][:, cc * 128:(cc + 1) * 128], xn[b][:, c * 128:(c + 1) * 128], cs['ident_f'][:],
                       NK(b) + ['c_ident_f'], [pk(bk)])
                CP('vector' if half == 0 else 'gpsimd' if False else 'vector',
                   uT[:, half * 4:(half + 1) * 4, tt * 128:(tt + 1) * 128],
                   ps[bk][:, :].rearrange("p (c t) -> p c t", c=4), [pk(bk)], [('uT', tt // 4)])
        if 'uT' in dbg_d and s == 0:
            CP('vector', WK[0][:], uT[:, 0, 0:512], [('uT', 0)], [('WK', 0)])
            dump('uT', WK[0][:], [('WK', 0)])

        ncm = (S - 32) // 16 + 1
        for g in range(2):
            wj = next_wst()
            load_w(wst[wj][:, :, 0:64], C_KV + 0 * 128 + g * 64, 64, ('wst', wj))
            load_w(wst[wj][:, :, 64:128], C_KV + 1 * 128 + g * 64, 64, ('wst', wj))
            wjb = next_wst()
            load_w(wst[wjb][:, :, 0:64], C_KV + 2 * 128 + g * 64, 64, ('wst', wjb))
            load_w(wst[wjb][:, :, 64:128], C_KV + 4 * 128 + g * 64, 64, ('wst', wjb))
            W1 = Vp[:, 0:4096].rearrange("p (l e) -> p l e", e=128)
            P.dma('gpsimd', W1, w1s_d, reads=(), writes=['Vp'])
            for blk in range(NQT):
                bk = misc_bank()
                proj_fm(wst[wj], ('wst', wj), 0, 128, blk, bk)
                CP('vector', kT1[:, blk * 512:(blk + 1) * 512], ps[bk][:, :], [pk(bk)], ['kT1'])
            wjc = next_wst()
            load_w(wst[wjc][:, :, 0:64], C_KV + 3 * 128 + g * 64, 64, ('wst', wjc))
            load_w(wst[wjc][:, :, 64:128], C_KV + 5 * 128 + g * 64, 64, ('wst', wjc))
            load_w(WQZ[:, :, 0:256], C_QA + g * 256, 256, 'WQZ')
            load_w(WQZ[:, :, 256:512], C_ZA + g * 256, 256, 'WQZ')
            for wh in range(2):
                r0 = 64 * wh
                bk = misc_bank()
                for l in range(32):
                    MM(ps[bk][:, 0:ncm], W1[r0:r0 + 64, l, :], kT1[r0:r0 + 64, l:l + 16 * (ncm - 1) + 1:16],
                       l == 0, l == 31, ['Vp', 'kT1'], [pk(bk)])
                bk2 = misc_bank()
                for l in range(32):
                    MM(ps[bk2][:, 0:1], W1[r0:r0 + 64, l, :], pet[r0:r0 + 64, l:l + 1], l == 0, l == 31,
                       ['Vp', 'pet'], [pk(bk2)])
                CP('vector', hb[:, 0:1], ps[bk2][:, 0:1], [pk(bk2)], ['hb'])
                TS('vector', hb[:, 1:2], hb[:, 0:1], -1.0, None, ALU.mult, None, ['hb'], ['hb'])
                w = WK[0]
                ACT(w[:, 0:ncm], ps[bk][:, 0:ncm], AF.Exp, [pk(bk), 'hb'], [('WK', 0)], bias=hb[:, 1:2], scale=-1.0)
                SIG1P(w[:, 0:ncm], 128, ('WK', 0))
                STT(hcm[:, wh, 0:ncm], ps[bk][:, 0:ncm], hb[:, 0:1], w[:, 0:ncm], ALU.add, ALU.mult,
                    [pk(bk), 'hb', ('WK', 0)], ['hcm'])
                if wh == 0:
                    bk3 = misc_bank()
                    MM(ps[bk3][0:64, 0:256], w2s[:, 0, :], hcm[:, 0, :], True, True, ['w2s', 'hcm'], [pk(bk3)])
                    CP('vector', kcT[0:64, :], ps[bk3][0:64, 0:256], [pk(bk3)], ['kcT'])
                else:
                    bk3 = misc_bank()
                    for a in range(2):
                        MM(ps[bk3][:, a * 64:(a + 1) * 64], hcm[:, 1, a * 128:(a + 1) * 128], w2s[:, 1, :], True, True,
                           ['w2s', 'hcm'], [pk(bk3)])
                    CP('vector', vcx[:, :, 0:64], ps[bk3][:, 0:128].rearrange("p (a d) -> p a d", a=2), [pk(bk3)],
                       ['vcx'])
            wj = wjb
            for blk in range(NQT):
                bk = misc_bank()
                proj_fm(wst[wj], ('wst', wj), 0, 128, blk, bk)
                CP('vector', kT0[0:64, blk * 512:(blk + 1) * 512], ps[bk][0:64, :], [pk(bk)], ['kT0'])
                CP('vector', kT1[0:64, blk * 512:(blk + 1) * 512], ps[bk][64:128, :], [pk(bk)], ['kT1'])
            P.dma('sync', kT0[64:128, 0:S], cd['eslc'][:, 0:S], reads=(), writes=['kT0'])
            MS('vector', kT1[64:128, :], 0.0, ['kT1'])
            MS('vector', kT1[64:65, :], 1.0, ['kT1'])
            for h4 in range(4):
                for b2 in range(2):
                    P.dma('sync', QA[b2][h4][64:65, :], cd['cq'][4 * g + h4:4 * g + h4 + 1, :], reads=(),
                          writes=[('QA', b2, h4)])

            def nsa_pre(g, i):
                b2 = i % 2
                for pp in range(2):
                    bk = misc_bank()
                    proj_fm(WQZ, 'WQZ', pp * 128, 128, i, bk)
                    CP('vector', QA[b2][2 * pp][0:64, :], ps[bk][0:64, :], [pk(bk)], [('QA', b2, 2 * pp)])
                    CP('vector', QA[b2][2 * pp + 1][0:64, :], ps[bk][64:128, :], [pk(bk)], [('QA', b2, 2 * pp + 1)])
                    yield
                bk = misc_bank()
                for c in range(8):
                    MM(ps[bk][0:24, :], wg24[:, c, :], uT[:, c, i * 512:(i + 1) * 512], c == 0, c == 7,
                       ['wg24', ('uT', i)], [pk(bk)])
                w = WK[0]
                ACT(w[0:24, :], ps[bk][0:24, :], AF.Exp, [pk(bk)], [('WK', 0)], scale=-1.0)
                SIG1P(w[0:24, :], 24, ('WK', 0))
                CP('vector', SGT[b2][0:24, :], w[0:24, :], [('WK', 0)], [('SGT', b2)])
                TT('vector', SGT[b2][32:56, :], w[0:24, :], SGT[b2][0:24, :], ALU.subtract, [('WK', 0), ('SGT', b2)],
                   [('SGT', b2)])
                yield
                a_list = [0] if (512 * i + 511) < (2048 + 31) else [0, 1]
                a_list = [a for a in a_list if a * 128 < ncm]
                for a in a_list:
                    thr = 512.0 * i - 2048.0 * a
                    TS('vector', XM[a][:], cs['xg'][:], thr, NEG, ALU.is_gt, ALU.mult, ['c_xg'], [('XM', a)])
                la = len(a_list)
                for h4 in range(4):
                    hl = h4 % 2
                    pp = h4 // 2
                    ha = 4 * g + h4
                    if hl == 0:
                        gb = misc_bank()
                        MM(ps[gb][:, :], cs['goh'][0:56, (g * 2 + pp) * 3 + 0, :], SGT[b2][0:56, :], True, True,
                           ['c_goh', ('SGT', b2)], [pk(gb)])
                        CP('vector', WKg[:], ps[gb][:, :], [pk(gb)], ['WKg'])
                    bkA = misc_bank()
                    bkB = misc_bank()
                    for ai, a in enumerate(a_list):
                        sb_ = ST[rr['st'] % 2]
                        rr['st'] += 1
                        pj = 4 + rr['ptc'] % 2
                        rr['ptc'] += 1
                        MM(ps[sb_][:, :], kcT[0:65, a * 128:(a + 1) * 128], QA[b2][h4][0:65, :], True, False,
                           ['kcT', ('QA', b2, h4)], [pk(sb_)])
                        MM(ps[sb_][:, :], cs['ident_b'][:], XM[a][:], False, True, ['c_ident_b', ('XM', a)], [pk(sb_)])
                        ACT(PT[pj][:], ps[sb_][:, :], AF.Exp, [pk(sb_)] + CK, [('PT', pj)],
                            bias=cs['clb'][:, ha, a, i:i + 1], scale=SCALE)
                        MM(ps[bkA][:, :], vcx[:, a, :], PT[pj][:], ai == 0, ai == la - 1, ['vcx', ('PT', pj)], [pk(bkA)])
                        MM(ps[bkB][0:64, :], cs['ov'][:, a, :], PT[pj][:], ai == 0, ai == la - 1, ['c_ov', ('PT', pj)],
                           [pk(bkB)])
                    wr = WK[3]
                    TS('vector', wr[0:64, :], ps[bkA][64:128, :], EPSL, None, ALU.max, None, [pk(bkA)], [('WK', 3)])
                    TS('vector', wr[64:128, :], ps[bkA][64:128, :], EPSL, None, ALU.max, None, [pk(bkA)], [('WK', 3)])
                    RCP('vector', wr[:, :], wr[:, :], [('WK', 3)], [('WK', 3)])
                    if h4 == 0:
                        TT('vector', impacc[0:64, :], ps[bkB][0:64, :], wr[0:64, :], ALU.mult, [pk(bkB), ('WK', 3)],
                           ['impacc'])
                    else:
                        TT('vector', WK[4][0:64, :], ps[bkB][0:64, :], wr[0:64, :], ALU.mult, [pk(bkB), ('WK', 3)],
                           [('WK', 4)])
                        TT('vector', impacc[0:64, :], impacc[0:64, :], WK[4][0:64, :], ALU.add, ['impacc', ('WK', 4)],
                           ['impacc'])
                    r0 = 64 * hl
                    TT('vector', WK[1][r0:r0 + 64, :], WKg[r0:r0 + 64, :], wr[r0:r0 + 64, :], ALU.mult, ['WKg', ('WK', 3)],
                       [('WK', 1)])
                    TT('vector', ACC[b2][pp][r0:r0 + 64, :], ps[bkA][0:64, :], WK[1][r0:r0 + 64, :], ALU.mult,
                       [pk(bkA), ('WK', 1)], [('xt', pp, b2)])
                    yield
                bk = misc_bank()
                for qs in range(4):
                    TR(ps[bk][:, qs * 64:(qs + 1) * 64], impacc[0:64, qs * 128:(qs + 1) * 128], cs['ident_f'][0:64, 0:64],
                       ['impacc', 'c_ident_f'], [pk(bk)])
                for qs in range(4):
                    ti = 4 * i + qs
                    TT('vector', T1[:, qs, :], ps[bk][:, qs * 64:(qs + 1) * 64],
                       cs['abase'][:, 64 - 2 * ti:128 - 2 * ti], ALU.add, [pk(bk), 'c_abase'], ['T1'])
                MS('vector', T1[:, :, 0:1], BIGF, ['T1'])
                yield
                for qs in range(4):
                    P.op('vector', (lambda q_: lambda e: e.max(out=M8a[:], in_=T1[:, q_, :]))(qs), ['T1'], ['M8a'])
                    P.op('vector', (lambda q_: lambda e: e.match_replace(out=T2[:, q_, :], in_to_replace=M8a[:],
                                                                        in_values=T1[:, q_, :], imm_value=-1e9))(qs),
                         ['T1', 'M8a'], ['T2'])
                    P.op('vector', (lambda q_: lambda e: e.max(out=M8b[:], in_=T2[:, q_, :]))(qs), ['T2'], ['M8b'])
                    TS('vector', T2[:, qs, :], T1[:, qs, :], M8b[:, 7:8], NEG, ALU.is_lt, ALU.mult, ['T1', 'M8b'], ['T2'])
                    yield
                for pp in range(2):
                    bk = misc_bank()
                    proj_fm(WQZ, 'WQZ', 256 + pp * 128, 128, i, bk)
                    silu_pair(bk, SZ[b2][pp], ('xn', pp, b2))
                    yield
                yield
                bk = misc_bank()
                for qs in range(4):
                    TR(ps[bk][0:64, qs * 128:(qs + 1) * 128], T2[:, qs, :], cs['ident_f'][:], ['T2', 'c_ident_f'],
                       [pk(bk)])
                for h4 in range(4):
                    STT(QA[b2][h4][64:128, :], cs['cqb'][64:128, :], -SL_HH[4 * g + h4] / SCALE, ps[bk][0:64, :],
                        ALU.mult, ALU.add, [pk(bk), 'c_cqb'], [('QA', b2, h4)])
                yield

            gen0 = nsa_pre(g, 0)
            wj = wjc
            MS('vector', Vp3[:, :, :].rearrange("p t (h c) -> p t h c", h=2)[:, :, :, 64:128], 1.0, ['Vp'])
            for t4 in range(NT // 4):
                bk = misc_bank()
                for q4 in range(4):
                    tt = t4 * 4 + q4
                    for c in range(8):
                        MM(ps[bk][:, q4 * 128:(q4 + 1) * 128], uT[:, c, tt * 128:(tt + 1) * 128], wst[wj][:, c, :],
                           c == 0, c == 7, [('wst', wj), ('uT', t4)], [pk(bk)])
                CP('vector', Vp3[:, t4 * 4:(t4 + 1) * 4, :].rearrange("p t (h c) -> p t h c", h=2)[:, :, :, 0:64],
                   ps[bk][:, :].rearrange("p (t h c) -> p t h c", h=2, c=64), [pk(bk)], ['Vp'])
                adv(gen0)
                adv(gen0)
            exhaust(gen0)
            for i in range(NQT):
                b2 = i % 2
                gen = nsa_pre(g, i + 1) if i + 1 < NQT else None
                for pp in range(2):
                    for br in (1, 2):
                        tiles = []
                        OB, LB = OLS[rr['ol'] % 2]
                        rr['ol'] += 1
                        if br == 1:
                            kts = list(range(0, 4 * i + 4))
                        else:
                            kts = [kt for kt in range(4 * i - 4, 4 * i + 4) if kt >= 0]
                        for kt in kts:
                            for hl in range(2):
                                h4 = 2 * pp + hl
                                ha = 4 * g + h4
                                hk = [k_ for k_ in kts if k_ >= kt_lo(ha, i)]
                                if kt not in hk:
                                    continue
                                ki = hk.index(kt)
                                nk = len(hk)
                                kT = kT0 if br == 1 else kT1
                                kkey = 'kT0' if br == 1 else 'kT1'
                                smm = [(kT[:, kt * 128:(kt + 1) * 128], QA[b2][h4][:, :], [kkey, ('QA', b2, h4)])]
                                if br == 1:
                                    if kt >= 4 * i:
                                        r = kt - 4 * i
                                        smm.append((cs['ident_b'][:], cs['cmb'][:, 384 - 128 * r:896 - 128 * r],
                                                    ['c_ident_b', 'c_cmb']))
                                else:
                                    r = kt - (4 * i - 4)
                                    if r < 4:
                                        smm.append((cs['ident_b'][:], cs['omb'][:, 384 - 128 * r:896 - 128 * r],
                                                    ['c_ident_b', 'c_omb']))
                                    else:
                                        r -= 4
                                        smm.append((cs['ident_b'][:], cs['cmb'][:, 384 - 128 * r:896 - 128 * r],
                                                    ['c_ident_b', 'c_cmb']))
                                vcol = 0 if br == 1 else 128
                                bnk = (OB, LB)[hl]
                                pv = [(ps[bnk][:, :], Vp3[:, kt, vcol:vcol + 128], None,
                                       ki == 0, ki == nk - 1, ['Vp'], pk(bnk))]
                                tiles.append(dict(smm=smm, bias=cs['alb'][:, ha, kt - 4 * i + 32:kt - 4 * i + 33], pv=pv))
                        attend(tiles, gen)
                        gb = misc_bank()
                        MM(ps[gb][:, :], cs['goh'][0:56, (g * 2 + pp) * 3 + br, :], SGT[b2][0:56, :], True, True,
                           ['c_goh', ('SGT', b2)], [pk(gb)])
                        combine_branch(ACC[b2][pp], ('xt', pp, b2), False, gb, (OB, LB))
                    yj = rr['wk'] % 2
                    rr['wk'] += 1
                    TT('vector', YST[yj][:], ACC[b2][pp], SZ[b2][pp], ALU.mult, [('xt', pp, b2), ('xn', pp, b2)],
                       [('YST', yj)])
                    P.dma('sync', ysc_d[g * 2 + pp, :, i * 512:(i + 1) * 512], YST[yj][:], reads=[('YST', yj)],
                          writes=[('ysc', g * 2 + pp, i)])
                exhaust(gen)

        nb = S // 256
        for j in range(4):
            wj = next_wst()
            load_w(wst[wj][:], C_QB + 512 + j * 128, 128, ('wst', wj))
            wjv = next_wst()
            load_w(wst[wjv][:], C_QB + 1024 + j * 128, 128, ('wst', wjv))
            load_w(WQZ[:, :, 0:128], C_QB + j * 128, 128, 'WQZ')
            load_w(WQZ[:, :, 128:256], C_ZB + j * 128, 128, 'WQZ')
            for blk in range(NQT):
                bk = misc_bank()
                proj_fm(wst[wj], ('wst', wj), 0, 128, blk, bk)
                CP('vector', kT0[0:64, blk * 512:(blk + 1) * 512], ps[bk][0:64, :], [pk(bk)], ['kT0'])
                CP('vector', kT1[0:64, blk * 512:(blk + 1) * 512], ps[bk][64:128, :], [pk(bk)], ['kT1'])
                P.op('vector', (lambda bk_, blk_: lambda e: e.reduce_sum(
                    out=kbarf[:, 2 * blk_:2 * blk_ + 2], in_=ps[bk_][:, :].rearrange("p (b t) -> p b t", b=2),
                    axis=AX.X))(bk, blk), [pk(bk)], ['kbarf'])
            for kT_, kk_ in (((kT0, 'kT0'), (kT1, 'kT1')) if j == 0 else ()):
                MS('vector', kT_[64:128, :], 0.0, [kk_])
                MS('vector', kT_[64:65, :], 1.0, [kk_])
                P.dma('sync', kT_[96:112, 0:S], cd['emoba'][:, 0:S], reads=(), writes=[kk_])
            TS('vector', kbar[0:64, 0, 0:nb], kbarf[0:64, 0:nb], 1.0 / 256, None, ALU.mult, None, ['kbarf'], ['kbar'])
            TS('vector', kbar[0:64, 1, 0:nb], kbarf[64:128, 0:nb], 1.0 / 256, None, ALU.mult, None, ['kbarf'], ['kbar'])
            for hl in range(2):
                for b2 in range(2):
                    if j == 0:
                        MS('vector', QA[b2][hl][64:128, :], 0.0, [('QA', b2, hl)])
                    P.dma('sync', QA[b2][hl][64:65, :], cd['cq'][8 + 2 * j + hl:8 + 2 * j + hl + 1, :], reads=(),
                          writes=[('QA', b2, hl)])

            def moba_pre(j, i):
                b2 = i % 2
                bk = misc_bank()
                proj_fm(WQZ, 'WQZ', 0, 128, i, bk)
                CP('vector', QA[b2][0][0:64, :], ps[bk][0:64, :], [pk(bk)], [('QA', b2, 0)])
                CP('vector', QA[b2][1][0:64, :], ps[bk][64:128, :], [pk(bk)], [('QA', b2, 1)])
                yield
                bk = misc_bank()
                for hl in range(2):
                    for qs in range(4):
                        MM(ps[bk][:, (hl * 4 + qs) * 16:(hl * 4 + qs) * 16 + nb], QA[b2][hl][0:64, qs * 128:(qs + 1) * 128],
                           kbar[0:64, hl, 0:nb], True, True, ['kbar', ('QA', b2, hl)], [pk(bk)])
                for hl in range(2):
                    for qs in range(4):
                        cur = (4 * i + qs) // 2
                        e8 = hl * 4 + qs
                        TT('vector', GT[:, e8, 0:nb], ps[bk][:, e8 * 16:e8 * 16 + nb], cs['bbase'][:, 16 - cur:16 - cur + nb],
                           ALU.add, [pk(bk), 'c_bbase'], ['GT'])
                        if nb < 16:
                            MS('vector', GT[:, e8, nb:16], -BIGF, ['GT'])
                yield
                for hl in range(2):
                    for qs in range(4):
                        cur = (4 * i + qs) // 2
                        e8 = hl * 4 + qs
                        P.op('vector', (lambda e_: lambda e: e.max(out=M8a[:], in_=GT[:, e_, :]))(e8), ['GT'], ['M8a'])
                        TS('vector', GT[:, e8, :], GT[:, e8, :], M8a[:, 2:3], NEG, ALU.is_lt, ALU.mult, ['GT', 'M8a'], ['GT'])
                        TT('vector', GT[:, e8, :], GT[:, e8, :], cs['obase'][:, 16 - cur:32 - cur], ALU.max,
                           ['GT', 'c_obase'], ['GT'])
                        yield
                bk = misc_bank()
                proj_fm(WQZ, 'WQZ', 128, 128, i, bk)
                silu_pair(bk, SZ[b2][0], ('xn', 0, b2))
                yield
                yield
                for hl in range(2):
                    bk = misc_bank()
                    for qs in range(4):
                        TR(ps[bk][0:16, qs * 128:(qs + 1) * 128], GT[:, hl * 4 + qs, :], cs['ident_f'][:],
                           ['GT', 'c_ident_f'], [pk(bk)])
                    CP('vector', QA[b2][hl][96:112, :], ps[bk][0:16, :], [pk(bk)], [('QA', b2, hl)])
                    yield

            gen0 = moba_pre(j, 0)
            wj = wjv
            MS('vector', Vp3[:, :, :].rearrange("p t (h c) -> p t h c", h=2)[:, :, :, 64:128], 1.0, ['Vp'])
            for t4 in range(NT // 4):
                bk = misc_bank()
                for q4 in range(4):
                    tt = t4 * 4 + q4
                    for c in range(8):
                        MM(ps[bk][:, q4 * 128:(q4 + 1) * 128], uT[:, c, tt * 128:(tt + 1) * 128], wst[wj][:, c, :],
                           c == 0, c == 7, [('wst', wj), ('uT', t4)], [pk(bk)])
                CP('vector', Vp3[:, t4 * 4:(t4 + 1) * 4, :].rearrange("p t (h c) -> p t h c", h=2)[:, :, :, 0:64],
                   ps[bk][:, :].rearrange("p (t h c) -> p t h c", h=2, c=64), [pk(bk)], ['Vp'])
                adv(gen0)
            exhaust(gen0)
            for i in range(NQT):
                b2 = i % 2
                gen = moba_pre(j, i + 1) if i + 1 < NQT else None
                tiles = []
                OB, LB = OLS[rr['ol'] % 2]
                rr['ol'] += 1
                kts = list(range(0, 4 * i + 4))
                for kt in kts:
                    for hl in range(2):
                        hh = 8 + 2 * j + hl
                        hk = [k_ for k_ in kts if k_ >= kt_lo(hh, i)]
                        if kt not in hk:
                            continue
                        ki = hk.index(kt)
                        nk = len(hk)
                        kT = kT0 if hl == 0 else kT1
                        kkey = 'kT0' if hl == 0 else 'kT1'
                        smm = [(kT[:, kt * 128:(kt + 1) * 128], QA[b2][hl][:, :], [kkey, ('QA', b2, hl)])]
                        if kt >= 4 * i:
                            smm.append((cs['ident_b'][:], cs['mmb'][:, kt - 4 * i, :], ['c_ident_b', 'c_mmb']))
                        bnk = (OB, LB)[hl]
                        pv = [(ps[bnk][:, :], Vp3[:, kt, 128 * hl:128 * hl + 128], None,
                               ki == 0, ki == nk - 1, ['Vp'], pk(bnk))]
                        tiles.append(dict(smm=smm, bias=cs['alb'][:, hh, kt - 4 * i + 32:kt - 4 * i + 33], pv=pv))
                attend(tiles, gen, step=2)
                combine_branch(ACC[b2][0], ('xt', 0, b2), True, None, (OB, LB))
                yj = rr['wk'] % 2
                rr['wk'] += 1
                TT('vector', YST[yj][:], ACC[b2][0], SZ[b2][0], ALU.mult, [('xt', 0, b2), ('xn', 0, b2)], [('YST', yj)])
                P.dma('sync', ysc_d[4 + j, :, i * 512:(i + 1) * 512], YST[yj][:], reads=[('YST', yj)],
                      writes=[('ysc', 4 + j, i)])
                exhaust(gen)

        P.op('sync', lambda e: e.nop(), (), ['Vp', ('wgs', 0), ('wgs', 1), 'WQZ', ('wab', 0), ('wab', 1), 'kT1', ('ysl', 0), ('ysl', 1)])
        P.dma('sync', gpost, npost_d, reads=(), writes=['impacc', 'WKg'])
        ysl2 = [ysl, Vp[:, SA:2 * SA].rearrange("p (c t) -> p c t", c=8)]

        def load_ysl(i_):
            P.dma('sync', ysl2[i_ % 2][:, :, 0:512], ysc_d[:, :, i_ * 512:(i_ + 1) * 512].rearrange("c p t -> p c t"),
                  reads=[('ysc', jj, i_) for jj in range(8)], writes=[('ysl', i_ % 2)])
        load_ysl(0)
        for i in range(NQT):
            if i + 1 < NQT:
                load_ysl(i + 1)
            for fc in range(8):
                gj = fc % 2
                P.dma('sync', wgs[:, gj, :, 0:128], wcols[:, :, C_MG + fc * 128:C_MG + (fc + 1) * 128], reads=WBK,
                      writes=[('wgs', gj)])
                P.dma('sync', wgs[:, gj, :, 128:256], wcols[:, :, C_MG + 1024 + fc * 128:C_MG + 1024 + (fc + 1) * 128],
                      reads=WBK, writes=[('wgs', gj)])
                for ab in range(2):
                    P.dma('sync', wab[:, gj, 4 * ab:4 * ab + 4, :],
                          wabf_d[ab].rearrange("(j p) n -> p j n", p=128)[:, :, fc * 128:(fc + 1) * 128],
                          reads=['wabf0', 'wabf1'], writes=[('wab', gj)])
                for ab in range(2):
                    bg = [6, 7, 0, 1][rr['g4'] % 4]
                    rr['g4'] += 1
                    for c in range(8):
                        MM(ps[bg][:, :], wgs[:, gj, c, ab * 128:(ab + 1) * 128], uT[:, c, i * 512:(i + 1) * 512],
                           c == 0, c == 7, [('wgs', gj), ('uT', i)], [pk(bg)])
                    w = WK[ab]
                    ACT(w[:], ps[bg][:, :], AF.Sigmoid, [pk(bg)], [('WK', ab)])
                    bm = OLS[fc % 2][ab]
                    for jj in range(4):
                        MM(ps[bm][:, :], wab[:, gj, 4 * ab + jj, :], ysl2[i % 2][:, 4 * ab + jj, 0:512], jj == 0, jj == 3,
                           [('wab', gj), ('ysl', i % 2)], [pk(bm)])
                    TT('vector', w[:], ps[bm][:, :], w[:], ALU.mult, [pk(bm), ('WK', ab)], [('WK', ab)])
                TT('vector', mT[:, fc, 0:512], WK[0][:], WK[1][:], ALU.add, [('WK', 0), ('WK', 1)], ['kT0'])
            for ts_ in range(4):
                tt = i * 4 + ts_
                b = tt % 2
                P.dma('sync', xt[b][:], x_d[s, tt * 128:(tt + 1) * 128, :], reads=(), writes=XK(b))
                banks = [ST[0], ST[1]]
                for hf in range(2):
                    for fc in range(8):
                        MM(ps[banks[hf]][:, :], mT[:, fc, ts_ * 128:(ts_ + 1) * 128], wo_s[:, fc, hf * 512:(hf + 1) * 512],
                           fc == 0, fc == 7, ['wo', 'kT0'], [pk(banks[hf])])
                for hf in range(2):
                    P.op('scalar', (lambda o_, i_: lambda e: e.copy(out=o_, in_=i_))(WK[2 + hf][:], ps[banks[hf]][:, :]),
                         [pk(banks[hf])], [('WK', 2 + hf)])
                    P.op('vector', (lambda o_, i_, a_: lambda e: e.scalar_tensor_tensor(
                        out=o_, in0=i_, scalar=1.0, in1=i_, op0=ALU.mult, op1=ALU.mult, accum_out=a_))(
                        YST[hf][:], WK[2 + hf][:], ssq2[:, hf:hf + 1]), [('WK', 2 + hf)], [('YST', hf), ('ssq2', hf)])
                TT('vector', ssq[:, b:b + 1], ssq2[:, 0:1], ssq2[:, 1:2], ALU.add, [('ssq2', 0), ('ssq2', 1)], [('ssq', b)])
                ACT(ssq[:, b:b + 1], ssq[:, b:b + 1], AF.Ln, [('ssq', b), 'eps'], [('ssq', b)], bias=eps_t[:, 0:1],
                    scale=1.0 / D)
                ACT(ssq[:, b:b + 1], ssq[:, b:b + 1], AF.Exp, [('ssq', b)], [('ssq', b)], scale=-0.5)
                for hf in range(2):
                    STT(xn[b][:, hf * 512:(hf + 1) * 512], WK[2 + hf][:], ssq[:, b:b + 1],
                        gpost[:, hf * 512:(hf + 1) * 512], ALU.mult, ALU.mult, [('WK', 2 + hf), ('ssq', b), 'impacc', 'WKg'],
                        [('xn', b, hf)])
                TT('gpsimd', xn[b][:], xn[b][:], xt[b][:], ALU.add, NK(b) + XK(b), NK(b))
                P.dma('sync', out_d[s, tt * 128:(tt + 1) * 128, :], xn[b][:], reads=NK(b), writes=[('out', tt)])
        P.op('sync', lambda e: e.nop(), (), ['Vp', ('wgs', 0), ('wgs', 1), 'WQZ', ('wab', 0), ('wab', 1), 'kT1', ('ysl', 0), ('ysl', 1)])
    P.finish()
    return nc, P


def host_inputs(inputs, S):
    c = make_consts()
    f = lambda a: np.ascontiguousarray(np.asarray(a, dtype=np.float32))
    w1k = f(inputs['cmp_w1_k'])[0].transpose(1, 0, 2)
    w1v = f(inputs['cmp_w1_v'])[0].transpose(1, 0, 2)
    shared = {
        'w_in': f(inputs['w_in'])[0],
        'w1s': np.ascontiguousarray(np.concatenate([w1k, w1v], 0)),
        'w2s': np.ascontiguousarray(np.stack([f(inputs['cmp_w2_k'])[0], f(inputs['cmp_w2_v'])[0]], 1)),
        'pet': np.ascontiguousarray(np.concatenate([f(inputs['cmp_pe_k'])[0].T, f(inputs['cmp_pe_v'])[0].T], 0)),
        'w_a': f(inputs['w_branch_a'])[0],
        'w_b': f(inputs['w_branch_b'])[0],
        'w_o': f(inputs['w_o'])[0],
        'npre_bc': np.ascontiguousarray(np.broadcast_to(f(inputs['norm_pre'])[0][None, :], (128, D))),
        'npost_bc': np.ascontiguousarray(np.broadcast_to(f(inputs['norm_post'])[0][None, :], (128, D))),
    }
    for k, v in c.items():
        shared['c_' + k] = v
    return shared


def kernel(**inputs):
    x = np.ascontiguousarray(np.asarray(inputs['x'], dtype=np.float32))
    B, S, _ = x.shape
    ncores = 8
    nseq = B // ncores
    nc, _ = build(S, nseq)
    shared = host_inputs(inputs, S)
    in_maps = []
    for c in range(ncores):
        m = dict(shared)
        m['x'] = np.ascontiguousarray(x[c * nseq:(c + 1) * nseq])
        in_maps.append(m)
    res = run_bass_kernel_spmd(nc, in_maps, core_ids=list(range(ncores)))
    return np.concatenate([np.asarray(r['out']) for r in res.results], axis=0).astype(np.float32)
```

```python
import contextlib
import numpy as np
import ml_dtypes
import concourse.bass as bass
import concourse.mybir as mybir
from concourse.bass_utils import run_bass_kernel_spmd

F32 = mybir.dt.float32
BF16 = mybir.dt.bfloat16
ALU = mybir.AluOpType
AF = mybir.ActivationFunctionType
AX = mybir.AxisListType
NPBF = ml_dtypes.bfloat16

D = 1024
DEBUG_SEP = False
NCOL = 5912
NEG = -30000.0
SCALE = 0.125
BIGF = 1.0e4
EPSL = 1.0e-18
C_QA, C_KV, C_GA, C_ZA, C_QB, C_ZB, C_MG = 0, 512, 1280, 1304, 1816, 3352, 3864
SLOPES = [2.0 ** (-(i + 1) / 2.0) for i in range(16)]
SL_HH = [SLOPES[2 * h] for h in range(8)] + [SLOPES[2 * h + 1] for h in range(8)]


class Prog:
    EPOCH = 20000
    NDMA = 8

    def __init__(self, nc):
        self.nc = nc
        self.ops = []
        self.lastw = {}
        self.readers = {}
        self.stack = contextlib.ExitStack()
        self.sb_bytes = 0

    def sb(self, name, shape, dtype):
        n = 1
        for s in shape[1:]:
            n *= s
        self.sb_bytes += n * (4 if dtype == F32 else 2)
        return self.stack.enter_context(self.nc.sbuf_tensor('sb_' + name, list(shape), dtype))

    def ps(self, name, shape=(128, 512), dtype=F32):
        return self.stack.enter_context(self.nc.psum_tensor(name, list(shape), dtype))

    def _deps(self, idx, reads, writes):
        deps = set()
        for k in reads:
            w = self.lastw.get(k)
            if w is not None:
                deps.add(w)
        for k in writes:
            w = self.lastw.get(k)
            if w is not None:
                deps.add(w)
            for r in self.readers.get(k, ()):
                deps.add(r)
        for k in reads:
            self.readers.setdefault(k, []).append(idx)
        for k in writes:
            self.lastw[k] = idx
            self.readers[k] = []
        deps.discard(idx)
        return deps

    def op(self, eng, fn, reads=(), writes=()):
        idx = len(self.ops)
        deps = self._deps(idx, reads, writes)
        self.ops.append(dict(eng=eng, fn=fn, deps=deps, dma=False, wkeys=set(writes), rkeys=set(reads)))
        return idx

    def dma(self, eng, out, in_, reads=(), writes=()):
        idx = len(self.ops)
        deps = self._deps(idx, reads, writes)
        self.ops.append(dict(eng=eng, fn=lambda e: e.dma_start(out=out, in_=in_), deps=deps, dma=True,
                             wkeys=set(writes), rkeys=set(reads)))
        return idx

    def finish(self):
        nc = self.nc
        ops = self.ops
        needed = set()
        for i, o in enumerate(ops):
            nd = set()
            for d in o['deps']:
                od = ops[d]
                if od['eng'] == o['eng'] and not od['dma'] and not o['dma']:
                    if o['eng'] == 'tensor':
                        continue
                    if not ((od['wkeys'] & o['rkeys']) or (od['wkeys'] & o['wkeys'])):
                        continue
                nd.add(d)
            o['deps'] = nd
            for d in nd:
                if not ops[d]['dma']:
                    needed.add(d)
        engs = ['tensor', 'vector', 'scalar', 'gpsimd', 'sync']
        cnt = {e: 0 for e in engs}
        dcnt = {e: 0 for e in engs}
        nep = {e: 0 for e in engs}
        for i, o in enumerate(ops):
            e = o['eng']
            if o['dma']:
                d = dcnt[e]
                dcnt[e] += 1
                o['sig'] = (('dma', e, d % self.NDMA), 16 * (d // self.NDMA + 1), 16)
                o['prev'] = (('dma', e, d % self.NDMA), 16 * (d // self.NDMA)) if d >= self.NDMA else None
            elif i in needed:
                c = cnt[e]
                cnt[e] += 1
                ep = c // self.EPOCH
                nep[e] = max(nep[e], ep + 1)
                o['sig'] = (('cmp', e, ep), c % self.EPOCH + 1, 1)
            else:
                o['sig'] = None
        sems = {}
        for e in engs:
            for ep in range(nep[e]):
                sems[('cmp', e, ep)] = self.stack.enter_context(nc.semaphore(f"s_{e}_{ep}"))
            for j in range(min(self.NDMA, dcnt[e])):
                sems[('dma', e, j)] = self.stack.enter_context(nc.semaphore(f"d_{e}_{j}"))
        self.n_instr = {e: 0 for e in engs}
        with nc.Block() as block:
            def make(ename):
                def body(eng):
                    waited = {}
                    lastdma = {}
                    for o in ops:
                        if o['eng'] != ename:
                            continue
                        best = {}
                        for d in o['deps']:
                            s = ops[d]['sig']
                            if s[1] > best.get(s[0], 0):
                                best[s[0]] = s[1]
                        if o['dma'] and o['prev'] is not None:
                            k, v = o['prev']
                            if v > best.get(k, 0):
                                best[k] = v
                        for k, v in best.items():
                            if waited.get(k, 0) >= v:
                                continue
                            eng.wait_ge(sems[k], v)
                            waited[k] = v
                            self.n_instr[ename] += 1
                        ins = o['fn'](eng)
                        self.n_instr[ename] += 1
                        if o['sig'] is not None:
                            ins.then_inc(sems[o['sig'][0]], o['sig'][2])
                            if o['dma']:
                                lastdma[o['sig'][0]] = o['sig'][1]
                    for k, v in lastdma.items():
                        if waited.get(k, 0) < v:
                            eng.wait_ge(sems[k], v)
                return body
            for ename in engs:
                if any(o['eng'] == ename for o in ops):
                    getattr(block, ename)(make(ename))
        self.stack.close()


def make_consts():
    c = {}
    c['ident_f'] = np.eye(128, dtype=np.float32)
    c['ident_b'] = np.eye(128).astype(NPBF)
    p = np.arange(128)[:, None]
    fp = np.arange(896)[None, :]
    c['cmb'] = np.where(p > fp - 384, NEG, 0.0).astype(NPBF)
    c['omb'] = np.where(fp - 384 >= p, NEG, 0.0).astype(NPBF)
    kk = np.arange(4)[None, :, None] * 128 + np.arange(128)[:, None, None]
    f = np.arange(512)[None, None, :]
    mm = np.where(kk // 256 == f // 256, np.where(kk > f, NEG, 0.0), np.where(f // 256 > kk // 256, 0.0, NEG))
    c['mmb'] = mm.astype(NPBF)
    k2 = np.arange(4096)[None, :]
    c['eslc'] = (k2 // 64 == np.arange(64)[:, None]).astype(NPBF)
    c['emoba'] = (k2 // 256 == np.arange(16)[:, None]).astype(NPBF)
    c['cqb'] = np.ascontiguousarray(np.broadcast_to(np.arange(512, dtype=np.float32)[None, :], (128, 512)))
    alb = np.zeros((128, 16, 36), np.float32)
    for hh in range(16):
        for di in range(36):
            alb[:, hh, di] = SL_HH[hh] * (np.arange(128) + 128.0 * (di - 32))
    c['alb'] = alb
    clb = np.zeros((128, 8, 2, 8), np.float32)
    for ha in range(8):
        for a in range(2):
            for i in range(8):
                clb[:, ha, a, i] = SL_HH[ha] * (2048.0 * a + 16.0 * np.arange(128) + 31.0 - 512.0 * i)
    c['clb'] = clb
    cq = np.zeros((16, 512), np.float32)
    for hh in range(16):
        cq[hh] = -SL_HH[hh] * np.arange(512) / SCALE
    c['cq'] = cq.astype(NPBF)
    goh = np.zeros((56, 12, 128), np.float32)
    for g in range(2):
        for pp in range(2):
            for k in range(3):
                idx = (g * 2 + pp) * 3 + k
                ra = k * 8 + g * 4 + 2 * pp
                goh[ra, idx, 0:64] = 1.0
                goh[32 + ra, idx, 0:64] = 1.0
                goh[ra + 1, idx, 64:128] = 1.0
                goh[32 + ra + 1, idx, 64:128] = 1.0
    c['goh'] = goh.astype(NPBF)
    n_cmp, n_slc = 255, 64
    cs = np.arange(n_cmp) * 16
    ce = cs + 31
    ss = np.arange(n_slc) * 64
    se = ss + 63
    ov = ((cs[:, None] <= se[None, :]) & (ce[:, None] >= ss[None, :])).astype(np.float32)
    ovp = np.zeros((256, 64), np.float32)
    ovp[:255] = ov
    c['ov'] = np.ascontiguousarray(ovp.reshape(2, 128, 64).transpose(1, 0, 2)).astype(NPBF)
    q = np.arange(128)[:, None]
    m = np.arange(128)[None, :]
    jr = m - 64
    cr = q // 64
    ab = np.where((jr == cr) | (jr == cr - 1), BIGF, np.where(jr > cr, -BIGF, 0.0))
    c['abase'] = ab.astype(np.float32)
    m2 = np.arange(32)[None, :]
    c['bbase'] = np.broadcast_to(np.where(m2 >= 16, -BIGF, 0.0), (128, 32)).astype(np.float32).copy()
    c['obase'] = np.broadcast_to(np.where(m2 == 16, 0.0, 2 * NEG), (128, 32)).astype(np.float32).copy()
    c['xg'] = (16.0 * np.arange(128)[:, None] + 31.0 - np.arange(512)[None, :]).astype(np.float32)
    return c


DRAM_ONLY = ('eslc', 'emoba', 'cq')
CONST_DT = dict(ident_f=F32, ident_b=BF16, cmb=BF16, omb=BF16, mmb=BF16, eslc=BF16, emoba=BF16, cqb=F32, alb=F32, clb=F32,
                cq=BF16, goh=BF16, ov=BF16, abase=F32, bbase=F32, obase=F32, xg=F32)


def build(S, NSEQ, dbg=()):
    nc = bass.Bass("TRN2", target_bir_lowering=False)
    NT = S // 128
    NQT = S // 512
    consts = make_consts()

    def din(name, shape, dt=F32):
        return nc.dram_tensor(name, list(shape), dt, kind="ExternalInput").ap()

    x_d = din("x", [NSEQ, S, D])
    win_d = din("w_in", [D, NCOL])
    w1s_d = din("w1s", [128, 32, 128])
    w2s_d = din("w2s", [128, 2, 64])
    pet_d = din("pet", [128, 32])
    wa_d = din("w_a", [512, D])
    wb_d = din("w_b", [512, D])
    wo_d = din("w_o", [D, D])
    npre_d = din("npre_bc", [128, D])
    npost_d = din("npost_bc", [128, D])
    cd = {k: din("c_" + k, list(v.shape), CONST_DT[k]) for k, v in consts.items()}
    out_d = nc.dram_tensor("out", [NSEQ, S, D], F32, kind="ExternalOutput").ap()
    wbf_d = nc.dram_tensor("wbf", [D, NCOL], BF16, kind="Internal").ap()
    ysc_d = nc.dram_tensor("yscr", [8, 128, S], BF16, kind="Internal").ap()
    wabf_d = nc.dram_tensor("wabf", [2, 512, D], BF16, kind="Internal").ap()
    dbg_d = {}
    for name, shape in dbg:
        dbg_d[name] = nc.dram_tensor("dbg_" + name, list(shape), F32, kind="ExternalOutput").ap()

    P = Prog(nc)
    cs = {k: P.sb("k_" + k, list(v.shape), CONST_DT[k]) for k, v in consts.items() if k not in DRAM_ONLY}
    uT = P.sb("uT", [128, 8, S], BF16)
    SA = max(S, 4096)
    kT0 = P.sb("kT0", [128, SA], BF16)
    kT1 = P.sb("kT1", [128, SA], BF16)
    Vp = P.sb("Vp", [128, 2 * SA], BF16)
    kcT = P.sb("kcT", [128, 256], BF16)
    vcx = P.sb("vcx", [128, 2, 128], BF16)
    hcm = P.sb("hcm", [128, 2, 256], BF16)
    QA = [[P.sb(f"QA{b}_{h}", [128, 512], BF16) for h in range(4)] for b in range(2)]
    PT = [P.sb(f"PT{j}", [128, 512], BF16) for j in range(6)]
    XM = [P.sb(f"XM{j}", [128, 512], BF16) for j in range(2)]
    WKB = P.sb("WKB", [128, 7, 512], F32)
    WK = [WKB[:, j, :] for j in range(5)]
    YST = [P.sb(f"YST{j}", [128, 512], BF16) for j in range(2)]
    SGT = [P.sb(f"SGT{j}", [64, 512], BF16) for j in range(2)]
    T1 = P.sb("T1", [128, 4, 64], F32)
    T2 = P.sb("T2", [128, 4, 64], F32)
    M8a = P.sb("M8a", [128, 8], F32)
    M8b = P.sb("M8b", [128, 8], F32)
    GT = P.sb("GT", [128, 8, 16], F32)
    kbarf = P.sb("kbarf", [128, 16], F32)
    kbar = P.sb("kbar", [64, 2, 16], BF16)
    wst = [P.sb(f"wst{j}", [128, 8, 128], BF16) for j in range(2)]
    WQZ = P.sb("WQZ", [128, 8, 512], BF16)
    wg24 = P.sb("wg24", [128, 8, 24], BF16)
    w2s = P.sb("w2s", [128, 2, 64], BF16)
    pet = P.sb("pet", [128, 32], BF16)
    hb = P.sb("hb", [128, 2], F32)
    xt = [P.sb(f"xt{j}", [128, D], F32) for j in range(2)]
    xn = [P.sb(f"xn{j}", [128, D], F32) for j in range(2)]
    gpre = WKB[:, 5:7, :].rearrange("p a n -> p (a n)")
    gpost = gpre
    ACC = [[xt[j][:, b_ * 512:(b_ + 1) * 512] for j in range(2)] for b_ in range(2)]
    SZ = [[xn[j][:, b_ * 512:(b_ + 1) * 512] for j in range(2)] for b_ in range(2)]
    if DEBUG_SEP:
        ACC = [[P.sb(f"ACCd{b_}{j}", [128, 512], F32)[:] for j in range(2)] for b_ in range(2)]
        SZ = [[P.sb(f"SZd{b_}{j}", [128, 512], F32)[:] for j in range(2)] for b_ in range(2)]
    impacc = WKB[:, 5, :]
    WKg = WKB[:, 6, :]
    ones1 = P.sb("ones1", [128, 1], F32)

    def XK(b_):
        return [('xt', b_, 0), ('xt', b_, 1)]

    def NK(b_):
        return [('xn', b_, 0), ('xn', b_, 1)]
    ssq = P.sb("ssq", [128, 2], F32)
    ssq2 = P.sb("ssq2", [128, 2], F32)
    eps_t = P.sb("eps_t", [128, 1], F32)
    ones_b = P.sb("ones_b", [128, 128], BF16)
    wab = WQZ[:].rearrange("p c (b n) -> p b c n", b=4)[:, 0:2]
    wo_s = P.sb("wo_s", [128, 8, D], BF16)
    ps = [P.ps(f"ps{j}") for j in range(8)]
    ST = [0, 1]
    OLS = [(2, 3), (4, 5)]
    MISC = [6, 7]
    Vp3 = Vp[:].rearrange("p (t c) -> p t c", c=256)
    mT = kT0[:].rearrange("p (c t) -> p c t", c=8)
    ysl = kT1[:].rearrange("p (c t) -> p c t", c=8)
    wgs = Vp[:, 0:SA].rearrange("p (b c n) -> p b c n", b=2, c=8)
    assert SA // 8 >= 512 and SA // 16 >= 256

    rr = {'misc': 0, 'st': 0, 'pt': 0, 'wst': 0, 'wk': 0, 'ol': 0, 'ptc': 0, 'g4': 0}

    def kt_lo(hh, i):
        lo = 0
        while SL_HH[hh] * (512 * i - 128 * lo - 127) > 110.0:
            lo += 1
        return lo

    def MM(out, lhsT, rhs, start, stop, reads, writes, tp=None):
        if tp is None:
            P.op('tensor', lambda e: e.matmul(out, lhsT=lhsT, rhs=rhs, start=start, stop=stop), reads, writes)
        else:
            P.op('tensor', lambda e: e.matmul(out, lhsT=lhsT, rhs=rhs, start=start, stop=stop, tile_position=tp),
                 reads, writes)

    def TR(out, in_, ident, reads, writes):
        P.op('tensor', lambda e: e.transpose(out, in_, ident), reads, writes)

    def ACT(out, in_, func, reads, writes, bias=None, scale=1.0):
        if bias is None:
            P.op('scalar', lambda e: e.activation(out=out, in_=in_, func=func, scale=scale), reads, writes)
        else:
            P.op('scalar', lambda e: e.activation(out=out, in_=in_, func=func, bias=bias, scale=scale), reads, writes)

    def TT(eng, out, in0, in1, op, reads, writes):
        P.op(eng, lambda e: e.tensor_tensor(out=out, in0=in0, in1=in1, op=op), reads, writes)

    def TS(eng, out, in0, s1, s2, op0, op1, reads, writes):
        if op1 is None:
            P.op(eng, lambda e: e.tensor_scalar(out=out, in0=in0, scalar1=s1, scalar2=None, op0=op0), reads, writes)
        else:
            P.op(eng, lambda e: e.tensor_scalar(out=out, in0=in0, scalar1=s1, scalar2=s2, op0=op0, op1=op1),
                 reads, writes)

    def STT(out, in0, scalar, in1, op0, op1, reads, writes):
        P.op('vector', lambda e: e.scalar_tensor_tensor(out=out, in0=in0, scalar=scalar, in1=in1, op0=op0, op1=op1),
             reads, writes)

    def CP(eng, out, in_, reads, writes):
        P.op(eng, lambda e: e.tensor_copy(out=out, in_=in_), reads, writes)

    def RCP(eng, out, in_, reads, writes):
        ACT(out, in_, AF.Ln, reads, writes)
        ACT(out, out, AF.Exp, writes, writes, scale=-1.0)

    def SIG1P(ap, np_, key):
        ACT(ap, ap, AF.Ln, [key, 'ones1'], [key], bias=ones1[0:np_, 0:1], scale=1.0)
        ACT(ap, ap, AF.Exp, [key], [key], scale=-1.0)

    def MS(eng, ap, val, writes):
        P.op(eng, lambda e: e.memset(ap, val), (), writes)

    def pk(j):
        return ('ps', j)

    def misc_bank():
        j = MISC[rr['misc'] % 2]
        rr['misc'] += 1
        return j

    def next_wst():
        j = rr['wst'] % 2
        rr['wst'] += 1
        return j

    wcols = wbf_d.rearrange("(c p) n -> p c n", p=128)
    WBK = [('wbf', r) for r in range(8)]

    def load_w(dst, col0, ncols, key):
        P.dma('sync', dst, wcols[:, :, col0:col0 + ncols], reads=WBK, writes=[key])

    def dump(name, src, reads):
        if name in dbg_d:
            P.dma('sync', dbg_d[name], src, reads=reads, writes=['dbg_' + name])

    for k in cs:
        P.dma('sync', cs[k][:], cd[k], reads=(), writes=['c_' + k])
    CK = ['c_alb', 'c_clb']
    for r in range(8):
        P.dma('gpsimd', wbf_d[r * 128:(r + 1) * 128, :], win_d[r * 128:(r + 1) * 128, :], reads=(), writes=[('wbf', r)])
    P.dma('gpsimd', wabf_d[0], wa_d, reads=(), writes=['wabf0'])
    P.dma('gpsimd', wabf_d[1], wb_d, reads=(), writes=['wabf1'])
    P.dma('gpsimd', wo_s[:], wo_d.rearrange("(j p) n -> p j n", p=128), reads=(), writes=['wo'])
    P.dma('gpsimd', w2s[:], w2s_d, reads=(), writes=['w2s'])
    P.dma('gpsimd', pet[:], pet_d, reads=(), writes=['pet'])
    load_w(wg24[:], C_GA, 24, 'wg24')
    MS('vector', eps_t[:], 1e-6, ['eps'])
    MS('vector', ones_b[:], 1.0, ['ones'])
    for b_ in range(2):
        MS('vector', SGT[b_][:], 0.0, [('SGT', b_)])
    MS('vector', vcx[:], 1.0, ['vcx'])
    MS('vector', ones1[:], 1.0, ['ones1'])
    MS('vector', kcT[64:65, :], 1.0, ['kcT'])
    MS('vector', hcm[:], 0.0, ['hcm'])

    def adv(gen):
        if gen is not None:
            try:
                next(gen)
            except StopIteration:
                pass

    def exhaust(gen):
        if gen is not None:
            for _ in gen:
                pass

    def attend(tiles, gen=None, step=3):
        pend = []
        for t in tiles:
            sb_ = ST[rr['st'] % 2]
            rr['st'] += 1
            pj = rr['pt'] % 4
            rr['pt'] += 1
            n = len(t['smm'])
            for m, (lh, rh, rd) in enumerate(t['smm']):
                MM(ps[sb_][:, :], lh, rh, m == 0, m == n - 1, rd, [pk(sb_)])
            ACT(PT[pj][:], ps[sb_][:, :], AF.Exp, [pk(sb_)] + CK, [('PT', pj)], bias=t['bias'], scale=SCALE)
            pend.append((t['pv'], pj))
            rr['tc'] = rr.get('tc', 0) + 1
            if rr['tc'] % step == 0:
                adv(gen)
            if len(pend) > 2:
                pv_, pj_ = pend.pop(0)
                for (o_, lh, tp, st_, sp_, rd, wk) in pv_:
                    MM(o_, lh, PT[pj_][:], st_, sp_, rd + [('PT', pj_)], [wk], tp=tp)
        for pv_, pj_ in pend:
            for (o_, lh, tp, st_, sp_, rd, wk) in pv_:
                MM(o_, lh, PT[pj_][:], st_, sp_, rd + [('PT', pj_)], [wk], tp=tp)

    def proj_fm(wtile, wkey, wc0, ncol, blk, bank):
        for c in range(8):
            MM(ps[bank][0:ncol, :], wtile[:, c, wc0:wc0 + ncol], uT[:, c, blk * 512:(blk + 1) * 512],
               c == 0, c == 7, [wkey, ('uT', blk)], [pk(bank)])

    def silu_pair(bank, dst, dkey):
        w = WK[0]
        ACT(w[:], ps[bank][:, :], AF.Exp, [pk(bank)], [('WK', 0)], scale=-1.0)
        SIG1P(w[:], 128, ('WK', 0))
        TT('vector', dst, ps[bank][:, :], w[:], ALU.mult, [pk(bank), ('WK', 0)], [dkey])

    def combine_branch(acc, akey, first, gbank, banks):
        w = WK[1]
        for hl in range(2):
            r0 = 64 * hl
            TS('vector', w[r0:r0 + 64, :], ps[banks[hl]][64:128, :], EPSL, None, ALU.max, None, [pk(banks[hl])],
               [('WK', 1)])
        RCP('vector', w[:], w[:], [('WK', 1)], [('WK', 1)])
        if gbank is not None:
            TT('vector', w[:], ps[gbank][:, :], w[:], ALU.mult, [pk(gbank), ('WK', 1)], [('WK', 1)])
        dst = acc if first else WK[2]
        dkey = akey if first else ('WK', 2)
        for hl in range(2):
            r0 = 64 * hl
            TT('vector', dst[r0:r0 + 64, :], ps[banks[hl]][0:64, :], w[r0:r0 + 64, :], ALU.mult,
               [pk(banks[hl]), ('WK', 1)], [dkey])
        if not first:
            TT('vector', acc, acc, WK[2][:], ALU.add, [akey, ('WK', 2)], [akey])

    for s in range(NSEQ):
        P.dma('sync', gpre, npre_d, reads=(), writes=['impacc', 'WKg'])
        for tt in range(NT):
            b = tt % 2
            P.dma('sync', xt[b][:], x_d[s, tt * 128:(tt + 1) * 128, :], reads=(), writes=XK(b))
            TT('vector', xn[b][:], xt[b][:], xt[b][:], ALU.mult, XK(b), NK(b))
            P.op('vector', (lambda bb: lambda e: e.reduce_sum(out=ssq[:, bb:bb + 1], in_=xn[bb][:], axis=AX.X))(b),
                 NK(b), [('ssq', b)])
            ACT(ssq[:, b:b + 1], ssq[:, b:b + 1], AF.Ln, [('ssq', b), 'eps'], [('ssq', b)], bias=eps_t[:, 0:1],
                scale=1.0 / D)
            ACT(ssq[:, b:b + 1], ssq[:, b:b + 1], AF.Exp, [('ssq', b)], [('ssq', b)], scale=-0.5)
            STT(xn[b][:], xt[b][:], ssq[:, b:b + 1], gpre, ALU.mult, ALU.mult, XK(b) + [('ssq', b), 'impacc', 'WKg'],
                NK(b))
            for half in range(2):
                bk = misc_bank()
                for cc in range(4):
                    c = half * 4 + cc
                    TR(ps[bk][:, cc * 128:(cc + 1) * 128], xn[b][:, c * 128:(c + 1) * 128], cs['ident_f'][:],
                       NK(b) + ['c_ident_f'], [pk(bk)])
                CP('vector' if half == 0 else 'gpsimd' if False else 'vector',
                   uT[:, half * 4:(half + 1) * 4, tt * 128:(tt + 1) * 128],
                   ps[bk][:, :].rearrange("p (c t) -> p c t", c=4), [pk(bk)], [('uT', tt // 4)])
        if 'uT' in dbg_d and s == 0:
            CP('vector', WK[0][:], uT[:, 0, 0:512], [('uT', 0)], [('WK', 0)])
            dump('uT', WK[0][:], [('WK', 0)])

        ncm = (S - 32) // 16 + 1
        for g in range(2):
            wj = next_wst()
            load_w(wst[wj][:, :, 0:64], C_KV + 0 * 128 + g * 64, 64, ('wst', wj))
            load_w(wst[wj][:, :, 64:128], C_KV + 1 * 128 + g * 64, 64, ('wst', wj))
            wjb = next_wst()
            load_w(wst[wjb][:, :, 0:64], C_KV + 2 * 128 + g * 64, 64, ('wst', wjb))
            load_w(wst[wjb][:, :, 64:128], C_KV + 4 * 128 + g * 64, 64, ('wst', wjb))
            W1 = Vp[:, 0:4096].rearrange("p (l e) -> p l e", e=128)
            P.dma('gpsimd', W1, w1s_d, reads=(), writes=['Vp'])
            for blk in range(NQT):
                bk = misc_bank()
                proj_fm(wst[wj], ('wst', wj), 0, 128, blk, bk)
                CP('vector', kT1[:, blk * 512:(blk + 1) * 512], ps[bk][:, :], [pk(bk)], ['kT1'])
            wjc = next_wst()
            load_w(wst[wjc][:, :, 0:64], C_KV + 3 * 128 + g * 64, 64, ('wst', wjc))
            load_w(wst[wjc][:, :, 64:128], C_KV + 5 * 128 + g * 64, 64, ('wst', wjc))
            load_w(WQZ[:, :, 0:256], C_QA + g * 256, 256, 'WQZ')
            load_w(WQZ[:, :, 256:512], C_ZA + g * 256, 256, 'WQZ')
            for wh in range(2):
                r0 = 64 * wh
                bk = misc_bank()
                for l in range(32):
                    MM(ps[bk][:, 0:ncm], W1[r0:r0 + 64, l, :], kT1[r0:r0 + 64, l:l + 16 * (ncm - 1) + 1:16],
                       l == 0, l == 31, ['Vp', 'kT1'], [pk(bk)])
                bk2 = misc_bank()
                for l in range(32):
                    MM(ps[bk2][:, 0:1], W1[r0:r0 + 64, l, :], pet[r0:r0 + 64, l:l + 1], l == 0, l == 31,
                       ['Vp', 'pet'], [pk(bk2)])
                CP('vector', hb[:, 0:1], ps[bk2][:, 0:1], [pk(bk2)], ['hb'])
                TS('vector', hb[:, 1:2], hb[:, 0:1], -1.0, None, ALU.mult, None, ['hb'], ['hb'])
                w = WK[0]
                ACT(w[:, 0:ncm], ps[bk][:, 0:ncm], AF.Exp, [pk(bk), 'hb'], [('WK', 0)], bias=hb[:, 1:2], scale=-1.0)
                SIG1P(w[:, 0:ncm], 128, ('WK', 0))
                STT(hcm[:, wh, 0:ncm], ps[bk][:, 0:ncm], hb[:, 0:1], w[:, 0:ncm], ALU.add, ALU.mult,
                    [pk(bk), 'hb', ('WK', 0)], ['hcm'])
                if wh == 0:
                    bk3 = misc_bank()
                    MM(ps[bk3][0:64, 0:256], w2s[:, 0, :], hcm[:, 0, :], True, True, ['w2s', 'hcm'], [pk(bk3)])
                    CP('vector', kcT[0:64, :], ps[bk3][0:64, 0:256], [pk(bk3)], ['kcT'])
                else:
                    bk3 = misc_bank()
                    for a in range(2):
                        MM(ps[bk3][:, a * 64:(a + 1) * 64], hcm[:, 1, a * 128:(a + 1) * 128], w2s[:, 1, :], True, True,
                           ['w2s', 'hcm'], [pk(bk3)])
                    CP('vector', vcx[:, :, 0:64], ps[bk3][:, 0:128].rearrange("p (a d) -> p a d", a=2), [pk(bk3)],
                       ['vcx'])
            wj = wjb
            for blk in range(NQT):
                bk = misc_bank()
                proj_fm(wst[wj], ('wst', wj), 0, 128, blk, bk)
                CP('vector', kT0[0:64, blk * 512:(blk + 1) * 512], ps[bk][0:64, :], [pk(bk)], ['kT0'])
                CP('vector', kT1[0:64, blk * 512:(blk + 1) * 512], ps[bk][64:128, :], [pk(bk)], ['kT1'])
            P.dma('sync', kT0[64:128, 0:S], cd['eslc'][:, 0:S], reads=(), writes=['kT0'])
            MS('vector', kT1[64:128, :], 0.0, ['kT1'])
            MS('vector', kT1[64:65, :], 1.0, ['kT1'])
            for h4 in range(4):
                for b2 in range(2):
                    P.dma('sync', QA[b2][h4][64:65, :], cd['cq'][4 * g + h4:4 * g + h4 + 1, :], reads=(),
                          writes=[('QA', b2, h4)])

            def nsa_pre(g, i):
                b2 = i % 2
                for pp in range(2):
                    bk = misc_bank()
                    proj_fm(WQZ, 'WQZ', pp * 128, 128, i, bk)
                    CP('vector', QA[b2][2 * pp][0:64, :], ps[bk][0:64, :], [pk(bk)], [('QA', b2, 2 * pp)])
                    CP('vector', QA[b2][2 * pp + 1][0:64, :], ps[bk][64:128, :], [pk(bk)], [('QA', b2, 2 * pp + 1)])
                    yield
                bk = misc_bank()
                for c in range(8):
                    MM(ps[bk][0:24, :], wg24[:, c, :], uT[:, c, i * 512:(i + 1) * 512], c == 0, c == 7,
                       ['wg24', ('uT', i)], [pk(bk)])
                w = WK[0]
                ACT(w[0:24, :], ps[bk][0:24, :], AF.Exp, [pk(bk)], [('WK', 0)], scale=-1.0)
                SIG1P(w[0:24, :], 24, ('WK', 0))
                CP('vector', SGT[b2][0:24, :], w[0:24, :], [('WK', 0)], [('SGT', b2)])
                TT('vector', SGT[b2][32:56, :], w[0:24, :], SGT[b2][0:24, :], ALU.subtract, [('WK', 0), ('SGT', b2)],
                   [('SGT', b2)])
                yield
                a_list = [0] if (512 * i + 511) < (2048 + 31) else [0, 1]
                a_list = [a for a in a_list if a * 128 < ncm]
                for a in a_list:
                    thr = 512.0 * i - 2048.0 * a
                    TS('vector', XM[a][:], cs['xg'][:], thr, NEG, ALU.is_gt, ALU.mult, ['c_xg'], [('XM', a)])
                la = len(a_list)
                for h4 in range(4):
                    hl = h4 % 2
                    pp = h4 // 2
                    ha = 4 * g + h4
                    if hl == 0:
                        gb = misc_bank()
                        MM(ps[gb][:, :], cs['goh'][0:56, (g * 2 + pp) * 3 + 0, :], SGT[b2][0:56, :], True, True,
                           ['c_goh', ('SGT', b2)], [pk(gb)])
                        CP('vector', WKg[:], ps[gb][:, :], [pk(gb)], ['WKg'])
                    bkA = misc_bank()
                    bkB = misc_bank()
                    for ai, a in enumerate(a_list):
                        sb_ = ST[rr['st'] % 2]
                        rr['st'] += 1
                        pj = 4 + rr['ptc'] % 2
                        rr['ptc'] += 1
                        MM(ps[sb_][:, :], kcT[0:65, a * 128:(a + 1) * 128], QA[b2][h4][0:65, :], True, False,
                           ['kcT', ('QA', b2, h4)], [pk(sb_)])
                        MM(ps[sb_][:, :], cs['ident_b'][:], XM[a][:], False, True, ['c_ident_b', ('XM', a)], [pk(sb_)])
                        ACT(PT[pj][:], ps[sb_][:, :], AF.Exp, [pk(sb_)] + CK, [('PT', pj)],
                            bias=cs['clb'][:, ha, a, i:i + 1], scale=SCALE)
                        MM(ps[bkA][:, :], vcx[:, a, :], PT[pj][:], ai == 0, ai == la - 1, ['vcx', ('PT', pj)], [pk(bkA)])
                        MM(ps[bkB][0:64, :], cs['ov'][:, a, :], PT[pj][:], ai == 0, ai == la - 1, ['c_ov', ('PT', pj)],
                           [pk(bkB)])
                    wr = WK[3]
                    TS('vector', wr[0:64, :], ps[bkA][64:128, :], EPSL, None, ALU.max, None, [pk(bkA)], [('WK', 3)])
                    TS('vector', wr[64:128, :], ps[bkA][64:128, :], EPSL, None, ALU.max, None, [pk(bkA)], [('WK', 3)])
                    RCP('vector', wr[:, :], wr[:, :], [('WK', 3)], [('WK', 3)])
                    if h4 == 0:
                        TT('vector', impacc[0:64, :], ps[bkB][0:64, :], wr[0:64, :], ALU.mult, [pk(bkB), ('WK', 3)],
                           ['impacc'])
                    else:
                        TT('vector', WK[4][0:64, :], ps[bkB][0:64, :], wr[0:64, :], ALU.mult, [pk(bkB), ('WK', 3)],
                           [('WK', 4)])
                        TT('vector', impacc[0:64, :], impacc[0:64, :], WK[4][0:64, :], ALU.add, ['impacc', ('WK', 4)],
                           ['impacc'])
                    r0 = 64 * hl
                    TT('vector', WK[1][r0:r0 + 64, :], WKg[r0:r0 + 64, :], wr[r0:r0 + 64, :], ALU.mult, ['WKg', ('WK', 3)],
                       [('WK', 1)])
                    TT('vector', ACC[b2][pp][r0:r0 + 64, :], ps[bkA][0:64, :], WK[1][r0:r0 + 64, :], ALU.mult,
                       [pk(bkA), ('WK', 1)], [('xt', pp, b2)])
                    yield
                bk = misc_bank()
                for qs in range(4):
                    TR(ps[bk][:, qs * 64:(qs + 1) * 64], impacc[0:64, qs * 128:(qs + 1) * 128], cs['ident_f'][0:64, 0:64],
                       ['impacc', 'c_ident_f'], [pk(bk)])
                for qs in range(4):
                    ti = 4 * i + qs
                    TT('vector', T1[:, qs, :], ps[bk][:, qs * 64:(qs + 1) * 64],
                       cs['abase'][:, 64 - 2 * ti:128 - 2 * ti], ALU.add, [pk(bk), 'c_abase'], ['T1'])
                MS('vector', T1[:, :, 0:1], BIGF, ['T1'])
                yield
                for qs in range(4):
                    P.op('vector', (lambda q_: lambda e: e.max(out=M8a[:], in_=T1[:, q_, :]))(qs), ['T1'], ['M8a'])
                    P.op('vector', (lambda q_: lambda e: e.match_replace(out=T2[:, q_, :], in_to_replace=M8a[:],
                                                                        in_values=T1[:, q_, :], imm_value=-1e9))(qs),
                         ['T1', 'M8a'], ['T2'])
                    P.op('vector', (lambda q_: lambda e: e.max(out=M8b[:], in_=T2[:, q_, :]))(qs), ['T2'], ['M8b'])
                    TS('vector', T2[:, qs, :], T1[:, qs, :], M8b[:, 7:8], NEG, ALU.is_lt, ALU.mult, ['T1', 'M8b'], ['T2'])
                    yield
                for pp in range(2):
                    bk = misc_bank()
                    proj_fm(WQZ, 'WQZ', 256 + pp * 128, 128, i, bk)
                    silu_pair(bk, SZ[b2][pp], ('xn', pp, b2))
                    yield
                yield
                bk = misc_bank()
                for qs in range(4):
                    TR(ps[bk][0:64, qs * 128:(qs + 1) * 128], T2[:, qs, :], cs['ident_f'][:], ['T2', 'c_ident_f'],
                       [pk(bk)])
                for h4 in range(4):
                    STT(QA[b2][h4][64:128, :], cs['cqb'][64:128, :], -SL_HH[4 * g + h4] / SCALE, ps[bk][0:64, :],
                        ALU.mult, ALU.add, [pk(bk), 'c_cqb'], [('QA', b2, h4)])
                yield

            gen0 = nsa_pre(g, 0)
            wj = wjc
            MS('vector', Vp3[:, :, :].rearrange("p t (h c) -> p t h c", h=2)[:, :, :, 64:128], 1.0, ['Vp'])
            for t4 in range(NT // 4):
                bk = misc_bank()
                for q4 in range(4):
                    tt = t4 * 4 + q4
                    for c in range(8):
                        MM(ps[bk][:, q4 * 128:(q4 + 1) * 128], uT[:, c, tt * 128:(tt + 1) * 128], wst[wj][:, c, :],
                           c == 0, c == 7, [('wst', wj), ('uT', t4)], [pk(bk)])
                CP('vector', Vp3[:, t4 * 4:(t4 + 1) * 4, :].rearrange("p t (h c) -> p t h c", h=2)[:, :, :, 0:64],
                   ps[bk][:, :].rearrange("p (t h c) -> p t h c", h=2, c=64), [pk(bk)], ['Vp'])
                adv(gen0)
                adv(gen0)
            exhaust(gen0)
            for i in range(NQT):
                b2 = i % 2
                gen = nsa_pre(g, i + 1) if i + 1 < NQT else None
                for pp in range(2):
                    for br in (1, 2):
                        tiles = []
                        OB, LB = OLS[rr['ol'] % 2]
                        rr['ol'] += 1
                        if br == 1:
                            kts = list(range(0, 4 * i + 4))
                        else:
                            kts = [kt for kt in range(4 * i - 4, 4 * i + 4) if kt >= 0]
                        for kt in kts:
                            for hl in range(2):
                                h4 = 2 * pp + hl
                                ha = 4 * g + h4
                                hk = [k_ for k_ in kts if k_ >= kt_lo(ha, i)]
                                if kt not in hk:
                                    continue
                                ki = hk.index(kt)
                                nk = len(hk)
                                kT = kT0 if br == 1 else kT1
                                kkey = 'kT0' if br == 1 else 'kT1'
                                smm = [(kT[:, kt * 128:(kt + 1) * 128], QA[b2][h4][:, :], [kkey, ('QA', b2, h4)])]
                                if br == 1:
                                    if kt >= 4 * i:
                                        r = kt - 4 * i
                                        smm.append((cs['ident_b'][:], cs['cmb'][:, 384 - 128 * r:896 - 128 * r],
                                                    ['c_ident_b', 'c_cmb']))
                                else:
                                    r = kt - (4 * i - 4)
                                    if r < 4:
                                        smm.append((cs['ident_b'][:], cs['omb'][:, 384 - 128 * r:896 - 128 * r],
                                                    ['c_ident_b', 'c_omb']))
                                    else:
                                        r -= 4
                                        smm.append((cs['ident_b'][:], cs['cmb'][:, 384 - 128 * r:896 - 128 * r],
                                                    ['c_ident_b', 'c_cmb']))
                                vcol = 0 if br == 1 else 128
                                bnk = (OB, LB)[hl]
                                pv = [(ps[bnk][:, :], Vp3[:, kt, vcol:vcol + 128], None,
                                       ki == 0, ki == nk - 1, ['Vp'], pk(bnk))]
                                tiles.append(dict(smm=smm, bias=cs['alb'][:, ha, kt - 4 * i + 32:kt - 4 * i + 33], pv=pv))
                        attend(tiles, gen)
                        gb = misc_bank()
                        MM(ps[gb][:, :], cs['goh'][0:56, (g * 2 + pp) * 3 + br, :], SGT[b2][0:56, :], True, True,
                           ['c_goh', ('SGT', b2)], [pk(gb)])
                        combine_branch(ACC[b2][pp], ('xt', pp, b2), False, gb, (OB, LB))
                    yj = rr['wk'] % 2
                    rr['wk'] += 1
                    TT('vector', YST[yj][:], ACC[b2][pp], SZ[b2][pp], ALU.mult, [('xt', pp, b2), ('xn', pp, b2)],
                       [('YST', yj)])
                    P.dma('gpsimd', ysc_d[g * 2 + pp, :, i * 512:(i + 1) * 512], YST[yj][:], reads=[('YST', yj)],
                          writes=[('ysc', g * 2 + pp, i)])
                exhaust(gen)

        nb = S // 256
        for j in range(4):
            wj = next_wst()
            load_w(wst[wj][:], C_QB + 512 + j * 128, 128, ('wst', wj))
            wjv = next_wst()
            load_w(wst[wjv][:], C_QB + 1024 + j * 128, 128, ('wst', wjv))
            load_w(WQZ[:, :, 0:128], C_QB + j * 128, 128, 'WQZ')
            load_w(WQZ[:, :, 128:256], C_ZB + j * 128, 128, 'WQZ')
            for blk in range(NQT):
                bk = misc_bank()
                proj_fm(wst[wj], ('wst', wj), 0, 128, blk, bk)
                CP('vector', kT0[0:64, blk * 512:(blk + 1) * 512], ps[bk][0:64, :], [pk(bk)], ['kT0'])
                CP('vector', kT1[0:64, blk * 512:(blk + 1) * 512], ps[bk][64:128, :], [pk(bk)], ['kT1'])
                P.op('vector', (lambda bk_, blk_: lambda e: e.reduce_sum(
                    out=kbarf[:, 2 * blk_:2 * blk_ + 2], in_=ps[bk_][:, :].rearrange("p (b t) -> p b t", b=2),
                    axis=AX.X))(bk, blk), [pk(bk)], ['kbarf'])
            for kT_, kk_ in (((kT0, 'kT0'), (kT1, 'kT1')) if j == 0 else ()):
                MS('vector', kT_[64:128, :], 0.0, [kk_])
                MS('vector', kT_[64:65, :], 1.0, [kk_])
                P.dma('sync', kT_[96:112, 0:S], cd['emoba'][:, 0:S], reads=(), writes=[kk_])
            TS('vector', kbar[0:64, 0, 0:nb], kbarf[0:64, 0:nb], 1.0 / 256, None, ALU.mult, None, ['kbarf'], ['kbar'])
            TS('vector', kbar[0:64, 1, 0:nb], kbarf[64:128, 0:nb], 1.0 / 256, None, ALU.mult, None, ['kbarf'], ['kbar'])
            for hl in range(2):
                for b2 in range(2):
                    if j == 0:
                        MS('vector', QA[b2][hl][64:128, :], 0.0, [('QA', b2, hl)])
                    P.dma('sync', QA[b2][hl][64:65, :], cd['cq'][8 + 2 * j + hl:8 + 2 * j + hl + 1, :], reads=(),
                          writes=[('QA', b2, hl)])

            def moba_pre(j, i):
                b2 = i % 2
                bk = misc_bank()
                proj_fm(WQZ, 'WQZ', 0, 128, i, bk)
                CP('vector', QA[b2][0][0:64, :], ps[bk][0:64, :], [pk(bk)], [('QA', b2, 0)])
                CP('vector', QA[b2][1][0:64, :], ps[bk][64:128, :], [pk(bk)], [('QA', b2, 1)])
                yield
                bk = misc_bank()
                for hl in range(2):
                    for qs in range(4):
                        MM(ps[bk][:, (hl * 4 + qs) * 16:(hl * 4 + qs) * 16 + nb], QA[b2][hl][0:64, qs * 128:(qs + 1) * 128],
                           kbar[0:64, hl, 0:nb], True, True, ['kbar', ('QA', b2, hl)], [pk(bk)])
                for hl in range(2):
                    for qs in range(4):
                        cur = (4 * i + qs) // 2
                        e8 = hl * 4 + qs
                        TT('vector', GT[:, e8, 0:nb], ps[bk][:, e8 * 16:e8 * 16 + nb], cs['bbase'][:, 16 - cur:16 - cur + nb],
                           ALU.add, [pk(bk), 'c_bbase'], ['GT'])
                        if nb < 16:
                            MS('vector', GT[:, e8, nb:16], -BIGF, ['GT'])
                yield
                for hl in range(2):
                    for qs in range(4):
                        cur = (4 * i + qs) // 2
                        e8 = hl * 4 + qs
                        P.op('vector', (lambda e_: lambda e: e.max(out=M8a[:], in_=GT[:, e_, :]))(e8), ['GT'], ['M8a'])
                        TS('vector', GT[:, e8, :], GT[:, e8, :], M8a[:, 2:3], NEG, ALU.is_lt, ALU.mult, ['GT', 'M8a'], ['GT'])
                        TT('vector', GT[:, e8, :], GT[:, e8, :], cs['obase'][:, 16 - cur:32 - cur], ALU.max,
                           ['GT', 'c_obase'], ['GT'])
                        yield
                bk = misc_bank()
                proj_fm(WQZ, 'WQZ', 128, 128, i, bk)
                silu_pair(bk, SZ[b2][0], ('xn', 0, b2))
                yield
                yield
                for hl in range(2):
                    bk = misc_bank()
                    for qs in range(4):
                        TR(ps[bk][0:16, qs * 128:(qs + 1) * 128], GT[:, hl * 4 + qs, :], cs['ident_f'][:],
                           ['GT', 'c_ident_f'], [pk(bk)])
                    CP('vector', QA[b2][hl][96:112, :], ps[bk][0:16, :], [pk(bk)], [('QA', b2, hl)])
                    yield

            gen0 = moba_pre(j, 0)
            wj = wjv
            MS('vector', Vp3[:, :, :].rearrange("p t (h c) -> p t h c", h=2)[:, :, :, 64:128], 1.0, ['Vp'])
            for t4 in range(NT // 4):
                bk = misc_bank()
                for q4 in range(4):
                    tt = t4 * 4 + q4
                    for c in range(8):
                        MM(ps[bk][:, q4 * 128:(q4 + 1) * 128], uT[:, c, tt * 128:(tt + 1) * 128], wst[wj][:, c, :],
                           c == 0, c == 7, [('wst', wj), ('uT', t4)], [pk(bk)])
                CP('vector', Vp3[:, t4 * 4:(t4 + 1) * 4, :].rearrange("p t (h c) -> p t h c", h=2)[:, :, :, 0:64],
                   ps[bk][:, :].rearrange("p (t h c) -> p t h c", h=2, c=64), [pk(bk)], ['Vp'])
                adv(gen0)
            exhaust(gen0)
            for i in range(NQT):
                b2 = i % 2
                gen = moba_pre(j, i + 1) if i + 1 < NQT else None
                tiles = []
                OB, LB = OLS[rr['ol'] % 2]
                rr['ol'] += 1
                kts = list(range(0, 4 * i + 4))
                for kt in kts:
                    for hl in range(2):
                        hh = 8 + 2 * j + hl
                        hk = [k_ for k_ in kts if k_ >= kt_lo(hh, i)]
                        if kt not in hk:
                            continue
                        ki = hk.index(kt)
                        nk = len(hk)
                        kT = kT0 if hl == 0 else kT1
                        kkey = 'kT0' if hl == 0 else 'kT1'
                        smm = [(kT[:, kt * 128:(kt + 1) * 128], QA[b2][hl][:, :], [kkey, ('QA', b2, hl)])]
                        if kt >= 4 * i:
                            smm.append((cs['ident_b'][:], cs['mmb'][:, kt - 4 * i, :], ['c_ident_b', 'c_mmb']))
                        bnk = (OB, LB)[hl]
                        pv = [(ps[bnk][:, :], Vp3[:, kt, 128 * hl:128 * hl + 128], None,
                               ki == 0, ki == nk - 1, ['Vp'], pk(bnk))]
                        tiles.append(dict(smm=smm, bias=cs['alb'][:, hh, kt - 4 * i + 32:kt - 4 * i + 33], pv=pv))
                attend(tiles, gen, step=2)
                combine_branch(ACC[b2][0], ('xt', 0, b2), True, None, (OB, LB))
                yj = rr['wk'] % 2
                rr['wk'] += 1
                TT('vector', YST[yj][:], ACC[b2][0], SZ[b2][0], ALU.mult, [('xt', 0, b2), ('xn', 0, b2)], [('YST', yj)])
                P.dma('gpsimd', ysc_d[4 + j, :, i * 512:(i + 1) * 512], YST[yj][:], reads=[('YST', yj)],
                      writes=[('ysc', 4 + j, i)])
                exhaust(gen)

        P.op('sync', lambda e: e.nop(), (), ['Vp', ('wgs', 0), ('wgs', 1), 'WQZ', ('wab', 0), ('wab', 1), 'kT1', ('ysl', 0), ('ysl', 1)])
        P.dma('sync', gpost, npost_d, reads=(), writes=['impacc', 'WKg'])
        ysl2 = [ysl, Vp[:, SA:2 * SA].rearrange("p (c t) -> p c t", c=8)]

        def load_ysl(i_):
            P.dma('sync', ysl2[i_ % 2][:, :, 0:512], ysc_d[:, :, i_ * 512:(i_ + 1) * 512].rearrange("c p t -> p c t"),
                  reads=[('ysc', jj, i_) for jj in range(8)], writes=[('ysl', i_ % 2)])
        load_ysl(0)
        for i in range(NQT):
            if i + 1 < NQT:
                load_ysl(i + 1)
            def load_fc(fc_):
                gj_ = fc_ % 2
                P.dma('sync', wgs[:, gj_, :, 0:128], wcols[:, :, C_MG + fc_ * 128:C_MG + (fc_ + 1) * 128], reads=WBK,
                      writes=[('wgs', gj_)])
                P.dma('sync', wgs[:, gj_, :, 128:256],
                      wcols[:, :, C_MG + 1024 + fc_ * 128:C_MG + 1024 + (fc_ + 1) * 128], reads=WBK, writes=[('wgs', gj_)])
                for ab_ in range(2):
                    P.dma('sync', wab[:, gj_, 4 * ab_:4 * ab_ + 4, :],
                          wabf_d[ab_].rearrange("(j p) n -> p j n", p=128)[:, :, fc_ * 128:(fc_ + 1) * 128],
                          reads=['wabf0', 'wabf1'], writes=[('wab', gj_)])
            if i == 0:
                load_fc(0)
                load_fc(1)
            for fc in range(8):
                gj = fc % 2
                for ab in range(2):
                    bg = [6, 7, 0, 1][rr['g4'] % 4]
                    rr['g4'] += 1
                    for c in range(8):
                        MM(ps[bg][:, :], wgs[:, gj, c, ab * 128:(ab + 1) * 128], uT[:, c, i * 512:(i + 1) * 512],
                           c == 0, c == 7, [('wgs', gj), ('uT', i)], [pk(bg)])
                    w = WK[ab]
                    ACT(w[:], ps[bg][:, :], AF.Sigmoid, [pk(bg)], [('WK', ab)])
                    bm = OLS[fc % 2][ab]
                    for jj in range(4):
                        MM(ps[bm][:, :], wab[:, gj, 4 * ab + jj, :], ysl2[i % 2][:, 4 * ab + jj, 0:512], jj == 0, jj == 3,
                           [('wab', gj), ('ysl', i % 2)], [pk(bm)])
                    TT('vector', w[:], ps[bm][:, :], w[:], ALU.mult, [pk(bm), ('WK', ab)], [('WK', ab)])
                TT('vector', mT[:, fc, 0:512], WK[0][:], WK[1][:], ALU.add, [('WK', 0), ('WK', 1)], ['kT0'])
                if fc + 2 < 8:
                    load_fc(fc + 2)
                elif i + 1 < NQT:
                    load_fc(fc + 2 - 8)
            for ts_ in range(4):
                tt = i * 4 + ts_
                b = tt % 2
                P.dma('sync', xt[b][:], x_d[s, tt * 128:(tt + 1) * 128, :], reads=(), writes=XK(b))
                banks = [ST[0], ST[1]]
                for hf in range(2):
                    for fc in range(8):
                        MM(ps[banks[hf]][:, :], mT[:, fc, ts_ * 128:(ts_ + 1) * 128], wo_s[:, fc, hf * 512:(hf + 1) * 512],
                           fc == 0, fc == 7, ['wo', 'kT0'], [pk(banks[hf])])
                for hf in range(2):
                    P.op('scalar', (lambda o_, i_: lambda e: e.copy(out=o_, in_=i_))(WK[2 + hf][:], ps[banks[hf]][:, :]),
                         [pk(banks[hf])], [('WK', 2 + hf)])
                    P.op('vector', (lambda o_, i_, a_: lambda e: e.scalar_tensor_tensor(
                        out=o_, in0=i_, scalar=1.0, in1=i_, op0=ALU.mult, op1=ALU.mult, accum_out=a_))(
                        YST[hf][:], WK[2 + hf][:], ssq2[:, hf:hf + 1]), [('WK', 2 + hf)], [('YST', hf), ('ssq2', hf)])
                TT('vector', ssq[:, b:b + 1], ssq2[:, 0:1], ssq2[:, 1:2], ALU.add, [('ssq2', 0), ('ssq2', 1)], [('ssq', b)])
                ACT(ssq[:, b:b + 1], ssq[:, b:b + 1], AF.Ln, [('ssq', b), 'eps'], [('ssq', b)], bias=eps_t[:, 0:1],
                    scale=1.0 / D)
                ACT(ssq[:, b:b + 1], ssq[:, b:b + 1], AF.Exp, [('ssq', b)], [('ssq', b)], scale=-0.5)
                for hf in range(2):
                    STT(xn[b][:, hf * 512:(hf + 1) * 512], WK[2 + hf][:], ssq[:, b:b + 1],
                        gpost[:, hf * 512:(hf + 1) * 512], ALU.mult, ALU.mult, [('WK', 2 + hf), ('ssq', b), 'impacc', 'WKg'],
                        [('xn', b, hf)])
                TT('gpsimd', xn[b][:], xn[b][:], xt[b][:], ALU.add, NK(b) + XK(b), NK(b))
                P.dma('gpsimd', out_d[s, tt * 128:(tt + 1) * 128, :], xn[b][:], reads=NK(b), writes=[('out', tt)])
        P.op('sync', lambda e: e.nop(), (), ['Vp', ('wgs', 0), ('wgs', 1), 'WQZ', ('wab', 0), ('wab', 1), 'kT1', ('ysl', 0), ('ysl', 1)])
    P.finish()
    return nc, P


def host_inputs(inputs, S):
    c = make_consts()
    f = lambda a: np.ascontiguousarray(np.asarray(a, dtype=np.float32))
    w1k = f(inputs['cmp_w1_k'])[0].transpose(1, 0, 2)
    w1v = f(inputs['cmp_w1_v'])[0].transpose(1, 0, 2)
    shared = {
        'w_in': f(inputs['w_in'])[0],
        'w1s': np.ascontiguousarray(np.concatenate([w1k, w1v], 0)),
        'w2s': np.ascontiguousarray(np.stack([f(inputs['cmp_w2_k'])[0], f(inputs['cmp_w2_v'])[0]], 1)),
        'pet': np.ascontiguousarray(np.concatenate([f(inputs['cmp_pe_k'])[0].T, f(inputs['cmp_pe_v'])[0].T], 0)),
        'w_a': f(inputs['w_branch_a'])[0],
        'w_b': f(inputs['w_branch_b'])[0],
        'w_o': f(inputs['w_o'])[0],
        'npre_bc': np.ascontiguousarray(np.broadcast_to(f(inputs['norm_pre'])[0][None, :], (128, D))),
        'npost_bc': np.ascontiguousarray(np.broadcast_to(f(inputs['norm_post'])[0][None, :], (128, D))),
    }
    for k, v in c.items():
        shared['c_' + k] = v
    return shared


def kernel(**inputs):
    x = np.ascontiguousarray(np.asarray(inputs['x'], dtype=np.float32))
    B, S, _ = x.shape
    ncores = 8
    nseq = B // ncores
    nc, _ = build(S, nseq)
    shared = host_inputs(inputs, S)
    in_maps = []
    for c in range(ncores):
        m = dict(shared)
        m['x'] = np.ascontiguousarray(x[c * nseq:(c + 1) * nseq])
        in_maps.append(m)
    res = run_bass_kernel_spmd(nc, in_maps, core_ids=list(range(ncores)))
    return np.concatenate([np.asarray(r['out']) for r in res.results], axis=0).astype(np.float32)
```

```python
import contextlib
import numpy as np
import ml_dtypes
import concourse.bass as bass
import concourse.mybir as mybir
from concourse.bass_utils import run_bass_kernel_spmd

F32 = mybir.dt.float32
BF16 = mybir.dt.bfloat16
ALU = mybir.AluOpType
AF = mybir.ActivationFunctionType
AX = mybir.AxisListType
NPBF = ml_dtypes.bfloat16

D = 1024
DEBUG_SEP = False
NCOL = 5912
NEG = -30000.0
SCALE = 0.125
BIGF = 1.0e4
EPSL = 1.0e-18
C_QA, C_KV, C_GA, C_ZA, C_QB, C_ZB, C_MG = 0, 512, 1280, 1304, 1816, 3352, 3864
SLOPES = [2.0 ** (-(i + 1) / 2.0) for i in range(16)]
SL_HH = [SLOPES[2 * h] for h in range(8)] + [SLOPES[2 * h + 1] for h in range(8)]


class Prog:
    EPOCH = 20000
    NDMA = 8

    def __init__(self, nc):
        self.nc = nc
        self.ops = []
        self.lastw = {}
        self.readers = {}
        self.stack = contextlib.ExitStack()
        self.sb_bytes = 0

    def sb(self, name, shape, dtype):
        n = 1
        for s in shape[1:]:
            n *= s
        self.sb_bytes += n * (4 if dtype == F32 else 2)
        return self.stack.enter_context(self.nc.sbuf_tensor('sb_' + name, list(shape), dtype))

    def ps(self, name, shape=(128, 512), dtype=F32):
        return self.stack.enter_context(self.nc.psum_tensor(name, list(shape), dtype))

    def _deps(self, idx, reads, writes):
        deps = set()
        for k in reads:
            w = self.lastw.get(k)
            if w is not None:
                deps.add(w)
        for k in writes:
            w = self.lastw.get(k)
            if w is not None:
                deps.add(w)
            for r in self.readers.get(k, ()):
                deps.add(r)
        for k in reads:
            self.readers.setdefault(k, []).append(idx)
        for k in writes:
            self.lastw[k] = idx
            self.readers[k] = []
        deps.discard(idx)
        return deps

    def op(self, eng, fn, reads=(), writes=()):
        idx = len(self.ops)
        deps = self._deps(idx, reads, writes)
        self.ops.append(dict(eng=eng, fn=fn, deps=deps, dma=False, wkeys=set(writes), rkeys=set(reads)))
        return idx

    def dma(self, eng, out, in_, reads=(), writes=()):
        idx = len(self.ops)
        deps = self._deps(idx, reads, writes)
        self.ops.append(dict(eng=eng, fn=lambda e: e.dma_start(out=out, in_=in_), deps=deps, dma=True,
                             wkeys=set(writes), rkeys=set(reads)))
        return idx

    def finish(self):
        nc = self.nc
        ops = self.ops
        needed = set()
        for i, o in enumerate(ops):
            nd = set()
            for d in o['deps']:
                od = ops[d]
                if od['eng'] == o['eng'] and not od['dma'] and not o['dma']:
                    if o['eng'] == 'tensor':
                        continue
                    if not ((od['wkeys'] & o['rkeys']) or (od['wkeys'] & o['wkeys'])):
                        continue
                nd.add(d)
            o['deps'] = nd
            for d in nd:
                if not ops[d]['dma']:
                    needed.add(d)
        engs = ['tensor', 'vector', 'scalar', 'gpsimd', 'sync']
        cnt = {e: 0 for e in engs}
        dcnt = {e: 0 for e in engs}
        nep = {e: 0 for e in engs}
        for i, o in enumerate(ops):
            e = o['eng']
            if o['dma']:
                d = dcnt[e]
                dcnt[e] += 1
                o['sig'] = (('dma', e, d % self.NDMA), 16 * (d // self.NDMA + 1), 16)
                o['prev'] = (('dma', e, d % self.NDMA), 16 * (d // self.NDMA)) if d >= self.NDMA else None
            elif i in needed:
                c = cnt[e]
                cnt[e] += 1
                ep = c // self.EPOCH
                nep[e] = max(nep[e], ep + 1)
                o['sig'] = (('cmp', e, ep), c % self.EPOCH + 1, 1)
            else:
                o['sig'] = None
        sems = {}
        for e in engs:
            for ep in range(nep[e]):
                sems[('cmp', e, ep)] = self.stack.enter_context(nc.semaphore(f"s_{e}_{ep}"))
            for j in range(min(self.NDMA, dcnt[e])):
                sems[('dma', e, j)] = self.stack.enter_context(nc.semaphore(f"d_{e}_{j}"))
        self.n_instr = {e: 0 for e in engs}
        with nc.Block() as block:
            def make(ename):
                def body(eng):
                    waited = {}
                    lastdma = {}
                    for o in ops:
                        if o['eng'] != ename:
                            continue
                        best = {}
                        for d in o['deps']:
                            s = ops[d]['sig']
                            if s[1] > best.get(s[0], 0):
                                best[s[0]] = s[1]
                        if o['dma'] and o['prev'] is not None:
                            k, v = o['prev']
                            if v > best.get(k, 0):
                                best[k] = v
                        for k, v in best.items():
                            if waited.get(k, 0) >= v:
                                continue
                            eng.wait_ge(sems[k], v)
                            waited[k] = v
                            self.n_instr[ename] += 1
                        ins = o['fn'](eng)
                        self.n_instr[ename] += 1
                        if o['sig'] is not None:
                            ins.then_inc(sems[o['sig'][0]], o['sig'][2])
                            if o['dma']:
                                lastdma[o['sig'][0]] = o['sig'][1]
                    for k, v in lastdma.items():
                        if waited.get(k, 0) < v:
                            eng.wait_ge(sems[k], v)
                return body
            for ename in engs:
                if any(o['eng'] == ename for o in ops):
                    getattr(block, ename)(make(ename))
        self.stack.close()


def make_consts():
    c = {}
    c['ident_f'] = np.eye(128, dtype=np.float32)
    c['ident_b'] = np.eye(128).astype(NPBF)
    p = np.arange(128)[:, None]
    fp = np.arange(896)[None, :]
    c['cmb'] = np.where(p > fp - 384, NEG, 0.0).astype(NPBF)
    c['omb'] = np.where(fp - 384 >= p, NEG, 0.0).astype(NPBF)
    kk = np.arange(4)[None, :, None] * 128 + np.arange(128)[:, None, None]
    f = np.arange(512)[None, None, :]
    mm = np.where(kk // 256 == f // 256, np.where(kk > f, NEG, 0.0), np.where(f // 256 > kk // 256, 0.0, NEG))
    c['mmb'] = mm.astype(NPBF)
    k2 = np.arange(4096)[None, :]
    c['eslc'] = (k2 // 64 == np.arange(64)[:, None]).astype(NPBF)
    c['emoba'] = (k2 // 256 == np.arange(16)[:, None]).astype(NPBF)
    c['cqb'] = np.ascontiguousarray(np.broadcast_to(np.arange(512, dtype=np.float32)[None, :], (128, 512)))
    alb = np.zeros((128, 16, 36), np.float32)
    for hh in range(16):
        for di in range(36):
            alb[:, hh, di] = SL_HH[hh] * (np.arange(128) + 128.0 * (di - 32))
    c['alb'] = alb
    clb = np.zeros((128, 8, 2, 8), np.float32)
    for ha in range(8):
        for a in range(2):
            for i in range(8):
                clb[:, ha, a, i] = SL_HH[ha] * (2048.0 * a + 16.0 * np.arange(128) + 31.0 - 512.0 * i)
    c['clb'] = clb
    cq = np.zeros((16, 512), np.float32)
    for hh in range(16):
        cq[hh] = -SL_HH[hh] * np.arange(512) / SCALE
    c['cq'] = cq.astype(NPBF)
    goh = np.zeros((56, 12, 128), np.float32)
    for g in range(2):
        for pp in range(2):
            for k in range(3):
                idx = (g * 2 + pp) * 3 + k
                ra = k * 8 + g * 4 + 2 * pp
                goh[ra, idx, 0:64] = 1.0
                goh[32 + ra, idx, 0:64] = 1.0
                goh[ra + 1, idx, 64:128] = 1.0
                goh[32 + ra + 1, idx, 64:128] = 1.0
    c['goh'] = goh.astype(NPBF)
    n_cmp, n_slc = 255, 64
    cs = np.arange(n_cmp) * 16
    ce = cs + 31
    ss = np.arange(n_slc) * 64
    se = ss + 63
    ov = ((cs[:, None] <= se[None, :]) & (ce[:, None] >= ss[None, :])).astype(np.float32)
    ovp = np.zeros((256, 64), np.float32)
    ovp[:255] = ov
    c['ov'] = np.ascontiguousarray(ovp.reshape(2, 128, 64).transpose(1, 0, 2)).astype(NPBF)
    q = np.arange(128)[:, None]
    m = np.arange(128)[None, :]
    jr = m - 64
    cr = q // 64
    ab = np.where((jr == cr) | (jr == cr - 1), BIGF, np.where(jr > cr, -BIGF, 0.0))
    c['abase'] = ab.astype(np.float32)
    m2 = np.arange(32)[None, :]
    c['bbase'] = np.broadcast_to(np.where(m2 >= 16, -BIGF, 0.0), (128, 32)).astype(np.float32).copy()
    c['obase'] = np.broadcast_to(np.where(m2 == 16, 0.0, 2 * NEG), (128, 32)).astype(np.float32).copy()
    c['xg'] = (16.0 * np.arange(128)[:, None] + 31.0 - np.arange(512)[None, :]).astype(np.float32)
    return c


DRAM_ONLY = ('eslc', 'emoba', 'cq')
CONST_DT = dict(ident_f=F32, ident_b=BF16, cmb=BF16, omb=BF16, mmb=BF16, eslc=BF16, emoba=BF16, cqb=F32, alb=F32, clb=F32,
                cq=BF16, goh=BF16, ov=BF16, abase=F32, bbase=F32, obase=F32, xg=F32)


def build(S, NSEQ, dbg=()):
    nc = bass.Bass("TRN2", target_bir_lowering=False)
    NT = S // 128
    NQT = S // 512
    consts = make_consts()

    def din(name, shape, dt=F32):
        return nc.dram_tensor(name, list(shape), dt, kind="ExternalInput").ap()

    x_d = din("x", [NSEQ, S, D])
    win_d = din("w_in", [D, NCOL])
    w1s_d = din("w1s", [128, 32, 128])
    w2s_d = din("w2s", [128, 2, 64])
    pet_d = din("pet", [128, 32])
    wa_d = din("w_a", [512, D])
    wb_d = din("w_b", [512, D])
    wo_d = din("w_o", [D, D])
    npre_d = din("npre_bc", [128, D])
    npost_d = din("npost_bc", [128, D])
    cd = {k: din("c_" + k, list(v.shape), CONST_DT[k]) for k, v in consts.items()}
    out_d = nc.dram_tensor("out", [NSEQ, S, D], F32, kind="ExternalOutput").ap()
    wbf_d = nc.dram_tensor("wbf", [D, NCOL], BF16, kind="Internal").ap()
    ysc_d = nc.dram_tensor("yscr", [8, 128, S], BF16, kind="Internal").ap()
    wabf_d = nc.dram_tensor("wabf", [2, 512, D], BF16, kind="Internal").ap()
    dbg_d = {}
    for name, shape in dbg:
        dbg_d[name] = nc.dram_tensor("dbg_" + name, list(shape), F32, kind="ExternalOutput").ap()

    P = Prog(nc)
    cs = {k: P.sb("k_" + k, list(v.shape), CONST_DT[k]) for k, v in consts.items() if k not in DRAM_ONLY}
    uT = P.sb("uT", [128, 8, S], BF16)
    SA = max(S, 4096)
    kT0 = P.sb("kT0", [128, SA], BF16)
    kT1 = P.sb("kT1", [128, SA], BF16)
    Vp = P.sb("Vp", [128, 2 * SA], BF16)
    kcT = P.sb("kcT", [128, 256], BF16)
    vcx = P.sb("vcx", [128, 2, 128], BF16)
    hcm = P.sb("hcm", [128, 2, 256], BF16)
    QA = [[P.sb(f"QA{b}_{h}", [128, 512], BF16) for h in range(4)] for b in range(2)]
    PT = [P.sb(f"PT{j}", [128, 512], BF16) for j in range(6)]
    XM = [P.sb(f"XM{j}", [128, 512], BF16) for j in range(2)]
    WKB = P.sb("WKB", [128, 7, 512], F32)
    WK = [WKB[:, j, :] for j in range(5)]
    YST = [P.sb(f"YST{j}", [128, 512], BF16) for j in range(2)]
    SGT = [P.sb(f"SGT{j}", [64, 512], BF16) for j in range(2)]
    T1 = P.sb("T1", [128, 4, 64], F32)
    T2 = P.sb("T2", [128, 4, 64], F32)
    M8a = P.sb("M8a", [128, 8], F32)
    M8b = P.sb("M8b", [128, 8], F32)
    GT = P.sb("GT", [128, 8, 16], F32)
    kbarf = P.sb("kbarf", [128, 16], F32)
    kbar = P.sb("kbar", [64, 2, 16], BF16)
    wst = [P.sb(f"wst{j}", [128, 8, 128], BF16) for j in range(2)]
    WQZ = P.sb("WQZ", [128, 8, 512], BF16)
    wg24 = P.sb("wg24", [128, 8, 24], BF16)
    w2s = P.sb("w2s", [128, 2, 64], BF16)
    pet = P.sb("pet", [128, 32], BF16)
    hb = P.sb("hb", [128, 2], F32)
    xt = [P.sb(f"xt{j}", [128, D], F32) for j in range(2)]
    xn = [P.sb(f"xn{j}", [128, D], F32) for j in range(2)]
    gpre = WKB[:, 5:7, :].rearrange("p a n -> p (a n)")
    gpost = gpre
    ACC = [[xt[j][:, b_ * 512:(b_ + 1) * 512] for j in range(2)] for b_ in range(2)]
    SZ = [[xn[j][:, b_ * 512:(b_ + 1) * 512] for j in range(2)] for b_ in range(2)]
    if DEBUG_SEP:
        ACC = [[P.sb(f"ACCd{b_}{j}", [128, 512], F32)[:] for j in range(2)] for b_ in range(2)]
        SZ = [[P.sb(f"SZd{b_}{j}", [128, 512], F32)[:] for j in range(2)] for b_ in range(2)]
    impacc = WKB[:, 5, :]
    WKg = WKB[:, 6, :]
    ones1 = P.sb("ones1", [128, 1], F32)

    def XK(b_):
        return [('xt', b_, 0), ('xt', b_, 1)]

    def NK(b_):
        return [('xn', b_, 0), ('xn', b_, 1)]
    ssq = P.sb("ssq", [128, 2], F32)
    ssq2 = P.sb("ssq2", [128, 2], F32)
    eps_t = P.sb("eps_t", [128, 1], F32)
    ones_b = P.sb("ones_b", [128, 128], BF16)
    wab = WQZ[:].rearrange("p c (b n) -> p b c n", b=4)[:, 0:2]
    wo_s = P.sb("wo_s", [128, 8, D], BF16)
    ps = [P.ps(f"ps{j}") for j in range(8)]
    ST = [0, 1]
    OLS = [(2, 3), (4, 5)]
    MISC = [6, 7]
    Vp3 = Vp[:].rearrange("p (t c) -> p t c", c=256)
    mT = kT0[:].rearrange("p (c t) -> p c t", c=8)
    ysl = kT1[:].rearrange("p (c t) -> p c t", c=8)
    wgs = Vp[:, 0:SA].rearrange("p (b c n) -> p b c n", b=2, c=8)
    assert SA // 8 >= 512 and SA // 16 >= 256

    rr = {'misc': 0, 'st': 0, 'pt': 0, 'wst': 0, 'wk': 0, 'ol': 0, 'ptc': 0, 'g4': 0}

    def kt_lo(hh, i):
        lo = 0
        while SL_HH[hh] * (512 * i - 128 * lo - 127) > 110.0:
            lo += 1
        return lo

    def MM(out, lhsT, rhs, start, stop, reads, writes, tp=None):
        if tp is None:
            P.op('tensor', lambda e: e.matmul(out, lhsT=lhsT, rhs=rhs, start=start, stop=stop), reads, writes)
        else:
            P.op('tensor', lambda e: e.matmul(out, lhsT=lhsT, rhs=rhs, start=start, stop=stop, tile_position=tp),
                 reads, writes)

    def TR(out, in_, ident, reads, writes):
        P.op('tensor', lambda e: e.transpose(out, in_, ident), reads, writes)

    def ACT(out, in_, func, reads, writes, bias=None, scale=1.0):
        if bias is None:
            P.op('scalar', lambda e: e.activation(out=out, in_=in_, func=func, scale=scale), reads, writes)
        else:
            P.op('scalar', lambda e: e.activation(out=out, in_=in_, func=func, bias=bias, scale=scale), reads, writes)

    def TT(eng, out, in0, in1, op, reads, writes):
        P.op(eng, lambda e: e.tensor_tensor(out=out, in0=in0, in1=in1, op=op), reads, writes)

    def TS(eng, out, in0, s1, s2, op0, op1, reads, writes):
        if op1 is None:
            P.op(eng, lambda e: e.tensor_scalar(out=out, in0=in0, scalar1=s1, scalar2=None, op0=op0), reads, writes)
        else:
            P.op(eng, lambda e: e.tensor_scalar(out=out, in0=in0, scalar1=s1, scalar2=s2, op0=op0, op1=op1),
                 reads, writes)

    def STT(out, in0, scalar, in1, op0, op1, reads, writes):
        P.op('vector', lambda e: e.scalar_tensor_tensor(out=out, in0=in0, scalar=scalar, in1=in1, op0=op0, op1=op1),
             reads, writes)

    def CP(eng, out, in_, reads, writes):
        P.op(eng, lambda e: e.tensor_copy(out=out, in_=in_), reads, writes)

    def RCP(eng, out, in_, reads, writes):
        ACT(out, in_, AF.Ln, reads, writes)
        ACT(out, out, AF.Exp, writes, writes, scale=-1.0)

    def SIG1P(ap, np_, key):
        ACT(ap, ap, AF.Ln, [key, 'ones1'], [key], bias=ones1[0:np_, 0:1], scale=1.0)
        ACT(ap, ap, AF.Exp, [key], [key], scale=-1.0)

    def MS(eng, ap, val, writes):
        P.op(eng, lambda e: e.memset(ap, val), (), writes)

    def pk(j):
        return ('ps', j)

    def misc_bank():
        j = MISC[rr['misc'] % 2]
        rr['misc'] += 1
        return j

    def next_wst():
        j = rr['wst'] % 2
        rr['wst'] += 1
        return j

    wcols = wbf_d.rearrange("(c p) n -> p c n", p=128)
    WBK = [('wbf', r) for r in range(8)]

    def load_w(dst, col0, ncols, key):
        P.dma('sync', dst, wcols[:, :, col0:col0 + ncols], reads=WBK, writes=[key])

    def dump(name, src, reads):
        if name in dbg_d:
            P.dma('sync', dbg_d[name], src, reads=reads, writes=['dbg_' + name])

    for k in cs:
        P.dma('sync', cs[k][:], cd[k], reads=(), writes=['c_' + k])
    CK = ['c_alb', 'c_clb']
    for r in range(8):
        P.dma('gpsimd', wbf_d[r * 128:(r + 1) * 128, :], win_d[r * 128:(r + 1) * 128, :], reads=(), writes=[('wbf', r)])
    P.dma('gpsimd', wabf_d[0], wa_d, reads=(), writes=['wabf0'])
    P.dma('gpsimd', wabf_d[1], wb_d, reads=(), writes=['wabf1'])
    P.dma('gpsimd', wo_s[:], wo_d.rearrange("(j p) n -> p j n", p=128), reads=(), writes=['wo'])
    P.dma('gpsimd', w2s[:], w2s_d, reads=(), writes=['w2s'])
    P.dma('gpsimd', pet[:], pet_d, reads=(), writes=['pet'])
    load_w(wg24[:], C_GA, 24, 'wg24')
    MS('vector', eps_t[:], 1e-6, ['eps'])
    MS('vector', ones_b[:], 1.0, ['ones'])
    for b_ in range(2):
        MS('vector', SGT[b_][:], 0.0, [('SGT', b_)])
    MS('vector', vcx[:], 1.0, ['vcx'])
    MS('vector', ones1[:], 1.0, ['ones1'])
    MS('vector', kcT[64:65, :], 1.0, ['kcT'])
    MS('vector', hcm[:], 0.0, ['hcm'])

    def adv(gen):
        if gen is not None:
            try:
                next(gen)
            except StopIteration:
                pass

    def exhaust(gen):
        if gen is not None:
            for _ in gen:
                pass

    def attend(tiles, gen=None, step=3):
        pend = []
        for t in tiles:
            sb_ = ST[rr['st'] % 2]
            rr['st'] += 1
            pj = rr['pt'] % 4
            rr['pt'] += 1
            n = len(t['smm'])
            for m, (lh, rh, rd) in enumerate(t['smm']):
                MM(ps[sb_][:, :], lh, rh, m == 0, m == n - 1, rd, [pk(sb_)])
            ACT(PT[pj][:], ps[sb_][:, :], AF.Exp, [pk(sb_)] + CK, [('PT', pj)], bias=t['bias'], scale=SCALE)
            pend.append((t['pv'], pj))
            rr['tc'] = rr.get('tc', 0) + 1
            if rr['tc'] % step == 0:
                adv(gen)
            if len(pend) > 2:
                pv_, pj_ = pend.pop(0)
                for (o_, lh, tp, st_, sp_, rd, wk) in pv_:
                    MM(o_, lh, PT[pj_][:], st_, sp_, rd + [('PT', pj_)], [wk], tp=tp)
        for pv_, pj_ in pend:
            for (o_, lh, tp, st_, sp_, rd, wk) in pv_:
                MM(o_, lh, PT[pj_][:], st_, sp_, rd + [('PT', pj_)], [wk], tp=tp)

    def proj_fm(wtile, wkey, wc0, ncol, blk, bank):
        for c in range(8):
            MM(ps[bank][0:ncol, :], wtile[:, c, wc0:wc0 + ncol], uT[:, c, blk * 512:(blk + 1) * 512],
               c == 0, c == 7, [wkey, ('uT', blk)], [pk(bank)])

    def silu_pair(bank, dst, dkey):
        w = WK[0]
        ACT(w[:], ps[bank][:, :], AF.Exp, [pk(bank)], [('WK', 0)], scale=-1.0)
        SIG1P(w[:], 128, ('WK', 0))
        TT('vector', dst, ps[bank][:, :], w[:], ALU.mult, [pk(bank), ('WK', 0)], [dkey])

    def combine_branch(acc, akey, first, gbank, banks):
        w = WK[1]
        for hl in range(2):
            r0 = 64 * hl
            TS('vector', w[r0:r0 + 64, :], ps[banks[hl]][64:128, :], EPSL, None, ALU.max, None, [pk(banks[hl])],
               [('WK', 1)])
        RCP('vector', w[:], w[:], [('WK', 1)], [('WK', 1)])
        if gbank is not None:
            TT('vector', w[:], ps[gbank][:, :], w[:], ALU.mult, [pk(gbank), ('WK', 1)], [('WK', 1)])
        dst = acc if first else WK[2]
        dkey = akey if first else ('WK', 2)
        for hl in range(2):
            r0 = 64 * hl
            TT('vector', dst[r0:r0 + 64, :], ps[banks[hl]][0:64, :], w[r0:r0 + 64, :], ALU.mult,
               [pk(banks[hl]), ('WK', 1)], [dkey])
        if not first:
            TT('vector', acc, acc, WK[2][:], ALU.add, [akey, ('WK', 2)], [akey])

    for s in range(NSEQ):
        P.dma('sync', gpre, npre_d, reads=(), writes=['impacc', 'WKg'])
        for tt in range(NT):
            b = tt % 2
            P.dma('sync', xt[b][:], x_d[s, tt * 128:(tt + 1) * 128, :], reads=(), writes=XK(b))
            P.op('vector', (lambda bb: lambda e: e.scalar_tensor_tensor(
                out=xn[bb][:], in0=xt[bb][:], scalar=1.0, in1=xt[bb][:], op0=ALU.mult, op1=ALU.mult,
                accum_out=ssq[:, bb:bb + 1]))(b), XK(b), NK(b) + [('ssq', b)])
            ACT(ssq[:, b:b + 1], ssq[:, b:b + 1], AF.Ln, [('ssq', b), 'eps'], [('ssq', b)], bias=eps_t[:, 0:1],
                scale=1.0 / D)
            ACT(ssq[:, b:b + 1], ssq[:, b:b + 1], AF.Exp, [('ssq', b)], [('ssq', b)], scale=-0.5)
            STT(xn[b][:], xt[b][:], ssq[:, b:b + 1], gpre, ALU.mult, ALU.mult, XK(b) + [('ssq', b), 'impacc', 'WKg'],
                NK(b))
            for half in range(2):
                bk = misc_bank()
                for cc in range(4):
                    c = half * 4 + cc
                    TR(ps[bk][:, cc * 128:(cc + 1) * 128], xn[b][:, c * 128:(c + 1) * 128], cs['ident_f'][:],
                       NK(b) + ['c_ident_f'], [pk(bk)])
                if half == 0:
                    CP('vector', uT[:, half * 4:(half + 1) * 4, tt * 128:(tt + 1) * 128],
                       ps[bk][:, :].rearrange("p (c t) -> p c t", c=4), [pk(bk)], [('uT', tt // 4)])
                else:
                    P.op('scalar', (lambda o_, i_: lambda e: e.copy(out=o_, in_=i_))(
                        uT[:, half * 4:(half + 1) * 4, tt * 128:(tt + 1) * 128],
                        ps[bk][:, :].rearrange("p (c t) -> p c t", c=4)), [pk(bk)], [('uT', tt // 4)])
        if 'uT' in dbg_d and s == 0:
            CP('vector', WK[0][:], uT[:, 0, 0:512], [('uT', 0)], [('WK', 0)])
            dump('uT', WK[0][:], [('WK', 0)])

        ncm = (S - 32) // 16 + 1
        for g in range(2):
            wj = next_wst()
            load_w(wst[wj][:, :, 0:64], C_KV + 0 * 128 + g * 64, 64, ('wst', wj))
            load_w(wst[wj][:, :, 64:128], C_KV + 1 * 128 + g * 64, 64, ('wst', wj))
            wjb = next_wst()
            load_w(wst[wjb][:, :, 0:64], C_KV + 2 * 128 + g * 64, 64, ('wst', wjb))
            load_w(wst[wjb][:, :, 64:128], C_KV + 4 * 128 + g * 64, 64, ('wst', wjb))
            W1 = Vp[:, 0:4096].rearrange("p (l e) -> p l e", e=128)
            P.dma('gpsimd', W1, w1s_d, reads=(), writes=['Vp'])
            for blk in range(NQT):
                bk = misc_bank()
                proj_fm(wst[wj], ('wst', wj), 0, 128, blk, bk)
                CP('vector', kT1[:, blk * 512:(blk + 1) * 512], ps[bk][:, :], [pk(bk)], ['kT1'])
            wjc = next_wst()
            load_w(wst[wjc][:, :, 0:64], C_KV + 3 * 128 + g * 64, 64, ('wst', wjc))
            load_w(wst[wjc][:, :, 64:128], C_KV + 5 * 128 + g * 64, 64, ('wst', wjc))
            load_w(WQZ[:, :, 0:256], C_QA + g * 256, 256, 'WQZ')
            load_w(WQZ[:, :, 256:512], C_ZA + g * 256, 256, 'WQZ')
            for wh in range(2):
                r0 = 64 * wh
                bk = misc_bank()
                for l in range(32):
                    MM(ps[bk][:, 0:ncm], W1[r0:r0 + 64, l, :], kT1[r0:r0 + 64, l:l + 16 * (ncm - 1) + 1:16],
                       l == 0, l == 31, ['Vp', 'kT1'], [pk(bk)])
                bk2 = misc_bank()
                for l in range(32):
                    MM(ps[bk2][:, 0:1], W1[r0:r0 + 64, l, :], pet[r0:r0 + 64, l:l + 1], l == 0, l == 31,
                       ['Vp', 'pet'], [pk(bk2)])
                CP('vector', hb[:, 0:1], ps[bk2][:, 0:1], [pk(bk2)], ['hb'])
                TS('vector', hb[:, 1:2], hb[:, 0:1], -1.0, None, ALU.mult, None, ['hb'], ['hb'])
                w = WK[0]
                ACT(w[:, 0:ncm], ps[bk][:, 0:ncm], AF.Exp, [pk(bk), 'hb'], [('WK', 0)], bias=hb[:, 1:2], scale=-1.0)
                SIG1P(w[:, 0:ncm], 128, ('WK', 0))
                STT(hcm[:, wh, 0:ncm], ps[bk][:, 0:ncm], hb[:, 0:1], w[:, 0:ncm], ALU.add, ALU.mult,
                    [pk(bk), 'hb', ('WK', 0)], ['hcm'])
                if wh == 0:
                    bk3 = misc_bank()
                    MM(ps[bk3][0:64, 0:256], w2s[:, 0, :], hcm[:, 0, :], True, True, ['w2s', 'hcm'], [pk(bk3)])
                    CP('vector', kcT[0:64, :], ps[bk3][0:64, 0:256], [pk(bk3)], ['kcT'])
                else:
                    bk3 = misc_bank()
                    for a in range(2):
                        MM(ps[bk3][:, a * 64:(a + 1) * 64], hcm[:, 1, a * 128:(a + 1) * 128], w2s[:, 1, :], True, True,
                           ['w2s', 'hcm'], [pk(bk3)])
                    CP('vector', vcx[:, :, 0:64], ps[bk3][:, 0:128].rearrange("p (a d) -> p a d", a=2), [pk(bk3)],
                       ['vcx'])
            wj = wjb
            for blk in range(NQT):
                bk = misc_bank()
                proj_fm(wst[wj], ('wst', wj), 0, 128, blk, bk)
                CP('vector', kT0[0:64, blk * 512:(blk + 1) * 512], ps[bk][0:64, :], [pk(bk)], ['kT0'])
                CP('vector', kT1[0:64, blk * 512:(blk + 1) * 512], ps[bk][64:128, :], [pk(bk)], ['kT1'])
            P.dma('sync', kT0[64:128, 0:S], cd['eslc'][:, 0:S], reads=(), writes=['kT0'])
            MS('vector', kT1[64:128, :], 0.0, ['kT1'])
            MS('vector', kT1[64:65, :], 1.0, ['kT1'])
            for h4 in range(4):
                for b2 in range(2):
                    P.dma('sync', QA[b2][h4][64:65, :], cd['cq'][4 * g + h4:4 * g + h4 + 1, :], reads=(),
                          writes=[('QA', b2, h4)])

            def nsa_pre(g, i):
                b2 = i % 2
                for pp in range(2):
                    bk = misc_bank()
                    proj_fm(WQZ, 'WQZ', pp * 128, 128, i, bk)
                    CP('vector', QA[b2][2 * pp][0:64, :], ps[bk][0:64, :], [pk(bk)], [('QA', b2, 2 * pp)])
                    CP('vector', QA[b2][2 * pp + 1][0:64, :], ps[bk][64:128, :], [pk(bk)], [('QA', b2, 2 * pp + 1)])
                    yield
                bk = misc_bank()
                for c in range(8):
                    MM(ps[bk][0:24, :], wg24[:, c, :], uT[:, c, i * 512:(i + 1) * 512], c == 0, c == 7,
                       ['wg24', ('uT', i)], [pk(bk)])
                w = WK[0]
                ACT(w[0:24, :], ps[bk][0:24, :], AF.Exp, [pk(bk)], [('WK', 0)], scale=-1.0)
                SIG1P(w[0:24, :], 24, ('WK', 0))
                CP('vector', SGT[b2][0:24, :], w[0:24, :], [('WK', 0)], [('SGT', b2)])
                TT('vector', SGT[b2][32:56, :], w[0:24, :], SGT[b2][0:24, :], ALU.subtract, [('WK', 0), ('SGT', b2)],
                   [('SGT', b2)])
                yield
                a_list = [0] if (512 * i + 511) < (2048 + 31) else [0, 1]
                a_list = [a for a in a_list if a * 128 < ncm]
                for a in a_list:
                    thr = 512.0 * i - 2048.0 * a
                    TS('vector', XM[a][:], cs['xg'][:], thr, NEG, ALU.is_gt, ALU.mult, ['c_xg'], [('XM', a)])
                la = len(a_list)
                for h4 in range(4):
                    hl = h4 % 2
                    pp = h4 // 2
                    ha = 4 * g + h4
                    if hl == 0:
                        gb = misc_bank()
                        MM(ps[gb][:, :], cs['goh'][0:56, (g * 2 + pp) * 3 + 0, :], SGT[b2][0:56, :], True, True,
                           ['c_goh', ('SGT', b2)], [pk(gb)])
                        CP('vector', WKg[:], ps[gb][:, :], [pk(gb)], ['WKg'])
                    bkA = misc_bank()
                    bkB = misc_bank()
                    for ai, a in enumerate(a_list):
                        sb_ = ST[rr['st'] % 2]
                        rr['st'] += 1
                        pj = 4 + rr['ptc'] % 2
                        rr['ptc'] += 1
                        MM(ps[sb_][:, :], kcT[0:65, a * 128:(a + 1) * 128], QA[b2][h4][0:65, :], True, False,
                           ['kcT', ('QA', b2, h4)], [pk(sb_)])
                        MM(ps[sb_][:, :], cs['ident_b'][:], XM[a][:], False, True, ['c_ident_b', ('XM', a)], [pk(sb_)])
                        ACT(PT[pj][:], ps[sb_][:, :], AF.Exp, [pk(sb_)] + CK, [('PT', pj)],
                            bias=cs['clb'][:, ha, a, i:i + 1], scale=SCALE)
                        MM(ps[bkA][:, :], vcx[:, a, :], PT[pj][:], ai == 0, ai == la - 1, ['vcx', ('PT', pj)], [pk(bkA)])
                        MM(ps[bkB][0:64, :], cs['ov'][:, a, :], PT[pj][:], ai == 0, ai == la - 1, ['c_ov', ('PT', pj)],
                           [pk(bkB)])
                    wr = WK[3]
                    TS('vector', wr[0:64, :], ps[bkA][64:128, :], EPSL, None, ALU.max, None, [pk(bkA)], [('WK', 3)])
                    TS('vector', wr[64:128, :], ps[bkA][64:128, :], EPSL, None, ALU.max, None, [pk(bkA)], [('WK', 3)])
                    RCP('vector', wr[:, :], wr[:, :], [('WK', 3)], [('WK', 3)])
                    if h4 == 0:
                        TT('vector', impacc[0:64, :], ps[bkB][0:64, :], wr[0:64, :], ALU.mult, [pk(bkB), ('WK', 3)],
                           ['impacc'])
                    else:
                        TT('vector', WK[4][0:64, :], ps[bkB][0:64, :], wr[0:64, :], ALU.mult, [pk(bkB), ('WK', 3)],
                           [('WK', 4)])
                        TT('vector', impacc[0:64, :], impacc[0:64, :], WK[4][0:64, :], ALU.add, ['impacc', ('WK', 4)],
                           ['impacc'])
                    r0 = 64 * hl
                    TT('vector', WK[1][r0:r0 + 64, :], WKg[r0:r0 + 64, :], wr[r0:r0 + 64, :], ALU.mult, ['WKg', ('WK', 3)],
                       [('WK', 1)])
                    TT('vector', ACC[b2][pp][r0:r0 + 64, :], ps[bkA][0:64, :], WK[1][r0:r0 + 64, :], ALU.mult,
                       [pk(bkA), ('WK', 1)], [('xt', pp, b2)])
                    yield
                bk = misc_bank()
                for qs in range(4):
                    TR(ps[bk][:, qs * 64:(qs + 1) * 64], impacc[0:64, qs * 128:(qs + 1) * 128], cs['ident_f'][0:64, 0:64],
                       ['impacc', 'c_ident_f'], [pk(bk)])
                for qs in range(4):
                    ti = 4 * i + qs
                    TT('vector', T1[:, qs, :], ps[bk][:, qs * 64:(qs + 1) * 64],
                       cs['abase'][:, 64 - 2 * ti:128 - 2 * ti], ALU.add, [pk(bk), 'c_abase'], ['T1'])
                MS('vector', T1[:, :, 0:1], BIGF, ['T1'])
                yield
                for qs in range(4):
                    P.op('vector', (lambda q_: lambda e: e.max(out=M8a[:], in_=T1[:, q_, :]))(qs), ['T1'], ['M8a'])
                    P.op('vector', (lambda q_: lambda e: e.match_replace(out=T2[:, q_, :], in_to_replace=M8a[:],
                                                                        in_values=T1[:, q_, :], imm_value=-1e9))(qs),
                         ['T1', 'M8a'], ['T2'])
                    P.op('vector', (lambda q_: lambda e: e.max(out=M8b[:], in_=T2[:, q_, :]))(qs), ['T2'], ['M8b'])
                    TS('vector', T2[:, qs, :], T1[:, qs, :], M8b[:, 7:8], NEG, ALU.is_lt, ALU.mult, ['T1', 'M8b'], ['T2'])
                    yield
                for pp in range(2):
                    bk = misc_bank()
                    proj_fm(WQZ, 'WQZ', 256 + pp * 128, 128, i, bk)
                    silu_pair(bk, SZ[b2][pp], ('xn', pp, b2))
                    yield
                yield
                bk = misc_bank()
                for qs in range(4):
                    TR(ps[bk][0:64, qs * 128:(qs + 1) * 128], T2[:, qs, :], cs['ident_f'][:], ['T2', 'c_ident_f'],
                       [pk(bk)])
                for h4 in range(4):
                    STT(QA[b2][h4][64:128, :], cs['cqb'][64:128, :], -SL_HH[4 * g + h4] / SCALE, ps[bk][0:64, :],
                        ALU.mult, ALU.add, [pk(bk), 'c_cqb'], [('QA', b2, h4)])
                yield

            gen0 = nsa_pre(g, 0)
            wj = wjc
            MS('vector', Vp3[:, :, :].rearrange("p t (h c) -> p t h c", h=2)[:, :, :, 64:128], 1.0, ['Vp'])
            for t4 in range(NT // 4):
                bk = misc_bank()
                for q4 in range(4):
                    tt = t4 * 4 + q4
                    for c in range(8):
                        MM(ps[bk][:, q4 * 128:(q4 + 1) * 128], uT[:, c, tt * 128:(tt + 1) * 128], wst[wj][:, c, :],
                           c == 0, c == 7, [('wst', wj), ('uT', t4)], [pk(bk)])
                CP('vector', Vp3[:, t4 * 4:(t4 + 1) * 4, :].rearrange("p t (h c) -> p t h c", h=2)[:, :, :, 0:64],
                   ps[bk][:, :].rearrange("p (t h c) -> p t h c", h=2, c=64), [pk(bk)], ['Vp'])
                adv(gen0)
                adv(gen0)
            exhaust(gen0)
            for i in range(NQT):
                b2 = i % 2
                gen = nsa_pre(g, i + 1) if i + 1 < NQT else None
                for pp in range(2):
                    for br in (1, 2):
                        tiles = []
                        OB, LB = OLS[rr['ol'] % 2]
                        rr['ol'] += 1
                        if br == 1:
                            kts = list(range(0, 4 * i + 4))
                        else:
                            kts = [kt for kt in range(4 * i - 4, 4 * i + 4) if kt >= 0]
                        for kt in kts:
                            for hl in range(2):
                                h4 = 2 * pp + hl
                                ha = 4 * g + h4
                                hk = [k_ for k_ in kts if k_ >= kt_lo(ha, i)]
                                if kt not in hk:
                                    continue
                                ki = hk.index(kt)
                                nk = len(hk)
                                kT = kT0 if br == 1 else kT1
                                kkey = 'kT0' if br == 1 else 'kT1'
                                smm = [(kT[:, kt * 128:(kt + 1) * 128], QA[b2][h4][:, :], [kkey, ('QA', b2, h4)])]
                                if br == 1:
                                    if kt >= 4 * i:
                                        r = kt - 4 * i
                                        smm.append((cs['ident_b'][:], cs['cmb'][:, 384 - 128 * r:896 - 128 * r],
                                                    ['c_ident_b', 'c_cmb']))
                                else:
                                    r = kt - (4 * i - 4)
                                    if r < 4:
                                        smm.append((cs['ident_b'][:], cs['omb'][:, 384 - 128 * r:896 - 128 * r],
                                                    ['c_ident_b', 'c_omb']))
                                    else:
                                        r -= 4
                                        smm.append((cs['ident_b'][:], cs['cmb'][:, 384 - 128 * r:896 - 128 * r],
                                                    ['c_ident_b', 'c_cmb']))
                                vcol = 0 if br == 1 else 128
                                bnk = (OB, LB)[hl]
                                pv = [(ps[bnk][:, :], Vp3[:, kt, vcol:vcol + 128], None,
                                       ki == 0, ki == nk - 1, ['Vp'], pk(bnk))]
                                tiles.append(dict(smm=smm, bias=cs['alb'][:, ha, kt - 4 * i + 32:kt - 4 * i + 33], pv=pv))
                        attend(tiles, gen)
                        gb = misc_bank()
                        MM(ps[gb][:, :], cs['goh'][0:56, (g * 2 + pp) * 3 + br, :], SGT[b2][0:56, :], True, True,
                           ['c_goh', ('SGT', b2)], [pk(gb)])
                        combine_branch(ACC[b2][pp], ('xt', pp, b2), False, gb, (OB, LB))
                    yj = rr['wk'] % 2
                    rr['wk'] += 1
                    TT('vector', YST[yj][:], ACC[b2][pp], SZ[b2][pp], ALU.mult, [('xt', pp, b2), ('xn', pp, b2)],
                       [('YST', yj)])
                    P.dma('gpsimd', ysc_d[g * 2 + pp, :, i * 512:(i + 1) * 512], YST[yj][:], reads=[('YST', yj)],
                          writes=[('ysc', g * 2 + pp, i)])
                exhaust(gen)

        nb = S // 256
        for j in range(4):
            wj = next_wst()
            load_w(wst[wj][:], C_QB + 512 + j * 128, 128, ('wst', wj))
            wjv = next_wst()
            load_w(wst[wjv][:], C_QB + 1024 + j * 128, 128, ('wst', wjv))
            load_w(WQZ[:, :, 0:128], C_QB + j * 128, 128, 'WQZ')
            load_w(WQZ[:, :, 128:256], C_ZB + j * 128, 128, 'WQZ')
            for blk in range(NQT):
                bk = misc_bank()
                proj_fm(wst[wj], ('wst', wj), 0, 128, blk, bk)
                CP('vector', kT0[0:64, blk * 512:(blk + 1) * 512], ps[bk][0:64, :], [pk(bk)], ['kT0'])
                CP('vector', kT1[0:64, blk * 512:(blk + 1) * 512], ps[bk][64:128, :], [pk(bk)], ['kT1'])
                P.op('vector', (lambda bk_, blk_: lambda e: e.reduce_sum(
                    out=kbarf[:, 2 * blk_:2 * blk_ + 2], in_=ps[bk_][:, :].rearrange("p (b t) -> p b t", b=2),
                    axis=AX.X))(bk, blk), [pk(bk)], ['kbarf'])
            for kT_, kk_ in (((kT0, 'kT0'), (kT1, 'kT1')) if j == 0 else ()):
                MS('vector', kT_[64:128, :], 0.0, [kk_])
                MS('vector', kT_[64:65, :], 1.0, [kk_])
                P.dma('sync', kT_[96:112, 0:S], cd['emoba'][:, 0:S], reads=(), writes=[kk_])
            TS('vector', kbar[0:64, 0, 0:nb], kbarf[0:64, 0:nb], 1.0 / 256, None, ALU.mult, None, ['kbarf'], ['kbar'])
            TS('vector', kbar[0:64, 1, 0:nb], kbarf[64:128, 0:nb], 1.0 / 256, None, ALU.mult, None, ['kbarf'], ['kbar'])
            for hl in range(2):
                for b2 in range(2):
                    if j == 0:
                        MS('vector', QA[b2][hl][64:128, :], 0.0, [('QA', b2, hl)])
                    P.dma('sync', QA[b2][hl][64:65, :], cd['cq'][8 + 2 * j + hl:8 + 2 * j + hl + 1, :], reads=(),
                          writes=[('QA', b2, hl)])

            def moba_pre(j, i):
                b2 = i % 2
                bk = misc_bank()
                proj_fm(WQZ, 'WQZ', 0, 128, i, bk)
                CP('vector', QA[b2][0][0:64, :], ps[bk][0:64, :], [pk(bk)], [('QA', b2, 0)])
                CP('vector', QA[b2][1][0:64, :], ps[bk][64:128, :], [pk(bk)], [('QA', b2, 1)])
                yield
                bk = misc_bank()
                for hl in range(2):
                    for qs in range(4):
                        MM(ps[bk][:, (hl * 4 + qs) * 16:(hl * 4 + qs) * 16 + nb], QA[b2][hl][0:64, qs * 128:(qs + 1) * 128],
                           kbar[0:64, hl, 0:nb], True, True, ['kbar', ('QA', b2, hl)], [pk(bk)])
                for hl in range(2):
                    for qs in range(4):
                        cur = (4 * i + qs) // 2
                        e8 = hl * 4 + qs
                        TT('vector', GT[:, e8, 0:nb], ps[bk][:, e8 * 16:e8 * 16 + nb], cs['bbase'][:, 16 - cur:16 - cur + nb],
                           ALU.add, [pk(bk), 'c_bbase'], ['GT'])
                        if nb < 16:
                            MS('vector', GT[:, e8, nb:16], -BIGF, ['GT'])
                yield
                for hl in range(2):
                    for qs in range(4):
                        cur = (4 * i + qs) // 2
                        e8 = hl * 4 + qs
                        P.op('vector', (lambda e_: lambda e: e.max(out=M8a[:], in_=GT[:, e_, :]))(e8), ['GT'], ['M8a'])
                        TS('vector', GT[:, e8, :], GT[:, e8, :], M8a[:, 2:3], NEG, ALU.is_lt, ALU.mult, ['GT', 'M8a'], ['GT'])
                        TT('vector', GT[:, e8, :], GT[:, e8, :], cs['obase'][:, 16 - cur:32 - cur], ALU.max,
                           ['GT', 'c_obase'], ['GT'])
                        yield
                bk = misc_bank()
                proj_fm(WQZ, 'WQZ', 128, 128, i, bk)
                silu_pair(bk, SZ[b2][0], ('xn', 0, b2))
                yield
                yield
                for hl in range(2):
                    bk = misc_bank()
                    for qs in range(4):
                        TR(ps[bk][0:16, qs * 128:(qs + 1) * 128], GT[:, hl * 4 + qs, :], cs['ident_f'][:],
                           ['GT', 'c_ident_f'], [pk(bk)])
                    CP('vector', QA[b2][hl][96:112, :], ps[bk][0:16, :], [pk(bk)], [('QA', b2, hl)])
                    yield

            gen0 = moba_pre(j, 0)
            wj = wjv
            MS('vector', Vp3[:, :, :].rearrange("p t (h c) -> p t h c", h=2)[:, :, :, 64:128], 1.0, ['Vp'])
            for t4 in range(NT // 4):
                bk = misc_bank()
                for q4 in range(4):
                    tt = t4 * 4 + q4
                    for c in range(8):
                        MM(ps[bk][:, q4 * 128:(q4 + 1) * 128], uT[:, c, tt * 128:(tt + 1) * 128], wst[wj][:, c, :],
                           c == 0, c == 7, [('wst', wj), ('uT', t4)], [pk(bk)])
                CP('vector', Vp3[:, t4 * 4:(t4 + 1) * 4, :].rearrange("p t (h c) -> p t h c", h=2)[:, :, :, 0:64],
                   ps[bk][:, :].rearrange("p (t h c) -> p t h c", h=2, c=64), [pk(bk)], ['Vp'])
                adv(gen0)
                adv(gen0)
            exhaust(gen0)
            for i in range(NQT):
                b2 = i % 2
                gen = moba_pre(j, i + 1) if i + 1 < NQT else None
                tiles = []
                OB, LB = OLS[rr['ol'] % 2]
                rr['ol'] += 1
                kts = list(range(0, 4 * i + 4))
                for kt in kts:
                    for hl in range(2):
                        hh = 8 + 2 * j + hl
                        hk = [k_ for k_ in kts if k_ >= kt_lo(hh, i)]
                        if kt not in hk:
                            continue
                        ki = hk.index(kt)
                        nk = len(hk)
                        kT = kT0 if hl == 0 else kT1
                        kkey = 'kT0' if hl == 0 else 'kT1'
                        smm = [(kT[:, kt * 128:(kt + 1) * 128], QA[b2][hl][:, :], [kkey, ('QA', b2, hl)])]
                        if kt >= 4 * i:
                            smm.append((cs['ident_b'][:], cs['mmb'][:, kt - 4 * i, :], ['c_ident_b', 'c_mmb']))
                        bnk = (OB, LB)[hl]
                        pv = [(ps[bnk][:, :], Vp3[:, kt, 128 * hl:128 * hl + 128], None,
                               ki == 0, ki == nk - 1, ['Vp'], pk(bnk))]
                        tiles.append(dict(smm=smm, bias=cs['alb'][:, hh, kt - 4 * i + 32:kt - 4 * i + 33], pv=pv))
                attend(tiles, gen, step=2)
                combine_branch(ACC[b2][0], ('xt', 0, b2), True, None, (OB, LB))
                yj = rr['wk'] % 2
                rr['wk'] += 1
                TT('vector', YST[yj][:], ACC[b2][0], SZ[b2][0], ALU.mult, [('xt', 0, b2), ('xn', 0, b2)], [('YST', yj)])
                P.dma('gpsimd', ysc_d[4 + j, :, i * 512:(i + 1) * 512], YST[yj][:], reads=[('YST', yj)],
                      writes=[('ysc', 4 + j, i)])
                exhaust(gen)

        P.op('sync', lambda e: e.nop(), (), ['Vp', ('wgs', 0), ('wgs', 1), 'WQZ', ('wab', 0), ('wab', 1), 'kT1', ('ysl', 0), ('ysl', 1)])
        P.dma('sync', gpost, npost_d, reads=(), writes=['impacc', 'WKg'])
        ysl2 = [ysl, Vp[:, SA:2 * SA].rearrange("p (c t) -> p c t", c=8)]

        def load_ysl(i_):
            P.dma('sync', ysl2[i_ % 2][:, :, 0:512], ysc_d[:, :, i_ * 512:(i_ + 1) * 512].rearrange("c p t -> p c t"),
                  reads=[('ysc', jj, i_) for jj in range(8)], writes=[('ysl', i_ % 2)])
        load_ysl(0)
        for i in range(NQT):
            if i + 1 < NQT:
                load_ysl(i + 1)
            def load_fc(fc_):
                gj_ = fc_ % 2
                P.dma('sync', wgs[:, gj_, :, 0:128], wcols[:, :, C_MG + fc_ * 128:C_MG + (fc_ + 1) * 128], reads=WBK,
                      writes=[('wgs', gj_)])
                P.dma('sync', wgs[:, gj_, :, 128:256],
                      wcols[:, :, C_MG + 1024 + fc_ * 128:C_MG + 1024 + (fc_ + 1) * 128], reads=WBK, writes=[('wgs', gj_)])
                for ab_ in range(2):
                    P.dma('sync', wab[:, gj_, 4 * ab_:4 * ab_ + 4, :],
                          wabf_d[ab_].rearrange("(j p) n -> p j n", p=128)[:, :, fc_ * 128:(fc_ + 1) * 128],
                          reads=['wabf0', 'wabf1'], writes=[('wab', gj_)])
            if i == 0:
                load_fc(0)
                load_fc(1)
            for fc in range(8):
                gj = fc % 2
                for ab in range(2):
                    bg = [6, 7, 0, 1][rr['g4'] % 4]
                    rr['g4'] += 1
                    for c in range(8):
                        MM(ps[bg][:, :], wgs[:, gj, c, ab * 128:(ab + 1) * 128], uT[:, c, i * 512:(i + 1) * 512],
                           c == 0, c == 7, [('wgs', gj), ('uT', i)], [pk(bg)])
                    w = WK[ab]
                    ACT(w[:], ps[bg][:, :], AF.Sigmoid, [pk(bg)], [('WK', ab)])
                    bm = OLS[fc % 2][ab]
                    for jj in range(4):
                        MM(ps[bm][:, :], wab[:, gj, 4 * ab + jj, :], ysl2[i % 2][:, 4 * ab + jj, 0:512], jj == 0, jj == 3,
                           [('wab', gj), ('ysl', i % 2)], [pk(bm)])
                    TT('vector', w[:], ps[bm][:, :], w[:], ALU.mult, [pk(bm), ('WK', ab)], [('WK', ab)])
                TT('vector', mT[:, fc, 0:512], WK[0][:], WK[1][:], ALU.add, [('WK', 0), ('WK', 1)], ['kT0'])
                if fc + 2 < 8:
                    load_fc(fc + 2)
                elif i + 1 < NQT:
                    load_fc(fc + 2 - 8)
            for ts_ in range(4):
                tt = i * 4 + ts_
                b = tt % 2
                P.dma('sync', xt[b][:], x_d[s, tt * 128:(tt + 1) * 128, :], reads=(), writes=XK(b))
                banks = [ST[0], ST[1]]
                for hf in range(2):
                    for fc in range(8):
                        MM(ps[banks[hf]][:, :], mT[:, fc, ts_ * 128:(ts_ + 1) * 128], wo_s[:, fc, hf * 512:(hf + 1) * 512],
                           fc == 0, fc == 7, ['wo', 'kT0'], [pk(banks[hf])])
                for hf in range(2):
                    P.op('scalar', (lambda o_, i_: lambda e: e.copy(out=o_, in_=i_))(WK[2 + hf][:], ps[banks[hf]][:, :]),
                         [pk(banks[hf])], [('WK', 2 + hf)])
                    P.op('vector', (lambda o_, i_, a_: lambda e: e.scalar_tensor_tensor(
                        out=o_, in0=i_, scalar=1.0, in1=i_, op0=ALU.mult, op1=ALU.mult, accum_out=a_))(
                        YST[hf][:], WK[2 + hf][:], ssq2[:, hf:hf + 1]), [('WK', 2 + hf)], [('YST', hf), ('ssq2', hf)])
                TT('vector', ssq[:, b:b + 1], ssq2[:, 0:1], ssq2[:, 1:2], ALU.add, [('ssq2', 0), ('ssq2', 1)], [('ssq', b)])
                ACT(ssq[:, b:b + 1], ssq[:, b:b + 1], AF.Ln, [('ssq', b), 'eps'], [('ssq', b)], bias=eps_t[:, 0:1],
                    scale=1.0 / D)
                ACT(ssq[:, b:b + 1], ssq[:, b:b + 1], AF.Exp, [('ssq', b)], [('ssq', b)], scale=-0.5)
                for hf in range(2):
                    STT(xn[b][:, hf * 512:(hf + 1) * 512], WK[2 + hf][:], ssq[:, b:b + 1],
                        gpost[:, hf * 512:(hf + 1) * 512], ALU.mult, ALU.mult, [('WK', 2 + hf), ('ssq', b), 'impacc', 'WKg'],
                        [('xn', b, hf)])
                TT('gpsimd', xn[b][:], xn[b][:], xt[b][:], ALU.add, NK(b) + XK(b), NK(b))
                P.dma('gpsimd', out_d[s, tt * 128:(tt + 1) * 128, :], xn[b][:], reads=NK(b), writes=[('out', tt)])
        P.op('sync', lambda e: e.nop(), (), ['Vp', ('wgs', 0), ('wgs', 1), 'WQZ', ('wab', 0), ('wab', 1), 'kT1', ('ysl', 0), ('ysl', 1)])
    P.finish()
    return nc, P


def host_inputs(inputs, S):
    c = make_consts()
    f = lambda a: np.ascontiguousarray(np.asarray(a, dtype=np.float32))
    w1k = f(inputs['cmp_w1_k'])[0].transpose(1, 0, 2)
    w1v = f(inputs['cmp_w1_v'])[0].transpose(1, 0, 2)
    shared = {
        'w_in': f(inputs['w_in'])[0],
        'w1s': np.ascontiguousarray(np.concatenate([w1k, w1v], 0)),
        'w2s': np.ascontiguousarray(np.stack([f(inputs['cmp_w2_k'])[0], f(inputs['cmp_w2_v'])[0]], 1)),
        'pet': np.ascontiguousarray(np.concatenate([f(inputs['cmp_pe_k'])[0].T, f(inputs['cmp_pe_v'])[0].T], 0)),
        'w_a': f(inputs['w_branch_a'])[0],
        'w_b': f(inputs['w_branch_b'])[0],
        'w_o': f(inputs['w_o'])[0],
        'npre_bc': np.ascontiguousarray(np.broadcast_to(f(inputs['norm_pre'])[0][None, :], (128, D))),
        'npost_bc': np.ascontiguousarray(np.broadcast_to(f(inputs['norm_post'])[0][None, :], (128, D))),
    }
    for k, v in c.items():
        shared['c_' + k] = v
    return shared


def kernel(**inputs):
    x = np.ascontiguousarray(np.asarray(inputs['x'], dtype=np.float32))
    B, S, _ = x.shape
    ncores = 8
    nseq = B // ncores
    nc, _ = build(S, nseq)
    shared = host_inputs(inputs, S)
    in_maps = []
    for c in range(ncores):
        m = dict(shared)
        m['x'] = np.ascontiguousarray(x[c * nseq:(c + 1) * nseq])
        in_maps.append(m)
    res = run_bass_kernel_spmd(nc, in_maps, core_ids=list(range(ncores)))
    return np.concatenate([np.asarray(r['out']) for r in res.results], axis=0).astype(np.float32)
```
